# Optimizing a Trainium2 kernel written in Bass

```python
import math
import jax
import jax.numpy as jnp
from jax import lax
import numpy as np

D_MODEL = 1024
BATCH = 2
SEQ = 8192
DEPTH = 2

GRID_W = 64
CTX_LEN = 256
HEAD_DIM = 64
ATTN_HEADS = 8
ATTN_KV_HEADS = 2
MLSTM_HEADS = 4
GDN_HEADS = 4
ATTN_W = ATTN_HEADS * HEAD_DIM
KV_W = ATTN_KV_HEADS * HEAD_DIM
MLSTM_W = MLSTM_HEADS * HEAD_DIM
GDN_W = GDN_HEADS * HEAD_DIM
MIX_W = ATTN_W + MLSTM_W + GDN_W
N_DIR = 2
CHUNK = 64
Q_BLOCK = 128
CONV_K = 5
ROPE_THETA = 10000.0
N_EXPERTS = 16
CAPACITY_FACTOR = 2
EXPERT_FF = 2048
N_MOD = 6
EPS = 1e-6
IN_SPLITS = (ATTN_W, KV_W, KV_W,
             MLSTM_W, MLSTM_W, MLSTM_W, MLSTM_W, N_DIR * MLSTM_HEADS, N_DIR * MLSTM_HEADS,
             GDN_W, GDN_W, GDN_W, GDN_W, N_DIR * GDN_HEADS, N_DIR * GDN_HEADS)
IN_W = ATTN_W + 2 * KV_W + 4 * MLSTM_W + 2 * N_DIR * MLSTM_HEADS + 4 * GDN_W + 2 * N_DIR * GDN_HEADS

kernel_name = 'hybrid_gqa_mlstm_gdn_ecmoe_dit'


def rms_norm(x, g):
    xf = x.astype(jnp.float32)
    y = xf * lax.rsqrt(jnp.mean(xf * xf, axis=-1, keepdims=True) + EPS)
    return (y * g.astype(jnp.float32)).astype(x.dtype)


def l2_norm(x):
    xf = x.astype(jnp.float32)
    return (xf * lax.rsqrt(jnp.sum(xf * xf, axis=-1, keepdims=True) + EPS)).astype(x.dtype)


def modulate(h, shift, scale):
    return h * (1 + scale) + shift


def split_cols(p):
    return jnp.split(p, np.cumsum(IN_SPLITS)[:-1].tolist(), axis=-1)


def flip_seq(t, on):
    return jnp.flip(t, axis=1) if on else t


def to_chunks(t):
    b, n, h = t.shape[:3]
    t = t.reshape(b, n // CHUNK, CHUNK, h, *t.shape[3:])
    return jnp.moveaxis(t, (1, 2), (0, 3))


def from_chunks(t):
    t = jnp.moveaxis(t, (0, 3), (1, 2))
    b, nc, l, h, d = t.shape
    return t.reshape(b, nc * l, h, d)


def axial_rope_tables(n):
    rows = n // GRID_W
    row = jnp.broadcast_to(jnp.arange(rows)[:, None], (rows, GRID_W)).reshape(-1)
    col = jnp.broadcast_to(jnp.arange(GRID_W)[None, :], (rows, GRID_W)).reshape(-1)
    n_freq = HEAD_DIM // 4
    inv = ROPE_THETA ** (-jnp.arange(n_freq, dtype=jnp.float32) / n_freq)
    ang = jnp.stack([row, col], axis=-1).astype(jnp.float32)[..., None] * inv
    return jnp.cos(ang), jnp.sin(ang)


def apply_axial_rope(x, cos, sin):
    b, n, h, d = x.shape
    xf = x.astype(jnp.float32).reshape(b, n, h, 2, 2, d // 4)
    x1, x2 = xf[..., 0, :], xf[..., 1, :]
    cs, sn = cos[:, None], sin[:, None]
    out = jnp.stack([x1 * cs - x2 * sn, x2 * cs + x1 * sn], axis=-2)
    return out.reshape(b, n, h, d).astype(x.dtype)


def attention_mixer(parts_x, parts_c, qn_g, kn_g, need_ctx):
    q_x, k_x, v_x = parts_x
    q_c, k_c, v_c = parts_c
    b, n, _ = q_x.shape
    m = q_c.shape[1]
    rep = ATTN_HEADS // ATTN_KV_HEADS
    scale = HEAD_DIM ** -0.5

    def heads(t, h):
        return t.reshape(t.shape[0], t.shape[1], h, HEAD_DIM)

    cos, sin = axial_rope_tables(n)
    qx = apply_axial_rope(rms_norm(heads(q_x, ATTN_HEADS), qn_g), cos, sin)
    kx = apply_axial_rope(rms_norm(heads(k_x, ATTN_KV_HEADS), kn_g), cos, sin)
    vx = heads(v_x, ATTN_KV_HEADS)
    kc = rms_norm(heads(k_c, ATTN_KV_HEADS), kn_g)
    vc = heads(v_c, ATTN_KV_HEADS)
    k_all = jnp.concatenate([kc, kx], axis=1)
    v_all = jnp.concatenate([vc, vx], axis=1)

    def attend(qb, kk, vv):
        s = jnp.einsum('bqgrd,bkgd->bgrqk', qb, kk).astype(jnp.float32) * scale
        p = jax.nn.softmax(s, axis=-1).astype(vv.dtype)
        return jnp.einsum('bgrqk,bkgd->bqgrd', p, vv)

    qxb = qx.reshape(b, n // Q_BLOCK, Q_BLOCK, ATTN_KV_HEADS, rep, HEAD_DIM).transpose(1, 0, 2, 3, 4, 5)
    out_x = lax.map(lambda qb: attend(qb, k_all, v_all), qxb)
    out_x = out_x.transpose(1, 0, 2, 3, 4, 5).reshape(b, n, ATTN_W)
    out_c = None
    if need_ctx:
        qc = rms_norm(heads(q_c, ATTN_HEADS), qn_g).reshape(b, m, ATTN_KV_HEADS, rep, HEAD_DIM)
        out_c = attend(qc, kc, vc).reshape(b, m, ATTN_W)
    return out_x, out_c


def mlstm_scan(q, k, v, i_pre, f_pre, state, with_out):
    causal = jnp.tril(jnp.ones((CHUNK, CHUNK), dtype=bool))

    def step(carry, inp):
        C, nv, m = carry
        qc, kc, vc, ic, fc = inp
        bcum = jnp.cumsum(jax.nn.log_sigmoid(fc), axis=-1)
        b_last = bcum[..., -1]
        w_end = b_last[..., None] - bcum + ic
        m_new = jnp.maximum(b_last + m, jnp.max(w_end, axis=-1))
        a_end = jnp.exp(w_end - m_new[..., None])
        dec = jnp.exp(b_last + m - m_new)
        C_new = dec[..., None, None] * C + jnp.einsum('bhl,bhld,bhle->bhde', a_end, kc, vc)
        n_new = dec[..., None] * nv + jnp.einsum('bhl,bhld->bhd', a_end, kc)
        if not with_out:
            return (C_new, n_new, m_new), None
        dmat = jnp.where(causal, bcum[..., :, None] - bcum[..., None, :] + ic[..., None, :], -jnp.inf)
        inter = bcum + m[..., None]
        m_t = jnp.maximum(inter, jnp.max(dmat, axis=-1))
        w_in = jnp.exp(inter - m_t)
        s = jnp.einsum('bhtd,bhsd->bhts', qc, kc) * jnp.exp(dmat - m_t[..., None])
        num = w_in[..., None] * jnp.einsum('bhtd,bhde->bhte', qc, C) + jnp.einsum('bhts,bhse->bhte', s, vc)
        den = w_in * jnp.einsum('bhtd,bhd->bht', qc, nv) + jnp.sum(s, axis=-1)
        h = num / jnp.maximum(jnp.abs(den), jnp.exp(-m_t))[..., None]
        return (C_new, n_new, m_new), h

    xs = (to_chunks(q), to_chunks(k), to_chunks(v), to_chunks(i_pre), to_chunks(f_pre))
    final, hs = lax.scan(step, state, xs)
    return final, (from_chunks(hs) if with_out else None)


def mlstm_mixer(parts_x, parts_c, i_bias, f_bias, out_g, need_ctx):
    dtype = parts_x[0].dtype

    def prep(parts):
        q, k, v, o, ig, fg = [p.astype(jnp.float32) for p in parts]
        b, n, _ = q.shape
        hd = lambda t: t.reshape(b, n, MLSTM_HEADS, HEAD_DIM)
        ig = ig.reshape(b, n, N_DIR, MLSTM_HEADS) + i_bias.astype(jnp.float32)
        fg = fg.reshape(b, n, N_DIR, MLSTM_HEADS) + f_bias.astype(jnp.float32)
        return hd(q), hd(k) * HEAD_DIM ** -0.5, hd(v), o, ig, fg

    qx, kx, vx, ox, ix, fx = prep(parts_x)
    qc, kc, vc, oc, ic, fc = prep(parts_c)
    b = qx.shape[0]
    zero = (jnp.zeros((b, MLSTM_HEADS, HEAD_DIM, HEAD_DIM), jnp.float32),
            jnp.zeros((b, MLSTM_HEADS, HEAD_DIM), jnp.float32),
            jnp.zeros((b, MLSTM_HEADS), jnp.float32))
    hx_sum, hc_sum = 0.0, 0.0
    for d in range(N_DIR):
        rv = d == 1
        st, hc = mlstm_scan(flip_seq(qc, rv), flip_seq(kc, rv), flip_seq(vc, rv),
                            flip_seq(ic[:, :, d], rv), flip_seq(fc[:, :, d], rv), zero, need_ctx)
        _, hx = mlstm_scan(flip_seq(qx, rv), flip_seq(kx, rv), flip_seq(vx, rv),
                           flip_seq(ix[:, :, d], rv), flip_seq(fx[:, :, d], rv), st, True)
        hx_sum = hx_sum + flip_seq(hx, rv)
        if need_ctx:
            hc_sum = hc_sum + flip_seq(hc, rv)

    def finish(h, o):
        bb, nn = h.shape[:2]
        return (jax.nn.sigmoid(o) * rms_norm(h, out_g).reshape(bb, nn, MLSTM_W)).astype(dtype)

    return finish(hx_sum, ox), (finish(hc_sum, oc) if need_ctx else None)


def short_conv(t, w):
    k, ch = w.shape
    return lax.conv_general_dilated(t, w.reshape(k, 1, ch).astype(t.dtype), window_strides=(1,),
                                    padding=[(k // 2, k // 2)], dimension_numbers=('NWC', 'WIO', 'NWC'),
                                    feature_group_count=ch)


def gdn_scan(q, k, v, g, beta, state, with_out):
    incl = jnp.tril(jnp.ones((CHUNK, CHUNK), dtype=bool))
    strict = jnp.tril(jnp.ones((CHUNK, CHUNK), dtype=bool), -1)
    eye = jnp.eye(CHUNK, dtype=jnp.float32)

    def step(S, inp):
        qc, kc, vc, gc, bc = inp
        G = jnp.cumsum(gc, axis=-1)
        decay = jnp.where(incl, jnp.exp(jnp.where(incl, G[..., :, None] - G[..., None, :], 0.0)), 0.0)
        kb = kc * bc[..., None]
        A = eye + jnp.where(strict, jnp.einsum('bhid,bhjd->bhij', kb, kc) * decay, 0.0)
        rhs = jnp.concatenate([vc * bc[..., None], kb * jnp.exp(G)[..., None]], axis=-1)
        sol = lax.linalg.triangular_solve(A, rhs, left_side=True, lower=True, unit_diagonal=True)
        u, w = sol[..., :HEAD_DIM], sol[..., HEAD_DIM:]
        v_new = u - jnp.einsum('bhld,bhde->bhle', w, S)
        G_last = G[..., -1]
        S_new = jnp.exp(G_last)[..., None, None] * S + jnp.einsum(
            'bhld,bhle->bhde', kc * jnp.exp(G_last[..., None] - G)[..., None], v_new)
        if not with_out:
            return S_new, None
        o = jnp.einsum('bhld,bhde->bhle', qc * jnp.exp(G)[..., None], S) + jnp.einsum(
            'bhts,bhse->bhte', jnp.einsum('bhtd,bhsd->bhts', qc, kc) * decay, v_new)
        return S_new, o

    xs = (to_chunks(q), to_chunks(k), to_chunks(v), to_chunks(g), to_chunks(beta))
    final, os_ = lax.scan(step, state, xs)
    return final, (from_chunks(os_) if with_out else None)


def gdn_mixer(parts_x, parts_c, conv_w, a_log, dt_bias, out_g, need_ctx):
    dtype = parts_x[0].dtype

    def prep(parts):
        q, k, v, z, a, bt = parts
        b, n, _ = q.shape
        qkv = jax.nn.silu(short_conv(jnp.concatenate([q, k, v], axis=-1), conv_w)).astype(jnp.float32)
        q, k, v = jnp.split(qkv, 3, axis=-1)
        hd = lambda t: t.reshape(b, n, GDN_HEADS, HEAD_DIM)
        q = l2_norm(hd(q)) * HEAD_DIM ** -0.5
        k = l2_norm(hd(k))
        g = -jnp.exp(a_log.astype(jnp.float32)) * jax.nn.softplus(
            a.astype(jnp.float32).reshape(b, n, N_DIR, GDN_HEADS) + dt_bias.astype(jnp.float32))
        beta = jax.nn.sigmoid(bt.astype(jnp.float32).reshape(b, n, N_DIR, GDN_HEADS))
        return q, k, hd(v), z, g, beta

    qx, kx, vx, zx, gx, bx = prep(parts_x)
    qc, kc, vc, zc, gc, bc = prep(parts_c)
    b = qx.shape[0]
    zero = jnp.zeros((b, GDN_HEADS, HEAD_DIM, HEAD_DIM), jnp.float32)
    ox_sum, oc_sum = 0.0, 0.0
    for d in range(N_DIR):
        rv = d == 1
        st, oc = gdn_scan(flip_seq(qc, rv), flip_seq(kc, rv), flip_seq(vc, rv),
                          flip_seq(gc[:, :, d], rv), flip_seq(bc[:, :, d], rv), zero, need_ctx)
        _, ox = gdn_scan(flip_seq(qx, rv), flip_seq(kx, rv), flip_seq(vx, rv),
                         flip_seq(gx[:, :, d], rv), flip_seq(bx[:, :, d], rv), st, True)
        ox_sum = ox_sum + flip_seq(ox, rv)
        if need_ctx:
            oc_sum = oc_sum + flip_seq(oc, rv)

    def finish(o, z):
        bb, nn = o.shape[:2]
        return (rms_norm(o, out_g).reshape(bb, nn, GDN_W) * jax.nn.silu(z.astype(jnp.float32))).astype(dtype)

    return finish(ox_sum, zx), (finish(oc_sum, zc) if need_ctx else None)


def expert_choice_ffn(h, router_w, w1, w3, w2):
    b, n, _ = h.shape
    cap = CAPACITY_FACTOR * n // N_EXPERTS
    aff = jax.nn.softmax((h @ router_w).astype(jnp.float32), axis=-1)
    vals, idx = lax.top_k(jnp.swapaxes(aff, 1, 2), cap)
    bidx = jnp.arange(b)[:, None, None]
    xg = h[bidx, idx]
    hid = jax.nn.silu(jnp.einsum('becd,edf->becf', xg, w1)) * jnp.einsum('becd,edf->becf', xg, w3)
    y = jnp.einsum('becf,efd->becd', hid, w2) * vals[..., None].astype(h.dtype)
    return jnp.zeros_like(h).at[bidx, idx].add(y)


def setup_inputs(seed: int = 0) -> dict:
    key = jax.random.key(seed)
    ks = jax.random.split(key, 26)
    f32 = jnp.float32
    L, D, E, F = DEPTH, D_MODEL, N_EXPERTS, EXPERT_FF
    nrm = lambda k, shape, s: jax.random.normal(k, shape, f32) * s
    gain = lambda k, shape: 1.0 + 0.02 * jax.random.normal(k, shape, f32)
    dt = jnp.exp(jax.random.uniform(ks[14], (L, N_DIR, GDN_HEADS), f32, math.log(1e-3), math.log(1e-1)))
    return {
        'x': nrm(ks[0], (BATCH, SEQ, D), 1.0),
        'c': nrm(ks[1], (BATCH, D), 1.0),
        'ctx': nrm(ks[2], (BATCH, CTX_LEN, D), 1.0),
        'c_ctx': nrm(ks[3], (D,), 1.0),
        'mod_w': nrm(ks[4], (L, D, N_MOD * D), 0.5 * D ** -0.5),
        'mod_b': nrm(ks[5], (L, N_MOD * D), 0.02),
        'norm1_g': gain(ks[6], (L, D)),
        'w_in': nrm(ks[7], (L, D, IN_W), D ** -0.5),
        'q_norm_g': gain(ks[8], (L, HEAD_DIM)),
        'k_norm_g': gain(ks[9], (L, HEAD_DIM)),
        'mlstm_i_bias': nrm(ks[10], (L, N_DIR, MLSTM_HEADS), 0.1),
        'mlstm_f_bias': jnp.linspace(3.0, 6.0, MLSTM_HEADS, dtype=f32) + nrm(ks[11], (L, N_DIR, MLSTM_HEADS), 0.1),
        'mlstm_out_g': gain(ks[12], (L, MLSTM_HEADS, HEAD_DIM)),
        'gdn_conv_w': nrm(ks[13], (L, CONV_K, 3 * GDN_W), CONV_K ** -0.5),
        'gdn_a_log': jnp.log(jax.random.uniform(ks[15], (L, N_DIR, GDN_HEADS), f32, 1.0, 16.0)),
        'gdn_dt_bias': dt + jnp.log(-jnp.expm1(-dt)),
        'gdn_out_g': gain(ks[16], (L, HEAD_DIM)),
        'w_out': nrm(ks[17], (L, MIX_W, D), MIX_W ** -0.5),
        'norm2_g': gain(ks[18], (L, D)),
        'router_w': nrm(ks[19], (L, D, E), D ** -0.5),
        'w1': nrm(ks[20], (L, E, D, F), D ** -0.5),
        'w3': nrm(ks[21], (L, E, D, F), D ** -0.5),
        'w2': nrm(ks[22], (L, E, F, D), F ** -0.5),
    }


def reference(x, c, ctx, c_ctx, mod_w, mod_b, norm1_g, w_in, q_norm_g, k_norm_g,
              mlstm_i_bias, mlstm_f_bias, mlstm_out_g, gdn_conv_w, gdn_a_log, gdn_dt_bias,
              gdn_out_g, w_out, norm2_g, router_w, w1, w3, w2):
    xc = ctx
    for l in range(DEPTH):
        need_ctx = l < DEPTH - 1
        mod = (jax.nn.silu(c) @ mod_w[l] + mod_b[l])[:, None, :]
        modc = jax.nn.silu(c_ctx) @ mod_w[l] + mod_b[l]
        sh1, sc1, g1, sh2, sc2, g2 = jnp.split(mod, N_MOD, axis=-1)
        sh1c, sc1c, g1c, sh2c, sc2c, g2c = jnp.split(modc, N_MOD, axis=-1)

        px = split_cols(modulate(rms_norm(x, norm1_g[l]), sh1, sc1) @ w_in[l])
        pc = split_cols(modulate(rms_norm(xc, norm1_g[l]), sh1c, sc1c) @ w_in[l])
        ax, ac = attention_mixer(px[0:3], pc[0:3], q_norm_g[l], k_norm_g[l], need_ctx)
        mx, mc = mlstm_mixer(px[3:9], pc[3:9], mlstm_i_bias[l], mlstm_f_bias[l], mlstm_out_g[l], need_ctx)
        gx, gc = gdn_mixer(px[9:15], pc[9:15], gdn_conv_w[l], gdn_a_log[l], gdn_dt_bias[l], gdn_out_g[l], need_ctx)

        x = x + g1 * (jnp.concatenate([ax, mx, gx], axis=-1) @ w_out[l])
        x = x + g2 * expert_choice_ffn(modulate(rms_norm(x, norm2_g[l]), sh2, sc2),
                                       router_w[l], w1[l], w3[l], w2[l])
        if need_ctx:
            xc = xc + g1c * (jnp.concatenate([ac, mc, gc], axis=-1) @ w_out[l])
            xc = xc + g2c * expert_choice_ffn(modulate(rms_norm(xc, norm2_g[l]), sh2c, sc2c),
                                              router_w[l], w1[l], w3[l], w2[l])
    return x
```

```python
import numpy as np
from contextlib import ExitStack
import concourse.bass as bass
import concourse.mybir as mybir
from concourse.bass_utils import run_bass_kernel_spmd

F32 = mybir.dt.float32
I32 = mybir.dt.int32
U32 = mybir.dt.uint32
ALU = mybir.AluOpType
AF = mybir.ActivationFunctionType
AX = mybir.AxisListType
EPS = 1e-6


class T:
    def __init__(self, k, ap, name):
        self.k = k; self.ap = ap; self.name = name
        self.w = None; self.r = {}
        self.dsem = {}; self.dn = 0; self.is_dram = False

    def __getitem__(self, idx):
        return self.ap[idx]

    def rearrange(self, *a, **kw):
        return self.ap.rearrange(*a, **kw)


class V:
    def __init__(s, t, a0, a1):
        s.t = t; s.base = t; s.a0 = a0; s.a1 = a1

    def __getitem__(s, idx):
        return s.t.ap[:, s.a0:s.a1][idx]


class K:
    def __init__(self):
        self.nc = bass.Bass("TRN2", target_bir_lowering=False)
        self.es = ExitStack()
        self.st = None
        nc = self.nc
        self.eng = {"pe": nc.tensor, "act": nc.scalar, "dve": nc.vector, "pool": nc.gpsimd, "sp": nc.sync}
        self.sem = {}; self.cnt = {}
        for e in ["pe", "act", "dve", "pool"]:
            self.sem[e] = self.es.enter_context(nc.semaphore("s_" + e)); self.cnt[e] = 0
        self.seen = {e: {} for e in self.eng}
        self.ntile = 0
        self.dsems = {}
        self.free_sems = {'sp': [], 'pool': []}
        self.stage_tiles = []
        self.nsem = 0

    def dram_in(self, name, shape, dt=F32):
        return self.nc.dram_tensor(name, list(shape), dt, kind="ExternalInput").ap()

    def dram_out(self, name, shape, dt=F32):
        t = T(self, self.nc.dram_tensor(name, list(shape), dt, kind="ExternalOutput").ap(), name); t.is_dram = True
        return t

    def dram_tmp(self, name, shape, dt=F32):
        t = T(self, self.nc.dram_tensor(name, list(shape), dt, kind="Internal").ap(), name); t.is_dram = True
        return t

    def sb(self, shape, dt=F32, name=None):
        self.ntile += 1
        name = (name or "t") + f"_{self.ntile}"
        stk = self.st if self.st is not None else self.es
        h = stk.enter_context(self.nc.sbuf_tensor("S_" + name, list(shape), dt))
        t = T(self, h[:], name)
        if self.st is not None:
            self.stage_tiles.append(t)
        return t

    def ps(self, shape, dt=F32, name=None):
        self.ntile += 1
        name = (name or "p") + f"_{self.ntile}"
        h = self.es.enter_context(self.nc.psum_tensor("P_" + name, list(shape), dt))
        return T(self, h[:], name)

    def stage_begin(self):
        assert self.st is None
        self.st = ExitStack(); self.stage_tiles = []

    def stage_end(self):
        self.barrier()
        for t in self.stage_tiles:
            for q_, sm_ in t.dsem.items():
                self.free_sems[q_].append(sm_)
        self.st.close(); self.st = None; self.stage_tiles = []

    def barrier(self):
        toks = [(self.sem[c], self.cnt[c]) for c in self.sem if self.cnt[c] > 0]
        toks += [(s, 16 * n) for (s, n) in self.dsems.values() if n > 0]
        for e in self.eng:
            self._wait(e, toks)

    def _get_dsem(self, t, q):
        if q not in t.dsem:
            if self.free_sems[q]:
                t.dsem[q] = self.free_sems[q].pop()
            else:
                self.nsem += 1
                sm_ = self.es.enter_context(self.nc.semaphore(f"d{self.nsem}{q}"))
                t.dsem[q] = sm_
                self.dsems[id(sm_)] = [sm_, 0]
        return t.dsem[q]

    def _deps(self, reads, writes):
        toks = []
        for t in reads:
            if t.w is not None:
                toks.append(t.w)
        for t in writes:
            if t.w is not None:
                toks.append(t.w)
            toks.extend(t.r.values())
        return toks

    def _wait(self, e, toks, skip_sem=None):
        eng = self.eng[e]
        best = {}
        for (s, v) in toks:
            if skip_sem is not None and s is skip_sem:
                continue
            if best.get(id(s), (None, 0))[1] < v:
                best[id(s)] = (s, v)
        for (s, v) in best.values():
            if self.seen[e].get(id(s), 0) < v:
                eng.wait_ge(s, v)
                self.seen[e][id(s)] = v

    def _mark(self, tok, reads, writes):
        for t in reads:
            t.r[id(tok[0])] = tok
        for t in writes:
            t.w = tok; t.r = {}

    def op(self, e, fn, reads, writes):
        reads = [getattr(t, "base", t) for t in reads if t is not None]
        writes = [getattr(t, "base", t) for t in writes]
        toks = self._deps(reads, writes)
        self._wait(e, toks, skip_sem=self.sem[e] if e == "pe" else None)
        ins = fn(self.eng[e])
        self.cnt[e] += 1
        ins.then_inc(self.sem[e], 1)
        tok = (self.sem[e], self.cnt[e])
        self._mark(tok, reads, writes)
        return tok

    def dma(self, out_t, out_ap, in_t, in_ap, q="sp", skip_w=False, indirect=None, **kw):
        reads = [in_t] if in_t is not None else []
        if indirect is not None and indirect.get("idx_t") is not None:
            reads.append(indirect["idx_t"])
        writes = [out_t] if out_t is not None else []
        toks = self._deps(reads, [] if skip_w else writes)
        self._wait(q, toks)
        if out_t is not None and not out_t.is_dram:
            owner = out_t
        elif in_t is not None and not in_t.is_dram:
            owner = in_t
        else:
            owner = out_t if out_t is not None else in_t
        sem = self._get_dsem(owner, q)
        ent = self.dsems[id(sem)]
        ent[1] += 1
        if indirect is None:
            ins = self.eng[q].dma_start(out=out_ap, in_=in_ap, **kw)
        else:
            ins = self.eng[q].indirect_dma_start(out=out_ap, out_offset=indirect.get("out_offset"), in_=in_ap,
                                                 in_offset=indirect.get("in_offset"), **kw)
        ins.then_inc(sem, 16)
        tok = (sem, 16 * ent[1])
        self._mark(tok, reads, writes)
        return tok

    def finish(self, out_tiles):
        self.barrier()

    def close(self):
        if self.st is not None:
            self.st.close()
        self.es.close()


CUM = [0, 512, 640, 768, 1024, 1280, 1536, 1792, 1800, 1808, 2064, 2320, 2576, 2832, 2840, 2848]
import os
T_ = int(os.environ.get('FZ_T', '8448')); NT_ = T_ // 128; NC_ = T_ // 64; NCTX = 256

OUT6 = dict(Qs=(0, 128), Ks=(128, 256), eb=(256, 258), sigo=(258, 322), qn=(322, 386), kn=(386, 450), kb=(450, 578),
            Rm=(578, 834), Qg=(834, 962), Kd=(962, 1090), G=(1090, 1092), eG=(1092, 1094), siluz=(1094, 1158), gv=(1158, 1222),
            ebl=(1222, 1224), eGl=(1224, 1226))
W6 = 1226
S8 = dict(QsT=(0, 64), WT=(64, 128), QgT=(128, 192), Ks=(192, 256), V1=(256, 321), AT=(321, 385), U=(385, 449), Kd=(449, 513),
          AqkT=(513, 577), sm=(577, 578), sg=(578, 579))
W8 = 579


def bcast_rows(k, PS, dst, src2, r, sel, n=1024):
    for nb in range(0, n, 512):
        ps = PS[(nb // 512) % 2]
        k.op("pe", lambda e: e.matmul(ps[:, 0:512], sel[:, r, :], src2[:, nb:nb+512], start=True, stop=True), [sel, src2], [ps])
        k.op("dve", lambda e: e.tensor_copy(dst[:, nb:nb+512], ps[:, 0:512]), [ps], [dst])


def e_mod(k, PS, ctsb, MODW, MODB, modrow):
    k.stage_begin()
    ws = [k.sb([128, 8, 512], name="mw") for _ in range(2)]
    bi = k.sb([2, 6144], name="mb")
    k.dma(bi, bi[:], None, MODB[:, :])
    for nb in range(12):
        w = ws[nb % 2]; ps = PS[nb % 2]
        k.dma(w, w[:], None, MODW.rearrange("(c p) n -> p c n", p=128)[:, :, nb*512:(nb+1)*512], q="sp" if nb % 2 == 0 else "pool")
        for c in range(8):
            k.op("pe", lambda e: e.matmul(ps[0:2, 0:512], ctsb[:, c, :], w[:, c, :], start=(c == 0), stop=(c == 7)), [ctsb, w], [ps])
        k.op("dve", lambda e: e.tensor_add(out=modrow[:, nb*512:(nb+1)*512], in0=ps[0:2, 0:512], in1=bi[:, nb*512:(nb+1)*512]), [ps, bi], [modrow])
    k.stage_end()


def e_normlin(k, PS, X, GREP, modrow, isc, ish, sel, ident, W, N, Y, H=None, softmax=False, YT=None, QKT=None, qk=None):
    k.stage_begin()
    g = k.sb([128, 1024], name="g"); k.dma(g, g[:], None, GREP[:, :])
    A = [k.sb([128, 1024], name="A") for _ in range(2)]; sh = [k.sb([128, 1024], name="sh") for _ in range(2)]
    for cls in range(2):
        r = 1 if cls == 0 else 0
        bcast_rows(k, PS, A[cls], V(modrow, isc*1024, (isc+1)*1024), r, sel)
        bcast_rows(k, PS, sh[cls], V(modrow, ish*1024, (ish+1)*1024), r, sel)
        k.op("dve", lambda e: e.scalar_tensor_tensor(out=A[cls][:], in0=A[cls][:], scalar=1.0, in1=g[:], op0=ALU.add, op1=ALU.mult), [A[cls], g], [A[cls]])
    w = k.sb([128, 8, N], name="w")
    for c in range(8):
        k.dma(w, w[:, c, :], None, W[c*128:(c+1)*128, :], q="sp" if c % 2 == 0 else "pool")
    xs = [k.sb([128, 1024], name="x") for _ in range(2)]; hs = [k.sb([128, 1024], name="h") for _ in range(2)]
    hT = [k.sb([128, 8, 128], name="hT") for _ in range(2)]
    ys = [k.sb([128, N], name="y") for _ in range(2)]
    st = [k.sb([128, 40], name="st") for _ in range(2)]
    if qk is not None:
        gn = k.sb([128, 640], name="gn"); k.dma(gn, gn[:], None, qk["GAIN"][:, :])
        QB = [dict(cs=k.sb([128, 320], name="cs"), sn=k.sb([128, 320], name="sn"), sq=k.sb([128, 640], name="sq"), o=k.sb([128, 640], name="o"),
                   t1=k.sb([128, 320], name="t1"), t2=k.sb([128, 320], name="t2"), oT=k.sb([128, 5, 128], name="oT")) for _ in range(1)]
        v5 = lambda ap: ap.rearrange("p (h a f j) -> p h a f j", h=10, a=2, f=2)
        v4 = lambda ap: ap.rearrange("p (h a j) -> p h a j", h=10, a=2)
    if YT is not None:
        yts = [k.sb([16, 128], name="yt") for _ in range(2)]
    nb_ = (N + 511) // 512
    pi = 0
    for rt in range(T_ // 128):
        cls = 0 if rt < 2 else 1
        x = xs[rt % 2]; h = hs[rt % 2]; s = st[rt % 2]; ht = hT[rt % 2]; y = ys[rt % 2]
        rows = slice(rt*128, (rt+1)*128)
        k.dma(x, x[:], X, X[rows, :])
        k.op("dve", lambda e: e.memset(s[:, 0:8], 0.0), [], [s])
        k.op("act", lambda e: e.activation(out=h[:], in_=x[:], func=AF.Square, accum_out=s[:, 0:1]), [x, s], [h, s])
        k.op("dve", lambda e: e.tensor_scalar(out=s[:, 1:2], in0=s[:, 0:1], scalar1=1.0/1024, scalar2=EPS, op0=ALU.mult, op1=ALU.add), [s], [s])
        k.op("act", lambda e: e.activation(out=s[:, 7:8], in_=s[:, 1:2], func=AF.Sqrt), [s], [s])
        k.op("dve", lambda e: e.reciprocal(out=s[:, 2:3], in_=s[:, 7:8]), [s], [s])
        k.op("dve", lambda e: e.scalar_tensor_tensor(out=h[:], in0=x[:], scalar=s[:, 2:3], in1=A[cls][:], op0=ALU.mult, op1=ALU.mult), [x, s, A[cls]], [h])
        k.op("pool", lambda e: e.tensor_add(out=h[:], in0=h[:], in1=sh[cls][:]), [h, sh[cls]], [h])
        if H is not None:
            k.dma(H, H[rows, :], h, h[:], q="pool", skip_w=True)
        for half in range(2):
            pt = PS[half]
            for c in range(4):
                cc = half*4 + c
                k.op("pe", lambda e: e.transpose(pt[:, c*128:(c+1)*128], h[:, cc*128:(cc+1)*128], ident[:]), [h, ident], [pt])
            if half == 0:
                k.op("act", lambda e: e.copy(ht[:, 0:4, :], pt[:].rearrange("p (c t) -> p c t", c=4)), [pt], [ht])
            else:
                k.op("dve", lambda e: e.tensor_copy(ht[:, 4:8, :], pt[:].rearrange("p (c t) -> p c t", c=4)), [pt], [ht])
        for b in range(nb_):
            n0 = b*512; n1 = min(N, n0+512); ps = PS[2 + pi % 4]; pi += 1
            for c in range(8):
                k.op("pe", lambda e: e.matmul(ps[:, 0:n1-n0], ht[:, c, :], w[:, c, n0:n1], start=(c == 0), stop=(c == 7)), [ht, w], [ps])
            if b % 2 == 0:
                k.op("dve", lambda e: e.tensor_copy(y[:, n0:n1], ps[:, 0:n1-n0]), [ps], [y])
            else:
                k.op("act", lambda e: e.copy(y[:, n0:n1], ps[:, 0:n1-n0]), [ps], [y])
        if softmax:
            k.op("dve", lambda e: e.reduce_max(out=s[:, 3:4], in_=y[:], axis=AX.X), [y], [s])
            k.op("dve", lambda e: e.tensor_scalar(out=s[:, 4:5], in0=s[:, 3:4], scalar1=-1.0, scalar2=None, op0=ALU.mult), [s], [s])
            k.op("dve", lambda e: e.memset(s[:, 5:6], 0.0), [], [s])
            k.op("act", lambda e: e.activation(out=y[:], in_=y[:], func=AF.Exp, bias=s[:, 4:5], scale=1.0, accum_out=s[:, 5:6]), [y, s], [y, s])
            k.op("dve", lambda e: e.reciprocal(out=s[:, 6:7], in_=s[:, 5:6]), [s], [s])
            k.op("dve", lambda e: e.tensor_scalar(out=y[:], in0=y[:], scalar1=s[:, 6:7], scalar2=None, op0=ALU.mult), [y, s], [y])
        if YT is not None:
            yt = yts[rt % 2]; pt = PS[6]
            k.op("pe", lambda e: e.transpose(pt[0:16, 0:128], y[:, 0:16], ident[:]), [y, ident], [pt])
            k.op("dve", lambda e: e.tensor_copy(yt[:], pt[0:16, 0:128]), [pt], [yt])
            k.dma(YT, YT[:, rows], yt, yt[:], q="pool", skip_w=True)
        if Y is not None:
            k.dma(Y, Y[rows, :], y, y[:], skip_w=True)
        if qk is not None:
            b = QB[0]; o = b['o']
            k.dma(b['cs'], b['cs'][:], None, qk["CS"][rows, :], q="pool"); k.dma(b['sn'], b['sn'][:], None, qk["SN"][rows, :], q="pool")
            xq = V(y, 0, 640)
            k.op("dve", lambda e: e.tensor_mul(out=b['sq'][:], in0=xq[:], in1=xq[:]), [y], [b['sq']])
            k.op("dve", lambda e: e.reduce_sum(out=s[:, 8:18], in_=b['sq'][:].rearrange("p (h d) -> p h d", h=10), axis=AX.X), [b['sq']], [s])
            k.op("dve", lambda e: e.tensor_scalar(out=s[:, 8:18], in0=s[:, 8:18], scalar1=1.0/64, scalar2=EPS, op0=ALU.mult, op1=ALU.add), [s], [s])
            k.op("act", lambda e: e.activation(out=s[:, 18:28], in_=s[:, 8:18], func=AF.Sqrt), [s], [s])
            k.op("dve", lambda e: e.reciprocal(out=s[:, 28:38], in_=s[:, 18:28]), [s], [s])
            for hh in range(10):
                k.op("dve" if hh % 2 == 0 else "pool", lambda e: e.tensor_scalar(out=b['sq'][:, hh*64:(hh+1)*64], in0=xq[:, hh*64:(hh+1)*64], scalar1=s[:, 28+hh:29+hh], scalar2=None, op0=ALU.mult), [y, s], [b['sq']])
            xn = b['sq']
            k.op("dve", lambda e: e.tensor_mul(out=xn[:], in0=xn[:], in1=gn[:]), [xn, gn], [xn])
            x1 = v5(xn[:])[:, :, :, 0, :]; x2 = v5(xn[:])[:, :, :, 1, :]
            o1 = v5(o[:])[:, :, :, 0, :]; o2 = v5(o[:])[:, :, :, 1, :]
            cs = v4(b['cs'][:]); sn = v4(b['sn'][:]); t1 = v4(b['t1'][:]); t2 = v4(b['t2'][:])
            k.op("dve", lambda e: e.tensor_mul(out=t1, in0=x1, in1=cs), [xn, b['cs']], [b['t1']])
            k.op("pool", lambda e: e.tensor_mul(out=t2, in0=x2, in1=sn), [xn, b['sn']], [b['t2']])
            k.op("dve", lambda e: e.tensor_sub(out=o1, in0=t1, in1=t2), [b['t1'], b['t2']], [o])
            k.op("dve", lambda e: e.tensor_mul(out=t1, in0=x2, in1=cs), [xn, b['cs']], [b['t1']])
            k.op("pool", lambda e: e.tensor_mul(out=t2, in0=x1, in1=sn), [xn, b['sn']], [b['t2']])
            k.op("dve", lambda e: e.tensor_add(out=o2, in0=t1, in1=t2), [b['t1'], b['t2']], [o])
            pt = PS[7]
            for c in range(4):
                k.op("pe", lambda e: e.transpose(pt[:, c*128:(c+1)*128], o[:, c*128:(c+1)*128], ident[:]), [o, ident], [pt])
            k.op("act", lambda e: e.copy(b['oT'][:, 0:4, :], pt[:].rearrange("p (c t) -> p c t", c=4)), [pt], [b['oT']])
            pt2 = PS[6]
            k.op("pe", lambda e: e.transpose(pt2[:, 0:128], o[:, 512:640], ident[:]), [o, ident], [pt2])
            k.op("dve", lambda e: e.tensor_copy(b['oT'][:, 4, :], pt2[:, 0:128]), [pt2], [b['oT']])
            k.dma(QKT, QKT.ap.rearrange("(c p) t -> p c t", p=128)[:, :, rows], b['oT'], b['oT'][:], q="pool", skip_w=True)
    k.stage_end()


def e_attn(k, PS, QKT, P, AXD):
    k.stage_begin()
    T = T_; NT = NT_
    kts = [k.sb([64, T], name="kt") for _ in range(2)]
    v1s = [k.sb([128, NT, 65], name="v1") for _ in range(2)]
    for g in range(2):
        k.dma(kts[g], kts[g][:], QKT, QKT[512 + g*64:512 + (g+1)*64, :])
        k.op("dve", lambda e: e.memset(v1s[g][:, :, 64:65], 1.0), [], [v1s[g]])
        k.dma(v1s[g], v1s[g][:, :, 0:64], P, P.ap.rearrange("(n p) c -> p n c", p=128)[:, :, 640 + g*64:640 + (g+1)*64], q="pool")
    qts = [k.sb([64, 512], name="q") for _ in range(2)]
    pts = [k.sb([128, 512], name="pt") for _ in range(3)]
    osb = [k.sb([128, 4, 64], name="osb") for _ in range(2)]
    rs = [k.sb([128, 8], name="rs") for _ in range(2)]
    pss = PS[0:2]; pso = PS[2:6]
    blocks = [(0, NCTX, 0, NCTX // 128)] + [(q0, 512, 0, NT) for q0 in range(NCTX, T, 512)]
    it = 0; bi = 0
    for h in range(8):
        g = h // 4; kt = kts[g]; v1 = v1s[g]
        for (q0, qn, k0, k1) in blocks:
            qt = qts[bi % 2]; ob = osb[bi % 2]; r = rs[bi % 2]; bi += 1
            nqs = qn // 128
            k.dma(qt, qt[:, 0:qn], QKT, QKT[h*64:(h+1)*64, q0:q0+qn])
            for kk in range(k0, k1):
                ps = pss[it % 2]; pt = pts[it % 3]; it += 1
                k.op("pe", lambda e: e.matmul(ps[:, 0:qn], kt[:, kk*128:(kk+1)*128], qt[:, 0:qn], start=True, stop=True), [kt, qt], [ps])
                k.op("act", lambda e: e.activation(out=pt[:, 0:qn], in_=ps[:, 0:qn], func=AF.Exp, scale=0.125), [ps], [pt])
                for qs in range(nqs):
                    k.op("pe", lambda e: e.matmul(pso[qs][:, 0:65], pt[:, qs*128:(qs+1)*128], v1[:, kk, :], start=(kk == k0), stop=(kk == k1-1)), [pt, v1], [pso[qs]])
            for qs in range(nqs):
                k.op("dve", lambda e: e.reciprocal(out=r[:, qs:qs+1], in_=pso[qs][:, 64:65]), [pso[qs]], [r])
                k.op("dve", lambda e: e.tensor_scalar(out=ob[:, qs, :], in0=pso[qs][:, 0:64], scalar1=r[:, qs:qs+1], scalar2=None, op0=ALU.mult), [pso[qs], r], [ob])
            k.dma(AXD, AXD[q0:q0+qn, h*64:(h+1)*64].rearrange("(s p) d -> p s d", p=128), ob, ob[:, 0:nqs, :], q="pool", skip_w=True)
    k.stage_end()


O6 = dict(Qs=(0, 128), sigo=(128, 192), qn=(192, 256), kn=(256, 320), kb=(320, 448), Rm=(448, 704), Qg=(704, 832), G=(832, 834),
          siluz=(834, 898), KG0=(898, 1028), KG1=(1028, 1158))
W6 = 1158
S8 = dict(QsT=(0, 64), WT=(64, 128), QgT=(128, 192), Ks=(192, 256), Kd=(256, 320), sm=(320, 321), sg=(321, 322), V1=(322, 387),
          AT=(387, 451), U=(451, 515), AqkT=(515, 579))
W8 = 579
FM_SRC = [(256, 320), (192, 256), (320, 384), (384, 448), (0, 64), (64, 128), None, None, (704, 768), (768, 832)]


def e_prep(k, PS, P, CB, CW, cc, ident, O6D, FMD):
    k.stage_begin()
    tf = cc[:, 2, :]; tr = cc[:, 3, :]; blk = cc[:, 8, :]
    cb = k.sb([128, 48], name="cb"); cw = k.sb([128, 5, 768], name="cw")
    k.dma(cb, cb[:, 0:32], None, CB[:, :]); k.dma(cw, cw[:], None, CW[:, :, :])
    k.op("act", lambda e: e.activation(out=cb[:, 32:40], in_=cb[:, 16:24], func=AF.Exp), [cb], [cb])
    k.op("dve", lambda e: e.tensor_scalar(out=cb[:, 32:40], in0=cb[:, 32:40], scalar1=-1.0, scalar2=None, op0=ALU.mult), [cb], [cb])
    NB = 2
    m4s = [k.sb([128, 1024], name="m4") for _ in range(NB)]; g4s = [k.sb([128, 1024], name="g4") for _ in range(NB)]
    mgs = [k.sb([128, 16], name="mg") for _ in range(NB)]; ggs = [k.sb([128, 16], name="gg") for _ in range(NB)]
    q5s = [k.sb([128, 5, 768], name="q5") for _ in range(NB)]
    cvs = [k.sb([128, 768], name="cv") for _ in range(NB)]; sqs = [k.sb([128, 512], name="sq") for _ in range(NB)]
    wks = [k.sb([128, 128], name="wk") for _ in range(NB)]
    outs = [[k.sb([128, W6], name="o6") for _ in range(4)] for _ in range(NB)]
    fmts = [[k.sb([64, 10, 128], name="fmt") for _ in range(4)] for _ in range(NB)]
    ksb = [[k.sb([128, 128], name="ksb") for _ in range(4)] for _ in range(NB)]
    psg = PS[0]
    Pt = P.rearrange("(n p) c -> n p c", p=128)
    segs = [(0, NCTX), (NCTX, T_)]
    for t in range(NT_):
        i = t % NB
        m4 = m4s[i]; g4 = g4s[i]; mg = mgs[i]; gg = ggs[i]; q5 = q5s[i]; c = cvs[i]; s2 = sqs[i]; s = wks[i]
        r0 = t*128
        k.dma(m4, m4[:], P, P[r0:r0+128, 768:1792]); k.dma(g4, g4[:], P, P[r0:r0+128, 1808:2832], q="pool")
        k.dma(mg, mg[:], P, P[r0:r0+128, 1792:1808]); k.dma(gg, gg[:], P, P[r0:r0+128, 2832:2848], q="pool")
        seg = segs[0] if r0 < NCTX else segs[1]
        edge = (r0 - 2 < seg[0]) or (r0 + 130 > seg[1])
        if edge:
            k.op("pool", lambda e: e.memset(q5[:], 0.0), [], [q5])
        for kk in range(5):
            a0 = r0 + kk - 2; a1 = a0 + 128
            lo = max(a0, seg[0]); hi = min(a1, seg[1])
            k.dma(q5, q5[lo-a0:hi-a0, kk, :], P, P[lo:hi, 1808:2576], q="sp" if kk % 2 == 0 else "pool")
        k.op("dve", lambda e: e.tensor_mul(out=q5[:], in0=q5[:], in1=cw[:]), [q5, cw], [q5])
        k.op("dve", lambda e: e.reduce_sum(out=c[:], in_=q5[:].rearrange("p k c -> p c k"), axis=AX.X), [q5], [c])
        k.op("act", lambda e: e.activation(out=c[:], in_=c[:], func=AF.Silu), [c], [c])
        k.op("dve", lambda e: e.tensor_mul(out=s2[:], in0=c[:, 0:512], in1=c[:, 0:512]), [c], [s2])
        k.op("dve", lambda e: e.reduce_sum(out=s[:, 64:72], in_=s2[:].rearrange("p (h d) -> p h d", h=8), axis=AX.X), [s2], [s])
        k.op("dve", lambda e: e.tensor_scalar(out=s[:, 64:72], in0=s[:, 64:72], scalar1=EPS, scalar2=None, op0=ALU.add), [s], [s])
        k.op("act", lambda e: e.activation(out=s[:, 72:80], in_=s[:, 64:72], func=AF.Sqrt), [s], [s])
        k.op("dve", lambda e: e.reciprocal(out=s[:, 80:88], in_=s[:, 72:80]), [s], [s])
        k.op("dve", lambda e: e.tensor_add(out=s[:, 0:8], in0=mg[:, 8:16], in1=cb[:, 8:16]), [mg, cb], [s])
        k.op("act", lambda e: e.activation(out=s[:, 8:16], in_=s[:, 0:8], func=AF.Exp, scale=-1.0), [s], [s])
        k.op("act", lambda e: e.activation(out=s[:, 8:16], in_=s[:, 8:16], func=AF.Ln, bias=1.0, scale=1.0), [s], [s])
        k.op("dve", lambda e: e.tensor_scalar(out=s[:, 8:16], in0=s[:, 8:16], scalar1=-1.0, scalar2=None, op0=ALU.mult), [s], [s])
        k.op("pe", lambda e: e.matmul(psg[:, 0:4], tf, s[:, 8:12], start=True, stop=True), [cc, s], [psg])
        k.op("pe", lambda e: e.matmul(psg[:, 4:8], tr, s[:, 12:16], start=True, stop=True), [cc, s], [psg])
        k.op("pe", lambda e: e.matmul(psg[:, 8:16], blk, s[:, 8:16], start=True, stop=True), [cc, s], [psg])
        k.op("dve", lambda e: e.tensor_copy(s[:, 16:24], psg[:, 0:8]), [psg], [s])
        k.op("act", lambda e: e.activation(out=s[:, 48:56], in_=psg[:, 8:16], func=AF.Exp), [psg], [s])
        k.op("act", lambda e: e.activation(out=s[:, 40:48], in_=s[:, 16:24], func=AF.Exp), [s], [s])
        k.op("dve", lambda e: e.tensor_add(out=s[:, 24:32], in0=mg[:, 0:8], in1=cb[:, 0:8]), [mg, cb], [s])
        k.op("dve", lambda e: e.tensor_sub(out=s[:, 24:32], in0=s[:, 24:32], in1=s[:, 16:24]), [s], [s])
        k.op("act", lambda e: e.activation(out=s[:, 32:40], in_=s[:, 24:32], func=AF.Exp), [s], [s])
        k.op("act", lambda e: e.activation(out=s[:, 88:96], in_=gg[:, 8:16], func=AF.Sigmoid), [gg], [s])
        k.op("dve", lambda e: e.tensor_add(out=s[:, 96:104], in0=gg[:, 0:8], in1=cb[:, 24:32]), [gg, cb], [s])
        k.op("act", lambda e: e.activation(out=s[:, 96:104], in_=s[:, 96:104], func=AF.Exp), [s], [s])
        k.op("act", lambda e: e.activation(out=s[:, 96:104], in_=s[:, 96:104], func=AF.Ln, bias=1.0, scale=1.0), [s], [s])
        k.op("dve", lambda e: e.tensor_mul(out=s[:, 104:112], in0=s[:, 96:104], in1=cb[:, 32:40]), [s, cb], [s])
        k.op("pe", lambda e: e.matmul(psg[:, 16:24], tf, s[:, 104:112], start=True, stop=True), [cc, s], [psg])
        k.op("pe", lambda e: e.matmul(psg[:, 24:32], tr, s[:, 104:112], start=True, stop=True), [cc, s], [psg])
        k.op("pe", lambda e: e.matmul(psg[:, 32:40], blk, s[:, 104:112], start=True, stop=True), [cc, s], [psg])
        k.op("dve", lambda e: e.tensor_copy(s[:, 112:116], psg[:, 16:20]), [psg], [s])
        k.op("dve", lambda e: e.tensor_copy(s[:, 116:120], psg[:, 28:32]), [psg], [s])
        k.op("dve", lambda e: e.tensor_sub(out=s[:, 120:124], in0=psg[:, 24:28], in1=s[:, 104:108]), [psg, s], [s])
        k.op("dve", lambda e: e.tensor_sub(out=s[:, 124:128], in0=psg[:, 20:24], in1=s[:, 108:112]), [psg, s], [s])
        k.op("act", lambda e: e.activation(out=s[:, 120:128], in_=s[:, 120:128], func=AF.Exp), [s], [s])
        k.op("act", lambda e: e.activation(out=s[:, 56:64], in_=s[:, 112:120], func=AF.Exp), [s], [s])
        k.op("act", lambda e: e.activation(out=s2[:, 0:8], in_=psg[:, 32:40], func=AF.Exp), [psg], [s2])
        for h in range(4):
            o = outs[i][h]; fmt = fmts[i][h]; kst = ksb[i][h]
            hs = slice(h*64, (h+1)*64)
            O = lambda n: o[:, O6[n][0]:O6[n][1]]
            for d in range(2):
                j = d*4 + h
                KG0 = O6["KG0"][0] + d*130
                k.op("dve", lambda e: e.tensor_scalar(out=o[:, d*64:(d+1)*64], in0=m4[:, hs], scalar1=s[:, 40+j:41+j], scalar2=None, op0=ALU.mult), [m4, s], [o])
                k.op("pool", lambda e: e.tensor_scalar(out=kst[:, d*64:(d+1)*64], in0=m4[:, 256+h*64:256+(h+1)*64], scalar1=s[:, 32+j:33+j], scalar2=0.125, op0=ALU.mult, op1=ALU.mult), [m4, s], [kst])
                k.op("pool", lambda e: e.tensor_copy(o[:, KG0:KG0+64], kst[:, d*64:(d+1)*64]), [kst], [o])
                k.op("dve", lambda e: e.tensor_copy(o[:, KG0+128:KG0+129], s[:, 48+j:49+j]), [s], [o])
                k.op("dve", lambda e: e.tensor_copy(o[:, KG0+129:KG0+130], s2[:, j:j+1]), [s2], [o])
            k.op("act", lambda e: e.activation(out=O("sigo"), in_=m4[:, 768+h*64:768+(h+1)*64], func=AF.Sigmoid), [m4], [o])
            k.op("dve", lambda e: e.tensor_scalar(out=O("qn"), in0=c[:, hs], scalar1=s[:, 80+h:81+h], scalar2=0.125, op0=ALU.mult, op1=ALU.mult), [c, s], [o])
            k.op("dve", lambda e: e.tensor_scalar(out=O("kn"), in0=c[:, 256+h*64:256+(h+1)*64], scalar1=s[:, 84+h:85+h], scalar2=None, op0=ALU.mult), [c, s], [o])
            gv = c[:, 512+h*64:512+(h+1)*64]
            for d in range(2):
                j = d*4 + h
                KG0 = O6["KG0"][0] + d*130
                kb = o[:, 320+d*64:320+(d+1)*64]
                k.op("dve", lambda e: e.tensor_scalar(out=kb, in0=O("kn"), scalar1=s[:, 88+j:89+j], scalar2=None, op0=ALU.mult), [o, s], [o])
                k.op("pool", lambda e: e.tensor_scalar(out=o[:, 448+d*128:448+d*128+64], in0=gv, scalar1=s[:, 88+j:89+j], scalar2=None, op0=ALU.mult), [c, s], [o])
                k.op("dve", lambda e: e.tensor_scalar(out=o[:, 448+d*128+64:448+(d+1)*128], in0=kb, scalar1=s[:, 56+j:57+j], scalar2=None, op0=ALU.mult), [o, s], [o])
                k.op("pool", lambda e: e.tensor_scalar(out=o[:, 704+d*64:704+(d+1)*64], in0=O("qn"), scalar1=s[:, 56+j:57+j], scalar2=None, op0=ALU.mult), [o, s], [o])
                k.op("dve", lambda e: e.tensor_scalar(out=o[:, KG0+64:KG0+128], in0=O("kn"), scalar1=s[:, 120+j:121+j], scalar2=None, op0=ALU.mult), [o, s], [o])
                k.op("dve", lambda e: e.tensor_copy(o[:, 832+d:833+d], s[:, 112+j:113+j]), [s], [o])
            k.op("act", lambda e: e.activation(out=O("siluz"), in_=g4[:, 768+h*64:768+(h+1)*64], func=AF.Silu), [g4], [o])
            k.dma(O6D, O6D[h, r0:r0+128, :], o, o[:], skip_w=True)
            pA = PS[1 + (h % 2)*3]; pB = PS[2 + (h % 2)*3]; pC = PS[3 + (h % 2)*3]
            for bi, src in enumerate(FM_SRC):
                if src is None:
                    src_ap = kst[:, (bi-6)*64:(bi-5)*64]; src_t = kst
                else:
                    src_ap = o[:, src[0]:src[1]]; src_t = o
                pp = (pA, pB, pC)[bi // 4]; off = (bi % 4)*128
                k.op("pe", lambda e: e.transpose(pp[0:64, off:off+128], src_ap, ident[:]), [src_t, ident], [pp])
            k.op("act", lambda e: e.copy(fmt[:, 0:4, :], pA[0:64, :].rearrange("p (c t) -> p c t", c=4)), [pA], [fmt])
            k.op("dve", lambda e: e.tensor_copy(fmt[:, 4:8, :], pB[0:64, :].rearrange("p (c t) -> p c t", c=4)), [pB], [fmt])
            k.op("act", lambda e: e.copy(fmt[:, 8:10, :], pC[0:64, 0:256].rearrange("p (c t) -> p c t", c=2)), [pC], [fmt])
            k.dma(FMD, FMD[h, t], fmt, fmt[:], q="pool", skip_w=True)
    k.stage_end()


def e_intra(k, PS, O6D, FMD, cc, O7D, O7T):
    k.stage_begin()
    ident, ones, trif, trir, trifs, trirs, ntrifs, ntrirs = [cc[:, i, :] for i in range(8)]
    class C: pass
    ch = []
    for c in range(4):
        o = C()
        o.P = [k.sb([128, 128], name="P") for _ in range(2)]; o.PT = [k.sb([128, 128], name="PT") for _ in range(2)]
        o.Y = [k.sb([128, 128], name="Y") for _ in range(2)]
        o.M1 = k.sb([128, 128], name="M1"); o.M2 = k.sb([128, 128], name="M2"); o.E2 = k.sb([128, 128], name="E2"); o.dg = k.sb([128, 128], name="dg")
        o.out = k.sb([128, 3, 128], name="out"); o.wt = k.sb([64, 128], name="wt")
        o.ps = [PS[c*2], PS[c*2+1]]
        ch.append(o)
    fms = [[k.sb([64, 8, 128], name="fm") for _ in range(2)] for _ in range(2)]
    tms = [[k.sb([128, 258], name="tm") for _ in range(2)] for _ in range(2)]
    for h in range(4):
      for tp in range(NT_ // 2):
        chains = []
        for j in range(2):
            t = tp*2 + j
            fm = fms[j][tp % 2]; tm = tms[j][tp % 2]
            k.dma(fm, fm[:], FMD, FMD[h, t, :, 0:8, :])
            k.dma(tm, tm[:, 0:256], O6D, O6D[h, t*128:(t+1)*128, 448:704], q="pool")
            k.dma(tm, tm[:, 256:258], O6D, O6D[h, t*128:(t+1)*128, 832:834], q="pool")
            for d in range(2):
                chains.append((ch[j*2+d], t, d, fm, tm))
        for (o, t, d, fm, tm) in chains:
            m1, m2, m3 = (ntrirs, ntrifs, trif) if d == 0 else (ntrifs, ntrirs, trir)
            gcol = tm[:, 256+d:257+d]
            k.op("dve", lambda e: e.tensor_scalar(out=o.dg[:], in0=ident, scalar1=gcol, scalar2=None, op0=ALU.mult), [cc, tm], [o.dg])
            k.op("pe", lambda e: e.matmul(o.ps[0][:, 0:128], ones, o.dg[:], start=True, stop=True), [cc, o.dg], [o.ps[0]])
            k.op("dve", lambda e: e.tensor_scalar(out=o.E2[:], in0=o.ps[0][:, 0:128], scalar1=gcol, scalar2=0.0, op0=ALU.subtract, op1=ALU.min), [o.ps[0], tm], [o.E2])
            k.op("dve", lambda e: e.tensor_scalar(out=o.M1[:], in0=o.ps[0][:, 0:128], scalar1=gcol, scalar2=0.0, op0=ALU.subtract, op1=ALU.max), [o.ps[0], tm], [o.M1])
            k.op("act", lambda e: e.activation(out=o.E2[:], in_=o.E2[:], func=AF.Exp), [o.E2], [o.E2])
            k.op("act", lambda e: e.activation(out=o.M1[:], in_=o.M1[:], func=AF.Exp, scale=-1.0), [o.M1], [o.M1])
            k.op("pool", lambda e: e.tensor_mul(out=o.M1[:], in0=o.M1[:], in1=m1), [o.M1, cc], [o.M1])
            k.op("pool", lambda e: e.tensor_mul(out=o.M2[:], in0=o.E2[:], in1=m2), [o.E2, cc], [o.M2])
            k.op("pool", lambda e: e.tensor_mul(out=o.E2[:], in0=o.E2[:], in1=m3), [o.E2, cc], [o.E2])
        for (o, t, d, fm, tm) in chains:
            mA = trif if d == 0 else trir
            knT = fm[:, 0, :]; qnT = fm[:, 1, :]; kbT = fm[:, 2+d, :]; qsT = fm[:, 4+d, :]; ksT = fm[:, 6+d, :]
            p0 = V(o.ps[0], 0, 128); p1 = V(o.ps[1], 0, 128)
            k.op("pe", lambda e: e.matmul(p0[:], kbT, knT, start=True, stop=True), [fm], [p0])
            k.op("pe", lambda e: e.matmul(p1[:], knT, kbT, start=True, stop=True), [fm], [p1])
            k.op("dve", lambda e: e.tensor_mul(out=o.P[0][:], in0=p0[:], in1=o.M1[:]), [p0, o.M1], [o.P[0]])
            k.op("dve", lambda e: e.tensor_mul(out=o.PT[0][:], in0=p1[:], in1=o.M2[:]), [p1, o.M2], [o.PT[0]])
            k.op("pe", lambda e: e.matmul(p0[:], knT, qnT, start=True, stop=True), [fm], [p0])
            k.op("pe", lambda e: e.matmul(p1[:], ksT, qsT, start=True, stop=True), [fm], [p1])
            k.op("dve", lambda e: e.tensor_mul(out=o.out[:, 1, :], in0=p0[:], in1=o.E2[:]), [p0, o.E2], [o.out])
            k.op("dve", lambda e: e.tensor_mul(out=o.out[:, 2, :], in0=p1[:], in1=mA), [p1, cc], [o.out])
            k.op("act", lambda e: e.copy(o.Y[0][:], tm[:, d*128:(d+1)*128]), [tm], [o.Y[0]])
        for m in range(6):
            a = m % 2; b = 1 - a
            for (o, t, d, fm, tm) in chains:
                p0 = V(o.ps[0], 0, 128); p1 = V(o.ps[1], 0, 128)
                k.op("pe", lambda e: e.matmul(p0[:], o.PT[a][:], o.Y[a][:], start=True, stop=True), [o.PT[a], o.Y[a]], [p0])
                dst = o.Y[b][:] if m < 5 else o.out[:, 0, :]
                dstT = o.Y[b] if m < 5 else o.out
                k.op("dve", lambda e: e.tensor_add(out=dst, in0=p0[:], in1=o.Y[a][:]), [p0, o.Y[a]], [dstT])
                if m < 5:
                    k.op("pe", lambda e: e.matmul(p1[:], o.P[a][:], o.PT[a][:], start=True, stop=True), [o.P[a], o.PT[a]], [p1])
                    k.op("act", lambda e: e.copy(o.PT[b][:], p1[:]), [p1], [o.PT[b]])
            if m < 4:
                for (o, t, d, fm, tm) in chains:
                    p0 = V(o.ps[0], 0, 128)
                    k.op("pe", lambda e: e.matmul(p0[:], o.PT[a][:], o.P[a][:], start=True, stop=True), [o.PT[a], o.P[a]], [p0])
                    k.op("act", lambda e: e.copy(o.P[b][:], p0[:]), [p0], [o.P[b]])
        for (o, t, d, fm, tm) in chains:
            k.dma(O7D, O7D[h, t*128:(t+1)*128, d, :, :], o.out, o.out[:], q="sp" if d == 0 else "pool", skip_w=True)
            p1 = V(o.ps[1], 0, 128)
            k.op("pe", lambda e: e.transpose(p1[0:64, :], o.out[:, 0, 64:128], ident), [o.out, cc], [p1])
            k.op("act", lambda e: e.copy(o.wt[:], p1[0:64, :]), [p1], [o.wt])
            k.dma(O7T, O7T[h, d, :, t*128:(t+1)*128], o.wt, o.wt[:], q="pool", skip_w=True)
    k.stage_end()


def e_stepglue(k, P, O6D, FMD, O7D, O7T, STEP):
    k.stage_begin()
    qi = 0
    def cp(dst, src, src_t):
        nonlocal qi
        k.dma(STEP, dst, src_t, src, q="sp" if qi % 2 == 0 else "pool", skip_w=True); qi += 1
    for h in range(4):
        S = STEP[h]
        S2 = S.rearrange("p (t two) d w -> p t two d w", two=2)
        o6c = O6D[h].rearrange("(c p) w -> p c w", p=64)
        o7c = O7D[h].rearrange("(c p) d j w -> p c d j w", p=64)
        o7t2 = O7D[h].rearrange("(t two p) d j w -> p t two d j w", two=2, p=64)
        pc = P.rearrange("(c p) w -> p c w", p=64)
        for d in range(2):
            for (blk_i, col) in ((4+d, 0), (8+d, 128)):
                for two in range(2):
                    cp(S2[:, :, two, d, col:col+64], FMD[h][:, :, blk_i, two*64:(two+1)*64].rearrange("t f k -> f t k"), FMD)
            cp(S[:, :, d, 64:128], O7T[h, d].rearrange("f (c k) -> f c k", k=64), O7T)
            kg0 = O6["KG0"][0] + d*130
            cp(S[:, :, d, 192:322], o6c[:, :, kg0:kg0+130], O6D)
            cp(S[:, :, d, 322:386], pc[:, :, 1280 + h*64:1280 + (h+1)*64], P)
            for two in range(2):
                cp(S2[:, :, two, d, 387:451], o7t2[:, :, two, d, 2, two*64:(two+1)*64], O7D)
                cp(S2[:, :, two, d, 515:579], o7t2[:, :, two, d, 1, two*64:(two+1)*64], O7D)
            cp(S[:, :, d, 451:515], o7c[:, :, d, 0, 0:64], O7D)
    k.stage_end()


def e_scan(k, PS, STEP, O8D):
    k.stage_begin()
    BS = 4; nblk = NC_ // BS
    bufs = [[k.sb([64, BS, W8], name="blk") for _ in range(2)] for _ in range(2)]
    obufs = [[k.sb([64, BS, 128], name="ob") for _ in range(2)] for _ in range(2)]
    C1 = [k.sb([64, 65], name="C1") for _ in range(2)]; Sg = [k.sb([64, 64], name="S") for _ in range(2)]
    tmpc = [k.sb([64, 65], name="tc") for _ in range(2)]; vnew = [k.sb([64, 64], name="vn") for _ in range(2)]
    rr = [k.sb([64, 4], name="rr") for _ in range(2)]
    psA = [PS[0], PS[1]]; psB = [PS[2], PS[3]]
    pso = [V(psA[d], 0, 65) for d in range(2)]; psc = [V(psA[d], 128, 193) for d in range(2)]
    psw = [V(psB[d], 0, 64) for d in range(2)]; pss = [V(psB[d], 128, 192) for d in range(2)]; psg = [V(psB[d], 256, 320) for d in range(2)]
    for h in range(4):
        for d in range(2):
            k.op("dve", lambda e: e.memset(C1[d][:], 0.0), [], [C1[d]])
            k.op("dve", lambda e: e.memset(Sg[d][:], 0.0), [], [Sg[d]])
        for b in range(nblk):
            mb = [b, 0 if b == 0 else nblk - b]
            B = [bufs[d][b % 2] for d in range(2)]; OB = [obufs[d][b % 2] for d in range(2)]
            for d in range(2):
                k.op("pool", lambda e: e.memset(B[d][:, :, 386:387], 1.0), [], [B[d]])
                k.dma(B[d], B[d][:, :, 0:386], STEP, STEP[h, :, mb[d]*BS:(mb[d]+1)*BS, d, 0:386], q="sp" if d == 0 else "pool")
                k.dma(B[d], B[d][:, :, 387:579], STEP, STEP[h, :, mb[d]*BS:(mb[d]+1)*BS, d, 387:579], q="sp" if d == 0 else "pool")
            for jj in range(BS):
                sl = [jj, BS - 1 - jj]
                F = lambda d, n: B[d][:, sl[d], S8[n][0]:S8[n][1]]
                R2 = range(2)
                for d in R2:
                    k.op("pe", lambda e: e.matmul(psw[d][:64], F(d, "WT"), Sg[d][:], start=True, stop=True), [B[d], Sg[d]], [psw[d]])
                for d in R2:
                    k.op("pe", lambda e: e.matmul(pso[d][:64], F(d, "QsT"), C1[d][:], start=True, stop=False), [B[d], C1[d]], [pso[d]])
                    k.op("pe", lambda e: e.matmul(pso[d][:64], F(d, "AT"), F(d, "V1"), start=False, stop=True), [B[d]], [pso[d]])
                    k.op("pe", lambda e: e.matmul(psc[d][:64], F(d, "Ks"), F(d, "V1"), start=True, stop=True), [B[d]], [psc[d]])
                for d in R2:
                    k.op("dve", lambda e: e.tensor_sub(out=vnew[d][:], in0=F(d, "U"), in1=psw[d][:64]), [B[d], psw[d]], [vnew[d]])
                for d in R2:
                    k.op("pe", lambda e: e.matmul(psg[d][:64], F(d, "QgT"), Sg[d][:], start=True, stop=False), [B[d], Sg[d]], [psg[d]])
                    k.op("pe", lambda e: e.matmul(psg[d][:64], F(d, "AqkT"), vnew[d][:], start=False, stop=True), [B[d], vnew[d]], [psg[d]])
                    k.op("pe", lambda e: e.matmul(pss[d][:64], F(d, "Kd"), vnew[d][:], start=True, stop=True), [B[d], vnew[d]], [pss[d]])
                for d in R2:
                    k.op("dve", lambda e: e.tensor_add(out=tmpc[d][:], in0=psc[d][:64], in1=C1[d][:]), [psc[d], C1[d]], [tmpc[d]])
                    k.op("dve", lambda e: e.tensor_scalar(out=C1[d][:], in0=tmpc[d][:], scalar1=F(d, "sm"), scalar2=None, op0=ALU.mult), [tmpc[d], B[d]], [C1[d]])
                    k.op("dve", lambda e: e.scalar_tensor_tensor(out=Sg[d][:], in0=Sg[d][:], scalar=F(d, "sg"), in1=pss[d][:64], op0=ALU.mult, op1=ALU.add), [Sg[d], B[d], pss[d]], [Sg[d]])
                for d in R2:
                    k.op("act", lambda e: e.activation(out=rr[d][:, 0:1], in_=pso[d][:64, 64:65], func=AF.Abs), [pso[d]], [rr[d]])
                    k.op("dve", lambda e: e.tensor_scalar(out=rr[d][:, 2:3], in0=rr[d][:, 0:1], scalar1=1.0, scalar2=None, op0=ALU.max), [rr[d]], [rr[d]])
                    k.op("dve", lambda e: e.reciprocal(out=rr[d][:, 1:2], in_=rr[d][:, 2:3]), [rr[d]], [rr[d]])
                    k.op("act", lambda e: e.activation(out=OB[d][:, sl[d], 0:64], in_=pso[d][:64, 0:64], func=AF.Copy, scale=rr[d][:, 1:2]), [pso[d], rr[d]], [OB[d]])
                    k.op("act", lambda e: e.copy(OB[d][:, sl[d], 64:128], psg[d][:64]), [psg[d]], [OB[d]])
            for d in range(2):
                k.dma(O8D, O8D[h, :, mb[d]*BS:(mb[d]+1)*BS, d, :], OB[d], OB[d][:], q="sp", skip_w=True)
    k.stage_end()


def e_outproj(k, PS, AXD, O8D, O6D, GAIN2, X, modrow, sel, ident, W, XMID):
    k.stage_begin()
    gn = k.sb([128, 512], name="gn"); w = k.sb([128, 8, 1024], name="w")
    gate = [k.sb([128, 1024], name="gate") for _ in range(2)]
    k.dma(gn, gn[:], None, GAIN2[:, :])
    for cls in range(2):
        bcast_rows(k, PS, gate[cls], V(modrow, 2*1024, 3*1024), 1 if cls == 0 else 0, sel)
    for c in range(8):
        k.dma(w, w[:, c, :], None, W[c*128:(c+1)*128, :], q="sp" if c % 2 == 0 else "pool")
    B = [dict(mix=k.sb([128, 1024], name="mix"), a=k.sb([128, 512], name="a"), b=k.sb([128, 512], name="b"), g=k.sb([128, 512], name="g"),
              sq=k.sb([128, 512], name="sq"), s=k.sb([128, 32], name="s"), x=k.sb([128, 1024], name="x"), mT=k.sb([128, 8, 128], name="mT"),
              o=k.sb([128, 1024], name="o")) for _ in range(2)]
    pi = 0
    for t in range(NT_):
        cls = 0 if t < 2 else 1
        b = B[t % 2]; mix = b['mix']; a = b['a']; bb = b['b']; g = b['g']; s = b['s']; x = b['x']; mT = b['mT']; o = b['o']
        rows = slice(t*128, (t+1)*128)
        k.dma(mix, mix[:, 0:512], AXD, AXD[rows, :])
        k.dma(x, x[:], X, X[rows, :])
        qi = 0
        for h in range(4):
            k.dma(g, g[:, h*64:(h+1)*64], O6D, O6D[h, rows, 128:192], q="pool")
            k.dma(g, g[:, 256+h*64:256+(h+1)*64], O6D, O6D[h, rows, 834:898], q="pool")
            for d in range(2):
                dst_t = a if d == 0 else bb
                for half in range(2):
                    dst = dst_t[half*64:(half+1)*64, :].rearrange("p (g hh w) -> p g hh w", g=2, hh=4)[:, :, h, :]
                    src = O8D[h, :, 2*t + half, d, :].rearrange("p (g w) -> p g w", g=2)
                    k.dma(dst_t, dst, O8D, src, q="sp" if qi % 2 == 0 else "pool"); qi += 1
        k.op("dve", lambda e: e.tensor_add(out=a[:], in0=a[:], in1=bb[:]), [a, bb], [a])
        k.op("dve", lambda e: e.tensor_mul(out=b['sq'][:], in0=a[:], in1=a[:]), [a], [b['sq']])
        k.op("dve", lambda e: e.reduce_sum(out=s[:, 0:8], in_=b['sq'][:].rearrange("p (h d) -> p h d", h=8), axis=AX.X), [b['sq']], [s])
        k.op("dve", lambda e: e.tensor_scalar(out=s[:, 0:8], in0=s[:, 0:8], scalar1=1.0/64, scalar2=EPS, op0=ALU.mult, op1=ALU.add), [s], [s])
        k.op("act", lambda e: e.activation(out=s[:, 8:16], in_=s[:, 0:8], func=AF.Sqrt), [s], [s])
        k.op("dve", lambda e: e.reciprocal(out=s[:, 16:24], in_=s[:, 8:16]), [s], [s])
        for h in range(8):
            k.op("dve" if h % 2 == 0 else "pool", lambda e: e.tensor_scalar(out=a[:, h*64:(h+1)*64], in0=a[:, h*64:(h+1)*64], scalar1=s[:, 16+h:17+h], scalar2=None, op0=ALU.mult), [a, s], [a])
        k.op("dve", lambda e: e.tensor_mul(out=a[:], in0=a[:], in1=gn[:]), [a, gn], [a])
        k.op("dve", lambda e: e.tensor_mul(out=mix[:, 512:1024], in0=a[:], in1=g[:]), [a, g], [mix])
        for half in range(2):
            pt = PS[half]
            for c in range(4):
                cc_ = half*4 + c
                k.op("pe", lambda e: e.transpose(pt[:, c*128:(c+1)*128], mix[:, cc_*128:(cc_+1)*128], ident[:]), [mix, ident], [pt])
            if half == 0:
                k.op("act", lambda e: e.copy(mT[:, 0:4, :], pt[:].rearrange("p (c t) -> p c t", c=4)), [pt], [mT])
            else:
                k.op("dve", lambda e: e.tensor_copy(mT[:, 4:8, :], pt[:].rearrange("p (c t) -> p c t", c=4)), [pt], [mT])
        for nb in range(2):
            ps = PS[2 + pi % 4]; pi += 1
            for c in range(8):
                k.op("pe", lambda e: e.matmul(ps[:], mT[:, c, :], w[:, c, nb*512:(nb+1)*512], start=(c == 0), stop=(c == 7)), [mT, w], [ps])
            k.op("dve", lambda e: e.tensor_mul(out=o[:, nb*512:(nb+1)*512], in0=ps[:], in1=gate[cls][:, nb*512:(nb+1)*512]), [ps, gate[cls]], [o])
        k.op("pool", lambda e: e.tensor_add(out=o[:], in0=o[:], in1=x[:]), [o, x], [o])
        k.dma(XMID, XMID[rows, :], o, o[:], skip_w=True)
    k.stage_end()


def e_topk(k, AFFT, VALS, IDX):
    k.stage_begin()
    for (c0, n, kk, o0, nm) in [(NCTX, T_ - NCTX, 1024, 0, "l"), (0, NCTX, 32, 1024, "c")]:
        a = k.sb([16, n], name="a" + nm); wk = k.sb([16, n], name="w" + nm)
        vals = k.sb([16, kk], name="v" + nm); idx = k.sb([16, kk], U32, name="i" + nm)
        k.dma(a, a[:], AFFT, AFFT[:, c0:c0+n])
        k.op("dve", lambda e: e.tensor_copy(wk[:], a[:]), [a], [wk])
        for r in range(kk // 8):
            k.op("dve", lambda e: e.max(out=vals[:, r*8:(r+1)*8], in_=wk[:]), [wk], [vals])
            k.op("dve", lambda e: e.max_index(out=idx[:, r*8:(r+1)*8], in_max=vals[:, r*8:(r+1)*8], in_values=wk[:]), [vals, wk], [idx])
            k.op("dve", lambda e: e.match_replace(out=wk[:], in_to_replace=vals[:, r*8:(r+1)*8], in_values=wk[:], imm_value=-1.0), [vals, wk], [wk])
        k.dma(VALS, VALS[:, o0:o0+kk], vals, vals[:], skip_w=True); k.dma(IDX, IDX[:, o0:o0+kk], idx, idx[:], skip_w=True)
    k.stage_end()


def e_expert(k, PS, H2, VALS, IDX, W1, W3, W2, ident, ACC):
    k.stage_begin()
    z = k.sb([128, 1024], name="z")
    k.op("dve", lambda e: e.memset(z[:], 0.0), [], [z])
    for t in range(NT_):
        k.dma(ACC, ACC[t*128:(t+1)*128, :], z, z[:], q="sp" if t % 2 == 0 else "pool", skip_w=True)
    RT = 9
    parts = [(0, 5), (5, 9)]
    PR = 5 * 128
    w2 = k.sb([128, 16, 1024], name="w2"); vl = k.sb([128, RT], name="vl"); ix = k.sb([128, RT], U32, name="ix")
    hid = k.sb([128, 16, PR], name="hid"); xt = k.sb([128, 8, PR], name="xt")
    xg = [k.sb([128, 1024], name="xg") for _ in range(2)]
    w1c = [k.sb([128, 8, 128], name="w1c") for _ in range(2)]; w3c = [k.sb([128, 8, 128], name="w3c") for _ in range(2)]
    sg = [k.sb([128, 512], name="sg") for _ in range(2)]
    ysb = [k.sb([128, 1024], name="ysb") for _ in range(2)]
    ph1 = [PS[0], PS[1]]; ph3 = [PS[2], PS[3]]; py = [PS[4], PS[5], PS[6], PS[7]]
    ci = 0; hi = 0; yi = 0; gi = 0
    first_scatter = True
    for e_ in range(16):
        k.dma(w2, w2[:], None, W2[e_].rearrange("(c p) n -> p c n", p=128), q="pool")
        k.op("dve", lambda e: e.memset(vl[:], 0.0), [], [vl])
        k.op("pool", lambda e: e.memset(ix[:], 0), [], [ix])
        k.dma(vl, vl[:, 0:8], VALS, VALS[e_, 0:1024].rearrange("(t p) -> p t", p=128), allow_slow_non_contiguous=True)
        k.dma(vl, vl[0:32, 8:9], VALS, VALS[e_, 1024:1056].rearrange("(t p) -> p t", p=32), allow_slow_non_contiguous=True)
        k.dma(ix, ix[:, 0:8], IDX, IDX[e_, 0:1024].rearrange("(t p) -> p t", p=128), allow_slow_non_contiguous=True)
        k.dma(ix, ix[0:32, 8:9], IDX, IDX[e_, 1024:1056].rearrange("(t p) -> p t", p=32), allow_slow_non_contiguous=True)
        for (t0, t1) in parts:
            nr = (t1 - t0) * 128
            for tt in range(t0, t1):
                g_ = xg[gi % 2]; gi += 1
                k.dma(g_, g_[:], H2, H2[:, :], q="pool", element_offset=(NCTX*1024 if tt < 8 else 0),
                      indirect=dict(in_offset=bass.IndirectOffsetOnAxis(ap=ix[:, tt:tt+1], axis=0), idx_t=ix))
                lr = (tt - t0) * 128
                for half in range(2):
                    pt = py[half]
                    for c in range(4):
                        cc_ = half*4 + c
                        k.op("pe", lambda e: e.transpose(pt[:, c*128:(c+1)*128], g_[:, cc_*128:(cc_+1)*128], ident[:]), [g_, ident], [pt])
                    k.op("act" if half == 0 else "dve", (lambda e: e.copy(xt[:, 0:4, lr:lr+128], pt[:].rearrange("p (c t) -> p c t", c=4))) if half == 0 else
                         (lambda e: e.tensor_copy(xt[:, 4:8, lr:lr+128], pt[:].rearrange("p (c t) -> p c t", c=4))), [pt], [xt])
            for fc in range(16):
                a1 = w1c[ci % 2]; a3 = w3c[ci % 2]; ci += 1
                k.dma(a1, a1[:], None, W1[e_].rearrange("(c p) f -> p c f", p=128)[:, :, fc*128:(fc+1)*128])
                k.dma(a3, a3[:], None, W3[e_].rearrange("(c p) f -> p c f", p=128)[:, :, fc*128:(fc+1)*128], q="pool")
                for rb in range(0, nr, 512):
                    rn = min(512, nr - rb)
                    p1 = ph1[hi % 2]; p3 = ph3[hi % 2]; s_ = sg[hi % 2]; hi += 1
                    for c in range(8):
                        k.op("pe", lambda e: e.matmul(p1[:, 0:rn], a1[:, c, :], xt[:, c, rb:rb+rn], start=(c == 0), stop=(c == 7)), [a1, xt], [p1])
                    for c in range(8):
                        k.op("pe", lambda e: e.matmul(p3[:, 0:rn], a3[:, c, :], xt[:, c, rb:rb+rn], start=(c == 0), stop=(c == 7)), [a3, xt], [p3])
                    k.op("act", lambda e: e.activation(out=s_[:, 0:rn], in_=p1[:, 0:rn], func=AF.Silu), [p1], [s_])
                    k.op("dve", lambda e: e.tensor_mul(out=hid[:, fc, rb:rb+rn], in0=s_[:, 0:rn], in1=p3[:, 0:rn]), [s_, p3], [hid])
            for tt in range(t0, t1):
                ys = ysb[yi % 2]; yi += 1
                lr = (tt - t0) * 128
                for nb in range(2):
                    pp = py[(yi*2 + nb) % 4]
                    for fc in range(16):
                        k.op("pe", lambda e: e.matmul(pp[:], hid[:, fc, lr:lr+128], w2[:, fc, nb*512:(nb+1)*512], start=(fc == 0), stop=(fc == 15)), [hid, w2], [pp])
                    if nb == 0:
                        k.op("dve", lambda e: e.tensor_scalar(out=ys[:, 0:512], in0=pp[:], scalar1=vl[:, tt:tt+1], scalar2=None, op0=ALU.mult), [pp, vl], [ys])
                    else:
                        k.op("act", lambda e: e.activation(out=ys[:, 512:1024], in_=pp[:], func=AF.Copy, scale=vl[:, tt:tt+1]), [pp, vl], [ys])
                if tt < 8:
                    k.dma(ACC, ACC[:, :], ys, ys[:], q="pool", compute_op=ALU.add, element_offset=NCTX*1024,
                          indirect=dict(out_offset=bass.IndirectOffsetOnAxis(ap=ix[:, tt:tt+1], axis=0), idx_t=ix))
                else:
                    k.dma(ACC, ACC[:, :], ys, ys[0:32, :], q="pool", compute_op=ALU.add, element_offset=0,
                          indirect=dict(out_offset=bass.IndirectOffsetOnAxis(ap=ix[0:32, tt:tt+1], axis=0), idx_t=ix))
    k.stage_end()


def e_combine(k, PS, XMID, ACC, modrow, sel, OUTT, out_rows0):
    k.stage_begin()
    gate = [k.sb([128, 1024], name="gate") for _ in range(2)]
    for cls in range(2):
        bcast_rows(k, PS, gate[cls], V(modrow, 5*1024, 6*1024), 1 if cls == 0 else 0, sel)
    xs = [k.sb([128, 1024], name="x") for _ in range(2)]; ac = [k.sb([128, 1024], name="ac") for _ in range(2)]
    for t in range(out_rows0 // 128, NT_):
        cls = 0 if t < 2 else 1
        x = xs[t % 2]; a = ac[t % 2]; rows = slice(t*128, (t+1)*128)
        k.dma(x, x[:], XMID, XMID[rows, :]); k.dma(a, a[:], ACC, ACC[rows, :], q="pool")
        k.op("dve", lambda e: e.tensor_mul(out=a[:], in0=a[:], in1=gate[cls][:]), [a, gate[cls]], [a])
        k.op("pool", lambda e: e.tensor_add(out=x[:], in0=x[:], in1=a[:]), [x, a], [x])
        k.dma(OUTT, OUTT[t*128 - out_rows0:(t+1)*128 - out_rows0, :], x, x[:], skip_w=True)
    k.stage_end()


def build_fused(nlayer=2, upto=None):
    k = K()
    TT = T_
    def IN(name, shape, dt=F32):
        t = T(k, k.dram_in(name, shape, dt), name); t.is_dram = True
        return t
    XCAT = IN("xcat", [TT, 1024]); CT = IN("ct", [128, 8, 2]); IDENT = IN("ident", [128, 128]); SEL = IN("sel", [2, 2, 128])
    CS = IN("cs", [TT, 320]); SN = IN("sn", [TT, 320]); CC = IN("cc", [9, 128, 128])
    L = []
    for l in range(nlayer):
        L.append(dict(MODW=IN(f"modw{l}", [1024, 6144]), MODB=IN(f"modb{l}", [2, 6144]), N1G=IN(f"n1g{l}", [128, 1024]), WIN=IN(f"win{l}", [1024, 2848]),
                      QKG=IN(f"qkg{l}", [128, 640]), CB=IN(f"cb{l}", [128, 32]), CW=IN(f"cw{l}", [128, 5, 768]), GAIN2=IN(f"gain2{l}", [128, 512]),
                      WOUT=IN(f"wout{l}", [1024, 1024]), N2G=IN(f"n2g{l}", [128, 1024]), RW=IN(f"rw{l}", [1024, 16]),
                      W1=IN(f"w1{l}", [16, 1024, 2048]), W3=IN(f"w3{l}", [16, 1024, 2048]), W2=IN(f"w2{l}", [16, 2048, 1024])))
    OUT = k.dram_out("out", [TT - NCTX, 1024])
    PS = [k.ps([128, 512], name=f"bank{i}") for i in range(8)]
    ident = k.sb([128, 128], name="ident"); sel = k.sb([2, 2, 128], name="sel"); ctsb = k.sb([128, 8, 2], name="ct")
    cc = k.sb([128, 9, 128], name="cc")
    modrow = k.sb([2, 6144], name="modrow")
    k.dma(ident, ident[:], None, IDENT[:, :]); k.dma(sel, sel[:], None, SEL[:, :, :]); k.dma(ctsb, ctsb[:], None, CT[:, :, :])
    for i in range(9):
        k.dma(cc, cc[:, i, :], None, CC[i])
    k.op("act", lambda e: e.activation(out=ctsb[:], in_=ctsb[:], func=AF.Silu), [ctsb], [ctsb])
    D = lambda n, s, dt=F32: k.dram_tmp(n, s, dt)
    P = D("P", [TT, 2848]); QKT = D("QKT", [640, TT]); AXD = D("AXD", [TT, 512]); O6D = D("O6D", [4, TT, W6]); FMD = D("FMD", [4, NT_, 64, 10, 128])
    O7D = D("O7D", [4, TT, 2, 3, 128]); O7T = D("O7T", [4, 2, 64, TT]); STEP = D("STEP", [4, 64, NC_, 2, W8]); O8D = D("O8D", [4, 64, NC_, 2, 128])
    XMID = D("XMID", [TT, 1024]); H2 = D("H2", [TT, 1024]); AFFT = D("AFFT", [16, TT]); VALS = D("VALS", [16, 1152]); IDX = D("IDX", [16, 1152], U32)
    ACC = D("ACC", [TT, 1024]); XN = D("XN", [TT, 1024])
    X = XCAT
    for l in range(nlayer):
        W = L[l]
        e_mod(k, PS, ctsb, W["MODW"], W["MODB"], modrow)
        e_normlin(k, PS, X, W["N1G"], modrow, 1, 0, sel, ident, W["WIN"], 2848, P, QKT=QKT, qk=dict(GAIN=W["QKG"], CS=CS, SN=SN))
        e_attn(k, PS, QKT, P, AXD)
        e_prep(k, PS, P, W["CB"], W["CW"], cc, ident, O6D, FMD)
        e_intra(k, PS, O6D, FMD, cc, O7D, O7T)
        e_stepglue(k, P, O6D, FMD, O7D, O7T, STEP)
        e_scan(k, PS, STEP, O8D)
        e_outproj(k, PS, AXD, O8D, O6D, W["GAIN2"], X, modrow, sel, ident, W["WOUT"], XMID)
        if upto == "xmid":
            k.stage_begin(); xx = [k.sb([128, 1024], name="cpy") for _ in range(2)]
            for t in range(2, NT_):
                k.dma(xx[t % 2], xx[t % 2][:], XMID, XMID[t*128:(t+1)*128, :]); k.dma(OUT, OUT[(t-2)*128:(t-1)*128, :], xx[t % 2], xx[t % 2][:], skip_w=True)
            k.stage_end(); break
        e_normlin(k, PS, XMID, W["N2G"], modrow, 4, 3, sel, ident, W["RW"], 16, None, H=H2, softmax=True, YT=AFFT)
        e_topk(k, AFFT, VALS, IDX)
        e_expert(k, PS, H2, VALS, IDX, W["W1"], W["W3"], W["W2"], ident, ACC)
        last = (l == nlayer - 1)
        e_combine(k, PS, XMID, ACC, modrow, sel, OUT if last else XN, NCTX if last else 0)
        X = XN
    k.finish([OUT])
    k.close()
    return k

def _rep(v, p=128):
    return np.ascontiguousarray(np.broadcast_to(np.asarray(v, np.float32)[None], (p,) + tuple(np.shape(v))))

def _consts():
    f32 = np.float32
    n = 8192
    row = (np.arange(n) // 64).astype(f32); col = (np.arange(n) % 64).astype(f32)
    inv = (10000.0 ** (-np.arange(16, dtype=f32) / 16)).astype(f32)
    ang = np.stack([row, col], -1)[..., None] * inv
    tile = lambda a: np.ascontiguousarray(np.broadcast_to(a[:, None], (a.shape[0], 10, 2, 16))).reshape(a.shape[0], 320)
    cs = np.concatenate([np.ones((256, 320), f32), tile(np.cos(ang).astype(f32))]); sn = np.concatenate([np.zeros((256, 320), f32), tile(np.sin(ang).astype(f32))])
    sel = np.zeros((2, 2, 128), f32); sel[0, 0] = 1; sel[1, 1] = 1
    s = np.arange(128)[:, None]; t = np.arange(128)[None, :]
    same = (s // 64) == (t // 64)
    trif = (same & (s <= t)).astype(f32); trir = (same & (s >= t)).astype(f32); eye = np.eye(128, dtype=f32)
    cc = np.stack([eye, np.ones((128, 128), f32), trif, trir, trif - eye, trir - eye, eye - trif, eye - trir, same.astype(f32)])
    return dict(cs=cs, sn=sn, sel=sel, cc=cc, ident=eye)

def make_in_maps(inp, nlayer=2):
    f32 = np.float32
    C = _consts()
    ims = []
    for b in range(2):
        ct = np.stack([inp['c'][b], inp['c_ctx']], 1).reshape(8, 128, 2).transpose(1, 0, 2)
        m = {"xcat": np.concatenate([inp['ctx'][b], inp['x'][b]], 0), "ct": np.ascontiguousarray(ct, dtype=f32)}
        m.update(C)
        for l in range(nlayer):
            cb = np.concatenate([inp['mlstm_i_bias'][l].reshape(8), inp['mlstm_f_bias'][l].reshape(8), inp['gdn_a_log'][l].reshape(8), inp['gdn_dt_bias'][l].reshape(8)])
            m.update({f"modw{l}": inp['mod_w'][l], f"modb{l}": _rep(inp['mod_b'][l], 2), f"n1g{l}": _rep(inp['norm1_g'][l]), f"win{l}": inp['w_in'][l],
                      f"qkg{l}": _rep(np.concatenate([np.tile(inp['q_norm_g'][l], 8), np.tile(inp['k_norm_g'][l], 2)])),
                      f"cb{l}": _rep(cb), f"cw{l}": _rep(inp['gdn_conv_w'][l]),
                      f"gain2{l}": _rep(np.concatenate([inp['mlstm_out_g'][l].reshape(256), np.tile(inp['gdn_out_g'][l], 4)])),
                      f"wout{l}": inp['w_out'][l], f"n2g{l}": _rep(inp['norm2_g'][l]), f"rw{l}": inp['router_w'][l],
                      f"w1{l}": inp['w1'][l], f"w3{l}": inp['w3'][l], f"w2{l}": inp['w2'][l]})
        ims.append({k_: np.ascontiguousarray(v, dtype=f32) for k_, v in m.items()})
    return ims

_K = {}
def kernel(**inputs):
    inp = {k_: np.asarray(v, np.float32) for k_, v in inputs.items()}
    if 2 not in _K:
        _K[2] = build_fused(2)
    res = run_bass_kernel_spmd(_K[2].nc, make_in_maps(inp, 2), core_ids=[0, 1])
    return np.ascontiguousarray(np.stack([res.results[b]["out"] for b in range(2)]).astype(np.float32))
```

```python
import numpy as np
from contextlib import ExitStack
import concourse.bass as bass
import concourse.mybir as mybir
from concourse.bass_utils import run_bass_kernel_spmd

F32 = mybir.dt.float32
I32 = mybir.dt.int32
U32 = mybir.dt.uint32
BF16 = mybir.dt.bfloat16
ALU = mybir.AluOpType
AF = mybir.ActivationFunctionType
AX = mybir.AxisListType
EPS = 1e-6


class T:
    def __init__(self, k, ap, name):
        self.k = k; self.ap = ap; self.name = name
        self.w = None; self.r = {}
        self.dsem = {}; self.dn = 0; self.is_dram = False

    def __getitem__(self, idx):
        return self.ap[idx]

    def rearrange(self, *a, **kw):
        return self.ap.rearrange(*a, **kw)


class V:
    def __init__(s, t, a0, a1):
        s.t = t; s.base = t; s.a0 = a0; s.a1 = a1

    def __getitem__(s, idx):
        return s.t.ap[:, s.a0:s.a1][idx]


class K:
    def __init__(self):
        self.nc = bass.Bass("TRN2", target_bir_lowering=False)
        self.es = ExitStack()
        self.st = None
        nc = self.nc
        self.es.enter_context(nc.allow_low_precision('bf16 matmul operands, fp32 PSUM accumulation'))
        self.eng = {"pe": nc.tensor, "act": nc.scalar, "dve": nc.vector, "pool": nc.gpsimd, "sp": nc.sync}
        self.sem = {}; self.cnt = {}
        for e in ["pe", "act", "dve", "pool"]:
            self.sem[e] = self.es.enter_context(nc.semaphore("s_" + e)); self.cnt[e] = 0
        self.seen = {e: {} for e in self.eng}
        self.ntile = 0
        self.dsems = {}
        self.free_sems = {'sp': [], 'pool': []}
        self.stage_tiles = []
        self.nsem = 0

    def dram_in(self, name, shape, dt=F32):
        return self.nc.dram_tensor(name, list(shape), dt, kind="ExternalInput").ap()

    def dram_out(self, name, shape, dt=F32):
        t = T(self, self.nc.dram_tensor(name, list(shape), dt, kind="ExternalOutput").ap(), name); t.is_dram = True
        return t

    def dram_tmp(self, name, shape, dt=F32):
        t = T(self, self.nc.dram_tensor(name, list(shape), dt, kind="Internal").ap(), name); t.is_dram = True
        return t

    def sb(self, shape, dt=F32, name=None):
        self.ntile += 1
        name = (name or "t") + f"_{self.ntile}"
        stk = self.st if self.st is not None else self.es
        h = stk.enter_context(self.nc.sbuf_tensor("S_" + name, list(shape), dt))
        t = T(self, h[:], name)
        if self.st is not None:
            self.stage_tiles.append(t)
        return t

    def ps(self, shape, dt=F32, name=None):
        self.ntile += 1
        name = (name or "p") + f"_{self.ntile}"
        h = self.es.enter_context(self.nc.psum_tensor("P_" + name, list(shape), dt))
        return T(self, h[:], name)

    def stage_begin(self):
        assert self.st is None
        self.st = ExitStack(); self.stage_tiles = []

    def stage_end(self):
        self.barrier()
        for t in self.stage_tiles:
            for q_, sm_ in t.dsem.items():
                self.free_sems[q_].append(sm_)
        self.st.close(); self.st = None; self.stage_tiles = []

    def barrier(self):
        toks = [(self.sem[c], self.cnt[c]) for c in self.sem if self.cnt[c] > 0]
        toks += [(s, 16 * n) for (s, n) in self.dsems.values() if n > 0]
        for e in self.eng:
            self._wait(e, toks)

    def _get_dsem(self, t, q):
        if q not in t.dsem:
            if self.free_sems[q]:
                t.dsem[q] = self.free_sems[q].pop()
            else:
                self.nsem += 1
                sm_ = self.es.enter_context(self.nc.semaphore(f"d{self.nsem}{q}"))
                t.dsem[q] = sm_
                self.dsems[id(sm_)] = [sm_, 0]
        return t.dsem[q]

    def _deps(self, reads, writes):
        toks = []
        for t in reads:
            if t.w is not None:
                toks.append(t.w)
        for t in writes:
            if t.w is not None:
                toks.append(t.w)
            toks.extend(t.r.values())
        return toks

    def _wait(self, e, toks, skip_sem=None):
        eng = self.eng[e]
        best = {}
        for (s, v) in toks:
            if skip_sem is not None and s is skip_sem:
                continue
            if best.get(id(s), (None, 0))[1] < v:
                best[id(s)] = (s, v)
        for (s, v) in best.values():
            if self.seen[e].get(id(s), 0) < v:
                eng.wait_ge(s, v)
                self.seen[e][id(s)] = v

    def _mark(self, tok, reads, writes):
        for t in reads:
            t.r[id(tok[0])] = tok
        for t in writes:
            t.w = tok; t.r = {}

    def op(self, e, fn, reads, writes):
        reads = [getattr(t, "base", t) for t in reads if t is not None]
        writes = [getattr(t, "base", t) for t in writes]
        toks = self._deps(reads, writes)
        self._wait(e, toks, skip_sem=self.sem[e] if e == "pe" else None)
        ins = fn(self.eng[e])
        self.cnt[e] += 1
        ins.then_inc(self.sem[e], 1)
        tok = (self.sem[e], self.cnt[e])
        self._mark(tok, reads, writes)
        return tok

    def dma(self, out_t, out_ap, in_t, in_ap, q="sp", skip_w=False, indirect=None, **kw):
        reads = [in_t] if in_t is not None else []
        if indirect is not None and indirect.get("idx_t") is not None:
            reads.append(indirect["idx_t"])
        writes = [out_t] if out_t is not None else []
        toks = self._deps(reads, [] if skip_w else writes)
        self._wait(q, toks)
        if out_t is not None and not out_t.is_dram:
            owner = out_t
        elif in_t is not None and not in_t.is_dram:
            owner = in_t
        else:
            owner = out_t if out_t is not None else in_t
        sem = self._get_dsem(owner, q)
        ent = self.dsems[id(sem)]
        ent[1] += 1
        if indirect is None:
            ins = self.eng[q].dma_start(out=out_ap, in_=in_ap, **kw)
        else:
            ins = self.eng[q].indirect_dma_start(out=out_ap, out_offset=indirect.get("out_offset"), in_=in_ap,
                                                 in_offset=indirect.get("in_offset"), **kw)
        ins.then_inc(sem, 16)
        tok = (sem, 16 * ent[1])
        self._mark(tok, reads, writes)
        return tok

    def finish(self, out_tiles):
        self.barrier()

    def close(self):
        if self.st is not None:
            self.st.close()
        self.es.close()


CUM = [0, 512, 640, 768, 1024, 1280, 1536, 1792, 1800, 1808, 2064, 2320, 2576, 2832, 2840, 2848]
import os
T_ = int(os.environ.get('FZ_T', '8448')); NT_ = T_ // 128; NC_ = T_ // 64; NCTX = 256

OUT6 = dict(Qs=(0, 128), Ks=(128, 256), eb=(256, 258), sigo=(258, 322), qn=(322, 386), kn=(386, 450), kb=(450, 578),
            Rm=(578, 834), Qg=(834, 962), Kd=(962, 1090), G=(1090, 1092), eG=(1092, 1094), siluz=(1094, 1158), gv=(1158, 1222),
            ebl=(1222, 1224), eGl=(1224, 1226))
W6 = 1226
S8 = dict(QsT=(0, 64), WT=(64, 128), QgT=(128, 192), Ks=(192, 256), V1=(256, 321), AT=(321, 385), U=(385, 449), Kd=(449, 513),
          AqkT=(513, 577), sm=(577, 578), sg=(578, 579))
W8 = 579


def bcast_rows(k, PS, dst, src2, r, sel, n=1024):
    for nb in range(0, n, 512):
        ps = PS[(nb // 512) % 2]
        k.op("pe", lambda e: e.matmul(ps[:, 0:512], sel[:, r, :], src2[:, nb:nb+512], start=True, stop=True), [sel, src2], [ps])
        k.op("dve", lambda e: e.tensor_copy(dst[:, nb:nb+512], ps[:, 0:512]), [ps], [dst])


def e_mod(k, PS, ctsb, MODW, MODB, modrow):
    k.stage_begin()
    ws = [k.sb([128, 8, 512], name="mw") for _ in range(2)]
    bi = k.sb([2, 6144], name="mb")
    k.dma(bi, bi[:], None, MODB[:, :])
    for nb in range(12):
        w = ws[nb % 2]; ps = PS[nb % 2]
        k.dma(w, w[:], None, MODW.rearrange("(c p) n -> p c n", p=128)[:, :, nb*512:(nb+1)*512], q="sp" if nb % 2 == 0 else "pool")
        for c in range(8):
            k.op("pe", lambda e: e.matmul(ps[0:2, 0:512], ctsb[:, c, :], w[:, c, :], start=(c == 0), stop=(c == 7)), [ctsb, w], [ps])
        k.op("dve", lambda e: e.tensor_add(out=modrow[:, nb*512:(nb+1)*512], in0=ps[0:2, 0:512], in1=bi[:, nb*512:(nb+1)*512]), [ps, bi], [modrow])
    k.stage_end()


def e_normlin(k, PS, X, GREP, modrow, isc, ish, sel, ident, W, N, Y, H=None, softmax=False, YT=None, QKT=None, qk=None):
    k.stage_begin()
    g = k.sb([128, 1024], name="g"); k.dma(g, g[:], None, GREP[:, :])
    A = [k.sb([128, 1024], name="A") for _ in range(2)]; sh = [k.sb([128, 1024], name="sh") for _ in range(2)]
    for cls in range(2):
        r = 1 if cls == 0 else 0
        bcast_rows(k, PS, A[cls], V(modrow, isc*1024, (isc+1)*1024), r, sel)
        bcast_rows(k, PS, sh[cls], V(modrow, ish*1024, (ish+1)*1024), r, sel)
        k.op("dve", lambda e: e.scalar_tensor_tensor(out=A[cls][:], in0=A[cls][:], scalar=1.0, in1=g[:], op0=ALU.add, op1=ALU.mult), [A[cls], g], [A[cls]])
    lowp = N > 16
    w = k.sb([128, 8, N], BF16 if lowp else F32, name="w")
    for c in range(8):
        k.dma(w, w[:, c, :], None, W[c*128:(c+1)*128, :], q="pool" if lowp else ("sp" if c % 2 == 0 else "pool"))
    xs = [k.sb([128, 1024], name="x") for _ in range(2)]; hs = [k.sb([128, 1024], name="h") for _ in range(2)]
    hT = [k.sb([128, 8, 128], BF16 if lowp else F32, name="hT") for _ in range(2)]
    ys = [k.sb([128, N], name="y") for _ in range(2)]
    st = [k.sb([128, 40], name="st") for _ in range(2)]
    if qk is not None:
        gn = k.sb([128, 640], name="gn"); k.dma(gn, gn[:], None, qk["GAIN"][:, :])
        QB = [dict(cs=k.sb([128, 320], name="cs"), sn=k.sb([128, 320], name="sn"), sq=k.sb([128, 640], name="sq"), o=k.sb([128, 640], name="o"),
                   t1=k.sb([128, 320], name="t1"), t2=k.sb([128, 320], name="t2"), oT=k.sb([128, 5, 128], BF16, name="oT")) for _ in range(1)]
        v5 = lambda ap: ap.rearrange("p (h a f j) -> p h a f j", h=10, a=2, f=2)
        v4 = lambda ap: ap.rearrange("p (h a j) -> p h a j", h=10, a=2)
    if YT is not None:
        yts = [k.sb([16, 128], name="yt") for _ in range(2)]
    nb_ = (N + 511) // 512
    pi = 0
    for rt in range(T_ // 128):
        cls = 0 if rt < 2 else 1
        x = xs[rt % 2]; h = hs[rt % 2]; s = st[rt % 2]; ht = hT[rt % 2]; y = ys[rt % 2]
        rows = slice(rt*128, (rt+1)*128)
        k.dma(x, x[:], X, X[rows, :])
        k.op("dve", lambda e: e.memset(s[:, 0:8], 0.0), [], [s])
        k.op("act", lambda e: e.activation(out=h[:], in_=x[:], func=AF.Square, accum_out=s[:, 0:1]), [x, s], [h, s])
        k.op("dve", lambda e: e.tensor_scalar(out=s[:, 1:2], in0=s[:, 0:1], scalar1=1.0/1024, scalar2=EPS, op0=ALU.mult, op1=ALU.add), [s], [s])
        k.op("act", lambda e: e.activation(out=s[:, 7:8], in_=s[:, 1:2], func=AF.Sqrt), [s], [s])
        k.op("dve", lambda e: e.reciprocal(out=s[:, 2:3], in_=s[:, 7:8]), [s], [s])
        k.op("dve", lambda e: e.scalar_tensor_tensor(out=h[:], in0=x[:], scalar=s[:, 2:3], in1=A[cls][:], op0=ALU.mult, op1=ALU.mult), [x, s, A[cls]], [h])
        k.op("pool", lambda e: e.tensor_add(out=h[:], in0=h[:], in1=sh[cls][:]), [h, sh[cls]], [h])
        if H is not None:
            k.dma(H, H[rows, :], h, h[:], q="pool", skip_w=True)
        for half in range(2):
            pt = PS[half]
            for c in range(4):
                cc = half*4 + c
                k.op("pe", lambda e: e.transpose(pt[:, c*128:(c+1)*128], h[:, cc*128:(cc+1)*128], ident[:]), [h, ident], [pt])
            if half == 0:
                k.op("act", lambda e: e.copy(ht[:, 0:4, :], pt[:].rearrange("p (c t) -> p c t", c=4)), [pt], [ht])
            else:
                k.op("dve", lambda e: e.tensor_copy(ht[:, 4:8, :], pt[:].rearrange("p (c t) -> p c t", c=4)), [pt], [ht])
        for b in range(nb_):
            n0 = b*512; n1 = min(N, n0+512); ps = PS[2 + pi % 4]; pi += 1
            for c in range(8):
                k.op("pe", lambda e: e.matmul(ps[:, 0:n1-n0], ht[:, c, :], w[:, c, n0:n1], start=(c == 0), stop=(c == 7)), [ht, w], [ps])
            if b % 2 == 0:
                k.op("dve", lambda e: e.tensor_copy(y[:, n0:n1], ps[:, 0:n1-n0]), [ps], [y])
            else:
                k.op("act", lambda e: e.copy(y[:, n0:n1], ps[:, 0:n1-n0]), [ps], [y])
        if softmax:
            k.op("dve", lambda e: e.reduce_max(out=s[:, 3:4], in_=y[:], axis=AX.X), [y], [s])
            k.op("dve", lambda e: e.tensor_scalar(out=s[:, 4:5], in0=s[:, 3:4], scalar1=-1.0, scalar2=None, op0=ALU.mult), [s], [s])
            k.op("dve", lambda e: e.memset(s[:, 5:6], 0.0), [], [s])
            k.op("act", lambda e: e.activation(out=y[:], in_=y[:], func=AF.Exp, bias=s[:, 4:5], scale=1.0, accum_out=s[:, 5:6]), [y, s], [y, s])
            k.op("dve", lambda e: e.reciprocal(out=s[:, 6:7], in_=s[:, 5:6]), [s], [s])
            k.op("dve", lambda e: e.tensor_scalar(out=y[:], in0=y[:], scalar1=s[:, 6:7], scalar2=None, op0=ALU.mult), [y, s], [y])
        if YT is not None:
            yt = yts[rt % 2]; pt = PS[6]
            k.op("pe", lambda e: e.transpose(pt[0:16, 0:128], y[:, 0:16], ident[:]), [y, ident], [pt])
            k.op("dve", lambda e: e.tensor_copy(yt[:], pt[0:16, 0:128]), [pt], [yt])
            k.dma(YT, YT[:, rows], yt, yt[:], q="pool", skip_w=True)
        if Y is not None:
            k.dma(Y, Y[rows, :], y, y[:], skip_w=True)
        if qk is not None:
            b = QB[0]; o = b['o']
            k.dma(b['cs'], b['cs'][:], None, qk["CS"][rows, :], q="pool"); k.dma(b['sn'], b['sn'][:], None, qk["SN"][rows, :], q="pool")
            xq = V(y, 0, 640)
            k.op("dve", lambda e: e.tensor_mul(out=b['sq'][:], in0=xq[:], in1=xq[:]), [y], [b['sq']])
            k.op("dve", lambda e: e.reduce_sum(out=s[:, 8:18], in_=b['sq'][:].rearrange("p (h d) -> p h d", h=10), axis=AX.X), [b['sq']], [s])
            k.op("dve", lambda e: e.tensor_scalar(out=s[:, 8:18], in0=s[:, 8:18], scalar1=1.0/64, scalar2=EPS, op0=ALU.mult, op1=ALU.add), [s], [s])
            k.op("act", lambda e: e.activation(out=s[:, 18:28], in_=s[:, 8:18], func=AF.Sqrt), [s], [s])
            k.op("dve", lambda e: e.reciprocal(out=s[:, 28:38], in_=s[:, 18:28]), [s], [s])
            for hh in range(10):
                k.op("dve" if hh % 2 == 0 else "pool", lambda e: e.tensor_scalar(out=b['sq'][:, hh*64:(hh+1)*64], in0=xq[:, hh*64:(hh+1)*64], scalar1=s[:, 28+hh:29+hh], scalar2=None, op0=ALU.mult), [y, s], [b['sq']])
            xn = b['sq']
            k.op("dve", lambda e: e.tensor_mul(out=xn[:], in0=xn[:], in1=gn[:]), [xn, gn], [xn])
            x1 = v5(xn[:])[:, :, :, 0, :]; x2 = v5(xn[:])[:, :, :, 1, :]
            o1 = v5(o[:])[:, :, :, 0, :]; o2 = v5(o[:])[:, :, :, 1, :]
            cs = v4(b['cs'][:]); sn = v4(b['sn'][:]); t1 = v4(b['t1'][:]); t2 = v4(b['t2'][:])
            k.op("dve", lambda e: e.tensor_mul(out=t1, in0=x1, in1=cs), [xn, b['cs']], [b['t1']])
            k.op("pool", lambda e: e.tensor_mul(out=t2, in0=x2, in1=sn), [xn, b['sn']], [b['t2']])
            k.op("dve", lambda e: e.tensor_sub(out=o1, in0=t1, in1=t2), [b['t1'], b['t2']], [o])
            k.op("dve", lambda e: e.tensor_mul(out=t1, in0=x2, in1=cs), [xn, b['cs']], [b['t1']])
            k.op("pool", lambda e: e.tensor_mul(out=t2, in0=x1, in1=sn), [xn, b['sn']], [b['t2']])
            k.op("dve", lambda e: e.tensor_add(out=o2, in0=t1, in1=t2), [b['t1'], b['t2']], [o])
            pt = PS[7]
            for c in range(4):
                k.op("pe", lambda e: e.transpose(pt[:, c*128:(c+1)*128], o[:, c*128:(c+1)*128], ident[:]), [o, ident], [pt])
            k.op("act", lambda e: e.copy(b['oT'][:, 0:4, :], pt[:].rearrange("p (c t) -> p c t", c=4)), [pt], [b['oT']])
            pt2 = PS[6]
            k.op("pe", lambda e: e.transpose(pt2[:, 0:128], o[:, 512:640], ident[:]), [o, ident], [pt2])
            k.op("dve", lambda e: e.tensor_copy(b['oT'][:, 4, :], pt2[:, 0:128]), [pt2], [b['oT']])
            k.dma(QKT, QKT.ap.rearrange("(c p) t -> p c t", p=128)[:, :, rows], b['oT'], b['oT'][:], q="pool", skip_w=True)
    k.stage_end()


def e_attn(k, PS, QKT, P, AXD):
    k.stage_begin()
    T = T_; NT = NT_
    kts = [k.sb([64, T], BF16, name="kt") for _ in range(2)]
    v1s = [k.sb([128, NT, 65], BF16, name="v1") for _ in range(2)]
    vtmp = k.sb([128, NT, 64], name="vtmp")
    for g in range(2):
        k.dma(kts[g], kts[g][:], QKT, QKT[512 + g*64:512 + (g+1)*64, :])
        k.op("dve", lambda e: e.memset(v1s[g][:, :, 64:65], 1.0), [], [v1s[g]])
        k.dma(vtmp, vtmp[:], P, P.ap.rearrange("(n p) c -> p n c", p=128)[:, :, 640 + g*64:640 + (g+1)*64], q="pool")
        k.op("dve", lambda e: e.tensor_copy(v1s[g][:, :, 0:64], vtmp[:]), [vtmp], [v1s[g]])
    qts = [k.sb([64, 512], BF16, name="q") for _ in range(2)]
    pts = [k.sb([128, 512], BF16, name="pt") for _ in range(3)]
    osb = [k.sb([128, 4, 64], name="osb") for _ in range(2)]
    rs = [k.sb([128, 8], name="rs") for _ in range(2)]
    pss = PS[0:2]; pso = PS[2:6]
    blocks = [(0, NCTX, 0, NCTX // 128)] + [(q0, 512, 0, NT) for q0 in range(NCTX, T, 512)]
    it = 0; bi = 0
    for h in range(8):
        g = h // 4; kt = kts[g]; v1 = v1s[g]
        for (q0, qn, k0, k1) in blocks:
            qt = qts[bi % 2]; ob = osb[bi % 2]; r = rs[bi % 2]; bi += 1
            nqs = qn // 128
            k.dma(qt, qt[:, 0:qn], QKT, QKT[h*64:(h+1)*64, q0:q0+qn])
            for kk in range(k0, k1):
                ps = pss[it % 2]; pt = pts[it % 3]; it += 1
                k.op("pe", lambda e: e.matmul(ps[:, 0:qn], kt[:, kk*128:(kk+1)*128], qt[:, 0:qn], start=True, stop=True), [kt, qt], [ps])
                k.op("act", lambda e: e.activation(out=pt[:, 0:qn], in_=ps[:, 0:qn], func=AF.Exp, scale=0.125), [ps], [pt])
                for qs in range(nqs):
                    k.op("pe", lambda e: e.matmul(pso[qs][:, 0:65], pt[:, qs*128:(qs+1)*128], v1[:, kk, :], start=(kk == k0), stop=(kk == k1-1)), [pt, v1], [pso[qs]])
            for qs in range(nqs):
                k.op("dve", lambda e: e.reciprocal(out=r[:, qs:qs+1], in_=pso[qs][:, 64:65]), [pso[qs]], [r])
                k.op("dve", lambda e: e.tensor_scalar(out=ob[:, qs, :], in0=pso[qs][:, 0:64], scalar1=r[:, qs:qs+1], scalar2=None, op0=ALU.mult), [pso[qs], r], [ob])
            k.dma(AXD, AXD[q0:q0+qn, h*64:(h+1)*64].rearrange("(s p) d -> p s d", p=128), ob, ob[:, 0:nqs, :], q="pool", skip_w=True)
    k.stage_end()


O6 = dict(Qs=(0, 128), sigo=(128, 192), qn=(192, 256), kn=(256, 320), kb=(320, 448), Rm=(448, 704), Qg=(704, 832), G=(832, 834),
          siluz=(834, 898), KG0=(898, 1028), KG1=(1028, 1158))
W6 = 1158
S8 = dict(QsT=(0, 64), WT=(64, 128), QgT=(128, 192), Ks=(192, 256), Kd=(256, 320), sm=(320, 321), sg=(321, 322), V1=(322, 387),
          AT=(387, 451), U=(451, 515), AqkT=(515, 579))
W8 = 579
FM_SRC = [(256, 320), (192, 256), (320, 384), (384, 448), (0, 64), (64, 128), None, None, (704, 768), (768, 832)]


def e_prep(k, PS, P, CB, CW, cc, ident, O6D, FMD):
    k.stage_begin()
    tf = cc[:, 2, :]; tr = cc[:, 3, :]; blk = cc[:, 8, :]
    cb = k.sb([128, 48], name="cb"); cw = k.sb([128, 5, 768], name="cw")
    k.dma(cb, cb[:, 0:32], None, CB[:, :]); k.dma(cw, cw[:], None, CW[:, :, :])
    k.op("act", lambda e: e.activation(out=cb[:, 32:40], in_=cb[:, 16:24], func=AF.Exp), [cb], [cb])
    k.op("dve", lambda e: e.tensor_scalar(out=cb[:, 32:40], in0=cb[:, 32:40], scalar1=-1.0, scalar2=None, op0=ALU.mult), [cb], [cb])
    NB = 2
    m4s = [k.sb([128, 1024], name="m4") for _ in range(NB)]; g4s = [k.sb([128, 1024], name="g4") for _ in range(NB)]
    mgs = [k.sb([128, 16], name="mg") for _ in range(NB)]; ggs = [k.sb([128, 16], name="gg") for _ in range(NB)]
    q5s = [k.sb([128, 5, 768], name="q5") for _ in range(NB)]
    cvs = [k.sb([128, 768], name="cv") for _ in range(NB)]; sqs = [k.sb([128, 512], name="sq") for _ in range(NB)]
    wks = [k.sb([128, 128], name="wk") for _ in range(NB)]
    outs = [[k.sb([128, W6], name="o6") for _ in range(4)] for _ in range(NB)]
    fmts = [[k.sb([64, 10, 128], name="fmt") for _ in range(4)] for _ in range(NB)]
    ksb = [[k.sb([128, 128], name="ksb") for _ in range(4)] for _ in range(NB)]
    psg = PS[0]
    Pt = P.rearrange("(n p) c -> n p c", p=128)
    segs = [(0, NCTX), (NCTX, T_)]
    for t in range(NT_):
        i = t % NB
        m4 = m4s[i]; g4 = g4s[i]; mg = mgs[i]; gg = ggs[i]; q5 = q5s[i]; c = cvs[i]; s2 = sqs[i]; s = wks[i]
        r0 = t*128
        k.dma(m4, m4[:], P, P[r0:r0+128, 768:1792]); k.dma(g4, g4[:], P, P[r0:r0+128, 1808:2832], q="pool")
        k.dma(mg, mg[:], P, P[r0:r0+128, 1792:1808]); k.dma(gg, gg[:], P, P[r0:r0+128, 2832:2848], q="pool")
        seg = segs[0] if r0 < NCTX else segs[1]
        edge = (r0 - 2 < seg[0]) or (r0 + 130 > seg[1])
        if edge:
            k.op("pool", lambda e: e.memset(q5[:], 0.0), [], [q5])
        for kk in range(5):
            a0 = r0 + kk - 2; a1 = a0 + 128
            lo = max(a0, seg[0]); hi = min(a1, seg[1])
            k.dma(q5, q5[lo-a0:hi-a0, kk, :], P, P[lo:hi, 1808:2576], q="sp" if kk % 2 == 0 else "pool")
        k.op("dve", lambda e: e.tensor_mul(out=q5[:], in0=q5[:], in1=cw[:]), [q5, cw], [q5])
        k.op("dve", lambda e: e.reduce_sum(out=c[:], in_=q5[:].rearrange("p k c -> p c k"), axis=AX.X), [q5], [c])
        k.op("act", lambda e: e.activation(out=c[:], in_=c[:], func=AF.Silu), [c], [c])
        k.op("dve", lambda e: e.tensor_mul(out=s2[:], in0=c[:, 0:512], in1=c[:, 0:512]), [c], [s2])
        k.op("dve", lambda e: e.reduce_sum(out=s[:, 64:72], in_=s2[:].rearrange("p (h d) -> p h d", h=8), axis=AX.X), [s2], [s])
        k.op("dve", lambda e: e.tensor_scalar(out=s[:, 64:72], in0=s[:, 64:72], scalar1=EPS, scalar2=None, op0=ALU.add), [s], [s])
        k.op("act", lambda e: e.activation(out=s[:, 72:80], in_=s[:, 64:72], func=AF.Sqrt), [s], [s])
        k.op("dve", lambda e: e.reciprocal(out=s[:, 80:88], in_=s[:, 72:80]), [s], [s])
        k.op("dve", lambda e: e.tensor_add(out=s[:, 0:8], in0=mg[:, 8:16], in1=cb[:, 8:16]), [mg, cb], [s])
        k.op("act", lambda e: e.activation(out=s[:, 8:16], in_=s[:, 0:8], func=AF.Exp, scale=-1.0), [s], [s])
        k.op("act", lambda e: e.activation(out=s[:, 8:16], in_=s[:, 8:16], func=AF.Ln, bias=1.0, scale=1.0), [s], [s])
        k.op("dve", lambda e: e.tensor_scalar(out=s[:, 8:16], in0=s[:, 8:16], scalar1=-1.0, scalar2=None, op0=ALU.mult), [s], [s])
        k.op("pe", lambda e: e.matmul(psg[:, 0:4], tf, s[:, 8:12], start=True, stop=True), [cc, s], [psg])
        k.op("pe", lambda e: e.matmul(psg[:, 4:8], tr, s[:, 12:16], start=True, stop=True), [cc, s], [psg])
        k.op("pe", lambda e: e.matmul(psg[:, 8:16], blk, s[:, 8:16], start=True, stop=True), [cc, s], [psg])
        k.op("dve", lambda e: e.tensor_copy(s[:, 16:24], psg[:, 0:8]), [psg], [s])
        k.op("act", lambda e: e.activation(out=s[:, 48:56], in_=psg[:, 8:16], func=AF.Exp), [psg], [s])
        k.op("act", lambda e: e.activation(out=s[:, 40:48], in_=s[:, 16:24], func=AF.Exp), [s], [s])
        k.op("dve", lambda e: e.tensor_add(out=s[:, 24:32], in0=mg[:, 0:8], in1=cb[:, 0:8]), [mg, cb], [s])
        k.op("dve", lambda e: e.tensor_sub(out=s[:, 24:32], in0=s[:, 24:32], in1=s[:, 16:24]), [s], [s])
        k.op("act", lambda e: e.activation(out=s[:, 32:40], in_=s[:, 24:32], func=AF.Exp), [s], [s])
        k.op("act", lambda e: e.activation(out=s[:, 88:96], in_=gg[:, 8:16], func=AF.Sigmoid), [gg], [s])
        k.op("dve", lambda e: e.tensor_add(out=s[:, 96:104], in0=gg[:, 0:8], in1=cb[:, 24:32]), [gg, cb], [s])
        k.op("act", lambda e: e.activation(out=s[:, 96:104], in_=s[:, 96:104], func=AF.Exp), [s], [s])
        k.op("act", lambda e: e.activation(out=s[:, 96:104], in_=s[:, 96:104], func=AF.Ln, bias=1.0, scale=1.0), [s], [s])
        k.op("dve", lambda e: e.tensor_mul(out=s[:, 104:112], in0=s[:, 96:104], in1=cb[:, 32:40]), [s, cb], [s])
        k.op("pe", lambda e: e.matmul(psg[:, 16:24], tf, s[:, 104:112], start=True, stop=True), [cc, s], [psg])
        k.op("pe", lambda e: e.matmul(psg[:, 24:32], tr, s[:, 104:112], start=True, stop=True), [cc, s], [psg])
        k.op("pe", lambda e: e.matmul(psg[:, 32:40], blk, s[:, 104:112], start=True, stop=True), [cc, s], [psg])
        k.op("dve", lambda e: e.tensor_copy(s[:, 112:116], psg[:, 16:20]), [psg], [s])
        k.op("dve", lambda e: e.tensor_copy(s[:, 116:120], psg[:, 28:32]), [psg], [s])
        k.op("dve", lambda e: e.tensor_sub(out=s[:, 120:124], in0=psg[:, 24:28], in1=s[:, 104:108]), [psg, s], [s])
        k.op("dve", lambda e: e.tensor_sub(out=s[:, 124:128], in0=psg[:, 20:24], in1=s[:, 108:112]), [psg, s], [s])
        k.op("act", lambda e: e.activation(out=s[:, 120:128], in_=s[:, 120:128], func=AF.Exp), [s], [s])
        k.op("act", lambda e: e.activation(out=s[:, 56:64], in_=s[:, 112:120], func=AF.Exp), [s], [s])
        k.op("act", lambda e: e.activation(out=s2[:, 0:8], in_=psg[:, 32:40], func=AF.Exp), [psg], [s2])
        for h in range(4):
            o = outs[i][h]; fmt = fmts[i][h]; kst = ksb[i][h]
            hs = slice(h*64, (h+1)*64)
            O = lambda n: o[:, O6[n][0]:O6[n][1]]
            for d in range(2):
                j = d*4 + h
                KG0 = O6["KG0"][0] + d*130
                k.op("dve", lambda e: e.tensor_scalar(out=o[:, d*64:(d+1)*64], in0=m4[:, hs], scalar1=s[:, 40+j:41+j], scalar2=None, op0=ALU.mult), [m4, s], [o])
                k.op("pool", lambda e: e.tensor_scalar(out=kst[:, d*64:(d+1)*64], in0=m4[:, 256+h*64:256+(h+1)*64], scalar1=s[:, 32+j:33+j], scalar2=0.125, op0=ALU.mult, op1=ALU.mult), [m4, s], [kst])
                k.op("pool", lambda e: e.tensor_copy(o[:, KG0:KG0+64], kst[:, d*64:(d+1)*64]), [kst], [o])
                k.op("dve", lambda e: e.tensor_copy(o[:, KG0+128:KG0+129], s[:, 48+j:49+j]), [s], [o])
                k.op("dve", lambda e: e.tensor_copy(o[:, KG0+129:KG0+130], s2[:, j:j+1]), [s2], [o])
            k.op("act", lambda e: e.activation(out=O("sigo"), in_=m4[:, 768+h*64:768+(h+1)*64], func=AF.Sigmoid), [m4], [o])
            k.op("dve", lambda e: e.tensor_scalar(out=O("qn"), in0=c[:, hs], scalar1=s[:, 80+h:81+h], scalar2=0.125, op0=ALU.mult, op1=ALU.mult), [c, s], [o])
            k.op("dve", lambda e: e.tensor_scalar(out=O("kn"), in0=c[:, 256+h*64:256+(h+1)*64], scalar1=s[:, 84+h:85+h], scalar2=None, op0=ALU.mult), [c, s], [o])
            gv = c[:, 512+h*64:512+(h+1)*64]
            for d in range(2):
                j = d*4 + h
                KG0 = O6["KG0"][0] + d*130
                kb = o[:, 320+d*64:320+(d+1)*64]
                k.op("dve", lambda e: e.tensor_scalar(out=kb, in0=O("kn"), scalar1=s[:, 88+j:89+j], scalar2=None, op0=ALU.mult), [o, s], [o])
                k.op("pool", lambda e: e.tensor_scalar(out=o[:, 448+d*128:448+d*128+64], in0=gv, scalar1=s[:, 88+j:89+j], scalar2=None, op0=ALU.mult), [c, s], [o])
                k.op("dve", lambda e: e.tensor_scalar(out=o[:, 448+d*128+64:448+(d+1)*128], in0=kb, scalar1=s[:, 56+j:57+j], scalar2=None, op0=ALU.mult), [o, s], [o])
                k.op("pool", lambda e: e.tensor_scalar(out=o[:, 704+d*64:704+(d+1)*64], in0=O("qn"), scalar1=s[:, 56+j:57+j], scalar2=None, op0=ALU.mult), [o, s], [o])
                k.op("dve", lambda e: e.tensor_scalar(out=o[:, KG0+64:KG0+128], in0=O("kn"), scalar1=s[:, 120+j:121+j], scalar2=None, op0=ALU.mult), [o, s], [o])
                k.op("dve", lambda e: e.tensor_copy(o[:, 832+d:833+d], s[:, 112+j:113+j]), [s], [o])
            k.op("act", lambda e: e.activation(out=O("siluz"), in_=g4[:, 768+h*64:768+(h+1)*64], func=AF.Silu), [g4], [o])
            k.dma(O6D, O6D[h, r0:r0+128, :], o, o[:], skip_w=True)
            pA = PS[1 + (h % 2)*3]; pB = PS[2 + (h % 2)*3]; pC = PS[3 + (h % 2)*3]
            for bi, src in enumerate(FM_SRC):
                if src is None:
                    src_ap = kst[:, (bi-6)*64:(bi-5)*64]; src_t = kst
                else:
                    src_ap = o[:, src[0]:src[1]]; src_t = o
                pp = (pA, pB, pC)[bi // 4]; off = (bi % 4)*128
                k.op("pe", lambda e: e.transpose(pp[0:64, off:off+128], src_ap, ident[:]), [src_t, ident], [pp])
            k.op("act", lambda e: e.copy(fmt[:, 0:4, :], pA[0:64, :].rearrange("p (c t) -> p c t", c=4)), [pA], [fmt])
            k.op("dve", lambda e: e.tensor_copy(fmt[:, 4:8, :], pB[0:64, :].rearrange("p (c t) -> p c t", c=4)), [pB], [fmt])
            k.op("act", lambda e: e.copy(fmt[:, 8:10, :], pC[0:64, 0:256].rearrange("p (c t) -> p c t", c=2)), [pC], [fmt])
            k.dma(FMD, FMD[h, t], fmt, fmt[:], q="pool", skip_w=True)
    k.stage_end()


def e_intra(k, PS, O6D, FMD, cc, O7D, O7T):
    k.stage_begin()
    ident, ones, trif, trir, trifs, trirs, ntrifs, ntrirs = [cc[:, i, :] for i in range(8)]
    class C: pass
    ch = []
    for c in range(4):
        o = C()
        o.P = [k.sb([128, 128], name="P") for _ in range(2)]; o.PT = [k.sb([128, 128], name="PT") for _ in range(2)]
        o.Y = [k.sb([128, 128], name="Y") for _ in range(2)]
        o.M1 = k.sb([128, 128], name="M1"); o.M2 = k.sb([128, 128], name="M2"); o.E2 = k.sb([128, 128], name="E2"); o.dg = k.sb([128, 128], name="dg")
        o.out = k.sb([128, 3, 128], name="out"); o.wt = k.sb([64, 128], name="wt")
        o.ps = [PS[c*2], PS[c*2+1]]
        ch.append(o)
    fms = [[k.sb([64, 8, 128], name="fm") for _ in range(2)] for _ in range(2)]
    tms = [[k.sb([128, 258], name="tm") for _ in range(2)] for _ in range(2)]
    for h in range(4):
      for tp in range(NT_ // 2):
        chains = []
        for j in range(2):
            t = tp*2 + j
            fm = fms[j][tp % 2]; tm = tms[j][tp % 2]
            k.dma(fm, fm[:], FMD, FMD[h, t, :, 0:8, :])
            k.dma(tm, tm[:, 0:256], O6D, O6D[h, t*128:(t+1)*128, 448:704], q="pool")
            k.dma(tm, tm[:, 256:258], O6D, O6D[h, t*128:(t+1)*128, 832:834], q="pool")
            for d in range(2):
                chains.append((ch[j*2+d], t, d, fm, tm))
        for (o, t, d, fm, tm) in chains:
            m1, m2, m3 = (ntrirs, ntrifs, trif) if d == 0 else (ntrifs, ntrirs, trir)
            gcol = tm[:, 256+d:257+d]
            k.op("dve", lambda e: e.tensor_scalar(out=o.dg[:], in0=ident, scalar1=gcol, scalar2=None, op0=ALU.mult), [cc, tm], [o.dg])
            k.op("pe", lambda e: e.matmul(o.ps[0][:, 0:128], ones, o.dg[:], start=True, stop=True), [cc, o.dg], [o.ps[0]])
            k.op("dve", lambda e: e.tensor_scalar(out=o.E2[:], in0=o.ps[0][:, 0:128], scalar1=gcol, scalar2=0.0, op0=ALU.subtract, op1=ALU.min), [o.ps[0], tm], [o.E2])
            k.op("dve", lambda e: e.tensor_scalar(out=o.M1[:], in0=o.ps[0][:, 0:128], scalar1=gcol, scalar2=0.0, op0=ALU.subtract, op1=ALU.max), [o.ps[0], tm], [o.M1])
            k.op("act", lambda e: e.activation(out=o.E2[:], in_=o.E2[:], func=AF.Exp), [o.E2], [o.E2])
            k.op("act", lambda e: e.activation(out=o.M1[:], in_=o.M1[:], func=AF.Exp, scale=-1.0), [o.M1], [o.M1])
            k.op("pool", lambda e: e.tensor_mul(out=o.M1[:], in0=o.M1[:], in1=m1), [o.M1, cc], [o.M1])
            k.op("pool", lambda e: e.tensor_mul(out=o.M2[:], in0=o.E2[:], in1=m2), [o.E2, cc], [o.M2])
            k.op("pool", lambda e: e.tensor_mul(out=o.E2[:], in0=o.E2[:], in1=m3), [o.E2, cc], [o.E2])
        for (o, t, d, fm, tm) in chains:
            mA = trif if d == 0 else trir
            knT = fm[:, 0, :]; qnT = fm[:, 1, :]; kbT = fm[:, 2+d, :]; qsT = fm[:, 4+d, :]; ksT = fm[:, 6+d, :]
            p0 = V(o.ps[0], 0, 128); p1 = V(o.ps[1], 0, 128)
            k.op("pe", lambda e: e.matmul(p0[:], kbT, knT, start=True, stop=True), [fm], [p0])
            k.op("pe", lambda e: e.matmul(p1[:], knT, kbT, start=True, stop=True), [fm], [p1])
            k.op("dve", lambda e: e.tensor_mul(out=o.P[0][:], in0=p0[:], in1=o.M1[:]), [p0, o.M1], [o.P[0]])
            k.op("dve", lambda e: e.tensor_mul(out=o.PT[0][:], in0=p1[:], in1=o.M2[:]), [p1, o.M2], [o.PT[0]])
            k.op("pe", lambda e: e.matmul(p0[:], knT, qnT, start=True, stop=True), [fm], [p0])
            k.op("pe", lambda e: e.matmul(p1[:], ksT, qsT, start=True, stop=True), [fm], [p1])
            k.op("dve", lambda e: e.tensor_mul(out=o.out[:, 1, :], in0=p0[:], in1=o.E2[:]), [p0, o.E2], [o.out])
            k.op("dve", lambda e: e.tensor_mul(out=o.out[:, 2, :], in0=p1[:], in1=mA), [p1, cc], [o.out])
            k.op("act", lambda e: e.copy(o.Y[0][:], tm[:, d*128:(d+1)*128]), [tm], [o.Y[0]])
        for m in range(6):
            a = m % 2; b = 1 - a
            for (o, t, d, fm, tm) in chains:
                p0 = V(o.ps[0], 0, 128); p1 = V(o.ps[1], 0, 128)
                k.op("pe", lambda e: e.matmul(p0[:], o.PT[a][:], o.Y[a][:], start=True, stop=True), [o.PT[a], o.Y[a]], [p0])
                dst = o.Y[b][:] if m < 5 else o.out[:, 0, :]
                dstT = o.Y[b] if m < 5 else o.out
                k.op("dve", lambda e: e.tensor_add(out=dst, in0=p0[:], in1=o.Y[a][:]), [p0, o.Y[a]], [dstT])
                if m < 5:
                    k.op("pe", lambda e: e.matmul(p1[:], o.P[a][:], o.PT[a][:], start=True, stop=True), [o.P[a], o.PT[a]], [p1])
                    k.op("act", lambda e: e.copy(o.PT[b][:], p1[:]), [p1], [o.PT[b]])
            if m < 4:
                for (o, t, d, fm, tm) in chains:
                    p0 = V(o.ps[0], 0, 128)
                    k.op("pe", lambda e: e.matmul(p0[:], o.PT[a][:], o.P[a][:], start=True, stop=True), [o.PT[a], o.P[a]], [p0])
                    k.op("act", lambda e: e.copy(o.P[b][:], p0[:]), [p0], [o.P[b]])
        for (o, t, d, fm, tm) in chains:
            k.dma(O7D, O7D[h, t*128:(t+1)*128, d, :, :], o.out, o.out[:], q="sp" if d == 0 else "pool", skip_w=True)
            p1 = V(o.ps[1], 0, 128)
            k.op("pe", lambda e: e.transpose(p1[0:64, :], o.out[:, 0, 64:128], ident), [o.out, cc], [p1])
            k.op("act", lambda e: e.copy(o.wt[:], p1[0:64, :]), [p1], [o.wt])
            k.dma(O7T, O7T[h, d, :, t*128:(t+1)*128], o.wt, o.wt[:], q="pool", skip_w=True)
    k.stage_end()


def e_stepglue(k, P, O6D, FMD, O7D, O7T, STEP):
    k.stage_begin()
    qi = 0
    def cp(dst, src, src_t):
        nonlocal qi
        k.dma(STEP, dst, src_t, src, q="sp" if qi % 2 == 0 else "pool", skip_w=True); qi += 1
    for h in range(4):
        S = STEP[h]
        S2 = S.rearrange("p (t two) d w -> p t two d w", two=2)
        o6c = O6D[h].rearrange("(c p) w -> p c w", p=64)
        o7c = O7D[h].rearrange("(c p) d j w -> p c d j w", p=64)
        o7t2 = O7D[h].rearrange("(t two p) d j w -> p t two d j w", two=2, p=64)
        pc = P.rearrange("(c p) w -> p c w", p=64)
        for d in range(2):
            for (blk_i, col) in ((4+d, 0), (8+d, 128)):
                for two in range(2):
                    cp(S2[:, :, two, d, col:col+64], FMD[h][:, :, blk_i, two*64:(two+1)*64].rearrange("t f k -> f t k"), FMD)
            cp(S[:, :, d, 64:128], O7T[h, d].rearrange("f (c k) -> f c k", k=64), O7T)
            kg0 = O6["KG0"][0] + d*130
            cp(S[:, :, d, 192:322], o6c[:, :, kg0:kg0+130], O6D)
            cp(S[:, :, d, 322:386], pc[:, :, 1280 + h*64:1280 + (h+1)*64], P)
            for two in range(2):
                cp(S2[:, :, two, d, 387:451], o7t2[:, :, two, d, 2, two*64:(two+1)*64], O7D)
                cp(S2[:, :, two, d, 515:579], o7t2[:, :, two, d, 1, two*64:(two+1)*64], O7D)
            cp(S[:, :, d, 451:515], o7c[:, :, d, 0, 0:64], O7D)
    k.stage_end()


def e_scan(k, PS, STEP, O8D):
    k.stage_begin()
    BS = 4; nblk = NC_ // BS
    bufs = [[k.sb([64, BS, W8], name="blk") for _ in range(2)] for _ in range(2)]
    obufs = [[k.sb([64, BS, 128], name="ob") for _ in range(2)] for _ in range(2)]
    C1 = [k.sb([64, 65], name="C1") for _ in range(2)]; Sg = [k.sb([64, 64], name="S") for _ in range(2)]
    tmpc = [k.sb([64, 65], name="tc") for _ in range(2)]; vnew = [k.sb([64, 64], name="vn") for _ in range(2)]
    rr = [k.sb([64, 4], name="rr") for _ in range(2)]
    psA = [PS[0], PS[1]]; psB = [PS[2], PS[3]]
    pso = [V(psA[d], 0, 65) for d in range(2)]; psc = [V(psA[d], 128, 193) for d in range(2)]
    psw = [V(psB[d], 0, 64) for d in range(2)]; pss = [V(psB[d], 128, 192) for d in range(2)]; psg = [V(psB[d], 256, 320) for d in range(2)]
    for h in range(4):
        for d in range(2):
            k.op("dve", lambda e: e.memset(C1[d][:], 0.0), [], [C1[d]])
            k.op("dve", lambda e: e.memset(Sg[d][:], 0.0), [], [Sg[d]])
        for b in range(nblk):
            mb = [b, 0 if b == 0 else nblk - b]
            B = [bufs[d][b % 2] for d in range(2)]; OB = [obufs[d][b % 2] for d in range(2)]
            for d in range(2):
                k.op("pool", lambda e: e.memset(B[d][:, :, 386:387], 1.0), [], [B[d]])
                k.dma(B[d], B[d][:, :, 0:386], STEP, STEP[h, :, mb[d]*BS:(mb[d]+1)*BS, d, 0:386], q="sp" if d == 0 else "pool")
                k.dma(B[d], B[d][:, :, 387:579], STEP, STEP[h, :, mb[d]*BS:(mb[d]+1)*BS, d, 387:579], q="sp" if d == 0 else "pool")
            for jj in range(BS):
                sl = [jj, BS - 1 - jj]
                F = lambda d, n: B[d][:, sl[d], S8[n][0]:S8[n][1]]
                R2 = range(2)
                for d in R2:
                    k.op("pe", lambda e: e.matmul(psw[d][:64], F(d, "WT"), Sg[d][:], start=True, stop=True), [B[d], Sg[d]], [psw[d]])
                for d in R2:
                    k.op("pe", lambda e: e.matmul(pso[d][:64], F(d, "QsT"), C1[d][:], start=True, stop=False), [B[d], C1[d]], [pso[d]])
                    k.op("pe", lambda e: e.matmul(pso[d][:64], F(d, "AT"), F(d, "V1"), start=False, stop=True), [B[d]], [pso[d]])
                    k.op("pe", lambda e: e.matmul(psc[d][:64], F(d, "Ks"), F(d, "V1"), start=True, stop=True), [B[d]], [psc[d]])
                for d in R2:
                    k.op("dve", lambda e: e.tensor_sub(out=vnew[d][:], in0=F(d, "U"), in1=psw[d][:64]), [B[d], psw[d]], [vnew[d]])
                for d in R2:
                    k.op("pe", lambda e: e.matmul(psg[d][:64], F(d, "QgT"), Sg[d][:], start=True, stop=False), [B[d], Sg[d]], [psg[d]])
                    k.op("pe", lambda e: e.matmul(psg[d][:64], F(d, "AqkT"), vnew[d][:], start=False, stop=True), [B[d], vnew[d]], [psg[d]])
                    k.op("pe", lambda e: e.matmul(pss[d][:64], F(d, "Kd"), vnew[d][:], start=True, stop=True), [B[d], vnew[d]], [pss[d]])
                for d in R2:
                    k.op("dve", lambda e: e.tensor_add(out=tmpc[d][:], in0=psc[d][:64], in1=C1[d][:]), [psc[d], C1[d]], [tmpc[d]])
                    k.op("dve", lambda e: e.tensor_scalar(out=C1[d][:], in0=tmpc[d][:], scalar1=F(d, "sm"), scalar2=None, op0=ALU.mult), [tmpc[d], B[d]], [C1[d]])
                    k.op("dve", lambda e: e.scalar_tensor_tensor(out=Sg[d][:], in0=Sg[d][:], scalar=F(d, "sg"), in1=pss[d][:64], op0=ALU.mult, op1=ALU.add), [Sg[d], B[d], pss[d]], [Sg[d]])
                for d in R2:
                    k.op("act", lambda e: e.activation(out=rr[d][:, 0:1], in_=pso[d][:64, 64:65], func=AF.Abs), [pso[d]], [rr[d]])
                    k.op("dve", lambda e: e.tensor_scalar(out=rr[d][:, 2:3], in0=rr[d][:, 0:1], scalar1=1.0, scalar2=None, op0=ALU.max), [rr[d]], [rr[d]])
                    k.op("dve", lambda e: e.reciprocal(out=rr[d][:, 1:2], in_=rr[d][:, 2:3]), [rr[d]], [rr[d]])
                    k.op("act", lambda e: e.activation(out=OB[d][:, sl[d], 0:64], in_=pso[d][:64, 0:64], func=AF.Copy, scale=rr[d][:, 1:2]), [pso[d], rr[d]], [OB[d]])
                    k.op("act", lambda e: e.copy(OB[d][:, sl[d], 64:128], psg[d][:64]), [psg[d]], [OB[d]])
            for d in range(2):
                k.dma(O8D, O8D[h, :, mb[d]*BS:(mb[d]+1)*BS, d, :], OB[d], OB[d][:], q="sp", skip_w=True)
    k.stage_end()


def e_outproj(k, PS, AXD, O8D, O6D, GAIN2, X, modrow, sel, ident, W, XMID):
    k.stage_begin()
    gn = k.sb([128, 512], name="gn"); w = k.sb([128, 8, 1024], BF16, name="w")
    gate = [k.sb([128, 1024], name="gate") for _ in range(2)]
    k.dma(gn, gn[:], None, GAIN2[:, :])
    for cls in range(2):
        bcast_rows(k, PS, gate[cls], V(modrow, 2*1024, 3*1024), 1 if cls == 0 else 0, sel)
    for c in range(8):
        k.dma(w, w[:, c, :], None, W[c*128:(c+1)*128, :], q="pool")
    B = [dict(mix=k.sb([128, 1024], name="mix"), a=k.sb([128, 512], name="a"), b=k.sb([128, 512], name="b"), g=k.sb([128, 512], name="g"),
              sq=k.sb([128, 512], name="sq"), s=k.sb([128, 32], name="s"), x=k.sb([128, 1024], name="x"), mT=k.sb([128, 8, 128], BF16, name="mT"),
              o=k.sb([128, 1024], name="o")) for _ in range(2)]
    pi = 0
    for t in range(NT_):
        cls = 0 if t < 2 else 1
        b = B[t % 2]; mix = b['mix']; a = b['a']; bb = b['b']; g = b['g']; s = b['s']; x = b['x']; mT = b['mT']; o = b['o']
        rows = slice(t*128, (t+1)*128)
        k.dma(mix, mix[:, 0:512], AXD, AXD[rows, :])
        k.dma(x, x[:], X, X[rows, :])
        qi = 0
        for h in range(4):
            k.dma(g, g[:, h*64:(h+1)*64], O6D, O6D[h, rows, 128:192], q="pool")
            k.dma(g, g[:, 256+h*64:256+(h+1)*64], O6D, O6D[h, rows, 834:898], q="pool")
            for d in range(2):
                dst_t = a if d == 0 else bb
                for half in range(2):
                    dst = dst_t[half*64:(half+1)*64, :].rearrange("p (g hh w) -> p g hh w", g=2, hh=4)[:, :, h, :]
                    src = O8D[h, :, 2*t + half, d, :].rearrange("p (g w) -> p g w", g=2)
                    k.dma(dst_t, dst, O8D, src, q="sp" if qi % 2 == 0 else "pool"); qi += 1
        k.op("dve", lambda e: e.tensor_add(out=a[:], in0=a[:], in1=bb[:]), [a, bb], [a])
        k.op("dve", lambda e: e.tensor_mul(out=b['sq'][:], in0=a[:], in1=a[:]), [a], [b['sq']])
        k.op("dve", lambda e: e.reduce_sum(out=s[:, 0:8], in_=b['sq'][:].rearrange("p (h d) -> p h d", h=8), axis=AX.X), [b['sq']], [s])
        k.op("dve", lambda e: e.tensor_scalar(out=s[:, 0:8], in0=s[:, 0:8], scalar1=1.0/64, scalar2=EPS, op0=ALU.mult, op1=ALU.add), [s], [s])
        k.op("act", lambda e: e.activation(out=s[:, 8:16], in_=s[:, 0:8], func=AF.Sqrt), [s], [s])
        k.op("dve", lambda e: e.reciprocal(out=s[:, 16:24], in_=s[:, 8:16]), [s], [s])
        for h in range(8):
            k.op("dve" if h % 2 == 0 else "pool", lambda e: e.tensor_scalar(out=a[:, h*64:(h+1)*64], in0=a[:, h*64:(h+1)*64], scalar1=s[:, 16+h:17+h], scalar2=None, op0=ALU.mult), [a, s], [a])
        k.op("dve", lambda e: e.tensor_mul(out=a[:], in0=a[:], in1=gn[:]), [a, gn], [a])
        k.op("dve", lambda e: e.tensor_mul(out=mix[:, 512:1024], in0=a[:], in1=g[:]), [a, g], [mix])
        for half in range(2):
            pt = PS[half]
            for c in range(4):
                cc_ = half*4 + c
                k.op("pe", lambda e: e.transpose(pt[:, c*128:(c+1)*128], mix[:, cc_*128:(cc_+1)*128], ident[:]), [mix, ident], [pt])
            if half == 0:
                k.op("act", lambda e: e.copy(mT[:, 0:4, :], pt[:].rearrange("p (c t) -> p c t", c=4)), [pt], [mT])
            else:
                k.op("dve", lambda e: e.tensor_copy(mT[:, 4:8, :], pt[:].rearrange("p (c t) -> p c t", c=4)), [pt], [mT])
        for nb in range(2):
            ps = PS[2 + pi % 4]; pi += 1
            for c in range(8):
                k.op("pe", lambda e: e.matmul(ps[:], mT[:, c, :], w[:, c, nb*512:(nb+1)*512], start=(c == 0), stop=(c == 7)), [mT, w], [ps])
            k.op("dve", lambda e: e.tensor_mul(out=o[:, nb*512:(nb+1)*512], in0=ps[:], in1=gate[cls][:, nb*512:(nb+1)*512]), [ps, gate[cls]], [o])
        k.op("pool", lambda e: e.tensor_add(out=o[:], in0=o[:], in1=x[:]), [o, x], [o])
        k.dma(XMID, XMID[rows, :], o, o[:], skip_w=True)
    k.stage_end()


def e_topk(k, AFFT, VALS, IDX):
    k.stage_begin()
    for (c0, n, kk, o0, nm) in [(NCTX, T_ - NCTX, 1024, 0, "l"), (0, NCTX, 32, 1024, "c")]:
        a = k.sb([16, n], name="a" + nm); wk = k.sb([16, n], name="w" + nm)
        vals = k.sb([16, kk], name="v" + nm); idx = k.sb([16, kk], U32, name="i" + nm)
        k.dma(a, a[:], AFFT, AFFT[:, c0:c0+n])
        k.op("dve", lambda e: e.tensor_copy(wk[:], a[:]), [a], [wk])
        for r in range(kk // 8):
            k.op("dve", lambda e: e.max(out=vals[:, r*8:(r+1)*8], in_=wk[:]), [wk], [vals])
            k.op("dve", lambda e: e.max_index(out=idx[:, r*8:(r+1)*8], in_max=vals[:, r*8:(r+1)*8], in_values=wk[:]), [vals, wk], [idx])
            k.op("dve", lambda e: e.match_replace(out=wk[:], in_to_replace=vals[:, r*8:(r+1)*8], in_values=wk[:], imm_value=-1.0), [vals, wk], [wk])
        k.dma(VALS, VALS[:, o0:o0+kk], vals, vals[:], skip_w=True); k.dma(IDX, IDX[:, o0:o0+kk], idx, idx[:], skip_w=True)
    k.stage_end()


def e_expert(k, PS, H2, VALS, IDX, W1, W3, W2, ident, ACC):
    k.stage_begin()
    z = k.sb([128, 1024], name="z")
    k.op("dve", lambda e: e.memset(z[:], 0.0), [], [z])
    for t in range(NT_):
        k.dma(ACC, ACC[t*128:(t+1)*128, :], z, z[:], q="sp" if t % 2 == 0 else "pool", skip_w=True)
    RT = 9
    parts = [(0, 9)]
    PR = 9 * 128
    w2 = k.sb([128, 16, 1024], BF16, name="w2"); vl = k.sb([128, RT], name="vl"); ix = k.sb([128, RT], U32, name="ix")
    hid = k.sb([128, 16, PR], BF16, name="hid"); xt = k.sb([128, 8, PR], BF16, name="xt")
    xg = [k.sb([128, 1024], name="xg") for _ in range(2)]
    w1c = [k.sb([128, 8, 128], BF16, name="w1c") for _ in range(3)]; w3c = [k.sb([128, 8, 128], BF16, name="w3c") for _ in range(3)]
    sg = [k.sb([128, 512], name="sg") for _ in range(2)]
    ysb = [k.sb([128, 1024], name="ysb") for _ in range(2)]
    ph1 = [PS[0], PS[1]]; ph3 = [PS[2], PS[3]]; py = [PS[4], PS[5], PS[6], PS[7]]
    ci = 0; hi = 0; yi = 0; gi = 0
    first_scatter = True
    for e_ in range(16):
        k.dma(w2, w2[:], None, W2[e_].rearrange("(c p) n -> p c n", p=128), q="pool")
        k.op("dve", lambda e: e.memset(vl[:], 0.0), [], [vl])
        k.op("pool", lambda e: e.memset(ix[:], 0), [], [ix])
        k.dma(vl, vl[:, 0:8], VALS, VALS[e_, 0:1024].rearrange("(t p) -> p t", p=128), allow_slow_non_contiguous=True)
        k.dma(vl, vl[0:32, 8:9], VALS, VALS[e_, 1024:1056].rearrange("(t p) -> p t", p=32), allow_slow_non_contiguous=True)
        k.dma(ix, ix[:, 0:8], IDX, IDX[e_, 0:1024].rearrange("(t p) -> p t", p=128), allow_slow_non_contiguous=True)
        k.dma(ix, ix[0:32, 8:9], IDX, IDX[e_, 1024:1056].rearrange("(t p) -> p t", p=32), allow_slow_non_contiguous=True)
        for (t0, t1) in parts:
            nr = (t1 - t0) * 128
            for tt in range(t0, t1):
                g_ = xg[gi % 2]; gi += 1
                k.dma(g_, g_[:], H2, H2[:, :], q="pool", element_offset=(NCTX*1024 if tt < 8 else 0),
                      indirect=dict(in_offset=bass.IndirectOffsetOnAxis(ap=ix[:, tt:tt+1], axis=0), idx_t=ix))
                lr = (tt - t0) * 128
                for half in range(2):
                    pt = py[half]
                    for c in range(4):
                        cc_ = half*4 + c
                        k.op("pe", lambda e: e.transpose(pt[:, c*128:(c+1)*128], g_[:, cc_*128:(cc_+1)*128], ident[:]), [g_, ident], [pt])
                    k.op("act" if half == 0 else "dve", (lambda e: e.copy(xt[:, 0:4, lr:lr+128], pt[:].rearrange("p (c t) -> p c t", c=4))) if half == 0 else
                         (lambda e: e.tensor_copy(xt[:, 4:8, lr:lr+128], pt[:].rearrange("p (c t) -> p c t", c=4))), [pt], [xt])
            for fc in range(16):
                a1 = w1c[ci % 3]; a3 = w3c[ci % 3]; ci += 1
                k.dma(a1, a1[:], None, W1[e_].rearrange("(c p) f -> p c f", p=128)[:, :, fc*128:(fc+1)*128], q="pool")
                k.dma(a3, a3[:], None, W3[e_].rearrange("(c p) f -> p c f", p=128)[:, :, fc*128:(fc+1)*128], q="pool")
                for rb in range(0, nr, 512):
                    rn = min(512, nr - rb)
                    p1 = ph1[hi % 2]; p3 = ph3[hi % 2]; s_ = sg[hi % 2]; hi += 1
                    for c in range(8):
                        k.op("pe", lambda e: e.matmul(p1[:, 0:rn], a1[:, c, :], xt[:, c, rb:rb+rn], start=(c == 0), stop=(c == 7)), [a1, xt], [p1])
                    for c in range(8):
                        k.op("pe", lambda e: e.matmul(p3[:, 0:rn], a3[:, c, :], xt[:, c, rb:rb+rn], start=(c == 0), stop=(c == 7)), [a3, xt], [p3])
                    k.op("act", lambda e: e.activation(out=s_[:, 0:rn], in_=p1[:, 0:rn], func=AF.Silu), [p1], [s_])
                    k.op("dve", lambda e: e.tensor_mul(out=hid[:, fc, rb:rb+rn], in0=s_[:, 0:rn], in1=p3[:, 0:rn]), [s_, p3], [hid])
            for tt in range(t0, t1):
                ys = ysb[yi % 2]; yi += 1
                lr = (tt - t0) * 128
                for nb in range(2):
                    pp = py[(yi*2 + nb) % 4]
                    for fc in range(16):
                        k.op("pe", lambda e: e.matmul(pp[:], hid[:, fc, lr:lr+128], w2[:, fc, nb*512:(nb+1)*512], start=(fc == 0), stop=(fc == 15)), [hid, w2], [pp])
                    if nb == 0:
                        k.op("dve", lambda e: e.tensor_scalar(out=ys[:, 0:512], in0=pp[:], scalar1=vl[:, tt:tt+1], scalar2=None, op0=ALU.mult), [pp, vl], [ys])
                    else:
                        k.op("act", lambda e: e.activation(out=ys[:, 512:1024], in_=pp[:], func=AF.Copy, scale=vl[:, tt:tt+1]), [pp, vl], [ys])
                if tt < 8:
                    k.dma(ACC, ACC[:, :], ys, ys[:], q="pool", compute_op=ALU.add, element_offset=NCTX*1024,
                          indirect=dict(out_offset=bass.IndirectOffsetOnAxis(ap=ix[:, tt:tt+1], axis=0), idx_t=ix))
                else:
                    k.dma(ACC, ACC[:, :], ys, ys[0:32, :], q="pool", compute_op=ALU.add, element_offset=0,
                          indirect=dict(out_offset=bass.IndirectOffsetOnAxis(ap=ix[0:32, tt:tt+1], axis=0), idx_t=ix))
    k.stage_end()


def e_combine(k, PS, XMID, ACC, modrow, sel, OUTT, out_rows0):
    k.stage_begin()
    gate = [k.sb([128, 1024], name="gate") for _ in range(2)]
    for cls in range(2):
        bcast_rows(k, PS, gate[cls], V(modrow, 5*1024, 6*1024), 1 if cls == 0 else 0, sel)
    xs = [k.sb([128, 1024], name="x") for _ in range(2)]; ac = [k.sb([128, 1024], name="ac") for _ in range(2)]
    for t in range(out_rows0 // 128, NT_):
        cls = 0 if t < 2 else 1
        x = xs[t % 2]; a = ac[t % 2]; rows = slice(t*128, (t+1)*128)
        k.dma(x, x[:], XMID, XMID[rows, :]); k.dma(a, a[:], ACC, ACC[rows, :], q="pool")
        k.op("dve", lambda e: e.tensor_mul(out=a[:], in0=a[:], in1=gate[cls][:]), [a, gate[cls]], [a])
        k.op("pool", lambda e: e.tensor_add(out=x[:], in0=x[:], in1=a[:]), [x, a], [x])
        k.dma(OUTT, OUTT[t*128 - out_rows0:(t+1)*128 - out_rows0, :], x, x[:], skip_w=True)
    k.stage_end()


def build_fused(nlayer=2, upto=None):
    k = K()
    TT = T_
    def IN(name, shape, dt=F32):
        t = T(k, k.dram_in(name, shape, dt), name); t.is_dram = True
        return t
    XCAT = IN("xcat", [TT, 1024]); CT = IN("ct", [128, 8, 2]); IDENT = IN("ident", [128, 128]); SEL = IN("sel", [2, 2, 128])
    CS = IN("cs", [TT, 320]); SN = IN("sn", [TT, 320]); CC = IN("cc", [9, 128, 128])
    L = []
    for l in range(nlayer):
        L.append(dict(MODW=IN(f"modw{l}", [1024, 6144]), MODB=IN(f"modb{l}", [2, 6144]), N1G=IN(f"n1g{l}", [128, 1024]), WIN=IN(f"win{l}", [1024, 2848]),
                      QKG=IN(f"qkg{l}", [128, 640]), CB=IN(f"cb{l}", [128, 32]), CW=IN(f"cw{l}", [128, 5, 768]), GAIN2=IN(f"gain2{l}", [128, 512]),
                      WOUT=IN(f"wout{l}", [1024, 1024]), N2G=IN(f"n2g{l}", [128, 1024]), RW=IN(f"rw{l}", [1024, 16]),
                      W1=IN(f"w1{l}", [16, 1024, 2048]), W3=IN(f"w3{l}", [16, 1024, 2048]), W2=IN(f"w2{l}", [16, 2048, 1024])))
    OUT = k.dram_out("out", [TT - NCTX, 1024])
    PS = [k.ps([128, 512], name=f"bank{i}") for i in range(8)]
    ident = k.sb([128, 128], name="ident"); sel = k.sb([2, 2, 128], name="sel"); ctsb = k.sb([128, 8, 2], name="ct")
    cc = k.sb([128, 9, 128], name="cc")
    modrow = k.sb([2, 6144], name="modrow")
    k.dma(ident, ident[:], None, IDENT[:, :]); k.dma(sel, sel[:], None, SEL[:, :, :]); k.dma(ctsb, ctsb[:], None, CT[:, :, :])
    for i in range(9):
        k.dma(cc, cc[:, i, :], None, CC[i])
    k.op("act", lambda e: e.activation(out=ctsb[:], in_=ctsb[:], func=AF.Silu), [ctsb], [ctsb])
    D = lambda n, s, dt=F32: k.dram_tmp(n, s, dt)
    P = D("P", [TT, 2848]); QKT = D("QKT", [640, TT], BF16); AXD = D("AXD", [TT, 512]); O6D = D("O6D", [4, TT, W6]); FMD = D("FMD", [4, NT_, 64, 10, 128])
    O7D = D("O7D", [4, TT, 2, 3, 128]); O7T = D("O7T", [4, 2, 64, TT]); STEP = D("STEP", [4, 64, NC_, 2, W8]); O8D = D("O8D", [4, 64, NC_, 2, 128])
    XMID = D("XMID", [TT, 1024]); H2 = D("H2", [TT, 1024]); AFFT = D("AFFT", [16, TT]); VALS = D("VALS", [16, 1152]); IDX = D("IDX", [16, 1152], U32)
    ACC = D("ACC", [TT, 1024]); XN = D("XN", [TT, 1024])
    X = XCAT
    for l in range(nlayer):
        W = L[l]
        e_mod(k, PS, ctsb, W["MODW"], W["MODB"], modrow)
        e_normlin(k, PS, X, W["N1G"], modrow, 1, 0, sel, ident, W["WIN"], 2848, P, QKT=QKT, qk=dict(GAIN=W["QKG"], CS=CS, SN=SN))
        e_attn(k, PS, QKT, P, AXD)
        e_prep(k, PS, P, W["CB"], W["CW"], cc, ident, O6D, FMD)
        e_intra(k, PS, O6D, FMD, cc, O7D, O7T)
        e_stepglue(k, P, O6D, FMD, O7D, O7T, STEP)
        e_scan(k, PS, STEP, O8D)
        e_outproj(k, PS, AXD, O8D, O6D, W["GAIN2"], X, modrow, sel, ident, W["WOUT"], XMID)
        if upto == "xmid":
            k.stage_begin(); xx = [k.sb([128, 1024], name="cpy") for _ in range(2)]
            for t in range(2, NT_):
                k.dma(xx[t % 2], xx[t % 2][:], XMID, XMID[t*128:(t+1)*128, :]); k.dma(OUT, OUT[(t-2)*128:(t-1)*128, :], xx[t % 2], xx[t % 2][:], skip_w=True)
            k.stage_end(); break
        e_normlin(k, PS, XMID, W["N2G"], modrow, 4, 3, sel, ident, W["RW"], 16, None, H=H2, softmax=True, YT=AFFT)
        e_topk(k, AFFT, VALS, IDX)
        e_expert(k, PS, H2, VALS, IDX, W["W1"], W["W3"], W["W2"], ident, ACC)
        last = (l == nlayer - 1)
        e_combine(k, PS, XMID, ACC, modrow, sel, OUT if last else XN, NCTX if last else 0)
        X = XN
    k.finish([OUT])
    k.close()
    return k

def _rep(v, p=128):
    return np.ascontiguousarray(np.broadcast_to(np.asarray(v, np.float32)[None], (p,) + tuple(np.shape(v))))

def _consts():
    f32 = np.float32
    n = 8192
    row = (np.arange(n) // 64).astype(f32); col = (np.arange(n) % 64).astype(f32)
    inv = (10000.0 ** (-np.arange(16, dtype=f32) / 16)).astype(f32)
    ang = np.stack([row, col], -1)[..., None] * inv
    tile = lambda a: np.ascontiguousarray(np.broadcast_to(a[:, None], (a.shape[0], 10, 2, 16))).reshape(a.shape[0], 320)
    cs = np.concatenate([np.ones((256, 320), f32), tile(np.cos(ang).astype(f32))]); sn = np.concatenate([np.zeros((256, 320), f32), tile(np.sin(ang).astype(f32))])
    sel = np.zeros((2, 2, 128), f32); sel[0, 0] = 1; sel[1, 1] = 1
    s = np.arange(128)[:, None]; t = np.arange(128)[None, :]
    same = (s // 64) == (t // 64)
    trif = (same & (s <= t)).astype(f32); trir = (same & (s >= t)).astype(f32); eye = np.eye(128, dtype=f32)
    cc = np.stack([eye, np.ones((128, 128), f32), trif, trir, trif - eye, trir - eye, eye - trif, eye - trir, same.astype(f32)])
    return dict(cs=cs, sn=sn, sel=sel, cc=cc, ident=eye)

def make_in_maps(inp, nlayer=2):
    f32 = np.float32
    C = _consts()
    ims = []
    for b in range(2):
        ct = np.stack([inp['c'][b], inp['c_ctx']], 1).reshape(8, 128, 2).transpose(1, 0, 2)
        m = {"xcat": np.concatenate([inp['ctx'][b], inp['x'][b]], 0), "ct": np.ascontiguousarray(ct, dtype=f32)}
        m.update(C)
        for l in range(nlayer):
            cb = np.concatenate([inp['mlstm_i_bias'][l].reshape(8), inp['mlstm_f_bias'][l].reshape(8), inp['gdn_a_log'][l].reshape(8), inp['gdn_dt_bias'][l].reshape(8)])
            m.update({f"modw{l}": inp['mod_w'][l], f"modb{l}": _rep(inp['mod_b'][l], 2), f"n1g{l}": _rep(inp['norm1_g'][l]), f"win{l}": inp['w_in'][l],
                      f"qkg{l}": _rep(np.concatenate([np.tile(inp['q_norm_g'][l], 8), np.tile(inp['k_norm_g'][l], 2)])),
                      f"cb{l}": _rep(cb), f"cw{l}": _rep(inp['gdn_conv_w'][l]),
                      f"gain2{l}": _rep(np.concatenate([inp['mlstm_out_g'][l].reshape(256), np.tile(inp['gdn_out_g'][l], 4)])),
                      f"wout{l}": inp['w_out'][l], f"n2g{l}": _rep(inp['norm2_g'][l]), f"rw{l}": inp['router_w'][l],
                      f"w1{l}": inp['w1'][l], f"w3{l}": inp['w3'][l], f"w2{l}": inp['w2'][l]})
        ims.append({k_: np.ascontiguousarray(v, dtype=f32) for k_, v in m.items()})
    return ims

_K = {}
def kernel(**inputs):
    inp = {k_: np.asarray(v, np.float32) for k_, v in inputs.items()}
    if 2 not in _K:
        _K[2] = build_fused(2)
    res = run_bass_kernel_spmd(_K[2].nc, make_in_maps(inp, 2), core_ids=[0, 1])
    return np.ascontiguousarray(np.stack([res.results[b]["out"] for b in range(2)]).astype(np.float32))
```

```python
import numpy as np
from contextlib import ExitStack
import concourse.bass as bass
import concourse.mybir as mybir
from concourse.bass_utils import run_bass_kernel_spmd

F32 = mybir.dt.float32
I32 = mybir.dt.int32
U32 = mybir.dt.uint32
BF16 = mybir.dt.bfloat16
ALU = mybir.AluOpType
AF = mybir.ActivationFunctionType
AX = mybir.AxisListType
EPS = 1e-6
SAME_ENGINE_FIFO = False


class T:
    def __init__(self, k, ap, name):
        self.k = k; self.ap = ap; self.name = name
        self.w = None; self.r = {}
        self.dsem = {}; self.dn = 0; self.is_dram = False

    def __getitem__(self, idx):
        return self.ap[idx]

    def rearrange(self, *a, **kw):
        return self.ap.rearrange(*a, **kw)


class V:
    def __init__(s, t, a0, a1):
        s.t = t; s.base = t; s.a0 = a0; s.a1 = a1

    def __getitem__(s, idx):
        return s.t.ap[:, s.a0:s.a1][idx]


class K:
    def __init__(self):
        self.nc = bass.Bass("TRN2", target_bir_lowering=False)
        self.es = ExitStack()
        self.st = None
        nc = self.nc
        self.es.enter_context(nc.allow_low_precision('bf16 matmul operands, fp32 PSUM accumulation'))
        self.eng = {"pe": nc.tensor, "act": nc.scalar, "dve": nc.vector, "pool": nc.gpsimd, "sp": nc.sync}
        self.sem = {}; self.cnt = {}
        for e in ["pe", "act", "dve", "pool"]:
            self.sem[e] = self.es.enter_context(nc.semaphore("s_" + e)); self.cnt[e] = 0
        self.seen = {e: {} for e in self.eng}
        self.ntile = 0
        self.dsems = {}
        self.free_sems = {'sp': [], 'pool': []}
        self.stage_tiles = []
        self.nsem = 0

    def dram_in(self, name, shape, dt=F32):
        return self.nc.dram_tensor(name, list(shape), dt, kind="ExternalInput").ap()

    def dram_out(self, name, shape, dt=F32):
        t = T(self, self.nc.dram_tensor(name, list(shape), dt, kind="ExternalOutput").ap(), name); t.is_dram = True
        return t

    def dram_tmp(self, name, shape, dt=F32):
        t = T(self, self.nc.dram_tensor(name, list(shape), dt, kind="Internal").ap(), name); t.is_dram = True
        return t

    def sb(self, shape, dt=F32, name=None):
        self.ntile += 1
        name = (name or "t") + f"_{self.ntile}"
        stk = self.st if self.st is not None else self.es
        h = stk.enter_context(self.nc.sbuf_tensor("S_" + name, list(shape), dt))
        t = T(self, h[:], name)
        if self.st is not None:
            self.stage_tiles.append(t)
        return t

    def ps(self, shape, dt=F32, name=None):
        self.ntile += 1
        name = (name or "p") + f"_{self.ntile}"
        h = self.es.enter_context(self.nc.psum_tensor("P_" + name, list(shape), dt))
        return T(self, h[:], name)

    def stage_begin(self):
        assert self.st is None
        self.st = ExitStack(); self.stage_tiles = []

    def stage_end(self):
        self.barrier()
        for t in self.stage_tiles:
            for q_, sm_ in t.dsem.items():
                self.free_sems[q_].append(sm_)
        self.st.close(); self.st = None; self.stage_tiles = []

    def barrier(self):
        toks = [(self.sem[c], self.cnt[c]) for c in self.sem if self.cnt[c] > 0]
        toks += [(s, 16 * n) for (s, n) in self.dsems.values() if n > 0]
        for e in self.eng:
            self._wait(e, toks)

    def _get_dsem(self, t, q):
        if q not in t.dsem:
            if self.free_sems[q]:
                t.dsem[q] = self.free_sems[q].pop()
            else:
                self.nsem += 1
                sm_ = self.es.enter_context(self.nc.semaphore(f"d{self.nsem}{q}"))
                t.dsem[q] = sm_
                self.dsems[id(sm_)] = [sm_, 0]
        return t.dsem[q]

    def _deps(self, reads, writes):
        toks = []
        for t in reads:
            if t.w is not None:
                toks.append(t.w)
        for t in writes:
            if t.w is not None:
                toks.append(t.w)
            toks.extend(t.r.values())
        return toks

    def _wait(self, e, toks, skip_sem=None):
        eng = self.eng[e]
        best = {}
        for (s, v) in toks:
            if skip_sem is not None and s is skip_sem:
                continue
            if best.get(id(s), (None, 0))[1] < v:
                best[id(s)] = (s, v)
        for (s, v) in best.values():
            if self.seen[e].get(id(s), 0) < v:
                eng.wait_ge(s, v)
                self.seen[e][id(s)] = v

    def _mark(self, tok, reads, writes):
        for t in reads:
            t.r[id(tok[0])] = tok
        for t in writes:
            t.w = tok; t.r = {}

    def op(self, e, fn, reads, writes):
        reads = [getattr(t, "base", t) for t in reads if t is not None]
        writes = [getattr(t, "base", t) for t in writes]
        toks = self._deps(reads, writes)
        self._wait(e, toks, skip_sem=self.sem[e] if (e == "pe" or SAME_ENGINE_FIFO) else None)
        ins = fn(self.eng[e])
        self.cnt[e] += 1
        ins.then_inc(self.sem[e], 1)
        tok = (self.sem[e], self.cnt[e])
        self._mark(tok, reads, writes)
        return tok

    def dma(self, out_t, out_ap, in_t, in_ap, q="sp", skip_w=False, indirect=None, **kw):
        reads = [in_t] if in_t is not None else []
        if indirect is not None and indirect.get("idx_t") is not None:
            reads.append(indirect["idx_t"])
        writes = [out_t] if out_t is not None else []
        toks = self._deps(reads, [] if skip_w else writes)
        self._wait(q, toks)
        if out_t is not None and not out_t.is_dram:
            owner = out_t
        elif in_t is not None and not in_t.is_dram:
            owner = in_t
        else:
            owner = out_t if out_t is not None else in_t
        sem = self._get_dsem(owner, q)
        ent = self.dsems[id(sem)]
        ent[1] += 1
        if indirect is None:
            ins = self.eng[q].dma_start(out=out_ap, in_=in_ap, **kw)
        else:
            ins = self.eng[q].indirect_dma_start(out=out_ap, out_offset=indirect.get("out_offset"), in_=in_ap,
                                                 in_offset=indirect.get("in_offset"), **kw)
        ins.then_inc(sem, 16)
        tok = (sem, 16 * ent[1])
        self._mark(tok, reads, writes)
        return tok

    def finish(self, out_tiles):
        self.barrier()

    def close(self):
        if self.st is not None:
            self.st.close()
        self.es.close()


CUM = [0, 512, 640, 768, 1024, 1280, 1536, 1792, 1800, 1808, 2064, 2320, 2576, 2832, 2840, 2848]
import os
T_ = int(os.environ.get('FZ_T', '8448')); NT_ = T_ // 128; NC_ = T_ // 64; NCTX = 256

OUT6 = dict(Qs=(0, 128), Ks=(128, 256), eb=(256, 258), sigo=(258, 322), qn=(322, 386), kn=(386, 450), kb=(450, 578),
            Rm=(578, 834), Qg=(834, 962), Kd=(962, 1090), G=(1090, 1092), eG=(1092, 1094), siluz=(1094, 1158), gv=(1158, 1222),
            ebl=(1222, 1224), eGl=(1224, 1226))
W6 = 1226
S8 = dict(QsT=(0, 64), WT=(64, 128), QgT=(128, 192), Ks=(192, 256), V1=(256, 321), AT=(321, 385), U=(385, 449), Kd=(449, 513),
          AqkT=(513, 577), sm=(577, 578), sg=(578, 579))
W8 = 579


def bcast_rows(k, PS, dst, src2, r, sel, n=1024):
    for nb in range(0, n, 512):
        ps = PS[(nb // 512) % 2]
        k.op("pe", lambda e: e.matmul(ps[:, 0:512], sel[:, r, :], src2[:, nb:nb+512], start=True, stop=True), [sel, src2], [ps])
        k.op("dve", lambda e: e.tensor_copy(dst[:, nb:nb+512], ps[:, 0:512]), [ps], [dst])


def e_mod(k, PS, ctsb, MODW, MODB, modrow):
    k.stage_begin()
    ws = [k.sb([128, 8, 512], name="mw") for _ in range(2)]
    bi = k.sb([2, 6144], name="mb")
    k.dma(bi, bi[:], None, MODB[:, :])
    for nb in range(12):
        w = ws[nb % 2]; ps = PS[nb % 2]
        k.dma(w, w[:], None, MODW.rearrange("(c p) n -> p c n", p=128)[:, :, nb*512:(nb+1)*512], q="sp" if nb % 2 == 0 else "pool")
        for c in range(8):
            k.op("pe", lambda e: e.matmul(ps[0:2, 0:512], ctsb[:, c, :], w[:, c, :], start=(c == 0), stop=(c == 7)), [ctsb, w], [ps])
        k.op("dve", lambda e: e.tensor_add(out=modrow[:, nb*512:(nb+1)*512], in0=ps[0:2, 0:512], in1=bi[:, nb*512:(nb+1)*512]), [ps, bi], [modrow])
    k.stage_end()


def e_normlin(k, PS, X, GREP, modrow, isc, ish, sel, ident, W, N, Y, H=None, softmax=False, YT=None, QKT=None, qk=None):
    k.stage_begin()
    g = k.sb([128, 1024], name="g"); k.dma(g, g[:], None, GREP[:, :])
    A = [k.sb([128, 1024], name="A") for _ in range(2)]; sh = [k.sb([128, 1024], name="sh") for _ in range(2)]
    for cls in range(2):
        r = 1 if cls == 0 else 0
        bcast_rows(k, PS, A[cls], V(modrow, isc*1024, (isc+1)*1024), r, sel)
        bcast_rows(k, PS, sh[cls], V(modrow, ish*1024, (ish+1)*1024), r, sel)
        k.op("dve", lambda e: e.scalar_tensor_tensor(out=A[cls][:], in0=A[cls][:], scalar=1.0, in1=g[:], op0=ALU.add, op1=ALU.mult), [A[cls], g], [A[cls]])
    lowp = N > 16
    w = k.sb([128, 8, N], BF16 if lowp else F32, name="w")
    for c in range(8):
        k.dma(w, w[:, c, :], None, W[c*128:(c+1)*128, :], q="pool" if lowp else ("sp" if c % 2 == 0 else "pool"))
    xs = [k.sb([128, 1024], name="x") for _ in range(2)]; hs = [k.sb([128, 1024], name="h") for _ in range(2)]
    hT = [k.sb([128, 8, 128], BF16 if lowp else F32, name="hT") for _ in range(2)]
    ys = [k.sb([128, N], name="y") for _ in range(2)]
    st = [k.sb([128, 40], name="st") for _ in range(2)]
    if qk is not None:
        gn = k.sb([128, 640], name="gn"); k.dma(gn, gn[:], None, qk["GAIN"][:, :])
        QB = [dict(cs=k.sb([128, 320], name="cs"), sn=k.sb([128, 320], name="sn"), sq=k.sb([128, 640], name="sq"), o=k.sb([128, 640], name="o"),
                   t1=k.sb([128, 320], name="t1"), t2=k.sb([128, 320], name="t2"), oT=k.sb([128, 5, 128], BF16, name="oT")) for _ in range(1)]
        v5 = lambda ap: ap.rearrange("p (h a f j) -> p h a f j", h=10, a=2, f=2)
        v4 = lambda ap: ap.rearrange("p (h a j) -> p h a j", h=10, a=2)
    if YT is not None:
        yts = [k.sb([16, 128], name="yt") for _ in range(2)]
    nb_ = (N + 511) // 512
    pi = 0
    for rt in range(T_ // 128):
        cls = 0 if rt < 2 else 1
        x = xs[rt % 2]; h = hs[rt % 2]; s = st[rt % 2]; ht = hT[rt % 2]; y = ys[rt % 2]
        rows = slice(rt*128, (rt+1)*128)
        k.dma(x, x[:], X, X[rows, :])
        k.op("dve", lambda e: e.memset(s[:, 0:8], 0.0), [], [s])
        k.op("act", lambda e: e.activation(out=h[:], in_=x[:], func=AF.Square, accum_out=s[:, 0:1]), [x, s], [h, s])
        k.op("dve", lambda e: e.tensor_scalar(out=s[:, 1:2], in0=s[:, 0:1], scalar1=1.0/1024, scalar2=EPS, op0=ALU.mult, op1=ALU.add), [s], [s])
        k.op("act", lambda e: e.activation(out=s[:, 7:8], in_=s[:, 1:2], func=AF.Sqrt), [s], [s])
        k.op("dve", lambda e: e.reciprocal(out=s[:, 2:3], in_=s[:, 7:8]), [s], [s])
        k.op("dve", lambda e: e.scalar_tensor_tensor(out=h[:], in0=x[:], scalar=s[:, 2:3], in1=A[cls][:], op0=ALU.mult, op1=ALU.mult), [x, s, A[cls]], [h])
        k.op("pool", lambda e: e.tensor_add(out=h[:], in0=h[:], in1=sh[cls][:]), [h, sh[cls]], [h])
        if H is not None:
            k.dma(H, H[rows, :], h, h[:], q="pool", skip_w=True)
        for half in range(2):
            pt = PS[half]
            for c in range(4):
                cc = half*4 + c
                k.op("pe", lambda e: e.transpose(pt[:, c*128:(c+1)*128], h[:, cc*128:(cc+1)*128], ident[:]), [h, ident], [pt])
            if half == 0:
                k.op("act", lambda e: e.copy(ht[:, 0:4, :], pt[:].rearrange("p (c t) -> p c t", c=4)), [pt], [ht])
            else:
                k.op("dve", lambda e: e.tensor_copy(ht[:, 4:8, :], pt[:].rearrange("p (c t) -> p c t", c=4)), [pt], [ht])
        for b in range(nb_):
            n0 = b*512; n1 = min(N, n0+512); ps = PS[2 + pi % 4]; pi += 1
            for c in range(8):
                k.op("pe", lambda e: e.matmul(ps[:, 0:n1-n0], ht[:, c, :], w[:, c, n0:n1], start=(c == 0), stop=(c == 7)), [ht, w], [ps])
            if b % 2 == 0:
                k.op("dve", lambda e: e.tensor_copy(y[:, n0:n1], ps[:, 0:n1-n0]), [ps], [y])
            else:
                k.op("act", lambda e: e.copy(y[:, n0:n1], ps[:, 0:n1-n0]), [ps], [y])
        if softmax:
            k.op("dve", lambda e: e.reduce_max(out=s[:, 3:4], in_=y[:], axis=AX.X), [y], [s])
            k.op("dve", lambda e: e.tensor_scalar(out=s[:, 4:5], in0=s[:, 3:4], scalar1=-1.0, scalar2=None, op0=ALU.mult), [s], [s])
            k.op("dve", lambda e: e.memset(s[:, 5:6], 0.0), [], [s])
            k.op("act", lambda e: e.activation(out=y[:], in_=y[:], func=AF.Exp, bias=s[:, 4:5], scale=1.0, accum_out=s[:, 5:6]), [y, s], [y, s])
            k.op("dve", lambda e: e.reciprocal(out=s[:, 6:7], in_=s[:, 5:6]), [s], [s])
            k.op("dve", lambda e: e.tensor_scalar(out=y[:], in0=y[:], scalar1=s[:, 6:7], scalar2=None, op0=ALU.mult), [y, s], [y])
        if YT is not None:
            yt = yts[rt % 2]; pt = PS[6]
            k.op("pe", lambda e: e.transpose(pt[0:16, 0:128], y[:, 0:16], ident[:]), [y, ident], [pt])
            k.op("dve", lambda e: e.tensor_copy(yt[:], pt[0:16, 0:128]), [pt], [yt])
            k.dma(YT, YT[:, rows], yt, yt[:], q="pool", skip_w=True)
        if Y is not None:
            k.dma(Y, Y[rows, :], y, y[:], skip_w=True)
        if qk is not None:
            b = QB[0]; o = b['o']
            k.dma(b['cs'], b['cs'][:], None, qk["CS"][rows, :], q="pool"); k.dma(b['sn'], b['sn'][:], None, qk["SN"][rows, :], q="pool")
            xq = V(y, 0, 640)
            k.op("dve", lambda e: e.tensor_mul(out=b['sq'][:], in0=xq[:], in1=xq[:]), [y], [b['sq']])
            k.op("dve", lambda e: e.reduce_sum(out=s[:, 8:18], in_=b['sq'][:].rearrange("p (h d) -> p h d", h=10), axis=AX.X), [b['sq']], [s])
            k.op("dve", lambda e: e.tensor_scalar(out=s[:, 8:18], in0=s[:, 8:18], scalar1=1.0/64, scalar2=EPS, op0=ALU.mult, op1=ALU.add), [s], [s])
            k.op("act", lambda e: e.activation(out=s[:, 18:28], in_=s[:, 8:18], func=AF.Sqrt), [s], [s])
            k.op("dve", lambda e: e.reciprocal(out=s[:, 28:38], in_=s[:, 18:28]), [s], [s])
            for hh in range(10):
                k.op("dve" if hh % 2 == 0 else "pool", lambda e: e.tensor_scalar(out=b['sq'][:, hh*64:(hh+1)*64], in0=xq[:, hh*64:(hh+1)*64], scalar1=s[:, 28+hh:29+hh], scalar2=None, op0=ALU.mult), [y, s], [b['sq']])
            xn = b['sq']
            k.op("dve", lambda e: e.tensor_mul(out=xn[:], in0=xn[:], in1=gn[:]), [xn, gn], [xn])
            x1 = v5(xn[:])[:, :, :, 0, :]; x2 = v5(xn[:])[:, :, :, 1, :]
            o1 = v5(o[:])[:, :, :, 0, :]; o2 = v5(o[:])[:, :, :, 1, :]
            cs = v4(b['cs'][:]); sn = v4(b['sn'][:]); t1 = v4(b['t1'][:]); t2 = v4(b['t2'][:])
            k.op("dve", lambda e: e.tensor_mul(out=t1, in0=x1, in1=cs), [xn, b['cs']], [b['t1']])
            k.op("pool", lambda e: e.tensor_mul(out=t2, in0=x2, in1=sn), [xn, b['sn']], [b['t2']])
            k.op("dve", lambda e: e.tensor_sub(out=o1, in0=t1, in1=t2), [b['t1'], b['t2']], [o])
            k.op("dve", lambda e: e.tensor_mul(out=t1, in0=x2, in1=cs), [xn, b['cs']], [b['t1']])
            k.op("pool", lambda e: e.tensor_mul(out=t2, in0=x1, in1=sn), [xn, b['sn']], [b['t2']])
            k.op("dve", lambda e: e.tensor_add(out=o2, in0=t1, in1=t2), [b['t1'], b['t2']], [o])
            pt = PS[7]
            for c in range(4):
                k.op("pe", lambda e: e.transpose(pt[:, c*128:(c+1)*128], o[:, c*128:(c+1)*128], ident[:]), [o, ident], [pt])
            k.op("act", lambda e: e.copy(b['oT'][:, 0:4, :], pt[:].rearrange("p (c t) -> p c t", c=4)), [pt], [b['oT']])
            pt2 = PS[6]
            k.op("pe", lambda e: e.transpose(pt2[:, 0:128], o[:, 512:640], ident[:]), [o, ident], [pt2])
            k.op("dve", lambda e: e.tensor_copy(b['oT'][:, 4, :], pt2[:, 0:128]), [pt2], [b['oT']])
            k.dma(QKT, QKT.ap.rearrange("(c p) t -> p c t", p=128)[:, :, rows], b['oT'], b['oT'][:], q="pool", skip_w=True)
    k.stage_end()


def e_attn(k, PS, QKT, P, AXD):
    k.stage_begin()
    T = T_; NT = NT_
    kts = [k.sb([64, T], BF16, name="kt") for _ in range(2)]
    v1s = [k.sb([128, NT, 65], BF16, name="v1") for _ in range(2)]
    vtmp = k.sb([128, NT, 64], name="vtmp")
    for g in range(2):
        k.dma(kts[g], kts[g][:], QKT, QKT[512 + g*64:512 + (g+1)*64, :])
        k.op("dve", lambda e: e.memset(v1s[g][:, :, 64:65], 1.0), [], [v1s[g]])
        k.dma(vtmp, vtmp[:], P, P.ap.rearrange("(n p) c -> p n c", p=128)[:, :, 640 + g*64:640 + (g+1)*64], q="pool")
        k.op("dve", lambda e: e.tensor_copy(v1s[g][:, :, 0:64], vtmp[:]), [vtmp], [v1s[g]])
    qts = [k.sb([64, 512], BF16, name="q") for _ in range(2)]
    pts = [k.sb([128, 512], BF16, name="pt") for _ in range(3)]
    osb = [k.sb([128, 4, 64], name="osb") for _ in range(2)]
    rs = [k.sb([128, 8], name="rs") for _ in range(2)]
    pss = PS[0:2]; pso = PS[2:6]
    blocks = [(0, NCTX, 0, NCTX // 128)] + [(q0, 512, 0, NT) for q0 in range(NCTX, T, 512)]
    it = 0; bi = 0
    pending = None
    for h in range(8):
        g = h // 4; kt = kts[g]; v1 = v1s[g]
        for (q0, qn, k0, k1) in blocks:
            qt = qts[bi % 2]; ob = osb[bi % 2]; r = rs[bi % 2]; bi += 1
            nqs = qn // 128
            k.dma(qt, qt[:, 0:qn], QKT, QKT[h*64:(h+1)*64, q0:q0+qn])
            for kk in range(k0, k1):
                ps = pss[it % 2]; pt = pts[it % 3]; it += 1
                k.op("pe", lambda e: e.matmul(ps[:, 0:qn], kt[:, kk*128:(kk+1)*128], qt[:, 0:qn], start=True, stop=True), [kt, qt], [ps])
                k.op("act", lambda e: e.activation(out=pt[:, 0:qn], in_=ps[:, 0:qn], func=AF.Exp, scale=0.125), [ps], [pt])
                if pending is not None:
                    pending()

                def mk(pt=pt, v1=v1, kk=kk, k0=k0, k1=k1, nqs=nqs, r=r, ob=ob, q0=q0, qn=qn, h=h):
                    def run():
                        for qs in range(nqs):
                            k.op("pe", lambda e: e.matmul(pso[qs][:, 0:65], pt[:, qs*128:(qs+1)*128], v1[:, kk, :], start=(kk == k0), stop=(kk == k1-1)), [pt, v1], [pso[qs]])
                        if kk == k1 - 1:
                            for qs in range(nqs):
                                k.op("dve", lambda e: e.reciprocal(out=r[:, qs:qs+1], in_=pso[qs][:, 64:65]), [pso[qs]], [r])
                                k.op("dve", lambda e: e.tensor_scalar(out=ob[:, qs, :], in0=pso[qs][:, 0:64], scalar1=r[:, qs:qs+1], scalar2=None, op0=ALU.mult), [pso[qs], r], [ob])
                            k.dma(AXD, AXD[q0:q0+qn, h*64:(h+1)*64].rearrange("(s p) d -> p s d", p=128), ob, ob[:, 0:nqs, :], q="pool", skip_w=True)
                    return run
                pending = mk()
    if pending is not None:
        pending()
    k.stage_end()


O6 = dict(Qs=(0, 128), sigo=(128, 192), qn=(192, 256), kn=(256, 320), kb=(320, 448), Rm=(448, 704), Qg=(704, 832), G=(832, 834),
          siluz=(834, 898), KG0=(898, 1028), KG1=(1028, 1158))
W6 = 1158
S8 = dict(QsT=(0, 64), WT=(64, 128), QgT=(128, 192), Ks=(192, 256), Kd=(256, 320), sm=(320, 321), sg=(321, 322), V1=(322, 387),
          AT=(387, 451), U=(451, 515), AqkT=(515, 579))
W8 = 579
FM_SRC = [(256, 320), (192, 256), (320, 384), (384, 448), (0, 64), (64, 128), None, None, (704, 768), (768, 832)]


def e_prep(k, PS, P, CB, CW, cc, ident, O6D, FMD):
    k.stage_begin()
    tf = cc[:, 2, :]; tr = cc[:, 3, :]; blk = cc[:, 8, :]
    cb = k.sb([128, 48], name="cb"); cw = k.sb([128, 5, 768], name="cw")
    k.dma(cb, cb[:, 0:32], None, CB[:, :]); k.dma(cw, cw[:], None, CW[:, :, :])
    k.op("act", lambda e: e.activation(out=cb[:, 32:40], in_=cb[:, 16:24], func=AF.Exp), [cb], [cb])
    k.op("dve", lambda e: e.tensor_scalar(out=cb[:, 32:40], in0=cb[:, 32:40], scalar1=-1.0, scalar2=None, op0=ALU.mult), [cb], [cb])
    NB = 2
    m4s = [k.sb([128, 1024], name="m4") for _ in range(NB)]; g4s = [k.sb([128, 1024], name="g4") for _ in range(NB)]
    mgs = [k.sb([128, 16], name="mg") for _ in range(NB)]; ggs = [k.sb([128, 16], name="gg") for _ in range(NB)]
    q5s = [k.sb([128, 5, 768], name="q5") for _ in range(NB)]
    cvs = [k.sb([128, 768], name="cv") for _ in range(NB)]; sqs = [k.sb([128, 512], name="sq") for _ in range(NB)]
    wks = [k.sb([128, 128], name="wk") for _ in range(NB)]
    outs = [[k.sb([128, W6], name="o6") for _ in range(4)] for _ in range(NB)]
    fmts = [[k.sb([64, 10, 128], name="fmt") for _ in range(4)] for _ in range(NB)]
    ksb = [[k.sb([128, 128], name="ksb") for _ in range(4)] for _ in range(NB)]
    psg = PS[0]
    Pt = P.rearrange("(n p) c -> n p c", p=128)
    segs = [(0, NCTX), (NCTX, T_)]
    for t in range(NT_):
        i = t % NB
        m4 = m4s[i]; g4 = g4s[i]; mg = mgs[i]; gg = ggs[i]; q5 = q5s[i]; c = cvs[i]; s2 = sqs[i]; s = wks[i]
        r0 = t*128
        k.dma(m4, m4[:], P, P[r0:r0+128, 768:1792]); k.dma(g4, g4[:], P, P[r0:r0+128, 1808:2832], q="pool")
        k.dma(mg, mg[:], P, P[r0:r0+128, 1792:1808]); k.dma(gg, gg[:], P, P[r0:r0+128, 2832:2848], q="pool")
        seg = segs[0] if r0 < NCTX else segs[1]
        edge = (r0 - 2 < seg[0]) or (r0 + 130 > seg[1])
        if edge:
            k.op("pool", lambda e: e.memset(q5[:], 0.0), [], [q5])
        for kk in range(5):
            a0 = r0 + kk - 2; a1 = a0 + 128
            lo = max(a0, seg[0]); hi = min(a1, seg[1])
            k.dma(q5, q5[lo-a0:hi-a0, kk, :], P, P[lo:hi, 1808:2576], q="sp" if kk % 2 == 0 else "pool")
        k.op("dve", lambda e: e.tensor_mul(out=q5[:], in0=q5[:], in1=cw[:]), [q5, cw], [q5])
        k.op("dve", lambda e: e.reduce_sum(out=c[:], in_=q5[:].rearrange("p k c -> p c k"), axis=AX.X), [q5], [c])
        k.op("act", lambda e: e.activation(out=c[:], in_=c[:], func=AF.Silu), [c], [c])
        k.op("dve", lambda e: e.tensor_mul(out=s2[:], in0=c[:, 0:512], in1=c[:, 0:512]), [c], [s2])
        k.op("dve", lambda e: e.reduce_sum(out=s[:, 64:72], in_=s2[:].rearrange("p (h d) -> p h d", h=8), axis=AX.X), [s2], [s])
        k.op("dve", lambda e: e.tensor_scalar(out=s[:, 64:72], in0=s[:, 64:72], scalar1=EPS, scalar2=None, op0=ALU.add), [s], [s])
        k.op("act", lambda e: e.activation(out=s[:, 72:80], in_=s[:, 64:72], func=AF.Sqrt), [s], [s])
        k.op("dve", lambda e: e.reciprocal(out=s[:, 80:88], in_=s[:, 72:80]), [s], [s])
        k.op("dve", lambda e: e.tensor_add(out=s[:, 0:8], in0=mg[:, 8:16], in1=cb[:, 8:16]), [mg, cb], [s])
        k.op("act", lambda e: e.activation(out=s[:, 8:16], in_=s[:, 0:8], func=AF.Exp, scale=-1.0), [s], [s])
        k.op("act", lambda e: e.activation(out=s[:, 8:16], in_=s[:, 8:16], func=AF.Ln, bias=1.0, scale=1.0), [s], [s])
        k.op("dve", lambda e: e.tensor_scalar(out=s[:, 8:16], in0=s[:, 8:16], scalar1=-1.0, scalar2=None, op0=ALU.mult), [s], [s])
        k.op("pe", lambda e: e.matmul(psg[:, 0:4], tf, s[:, 8:12], start=True, stop=True), [cc, s], [psg])
        k.op("pe", lambda e: e.matmul(psg[:, 4:8], tr, s[:, 12:16], start=True, stop=True), [cc, s], [psg])
        k.op("pe", lambda e: e.matmul(psg[:, 8:16], blk, s[:, 8:16], start=True, stop=True), [cc, s], [psg])
        k.op("dve", lambda e: e.tensor_copy(s[:, 16:24], psg[:, 0:8]), [psg], [s])
        k.op("act", lambda e: e.activation(out=s[:, 48:56], in_=psg[:, 8:16], func=AF.Exp), [psg], [s])
        k.op("act", lambda e: e.activation(out=s[:, 40:48], in_=s[:, 16:24], func=AF.Exp), [s], [s])
        k.op("dve", lambda e: e.tensor_add(out=s[:, 24:32], in0=mg[:, 0:8], in1=cb[:, 0:8]), [mg, cb], [s])
        k.op("dve", lambda e: e.tensor_sub(out=s[:, 24:32], in0=s[:, 24:32], in1=s[:, 16:24]), [s], [s])
        k.op("act", lambda e: e.activation(out=s[:, 32:40], in_=s[:, 24:32], func=AF.Exp), [s], [s])
        k.op("act", lambda e: e.activation(out=s[:, 88:96], in_=gg[:, 8:16], func=AF.Sigmoid), [gg], [s])
        k.op("dve", lambda e: e.tensor_add(out=s[:, 96:104], in0=gg[:, 0:8], in1=cb[:, 24:32]), [gg, cb], [s])
        k.op("act", lambda e: e.activation(out=s[:, 96:104], in_=s[:, 96:104], func=AF.Exp), [s], [s])
        k.op("act", lambda e: e.activation(out=s[:, 96:104], in_=s[:, 96:104], func=AF.Ln, bias=1.0, scale=1.0), [s], [s])
        k.op("dve", lambda e: e.tensor_mul(out=s[:, 104:112], in0=s[:, 96:104], in1=cb[:, 32:40]), [s, cb], [s])
        k.op("pe", lambda e: e.matmul(psg[:, 16:24], tf, s[:, 104:112], start=True, stop=True), [cc, s], [psg])
        k.op("pe", lambda e: e.matmul(psg[:, 24:32], tr, s[:, 104:112], start=True, stop=True), [cc, s], [psg])
        k.op("pe", lambda e: e.matmul(psg[:, 32:40], blk, s[:, 104:112], start=True, stop=True), [cc, s], [psg])
        k.op("dve", lambda e: e.tensor_copy(s[:, 112:116], psg[:, 16:20]), [psg], [s])
        k.op("dve", lambda e: e.tensor_copy(s[:, 116:120], psg[:, 28:32]), [psg], [s])
        k.op("dve", lambda e: e.tensor_sub(out=s[:, 120:124], in0=psg[:, 24:28], in1=s[:, 104:108]), [psg, s], [s])
        k.op("dve", lambda e: e.tensor_sub(out=s[:, 124:128], in0=psg[:, 20:24], in1=s[:, 108:112]), [psg, s], [s])
        k.op("act", lambda e: e.activation(out=s[:, 120:128], in_=s[:, 120:128], func=AF.Exp), [s], [s])
        k.op("act", lambda e: e.activation(out=s[:, 56:64], in_=s[:, 112:120], func=AF.Exp), [s], [s])
        k.op("act", lambda e: e.activation(out=s2[:, 0:8], in_=psg[:, 32:40], func=AF.Exp), [psg], [s2])
        for h in range(4):
            o = outs[i][h]; fmt = fmts[i][h]; kst = ksb[i][h]
            hs = slice(h*64, (h+1)*64)
            O = lambda n: o[:, O6[n][0]:O6[n][1]]
            for d in range(2):
                j = d*4 + h
                KG0 = O6["KG0"][0] + d*130
                k.op("dve", lambda e: e.tensor_scalar(out=o[:, d*64:(d+1)*64], in0=m4[:, hs], scalar1=s[:, 40+j:41+j], scalar2=None, op0=ALU.mult), [m4, s], [o])
                k.op("pool", lambda e: e.tensor_scalar(out=kst[:, d*64:(d+1)*64], in0=m4[:, 256+h*64:256+(h+1)*64], scalar1=s[:, 32+j:33+j], scalar2=0.125, op0=ALU.mult, op1=ALU.mult), [m4, s], [kst])
                k.op("pool", lambda e: e.tensor_copy(o[:, KG0:KG0+64], kst[:, d*64:(d+1)*64]), [kst], [o])
                k.op("dve", lambda e: e.tensor_copy(o[:, KG0+128:KG0+129], s[:, 48+j:49+j]), [s], [o])
                k.op("dve", lambda e: e.tensor_copy(o[:, KG0+129:KG0+130], s2[:, j:j+1]), [s2], [o])
            k.op("act", lambda e: e.activation(out=O("sigo"), in_=m4[:, 768+h*64:768+(h+1)*64], func=AF.Sigmoid), [m4], [o])
            k.op("dve", lambda e: e.tensor_scalar(out=O("qn"), in0=c[:, hs], scalar1=s[:, 80+h:81+h], scalar2=0.125, op0=ALU.mult, op1=ALU.mult), [c, s], [o])
            k.op("dve", lambda e: e.tensor_scalar(out=O("kn"), in0=c[:, 256+h*64:256+(h+1)*64], scalar1=s[:, 84+h:85+h], scalar2=None, op0=ALU.mult), [c, s], [o])
            gv = c[:, 512+h*64:512+(h+1)*64]
            for d in range(2):
                j = d*4 + h
                KG0 = O6["KG0"][0] + d*130
                kb = o[:, 320+d*64:320+(d+1)*64]
                k.op("dve", lambda e: e.tensor_scalar(out=kb, in0=O("kn"), scalar1=s[:, 88+j:89+j], scalar2=None, op0=ALU.mult), [o, s], [o])
                k.op("pool", lambda e: e.tensor_scalar(out=o[:, 448+d*128:448+d*128+64], in0=gv, scalar1=s[:, 88+j:89+j], scalar2=None, op0=ALU.mult), [c, s], [o])
                k.op("dve", lambda e: e.tensor_scalar(out=o[:, 448+d*128+64:448+(d+1)*128], in0=kb, scalar1=s[:, 56+j:57+j], scalar2=None, op0=ALU.mult), [o, s], [o])
                k.op("pool", lambda e: e.tensor_scalar(out=o[:, 704+d*64:704+(d+1)*64], in0=O("qn"), scalar1=s[:, 56+j:57+j], scalar2=None, op0=ALU.mult), [o, s], [o])
                k.op("dve", lambda e: e.tensor_scalar(out=o[:, KG0+64:KG0+128], in0=O("kn"), scalar1=s[:, 120+j:121+j], scalar2=None, op0=ALU.mult), [o, s], [o])
                k.op("dve", lambda e: e.tensor_copy(o[:, 832+d:833+d], s[:, 112+j:113+j]), [s], [o])
            k.op("act", lambda e: e.activation(out=O("siluz"), in_=g4[:, 768+h*64:768+(h+1)*64], func=AF.Silu), [g4], [o])
            k.dma(O6D, O6D[h, r0:r0+128, :], o, o[:], skip_w=True)
            pA = PS[1 + (h % 2)*3]; pB = PS[2 + (h % 2)*3]; pC = PS[3 + (h % 2)*3]
            for bi, src in enumerate(FM_SRC):
                if src is None:
                    src_ap = kst[:, (bi-6)*64:(bi-5)*64]; src_t = kst
                else:
                    src_ap = o[:, src[0]:src[1]]; src_t = o
                pp = (pA, pB, pC)[bi // 4]; off = (bi % 4)*128
                k.op("pe", lambda e: e.transpose(pp[0:64, off:off+128], src_ap, ident[:]), [src_t, ident], [pp])
            k.op("act", lambda e: e.copy(fmt[:, 0:4, :], pA[0:64, :].rearrange("p (c t) -> p c t", c=4)), [pA], [fmt])
            k.op("dve", lambda e: e.tensor_copy(fmt[:, 4:8, :], pB[0:64, :].rearrange("p (c t) -> p c t", c=4)), [pB], [fmt])
            k.op("act", lambda e: e.copy(fmt[:, 8:10, :], pC[0:64, 0:256].rearrange("p (c t) -> p c t", c=2)), [pC], [fmt])
            k.dma(FMD, FMD[h, t], fmt, fmt[:], q="pool", skip_w=True)
    k.stage_end()


def e_intra(k, PS, O6D, FMD, cc, O7D, O7T):
    k.stage_begin()
    ident, ones, trif, trir, trifs, trirs, ntrifs, ntrirs = [cc[:, i, :] for i in range(8)]
    class C: pass
    ch = []
    for c in range(4):
        o = C()
        o.P = [k.sb([128, 128], name="P") for _ in range(2)]; o.PT = [k.sb([128, 128], name="PT") for _ in range(2)]
        o.Y = [k.sb([128, 128], name="Y") for _ in range(2)]
        o.M1 = k.sb([128, 128], name="M1"); o.M2 = k.sb([128, 128], name="M2"); o.E2 = k.sb([128, 128], name="E2"); o.dg = k.sb([128, 128], name="dg")
        o.out = k.sb([128, 3, 128], name="out"); o.wt = k.sb([64, 128], name="wt")
        o.ps = [PS[c*2], PS[c*2+1]]
        ch.append(o)
    fms = [[k.sb([64, 8, 128], name="fm") for _ in range(2)] for _ in range(2)]
    tms = [[k.sb([128, 258], name="tm") for _ in range(2)] for _ in range(2)]
    for h in range(4):
      for tp in range(NT_ // 2):
        chains = []
        for j in range(2):
            t = tp*2 + j
            fm = fms[j][tp % 2]; tm = tms[j][tp % 2]
            k.dma(fm, fm[:], FMD, FMD[h, t, :, 0:8, :])
            k.dma(tm, tm[:, 0:256], O6D, O6D[h, t*128:(t+1)*128, 448:704], q="pool")
            k.dma(tm, tm[:, 256:258], O6D, O6D[h, t*128:(t+1)*128, 832:834], q="pool")
            for d in range(2):
                chains.append((ch[j*2+d], t, d, fm, tm))
        for (o, t, d, fm, tm) in chains:
            m1, m2, m3 = (ntrirs, ntrifs, trif) if d == 0 else (ntrifs, ntrirs, trir)
            gcol = tm[:, 256+d:257+d]
            k.op("dve", lambda e: e.tensor_scalar(out=o.dg[:], in0=ident, scalar1=gcol, scalar2=None, op0=ALU.mult), [cc, tm], [o.dg])
            k.op("pe", lambda e: e.matmul(o.ps[0][:, 0:128], ones, o.dg[:], start=True, stop=True), [cc, o.dg], [o.ps[0]])
            k.op("dve", lambda e: e.tensor_scalar(out=o.E2[:], in0=o.ps[0][:, 0:128], scalar1=gcol, scalar2=0.0, op0=ALU.subtract, op1=ALU.min), [o.ps[0], tm], [o.E2])
            k.op("dve", lambda e: e.tensor_scalar(out=o.M1[:], in0=o.ps[0][:, 0:128], scalar1=gcol, scalar2=0.0, op0=ALU.subtract, op1=ALU.max), [o.ps[0], tm], [o.M1])
            k.op("act", lambda e: e.activation(out=o.E2[:], in_=o.E2[:], func=AF.Exp), [o.E2], [o.E2])
            k.op("act", lambda e: e.activation(out=o.M1[:], in_=o.M1[:], func=AF.Exp, scale=-1.0), [o.M1], [o.M1])
            k.op("pool", lambda e: e.tensor_mul(out=o.M1[:], in0=o.M1[:], in1=m1), [o.M1, cc], [o.M1])
            k.op("pool", lambda e: e.tensor_mul(out=o.M2[:], in0=o.E2[:], in1=m2), [o.E2, cc], [o.M2])
            k.op("pool", lambda e: e.tensor_mul(out=o.E2[:], in0=o.E2[:], in1=m3), [o.E2, cc], [o.E2])
        for (o, t, d, fm, tm) in chains:
            mA = trif if d == 0 else trir
            knT = fm[:, 0, :]; qnT = fm[:, 1, :]; kbT = fm[:, 2+d, :]; qsT = fm[:, 4+d, :]; ksT = fm[:, 6+d, :]
            p0 = V(o.ps[0], 0, 128); p1 = V(o.ps[1], 0, 128)
            k.op("pe", lambda e: e.matmul(p0[:], kbT, knT, start=True, stop=True), [fm], [p0])
            k.op("pe", lambda e: e.matmul(p1[:], knT, kbT, start=True, stop=True), [fm], [p1])
            k.op("dve", lambda e: e.tensor_mul(out=o.P[0][:], in0=p0[:], in1=o.M1[:]), [p0, o.M1], [o.P[0]])
            k.op("dve", lambda e: e.tensor_mul(out=o.PT[0][:], in0=p1[:], in1=o.M2[:]), [p1, o.M2], [o.PT[0]])
            k.op("pe", lambda e: e.matmul(p0[:], knT, qnT, start=True, stop=True), [fm], [p0])
            k.op("pe", lambda e: e.matmul(p1[:], ksT, qsT, start=True, stop=True), [fm], [p1])
            k.op("dve", lambda e: e.tensor_mul(out=o.out[:, 1, :], in0=p0[:], in1=o.E2[:]), [p0, o.E2], [o.out])
            k.op("dve", lambda e: e.tensor_mul(out=o.out[:, 2, :], in0=p1[:], in1=mA), [p1, cc], [o.out])
            k.op("act", lambda e: e.copy(o.Y[0][:], tm[:, d*128:(d+1)*128]), [tm], [o.Y[0]])
        for m in range(6):
            a = m % 2; b = 1 - a
            for (o, t, d, fm, tm) in chains:
                p0 = V(o.ps[0], 0, 128); p1 = V(o.ps[1], 0, 128)
                k.op("pe", lambda e: e.matmul(p0[:], o.PT[a][:], o.Y[a][:], start=True, stop=True), [o.PT[a], o.Y[a]], [p0])
                dst = o.Y[b][:] if m < 5 else o.out[:, 0, :]
                dstT = o.Y[b] if m < 5 else o.out
                k.op("dve", lambda e: e.tensor_add(out=dst, in0=p0[:], in1=o.Y[a][:]), [p0, o.Y[a]], [dstT])
                if m < 5:
                    k.op("pe", lambda e: e.matmul(p1[:], o.P[a][:], o.PT[a][:], start=True, stop=True), [o.P[a], o.PT[a]], [p1])
                    k.op("act", lambda e: e.copy(o.PT[b][:], p1[:]), [p1], [o.PT[b]])
            if m < 4:
                for (o, t, d, fm, tm) in chains:
                    p0 = V(o.ps[0], 0, 128)
                    k.op("pe", lambda e: e.matmul(p0[:], o.PT[a][:], o.P[a][:], start=True, stop=True), [o.PT[a], o.P[a]], [p0])
                    k.op("act", lambda e: e.copy(o.P[b][:], p0[:]), [p0], [o.P[b]])
        for (o, t, d, fm, tm) in chains:
            k.dma(O7D, O7D[h, t*128:(t+1)*128, d, :, :], o.out, o.out[:], q="sp" if d == 0 else "pool", skip_w=True)
            p1 = V(o.ps[1], 0, 128)
            k.op("pe", lambda e: e.transpose(p1[0:64, :], o.out[:, 0, 64:128], ident), [o.out, cc], [p1])
            k.op("act", lambda e: e.copy(o.wt[:], p1[0:64, :]), [p1], [o.wt])
            k.dma(O7T, O7T[h, d, :, t*128:(t+1)*128], o.wt, o.wt[:], q="pool", skip_w=True)
    k.stage_end()


def e_stepglue(k, P, O6D, FMD, O7D, O7T, STEP):
    k.stage_begin()
    qi = 0
    def cp(dst, src, src_t):
        nonlocal qi
        k.dma(STEP, dst, src_t, src, q="sp" if qi % 2 == 0 else "pool", skip_w=True); qi += 1
    for h in range(4):
        S = STEP[h]
        S2 = S.rearrange("p (t two) d w -> p t two d w", two=2)
        o6c = O6D[h].rearrange("(c p) w -> p c w", p=64)
        o7c = O7D[h].rearrange("(c p) d j w -> p c d j w", p=64)
        o7t2 = O7D[h].rearrange("(t two p) d j w -> p t two d j w", two=2, p=64)
        pc = P.rearrange("(c p) w -> p c w", p=64)
        for d in range(2):
            for (blk_i, col) in ((4+d, 0), (8+d, 128)):
                for two in range(2):
                    cp(S2[:, :, two, d, col:col+64], FMD[h][:, :, blk_i, two*64:(two+1)*64].rearrange("t f k -> f t k"), FMD)
            cp(S[:, :, d, 64:128], O7T[h, d].rearrange("f (c k) -> f c k", k=64), O7T)
            kg0 = O6["KG0"][0] + d*130
            cp(S[:, :, d, 192:322], o6c[:, :, kg0:kg0+130], O6D)
            cp(S[:, :, d, 322:386], pc[:, :, 1280 + h*64:1280 + (h+1)*64], P)
            for two in range(2):
                cp(S2[:, :, two, d, 387:451], o7t2[:, :, two, d, 2, two*64:(two+1)*64], O7D)
                cp(S2[:, :, two, d, 515:579], o7t2[:, :, two, d, 1, two*64:(two+1)*64], O7D)
            cp(S[:, :, d, 451:515], o7c[:, :, d, 0, 0:64], O7D)
    k.stage_end()


def e_scan(k, PS, STEP, O8D):
    k.stage_begin()
    BS = 4; nblk = NC_ // BS
    bufs = [[k.sb([64, BS, W8], name="blk") for _ in range(2)] for _ in range(2)]
    obufs = [[k.sb([64, BS, 128], name="ob") for _ in range(2)] for _ in range(2)]
    C1 = [k.sb([64, 65], name="C1") for _ in range(2)]; Sg = [k.sb([64, 64], name="S") for _ in range(2)]
    tmpc = [k.sb([64, 65], name="tc") for _ in range(2)]; vnew = [k.sb([64, 64], name="vn") for _ in range(2)]
    rr = [k.sb([64, 4], name="rr") for _ in range(2)]
    psA = [PS[0], PS[1]]; psB = [PS[2], PS[3]]
    pso = [V(psA[d], 0, 65) for d in range(2)]; psc = [V(psA[d], 128, 193) for d in range(2)]
    psw = [V(psB[d], 0, 64) for d in range(2)]; pss = [V(psB[d], 128, 192) for d in range(2)]; psg = [V(psB[d], 256, 320) for d in range(2)]
    for h in range(4):
        for d in range(2):
            k.op("dve", lambda e: e.memset(C1[d][:], 0.0), [], [C1[d]])
            k.op("dve", lambda e: e.memset(Sg[d][:], 0.0), [], [Sg[d]])
        for b in range(nblk):
            mb = [b, 0 if b == 0 else nblk - b]
            B = [bufs[d][b % 2] for d in range(2)]; OB = [obufs[d][b % 2] for d in range(2)]
            for d in range(2):
                k.op("pool", lambda e: e.memset(B[d][:, :, 386:387], 1.0), [], [B[d]])
                k.dma(B[d], B[d][:, :, 0:386], STEP, STEP[h, :, mb[d]*BS:(mb[d]+1)*BS, d, 0:386], q="sp" if d == 0 else "pool")
                k.dma(B[d], B[d][:, :, 387:579], STEP, STEP[h, :, mb[d]*BS:(mb[d]+1)*BS, d, 387:579], q="sp" if d == 0 else "pool")
            for jj in range(BS):
                sl = [jj, BS - 1 - jj]
                F = lambda d, n: B[d][:, sl[d], S8[n][0]:S8[n][1]]
                R2 = range(2)
                for d in R2:
                    k.op("pe", lambda e: e.matmul(psw[d][:64], F(d, "WT"), Sg[d][:], start=True, stop=True), [B[d], Sg[d]], [psw[d]])
                for d in R2:
                    k.op("pe", lambda e: e.matmul(pso[d][:64], F(d, "QsT"), C1[d][:], start=True, stop=False), [B[d], C1[d]], [pso[d]])
                    k.op("pe", lambda e: e.matmul(pso[d][:64], F(d, "AT"), F(d, "V1"), start=False, stop=True), [B[d]], [pso[d]])
                    k.op("pe", lambda e: e.matmul(psc[d][:64], F(d, "Ks"), F(d, "V1"), start=True, stop=True), [B[d]], [psc[d]])
                for d in R2:
                    k.op("dve", lambda e: e.tensor_sub(out=vnew[d][:], in0=F(d, "U"), in1=psw[d][:64]), [B[d], psw[d]], [vnew[d]])
                for d in R2:
                    k.op("pe", lambda e: e.matmul(psg[d][:64], F(d, "QgT"), Sg[d][:], start=True, stop=False), [B[d], Sg[d]], [psg[d]])
                    k.op("pe", lambda e: e.matmul(psg[d][:64], F(d, "AqkT"), vnew[d][:], start=False, stop=True), [B[d], vnew[d]], [psg[d]])
                    k.op("pe", lambda e: e.matmul(pss[d][:64], F(d, "Kd"), vnew[d][:], start=True, stop=True), [B[d], vnew[d]], [pss[d]])
                for d in R2:
                    k.op("dve", lambda e: e.tensor_add(out=tmpc[d][:], in0=psc[d][:64], in1=C1[d][:]), [psc[d], C1[d]], [tmpc[d]])
                    k.op("dve", lambda e: e.tensor_scalar(out=C1[d][:], in0=tmpc[d][:], scalar1=F(d, "sm"), scalar2=None, op0=ALU.mult), [tmpc[d], B[d]], [C1[d]])
                    k.op("dve", lambda e: e.scalar_tensor_tensor(out=Sg[d][:], in0=Sg[d][:], scalar=F(d, "sg"), in1=pss[d][:64], op0=ALU.mult, op1=ALU.add), [Sg[d], B[d], pss[d]], [Sg[d]])
                for d in R2:
                    k.op("act", lambda e: e.activation(out=rr[d][:, 0:1], in_=pso[d][:64, 64:65], func=AF.Abs), [pso[d]], [rr[d]])
                    k.op("dve", lambda e: e.tensor_scalar(out=rr[d][:, 2:3], in0=rr[d][:, 0:1], scalar1=1.0, scalar2=None, op0=ALU.max), [rr[d]], [rr[d]])
                    k.op("dve", lambda e: e.reciprocal(out=rr[d][:, 1:2], in_=rr[d][:, 2:3]), [rr[d]], [rr[d]])
                    k.op("act", lambda e: e.activation(out=OB[d][:, sl[d], 0:64], in_=pso[d][:64, 0:64], func=AF.Copy, scale=rr[d][:, 1:2]), [pso[d], rr[d]], [OB[d]])
                    k.op("act", lambda e: e.copy(OB[d][:, sl[d], 64:128], psg[d][:64]), [psg[d]], [OB[d]])
            for d in range(2):
                k.dma(O8D, O8D[h, :, mb[d]*BS:(mb[d]+1)*BS, d, :], OB[d], OB[d][:], q="sp", skip_w=True)
    k.stage_end()


def e_outproj(k, PS, AXD, O8D, O6D, GAIN2, X, modrow, sel, ident, W, XMID):
    k.stage_begin()
    gn = k.sb([128, 512], name="gn"); w = k.sb([128, 8, 1024], BF16, name="w")
    gate = [k.sb([128, 1024], name="gate") for _ in range(2)]
    k.dma(gn, gn[:], None, GAIN2[:, :])
    for cls in range(2):
        bcast_rows(k, PS, gate[cls], V(modrow, 2*1024, 3*1024), 1 if cls == 0 else 0, sel)
    for c in range(8):
        k.dma(w, w[:, c, :], None, W[c*128:(c+1)*128, :], q="pool")
    B = [dict(mix=k.sb([128, 1024], name="mix"), a=k.sb([128, 512], name="a"), b=k.sb([128, 512], name="b"), g=k.sb([128, 512], name="g"),
              sq=k.sb([128, 512], name="sq"), s=k.sb([128, 32], name="s"), x=k.sb([128, 1024], name="x"), mT=k.sb([128, 8, 128], BF16, name="mT"),
              o=k.sb([128, 1024], name="o")) for _ in range(2)]
    pi = 0
    for t in range(NT_):
        cls = 0 if t < 2 else 1
        b = B[t % 2]; mix = b['mix']; a = b['a']; bb = b['b']; g = b['g']; s = b['s']; x = b['x']; mT = b['mT']; o = b['o']
        rows = slice(t*128, (t+1)*128)
        k.dma(mix, mix[:, 0:512], AXD, AXD[rows, :])
        k.dma(x, x[:], X, X[rows, :])
        qi = 0
        for h in range(4):
            k.dma(g, g[:, h*64:(h+1)*64], O6D, O6D[h, rows, 128:192], q="pool")
            k.dma(g, g[:, 256+h*64:256+(h+1)*64], O6D, O6D[h, rows, 834:898], q="pool")
            for d in range(2):
                dst_t = a if d == 0 else bb
                for half in range(2):
                    dst = dst_t[half*64:(half+1)*64, :].rearrange("p (g hh w) -> p g hh w", g=2, hh=4)[:, :, h, :]
                    src = O8D[h, :, 2*t + half, d, :].rearrange("p (g w) -> p g w", g=2)
                    k.dma(dst_t, dst, O8D, src, q="sp" if qi % 2 == 0 else "pool"); qi += 1
        k.op("dve", lambda e: e.tensor_add(out=a[:], in0=a[:], in1=bb[:]), [a, bb], [a])
        k.op("dve", lambda e: e.tensor_mul(out=b['sq'][:], in0=a[:], in1=a[:]), [a], [b['sq']])
        k.op("dve", lambda e: e.reduce_sum(out=s[:, 0:8], in_=b['sq'][:].rearrange("p (h d) -> p h d", h=8), axis=AX.X), [b['sq']], [s])
        k.op("dve", lambda e: e.tensor_scalar(out=s[:, 0:8], in0=s[:, 0:8], scalar1=1.0/64, scalar2=EPS, op0=ALU.mult, op1=ALU.add), [s], [s])
        k.op("act", lambda e: e.activation(out=s[:, 8:16], in_=s[:, 0:8], func=AF.Sqrt), [s], [s])
        k.op("dve", lambda e: e.reciprocal(out=s[:, 16:24], in_=s[:, 8:16]), [s], [s])
        for h in range(8):
            k.op("dve" if h % 2 == 0 else "pool", lambda e: e.tensor_scalar(out=a[:, h*64:(h+1)*64], in0=a[:, h*64:(h+1)*64], scalar1=s[:, 16+h:17+h], scalar2=None, op0=ALU.mult), [a, s], [a])
        k.op("dve", lambda e: e.tensor_mul(out=a[:], in0=a[:], in1=gn[:]), [a, gn], [a])
        k.op("dve", lambda e: e.tensor_mul(out=mix[:, 512:1024], in0=a[:], in1=g[:]), [a, g], [mix])
        for half in range(2):
            pt = PS[half]
            for c in range(4):
                cc_ = half*4 + c
                k.op("pe", lambda e: e.transpose(pt[:, c*128:(c+1)*128], mix[:, cc_*128:(cc_+1)*128], ident[:]), [mix, ident], [pt])
            if half == 0:
                k.op("act", lambda e: e.copy(mT[:, 0:4, :], pt[:].rearrange("p (c t) -> p c t", c=4)), [pt], [mT])
            else:
                k.op("dve", lambda e: e.tensor_copy(mT[:, 4:8, :], pt[:].rearrange("p (c t) -> p c t", c=4)), [pt], [mT])
        for nb in range(2):
            ps = PS[2 + pi % 4]; pi += 1
            for c in range(8):
                k.op("pe", lambda e: e.matmul(ps[:], mT[:, c, :], w[:, c, nb*512:(nb+1)*512], start=(c == 0), stop=(c == 7)), [mT, w], [ps])
            k.op("dve", lambda e: e.tensor_mul(out=o[:, nb*512:(nb+1)*512], in0=ps[:], in1=gate[cls][:, nb*512:(nb+1)*512]), [ps, gate[cls]], [o])
        k.op("pool", lambda e: e.tensor_add(out=o[:], in0=o[:], in1=x[:]), [o, x], [o])
        k.dma(XMID, XMID[rows, :], o, o[:], skip_w=True)
    k.stage_end()


def e_topk(k, AFFT, VALS, IDX):
    k.stage_begin()
    for (c0, n, kk, o0, nm) in [(NCTX, T_ - NCTX, 1024, 0, "l"), (0, NCTX, 32, 1024, "c")]:
        a = k.sb([16, n], name="a" + nm); wk = k.sb([16, n], name="w" + nm)
        vals = k.sb([16, kk], name="v" + nm); idx = k.sb([16, kk], U32, name="i" + nm)
        k.dma(a, a[:], AFFT, AFFT[:, c0:c0+n])
        k.op("dve", lambda e: e.tensor_copy(wk[:], a[:]), [a], [wk])
        for r in range(kk // 8):
            k.op("dve", lambda e: e.max(out=vals[:, r*8:(r+1)*8], in_=wk[:]), [wk], [vals])
            k.op("dve", lambda e: e.max_index(out=idx[:, r*8:(r+1)*8], in_max=vals[:, r*8:(r+1)*8], in_values=wk[:]), [vals, wk], [idx])
            k.op("dve", lambda e: e.match_replace(out=wk[:], in_to_replace=vals[:, r*8:(r+1)*8], in_values=wk[:], imm_value=-1.0), [vals, wk], [wk])
        k.dma(VALS, VALS[:, o0:o0+kk], vals, vals[:], skip_w=True); k.dma(IDX, IDX[:, o0:o0+kk], idx, idx[:], skip_w=True)
    k.stage_end()


def e_expert(k, PS, H2, VALS, IDX, W1, W3, W2, ident, ACC):
    k.stage_begin()
    z = k.sb([128, 1024], name="z")
    k.op("dve", lambda e: e.memset(z[:], 0.0), [], [z])
    for t in range(NT_):
        k.dma(ACC, ACC[t*128:(t+1)*128, :], z, z[:], q="sp" if t % 2 == 0 else "pool", skip_w=True)
    RT = 9
    parts = [(0, 9)]
    PR = 9 * 128
    w2 = k.sb([128, 16, 1024], BF16, name="w2"); vl = k.sb([128, RT], name="vl"); ix = k.sb([128, RT], U32, name="ix")
    hid = k.sb([128, 16, PR], BF16, name="hid"); xt = k.sb([128, 8, PR], BF16, name="xt")
    xg = [k.sb([128, 1024], name="xg") for _ in range(2)]
    w1c = [k.sb([128, 8, 128], BF16, name="w1c") for _ in range(3)]; w3c = [k.sb([128, 8, 128], BF16, name="w3c") for _ in range(3)]
    sg = [k.sb([128, 512], name="sg") for _ in range(2)]
    ysb = [k.sb([128, 1024], name="ysb") for _ in range(2)]
    ph1 = [PS[0], PS[1]]; ph3 = [PS[2], PS[3]]; py = [PS[4], PS[5], PS[6], PS[7]]
    ci = 0; hi = 0; yi = 0; gi = 0
    first_scatter = True
    for e_ in range(16):
        k.dma(w2, w2[:], None, W2[e_].rearrange("(c p) n -> p c n", p=128), q="pool")
        k.op("dve", lambda e: e.memset(vl[:], 0.0), [], [vl])
        k.op("pool", lambda e: e.memset(ix[:], 0), [], [ix])
        k.dma(vl, vl[:, 0:8], VALS, VALS[e_, 0:1024].rearrange("(t p) -> p t", p=128), allow_slow_non_contiguous=True)
        k.dma(vl, vl[0:32, 8:9], VALS, VALS[e_, 1024:1056].rearrange("(t p) -> p t", p=32), allow_slow_non_contiguous=True)
        k.dma(ix, ix[:, 0:8], IDX, IDX[e_, 0:1024].rearrange("(t p) -> p t", p=128), allow_slow_non_contiguous=True)
        k.dma(ix, ix[0:32, 8:9], IDX, IDX[e_, 1024:1056].rearrange("(t p) -> p t", p=32), allow_slow_non_contiguous=True)
        for (t0, t1) in parts:
            nr = (t1 - t0) * 128
            for tt in range(t0, t1):
                g_ = xg[gi % 2]; gi += 1
                k.dma(g_, g_[:], H2, H2[:, :], q="pool", element_offset=(NCTX*1024 if tt < 8 else 0),
                      indirect=dict(in_offset=bass.IndirectOffsetOnAxis(ap=ix[:, tt:tt+1], axis=0), idx_t=ix))
                lr = (tt - t0) * 128
                for half in range(2):
                    pt = py[half]
                    for c in range(4):
                        cc_ = half*4 + c
                        k.op("pe", lambda e: e.transpose(pt[:, c*128:(c+1)*128], g_[:, cc_*128:(cc_+1)*128], ident[:]), [g_, ident], [pt])
                    k.op("act" if half == 0 else "dve", (lambda e: e.copy(xt[:, 0:4, lr:lr+128], pt[:].rearrange("p (c t) -> p c t", c=4))) if half == 0 else
                         (lambda e: e.tensor_copy(xt[:, 4:8, lr:lr+128], pt[:].rearrange("p (c t) -> p c t", c=4))), [pt], [xt])
            for fc in range(16):
                a1 = w1c[ci % 3]; a3 = w3c[ci % 3]; ci += 1
                k.dma(a1, a1[:], None, W1[e_].rearrange("(c p) f -> p c f", p=128)[:, :, fc*128:(fc+1)*128], q="pool")
                k.dma(a3, a3[:], None, W3[e_].rearrange("(c p) f -> p c f", p=128)[:, :, fc*128:(fc+1)*128], q="pool")
                for rb in range(0, nr, 512):
                    rn = min(512, nr - rb)
                    p1 = ph1[hi % 2]; p3 = ph3[hi % 2]; s_ = sg[hi % 2]; hi += 1
                    for c in range(8):
                        k.op("pe", lambda e: e.matmul(p1[:, 0:rn], a1[:, c, :], xt[:, c, rb:rb+rn], start=(c == 0), stop=(c == 7)), [a1, xt], [p1])
                    for c in range(8):
                        k.op("pe", lambda e: e.matmul(p3[:, 0:rn], a3[:, c, :], xt[:, c, rb:rb+rn], start=(c == 0), stop=(c == 7)), [a3, xt], [p3])
                    k.op("act", lambda e: e.activation(out=s_[:, 0:rn], in_=p1[:, 0:rn], func=AF.Silu), [p1], [s_])
                    k.op("dve", lambda e: e.tensor_mul(out=hid[:, fc, rb:rb+rn], in0=s_[:, 0:rn], in1=p3[:, 0:rn]), [s_, p3], [hid])
            for tt in range(t0, t1):
                ys = ysb[yi % 2]; yi += 1
                lr = (tt - t0) * 128
                for nb in range(2):
                    pp = py[(yi*2 + nb) % 4]
                    for fc in range(16):
                        k.op("pe", lambda e: e.matmul(pp[:], hid[:, fc, lr:lr+128], w2[:, fc, nb*512:(nb+1)*512], start=(fc == 0), stop=(fc == 15)), [hid, w2], [pp])
                    if nb == 0:
                        k.op("dve", lambda e: e.tensor_scalar(out=ys[:, 0:512], in0=pp[:], scalar1=vl[:, tt:tt+1], scalar2=None, op0=ALU.mult), [pp, vl], [ys])
                    else:
                        k.op("act", lambda e: e.activation(out=ys[:, 512:1024], in_=pp[:], func=AF.Copy, scale=vl[:, tt:tt+1]), [pp, vl], [ys])
                if tt < 8:
                    k.dma(ACC, ACC[:, :], ys, ys[:], q="pool", compute_op=ALU.add, element_offset=NCTX*1024,
                          indirect=dict(out_offset=bass.IndirectOffsetOnAxis(ap=ix[:, tt:tt+1], axis=0), idx_t=ix))
                else:
                    k.dma(ACC, ACC[:, :], ys, ys[0:32, :], q="pool", compute_op=ALU.add, element_offset=0,
                          indirect=dict(out_offset=bass.IndirectOffsetOnAxis(ap=ix[0:32, tt:tt+1], axis=0), idx_t=ix))
    k.stage_end()


def e_combine(k, PS, XMID, ACC, modrow, sel, OUTT, out_rows0):
    k.stage_begin()
    gate = [k.sb([128, 1024], name="gate") for _ in range(2)]
    for cls in range(2):
        bcast_rows(k, PS, gate[cls], V(modrow, 5*1024, 6*1024), 1 if cls == 0 else 0, sel)
    xs = [k.sb([128, 1024], name="x") for _ in range(2)]; ac = [k.sb([128, 1024], name="ac") for _ in range(2)]
    for t in range(out_rows0 // 128, NT_):
        cls = 0 if t < 2 else 1
        x = xs[t % 2]; a = ac[t % 2]; rows = slice(t*128, (t+1)*128)
        k.dma(x, x[:], XMID, XMID[rows, :]); k.dma(a, a[:], ACC, ACC[rows, :], q="pool")
        k.op("dve", lambda e: e.tensor_mul(out=a[:], in0=a[:], in1=gate[cls][:]), [a, gate[cls]], [a])
        k.op("pool", lambda e: e.tensor_add(out=x[:], in0=x[:], in1=a[:]), [x, a], [x])
        k.dma(OUTT, OUTT[t*128 - out_rows0:(t+1)*128 - out_rows0, :], x, x[:], skip_w=True)
    k.stage_end()


def build_fused(nlayer=2, upto=None):
    k = K()
    TT = T_
    def IN(name, shape, dt=F32):
        t = T(k, k.dram_in(name, shape, dt), name); t.is_dram = True
        return t
    XCAT = IN("xcat", [TT, 1024]); CT = IN("ct", [128, 8, 2]); IDENT = IN("ident", [128, 128]); SEL = IN("sel", [2, 2, 128])
    CS = IN("cs", [TT, 320]); SN = IN("sn", [TT, 320]); CC = IN("cc", [9, 128, 128])
    L = []
    for l in range(nlayer):
        L.append(dict(MODW=IN(f"modw{l}", [1024, 6144]), MODB=IN(f"modb{l}", [2, 6144]), N1G=IN(f"n1g{l}", [128, 1024]), WIN=IN(f"win{l}", [1024, 2848]),
                      QKG=IN(f"qkg{l}", [128, 640]), CB=IN(f"cb{l}", [128, 32]), CW=IN(f"cw{l}", [128, 5, 768]), GAIN2=IN(f"gain2{l}", [128, 512]),
                      WOUT=IN(f"wout{l}", [1024, 1024]), N2G=IN(f"n2g{l}", [128, 1024]), RW=IN(f"rw{l}", [1024, 16]),
                      W1=IN(f"w1{l}", [16, 1024, 2048]), W3=IN(f"w3{l}", [16, 1024, 2048]), W2=IN(f"w2{l}", [16, 2048, 1024])))
    OUT = k.dram_out("out", [TT - NCTX, 1024])
    PS = [k.ps([128, 512], name=f"bank{i}") for i in range(8)]
    ident = k.sb([128, 128], name="ident"); sel = k.sb([2, 2, 128], name="sel"); ctsb = k.sb([128, 8, 2], name="ct")
    cc = k.sb([128, 9, 128], name="cc")
    modrow = k.sb([2, 6144], name="modrow")
    k.dma(ident, ident[:], None, IDENT[:, :]); k.dma(sel, sel[:], None, SEL[:, :, :]); k.dma(ctsb, ctsb[:], None, CT[:, :, :])
    for i in range(9):
        k.dma(cc, cc[:, i, :], None, CC[i])
    k.op("act", lambda e: e.activation(out=ctsb[:], in_=ctsb[:], func=AF.Silu), [ctsb], [ctsb])
    D = lambda n, s, dt=F32: k.dram_tmp(n, s, dt)
    P = D("P", [TT, 2848]); QKT = D("QKT", [640, TT], BF16); AXD = D("AXD", [TT, 512]); O6D = D("O6D", [4, TT, W6]); FMD = D("FMD", [4, NT_, 64, 10, 128])
    O7D = D("O7D", [4, TT, 2, 3, 128]); O7T = D("O7T", [4, 2, 64, TT]); STEP = D("STEP", [4, 64, NC_, 2, W8]); O8D = D("O8D", [4, 64, NC_, 2, 128])
    XMID = D("XMID", [TT, 1024]); H2 = D("H2", [TT, 1024]); AFFT = D("AFFT", [16, TT]); VALS = D("VALS", [16, 1152]); IDX = D("IDX", [16, 1152], U32)
    ACC = D("ACC", [TT, 1024]); XN = D("XN", [TT, 1024])
    X = XCAT
    for l in range(nlayer):
        W = L[l]
        e_mod(k, PS, ctsb, W["MODW"], W["MODB"], modrow)
        e_normlin(k, PS, X, W["N1G"], modrow, 1, 0, sel, ident, W["WIN"], 2848, P, QKT=QKT, qk=dict(GAIN=W["QKG"], CS=CS, SN=SN))
        e_attn(k, PS, QKT, P, AXD)
        e_prep(k, PS, P, W["CB"], W["CW"], cc, ident, O6D, FMD)
        e_intra(k, PS, O6D, FMD, cc, O7D, O7T)
        e_stepglue(k, P, O6D, FMD, O7D, O7T, STEP)
        e_scan(k, PS, STEP, O8D)
        e_outproj(k, PS, AXD, O8D, O6D, W["GAIN2"], X, modrow, sel, ident, W["WOUT"], XMID)
        if upto == "xmid":
            k.stage_begin(); xx = [k.sb([128, 1024], name="cpy") for _ in range(2)]
            for t in range(2, NT_):
                k.dma(xx[t % 2], xx[t % 2][:], XMID, XMID[t*128:(t+1)*128, :]); k.dma(OUT, OUT[(t-2)*128:(t-1)*128, :], xx[t % 2], xx[t % 2][:], skip_w=True)
            k.stage_end(); break
        e_normlin(k, PS, XMID, W["N2G"], modrow, 4, 3, sel, ident, W["RW"], 16, None, H=H2, softmax=True, YT=AFFT)
        e_topk(k, AFFT, VALS, IDX)
        e_expert(k, PS, H2, VALS, IDX, W["W1"], W["W3"], W["W2"], ident, ACC)
        last = (l == nlayer - 1)
        e_combine(k, PS, XMID, ACC, modrow, sel, OUT if last else XN, NCTX if last else 0)
        X = XN
    k.finish([OUT])
    k.close()
    return k

def _rep(v, p=128):
    return np.ascontiguousarray(np.broadcast_to(np.asarray(v, np.float32)[None], (p,) + tuple(np.shape(v))))

def _consts():
    f32 = np.float32
    n = 8192
    row = (np.arange(n) // 64).astype(f32); col = (np.arange(n) % 64).astype(f32)
    inv = (10000.0 ** (-np.arange(16, dtype=f32) / 16)).astype(f32)
    ang = np.stack([row, col], -1)[..., None] * inv
    tile = lambda a: np.ascontiguousarray(np.broadcast_to(a[:, None], (a.shape[0], 10, 2, 16))).reshape(a.shape[0], 320)
    cs = np.concatenate([np.ones((256, 320), f32), tile(np.cos(ang).astype(f32))]); sn = np.concatenate([np.zeros((256, 320), f32), tile(np.sin(ang).astype(f32))])
    sel = np.zeros((2, 2, 128), f32); sel[0, 0] = 1; sel[1, 1] = 1
    s = np.arange(128)[:, None]; t = np.arange(128)[None, :]
    same = (s // 64) == (t // 64)
    trif = (same & (s <= t)).astype(f32); trir = (same & (s >= t)).astype(f32); eye = np.eye(128, dtype=f32)
    cc = np.stack([eye, np.ones((128, 128), f32), trif, trir, trif - eye, trir - eye, eye - trif, eye - trir, same.astype(f32)])
    return dict(cs=cs, sn=sn, sel=sel, cc=cc, ident=eye)

def make_in_maps(inp, nlayer=2):
    f32 = np.float32
    C = _consts()
    ims = []
    for b in range(2):
        ct = np.stack([inp['c'][b], inp['c_ctx']], 1).reshape(8, 128, 2).transpose(1, 0, 2)
        m = {"xcat": np.concatenate([inp['ctx'][b], inp['x'][b]], 0), "ct": np.ascontiguousarray(ct, dtype=f32)}
        m.update(C)
        for l in range(nlayer):
            cb = np.concatenate([inp['mlstm_i_bias'][l].reshape(8), inp['mlstm_f_bias'][l].reshape(8), inp['gdn_a_log'][l].reshape(8), inp['gdn_dt_bias'][l].reshape(8)])
            m.update({f"modw{l}": inp['mod_w'][l], f"modb{l}": _rep(inp['mod_b'][l], 2), f"n1g{l}": _rep(inp['norm1_g'][l]), f"win{l}": inp['w_in'][l],
                      f"qkg{l}": _rep(np.concatenate([np.tile(inp['q_norm_g'][l], 8), np.tile(inp['k_norm_g'][l], 2)])),
                      f"cb{l}": _rep(cb), f"cw{l}": _rep(inp['gdn_conv_w'][l]),
                      f"gain2{l}": _rep(np.concatenate([inp['mlstm_out_g'][l].reshape(256), np.tile(inp['gdn_out_g'][l], 4)])),
                      f"wout{l}": inp['w_out'][l], f"n2g{l}": _rep(inp['norm2_g'][l]), f"rw{l}": inp['router_w'][l],
                      f"w1{l}": inp['w1'][l], f"w3{l}": inp['w3'][l], f"w2{l}": inp['w2'][l]})
        ims.append({k_: np.ascontiguousarray(v, dtype=f32) for k_, v in m.items()})
    return ims

_K = {}
def kernel(**inputs):
    inp = {k_: np.asarray(v, np.float32) for k_, v in inputs.items()}
    if 2 not in _K:
        _K[2] = build_fused(2)
    res = run_bass_kernel_spmd(_K[2].nc, make_in_maps(inp, 2), core_ids=[0, 1])
    return np.ascontiguousarray(np.stack([res.results[b]["out"] for b in range(2)]).astype(np.float32))
```

```python
import numpy as np
import threading
from contextlib import ExitStack
import concourse.bass as bass
import concourse.mybir as mybir
from concourse.bass_utils import run_bass_kernel_spmd

F32 = mybir.dt.float32
I32 = mybir.dt.int32
U32 = mybir.dt.uint32
BF16 = mybir.dt.bfloat16
ALU = mybir.AluOpType
AF = mybir.ActivationFunctionType
AX = mybir.AxisListType
EPS = 1e-6
SAME_ENGINE_FIFO = False


class T:
    def __init__(self, k, ap, name):
        self.k = k; self.ap = ap; self.name = name
        self.w = None; self.r = {}
        self.dsem = {}; self.dn = 0; self.is_dram = False

    def __getitem__(self, idx):
        return self.ap[idx]

    def rearrange(self, *a, **kw):
        return self.ap.rearrange(*a, **kw)


class V:
    def __init__(s, t, a0, a1):
        s.t = t; s.base = t; s.a0 = a0; s.a1 = a1

    def __getitem__(s, idx):
        return s.t.ap[:, s.a0:s.a1][idx]


class K:
    def __init__(self):
        self.nc = bass.Bass("TRN2", target_bir_lowering=False)
        self.es = ExitStack()
        self.st = None
        nc = self.nc
        self.es.enter_context(nc.allow_low_precision('bf16 matmul operands, fp32 PSUM accumulation'))
        self.eng = {"pe": nc.tensor, "act": nc.scalar, "dve": nc.vector, "pool": nc.gpsimd, "sp": nc.sync}
        self.sem = {}; self.cnt = {}
        for e in ["pe", "act", "dve", "pool"]:
            self.sem[e] = self.es.enter_context(nc.semaphore("s_" + e)); self.cnt[e] = 0
        self.seen = {e: {} for e in self.eng}
        self.ntile = 0
        self.dsems = {}
        self.free_sems = {'sp': [], 'pool': []}
        self.stage_tiles = []
        self.nsem = 0

    def dram_in(self, name, shape, dt=F32):
        return self.nc.dram_tensor(name, list(shape), dt, kind="ExternalInput").ap()

    def dram_out(self, name, shape, dt=F32):
        t = T(self, self.nc.dram_tensor(name, list(shape), dt, kind="ExternalOutput").ap(), name); t.is_dram = True
        return t

    def dram_tmp(self, name, shape, dt=F32):
        t = T(self, self.nc.dram_tensor(name, list(shape), dt, kind="Internal").ap(), name); t.is_dram = True
        return t

    def sb(self, shape, dt=F32, name=None):
        self.ntile += 1
        name = (name or "t") + f"_{self.ntile}"
        stk = self.st if self.st is not None else self.es
        h = stk.enter_context(self.nc.sbuf_tensor("S_" + name, list(shape), dt))
        t = T(self, h[:], name)
        if self.st is not None:
            self.stage_tiles.append(t)
        return t

    def ps(self, shape, dt=F32, name=None):
        self.ntile += 1
        name = (name or "p") + f"_{self.ntile}"
        h = self.es.enter_context(self.nc.psum_tensor("P_" + name, list(shape), dt))
        return T(self, h[:], name)

    def stage_begin(self):
        assert self.st is None
        self.st = ExitStack(); self.stage_tiles = []

    def stage_end(self):
        self.barrier()
        for t in self.stage_tiles:
            for q_, sm_ in t.dsem.items():
                self.free_sems[q_].append(sm_)
        self.st.close(); self.st = None; self.stage_tiles = []

    def barrier(self):
        toks = [(self.sem[c], self.cnt[c]) for c in self.sem if self.cnt[c] > 0]
        toks += [(s, 16 * n) for (s, n) in self.dsems.values() if n > 0]
        for e in self.eng:
            self._wait(e, toks)

    def _get_dsem(self, t, q):
        if q not in t.dsem:
            if self.free_sems[q]:
                t.dsem[q] = self.free_sems[q].pop()
            else:
                self.nsem += 1
                sm_ = self.es.enter_context(self.nc.semaphore(f"d{self.nsem}{q}"))
                t.dsem[q] = sm_
                self.dsems[id(sm_)] = [sm_, 0]
        return t.dsem[q]

    def _deps(self, reads, writes):
        toks = []
        for t in reads:
            if t.w is not None:
                toks.append(t.w)
        for t in writes:
            if t.w is not None:
                toks.append(t.w)
            toks.extend(t.r.values())
        return toks

    def _wait(self, e, toks, skip_sem=None):
        eng = self.eng[e]
        best = {}
        for (s, v) in toks:
            if skip_sem is not None and s is skip_sem:
                continue
            if best.get(id(s), (None, 0))[1] < v:
                best[id(s)] = (s, v)
        for (s, v) in best.values():
            if self.seen[e].get(id(s), 0) < v:
                eng.wait_ge(s, v)
                self.seen[e][id(s)] = v

    def _mark(self, tok, reads, writes):
        for t in reads:
            t.r[id(tok[0])] = tok
        for t in writes:
            t.w = tok; t.r = {}

    def op(self, e, fn, reads, writes):
        reads = [getattr(t, "base", t) for t in reads if t is not None]
        writes = [getattr(t, "base", t) for t in writes]
        toks = self._deps(reads, writes)
        self._wait(e, toks, skip_sem=self.sem[e] if (e == "pe" or SAME_ENGINE_FIFO) else None)
        ins = fn(self.eng[e])
        self.cnt[e] += 1
        ins.then_inc(self.sem[e], 1)
        tok = (self.sem[e], self.cnt[e])
        self._mark(tok, reads, writes)
        self._yield()
        return tok

    def dma(self, out_t, out_ap, in_t, in_ap, q="sp", skip_w=False, indirect=None, **kw):
        reads = [in_t] if in_t is not None else []
        if indirect is not None and indirect.get("idx_t") is not None:
            reads.append(indirect["idx_t"])
        writes = [out_t] if out_t is not None else []
        toks = self._deps(reads, [] if skip_w else writes)
        self._wait(q, toks)
        if out_t is not None and not out_t.is_dram:
            owner = out_t
        elif in_t is not None and not in_t.is_dram:
            owner = in_t
        else:
            owner = out_t if out_t is not None else in_t
        sem = self._get_dsem(owner, q)
        ent = self.dsems[id(sem)]
        ent[1] += 1
        if indirect is None:
            ins = self.eng[q].dma_start(out=out_ap, in_=in_ap, **kw)
        else:
            ins = self.eng[q].indirect_dma_start(out=out_ap, out_offset=indirect.get("out_offset"), in_=in_ap,
                                                 in_offset=indirect.get("in_offset"), **kw)
        ins.then_inc(sem, 16)
        tok = (sem, 16 * ent[1])
        self._mark(tok, reads, writes)
        self._yield()
        return tok

    def _yield(self):
        il = getattr(self, "_il", None)
        if il is None:
            return
        me = threading.current_thread()
        if me not in il["threads"]:
            return
        with il["cv"]:
            i = il["threads"].index(me)
            self._pass_turn(il, i)
            while il["turn"] is not me:
                il["cv"].wait()

    def _pass_turn(self, il, i):
        n = len(il["threads"])
        for d in range(1, n + 1):
            t = il["threads"][(i + d) % n]
            if t in il["live"]:
                il["turn"] = t
                break
        il["cv"].notify_all()

    def interleave(self, fns):
        if len(fns) == 1:
            fns[0](); return
        il = {"cv": threading.Condition(), "threads": [], "live": set(), "turn": None, "err": []}
        def runner(fn):
            me = threading.current_thread()
            with il["cv"]:
                while il["turn"] is not me:
                    il["cv"].wait()
            try:
                fn()
            except BaseException as e:
                il["err"].append(e)
            with il["cv"]:
                il["live"].discard(me)
                if il["live"]:
                    self._pass_turn(il, il["threads"].index(me))
                else:
                    il["turn"] = None; il["cv"].notify_all()
        ths = [threading.Thread(target=runner, args=(fn,)) for fn in fns]
        il["threads"] = ths; il["live"] = set(ths)
        self._il = il
        for t in ths:
            t.start()
        with il["cv"]:
            il["turn"] = ths[0]; il["cv"].notify_all()
        for t in ths:
            t.join()
        self._il = None
        if il["err"]:
            raise il["err"][0]

    def finish(self, out_tiles):
        self.barrier()

    def close(self):
        if self.st is not None:
            self.st.close()
        self.es.close()


CUM = [0, 512, 640, 768, 1024, 1280, 1536, 1792, 1800, 1808, 2064, 2320, 2576, 2832, 2840, 2848]
import os
T_ = int(os.environ.get('FZ_T', '8448')); NT_ = T_ // 128; NC_ = T_ // 64; NCTX = 256

OUT6 = dict(Qs=(0, 128), Ks=(128, 256), eb=(256, 258), sigo=(258, 322), qn=(322, 386), kn=(386, 450), kb=(450, 578),
            Rm=(578, 834), Qg=(834, 962), Kd=(962, 1090), G=(1090, 1092), eG=(1092, 1094), siluz=(1094, 1158), gv=(1158, 1222),
            ebl=(1222, 1224), eGl=(1224, 1226))
W6 = 1226
S8 = dict(QsT=(0, 64), WT=(64, 128), QgT=(128, 192), Ks=(192, 256), V1=(256, 321), AT=(321, 385), U=(385, 449), Kd=(449, 513),
          AqkT=(513, 577), sm=(577, 578), sg=(578, 579))
W8 = 579


def bcast_rows(k, PS, dst, src2, r, sel, n=1024):
    for nb in range(0, n, 512):
        ps = PS[(nb // 512) % 2]
        k.op("pe", lambda e: e.matmul(ps[:, 0:512], sel[:, r, :], src2[:, nb:nb+512], start=True, stop=True), [sel, src2], [ps])
        k.op("dve", lambda e: e.tensor_copy(dst[:, nb:nb+512], ps[:, 0:512]), [ps], [dst])


def e_mod(k, PS, ctsb, MODW, MODB, modrow):
    k.stage_begin()
    ws = [k.sb([128, 8, 512], name="mw") for _ in range(2)]
    bi = k.sb([2, 6144], name="mb")
    k.dma(bi, bi[:], None, MODB[:, :])
    for nb in range(12):
        w = ws[nb % 2]; ps = PS[nb % 2]
        k.dma(w, w[:], None, MODW.rearrange("(c p) n -> p c n", p=128)[:, :, nb*512:(nb+1)*512], q="sp" if nb % 2 == 0 else "pool")
        for c in range(8):
            k.op("pe", lambda e: e.matmul(ps[0:2, 0:512], ctsb[:, c, :], w[:, c, :], start=(c == 0), stop=(c == 7)), [ctsb, w], [ps])
        k.op("dve", lambda e: e.tensor_add(out=modrow[:, nb*512:(nb+1)*512], in0=ps[0:2, 0:512], in1=bi[:, nb*512:(nb+1)*512]), [ps, bi], [modrow])
    k.stage_end()


def e_normlin(k, PS, X, GREP, modrow, isc, ish, sel, ident, W, N, Y, H=None, softmax=False, YT=None, QKT=None, qk=None):
    k.stage_begin()
    g = k.sb([128, 1024], name="g"); k.dma(g, g[:], None, GREP[:, :])
    A = [k.sb([128, 1024], name="A") for _ in range(2)]; sh = [k.sb([128, 1024], name="sh") for _ in range(2)]
    for cls in range(2):
        r = 1 if cls == 0 else 0
        bcast_rows(k, PS, A[cls], V(modrow, isc*1024, (isc+1)*1024), r, sel)
        bcast_rows(k, PS, sh[cls], V(modrow, ish*1024, (ish+1)*1024), r, sel)
        k.op("dve", lambda e: e.scalar_tensor_tensor(out=A[cls][:], in0=A[cls][:], scalar=1.0, in1=g[:], op0=ALU.add, op1=ALU.mult), [A[cls], g], [A[cls]])
    lowp = N > 16
    w = k.sb([128, 8, N], BF16 if lowp else F32, name="w")
    for c in range(8):
        k.dma(w, w[:, c, :], None, W[c*128:(c+1)*128, :], q="pool" if lowp else ("sp" if c % 2 == 0 else "pool"))
    xs = [k.sb([128, 1024], name="x") for _ in range(2)]; hs = [k.sb([128, 1024], name="h") for _ in range(2)]
    hT = [k.sb([128, 8, 128], BF16 if lowp else F32, name="hT") for _ in range(2)]
    ys = [k.sb([128, N], name="y") for _ in range(2)]
    st = [k.sb([128, 40], name="st") for _ in range(2)]
    if qk is not None:
        gn = k.sb([128, 640], name="gn"); k.dma(gn, gn[:], None, qk["GAIN"][:, :])
        QB = [dict(cs=k.sb([128, 320], name="cs"), sn=k.sb([128, 320], name="sn"), sq=k.sb([128, 640], name="sq"), o=k.sb([128, 640], name="o"),
                   t1=k.sb([128, 320], name="t1"), t2=k.sb([128, 320], name="t2"), oT=k.sb([128, 5, 128], BF16, name="oT")) for _ in range(1)]
        v5 = lambda ap: ap.rearrange("p (h a f j) -> p h a f j", h=10, a=2, f=2)
        v4 = lambda ap: ap.rearrange("p (h a j) -> p h a j", h=10, a=2)
    if YT is not None:
        yts = [k.sb([16, 128], name="yt") for _ in range(2)]
    nb_ = (N + 511) // 512
    pi = 0
    for rt in range(T_ // 128):
        cls = 0 if rt < 2 else 1
        x = xs[rt % 2]; h = hs[rt % 2]; s = st[rt % 2]; ht = hT[rt % 2]; y = ys[rt % 2]
        rows = slice(rt*128, (rt+1)*128)
        k.dma(x, x[:], X, X[rows, :])
        k.op("dve", lambda e: e.memset(s[:, 0:8], 0.0), [], [s])
        k.op("act", lambda e: e.activation(out=h[:], in_=x[:], func=AF.Square, accum_out=s[:, 0:1]), [x, s], [h, s])
        k.op("dve", lambda e: e.tensor_scalar(out=s[:, 1:2], in0=s[:, 0:1], scalar1=1.0/1024, scalar2=EPS, op0=ALU.mult, op1=ALU.add), [s], [s])
        k.op("act", lambda e: e.activation(out=s[:, 7:8], in_=s[:, 1:2], func=AF.Sqrt), [s], [s])
        k.op("dve", lambda e: e.reciprocal(out=s[:, 2:3], in_=s[:, 7:8]), [s], [s])
        k.op("dve", lambda e: e.scalar_tensor_tensor(out=h[:], in0=x[:], scalar=s[:, 2:3], in1=A[cls][:], op0=ALU.mult, op1=ALU.mult), [x, s, A[cls]], [h])
        k.op("pool", lambda e: e.tensor_add(out=h[:], in0=h[:], in1=sh[cls][:]), [h, sh[cls]], [h])
        if H is not None:
            k.dma(H, H[rows, :], h, h[:], q="pool", skip_w=True)
        for half in range(2):
            pt = PS[half]
            for c in range(4):
                cc = half*4 + c
                k.op("pe", lambda e: e.transpose(pt[:, c*128:(c+1)*128], h[:, cc*128:(cc+1)*128], ident[:]), [h, ident], [pt])
            if half == 0:
                k.op("act", lambda e: e.copy(ht[:, 0:4, :], pt[:].rearrange("p (c t) -> p c t", c=4)), [pt], [ht])
            else:
                k.op("dve", lambda e: e.tensor_copy(ht[:, 4:8, :], pt[:].rearrange("p (c t) -> p c t", c=4)), [pt], [ht])
        for b in range(nb_):
            n0 = b*512; n1 = min(N, n0+512); ps = PS[2 + pi % 4]; pi += 1
            for c in range(8):
                k.op("pe", lambda e: e.matmul(ps[:, 0:n1-n0], ht[:, c, :], w[:, c, n0:n1], start=(c == 0), stop=(c == 7)), [ht, w], [ps])
            if b % 2 == 0:
                k.op("dve", lambda e: e.tensor_copy(y[:, n0:n1], ps[:, 0:n1-n0]), [ps], [y])
            else:
                k.op("act", lambda e: e.copy(y[:, n0:n1], ps[:, 0:n1-n0]), [ps], [y])
        if softmax:
            k.op("dve", lambda e: e.reduce_max(out=s[:, 3:4], in_=y[:], axis=AX.X), [y], [s])
            k.op("dve", lambda e: e.tensor_scalar(out=s[:, 4:5], in0=s[:, 3:4], scalar1=-1.0, scalar2=None, op0=ALU.mult), [s], [s])
            k.op("dve", lambda e: e.memset(s[:, 5:6], 0.0), [], [s])
            k.op("act", lambda e: e.activation(out=y[:], in_=y[:], func=AF.Exp, bias=s[:, 4:5], scale=1.0, accum_out=s[:, 5:6]), [y, s], [y, s])
            k.op("dve", lambda e: e.reciprocal(out=s[:, 6:7], in_=s[:, 5:6]), [s], [s])
            k.op("dve", lambda e: e.tensor_scalar(out=y[:], in0=y[:], scalar1=s[:, 6:7], scalar2=None, op0=ALU.mult), [y, s], [y])
        if YT is not None:
            yt = yts[rt % 2]; pt = PS[6]
            k.op("pe", lambda e: e.transpose(pt[0:16, 0:128], y[:, 0:16], ident[:]), [y, ident], [pt])
            k.op("dve", lambda e: e.tensor_copy(yt[:], pt[0:16, 0:128]), [pt], [yt])
            k.dma(YT, YT[:, rows], yt, yt[:], q="pool", skip_w=True)
        if Y is not None:
            k.dma(Y, Y[rows, :], y, y[:], skip_w=True)
        if qk is not None:
            b = QB[0]; o = b['o']
            k.dma(b['cs'], b['cs'][:], None, qk["CS"][rows, :], q="pool"); k.dma(b['sn'], b['sn'][:], None, qk["SN"][rows, :], q="pool")
            xq = V(y, 0, 640)
            k.op("dve", lambda e: e.tensor_mul(out=b['sq'][:], in0=xq[:], in1=xq[:]), [y], [b['sq']])
            k.op("dve", lambda e: e.reduce_sum(out=s[:, 8:18], in_=b['sq'][:].rearrange("p (h d) -> p h d", h=10), axis=AX.X), [b['sq']], [s])
            k.op("dve", lambda e: e.tensor_scalar(out=s[:, 8:18], in0=s[:, 8:18], scalar1=1.0/64, scalar2=EPS, op0=ALU.mult, op1=ALU.add), [s], [s])
            k.op("act", lambda e: e.activation(out=s[:, 18:28], in_=s[:, 8:18], func=AF.Sqrt), [s], [s])
            k.op("dve", lambda e: e.reciprocal(out=s[:, 28:38], in_=s[:, 18:28]), [s], [s])
            for hh in range(10):
                k.op("dve" if hh % 2 == 0 else "pool", lambda e: e.tensor_scalar(out=b['sq'][:, hh*64:(hh+1)*64], in0=xq[:, hh*64:(hh+1)*64], scalar1=s[:, 28+hh:29+hh], scalar2=None, op0=ALU.mult), [y, s], [b['sq']])
            xn = b['sq']
            k.op("dve", lambda e: e.tensor_mul(out=xn[:], in0=xn[:], in1=gn[:]), [xn, gn], [xn])
            x1 = v5(xn[:])[:, :, :, 0, :]; x2 = v5(xn[:])[:, :, :, 1, :]
            o1 = v5(o[:])[:, :, :, 0, :]; o2 = v5(o[:])[:, :, :, 1, :]
            cs = v4(b['cs'][:]); sn = v4(b['sn'][:]); t1 = v4(b['t1'][:]); t2 = v4(b['t2'][:])
            k.op("dve", lambda e: e.tensor_mul(out=t1, in0=x1, in1=cs), [xn, b['cs']], [b['t1']])
            k.op("pool", lambda e: e.tensor_mul(out=t2, in0=x2, in1=sn), [xn, b['sn']], [b['t2']])
            k.op("dve", lambda e: e.tensor_sub(out=o1, in0=t1, in1=t2), [b['t1'], b['t2']], [o])
            k.op("dve", lambda e: e.tensor_mul(out=t1, in0=x2, in1=cs), [xn, b['cs']], [b['t1']])
            k.op("pool", lambda e: e.tensor_mul(out=t2, in0=x1, in1=sn), [xn, b['sn']], [b['t2']])
            k.op("dve", lambda e: e.tensor_add(out=o2, in0=t1, in1=t2), [b['t1'], b['t2']], [o])
            pt = PS[7]
            for c in range(4):
                k.op("pe", lambda e: e.transpose(pt[:, c*128:(c+1)*128], o[:, c*128:(c+1)*128], ident[:]), [o, ident], [pt])
            k.op("act", lambda e: e.copy(b['oT'][:, 0:4, :], pt[:].rearrange("p (c t) -> p c t", c=4)), [pt], [b['oT']])
            pt2 = PS[6]
            k.op("pe", lambda e: e.transpose(pt2[:, 0:128], o[:, 512:640], ident[:]), [o, ident], [pt2])
            k.op("dve", lambda e: e.tensor_copy(b['oT'][:, 4, :], pt2[:, 0:128]), [pt2], [b['oT']])
            k.dma(QKT, QKT.ap.rearrange("(c p) t -> p c t", p=128)[:, :, rows], b['oT'], b['oT'][:], q="pool", skip_w=True)
    k.stage_end()


def e_attn(k, PS, QKT, P, AXD):
    k.stage_begin()
    T = T_; NT = NT_
    kts = [k.sb([64, T], BF16, name="kt") for _ in range(2)]
    v1s = [k.sb([128, NT, 65], BF16, name="v1") for _ in range(2)]
    vtmp = k.sb([128, NT, 64], name="vtmp")
    for g in range(2):
        k.dma(kts[g], kts[g][:], QKT, QKT[512 + g*64:512 + (g+1)*64, :])
        k.op("dve", lambda e: e.memset(v1s[g][:, :, 64:65], 1.0), [], [v1s[g]])
        k.dma(vtmp, vtmp[:], P, P.ap.rearrange("(n p) c -> p n c", p=128)[:, :, 640 + g*64:640 + (g+1)*64], q="pool")
        k.op("dve", lambda e: e.tensor_copy(v1s[g][:, :, 0:64], vtmp[:]), [vtmp], [v1s[g]])
    qts = [k.sb([64, 512], BF16, name="q") for _ in range(2)]
    pts = [k.sb([128, 512], BF16, name="pt") for _ in range(3)]
    osb = [k.sb([128, 4, 64], name="osb") for _ in range(2)]
    rs = [k.sb([128, 8], name="rs") for _ in range(2)]
    pss = PS[0:2]; pso = PS[2:6]
    blocks = [(0, NCTX, 0, NCTX // 128)] + [(q0, 512, 0, NT) for q0 in range(NCTX, T, 512)]
    it = 0; bi = 0
    pending = None
    for h in range(8):
        g = h // 4; kt = kts[g]; v1 = v1s[g]
        for (q0, qn, k0, k1) in blocks:
            qt = qts[bi % 2]; ob = osb[bi % 2]; r = rs[bi % 2]; bi += 1
            nqs = qn // 128
            k.dma(qt, qt[:, 0:qn], QKT, QKT[h*64:(h+1)*64, q0:q0+qn])
            for kk in range(k0, k1):
                ps = pss[it % 2]; pt = pts[it % 3]; it += 1
                k.op("pe", lambda e: e.matmul(ps[:, 0:qn], kt[:, kk*128:(kk+1)*128], qt[:, 0:qn], start=True, stop=True), [kt, qt], [ps])
                k.op("act", lambda e: e.activation(out=pt[:, 0:qn], in_=ps[:, 0:qn], func=AF.Exp, scale=0.125), [ps], [pt])
                if pending is not None:
                    pending()

                def mk(pt=pt, v1=v1, kk=kk, k0=k0, k1=k1, nqs=nqs, r=r, ob=ob, q0=q0, qn=qn, h=h):
                    def run():
                        for qs in range(nqs):
                            k.op("pe", lambda e: e.matmul(pso[qs][:, 0:65], pt[:, qs*128:(qs+1)*128], v1[:, kk, :], start=(kk == k0), stop=(kk == k1-1)), [pt, v1], [pso[qs]])
                        if kk == k1 - 1:
                            for qs in range(nqs):
                                k.op("dve", lambda e: e.reciprocal(out=r[:, qs:qs+1], in_=pso[qs][:, 64:65]), [pso[qs]], [r])
                                k.op("dve", lambda e: e.tensor_scalar(out=ob[:, qs, :], in0=pso[qs][:, 0:64], scalar1=r[:, qs:qs+1], scalar2=None, op0=ALU.mult), [pso[qs], r], [ob])
                            k.dma(AXD, AXD[q0:q0+qn, h*64:(h+1)*64].rearrange("(s p) d -> p s d", p=128), ob, ob[:, 0:nqs, :], q="pool", skip_w=True)
                    return run
                pending = mk()
    if pending is not None:
        pending()
    k.stage_end()


O6 = dict(Qs=(0, 128), sigo=(128, 192), qn=(192, 256), kn=(256, 320), kb=(320, 448), Rm=(448, 704), Qg=(704, 832), G=(832, 834),
          siluz=(834, 898), KG0=(898, 1028), KG1=(1028, 1158))
W6 = 1158
S8 = dict(QsT=(0, 64), WT=(64, 128), QgT=(128, 192), Ks=(192, 256), Kd=(256, 320), sm=(320, 321), sg=(321, 322), V1=(322, 387),
          AT=(387, 451), U=(451, 515), AqkT=(515, 579))
W8 = 579
FM_SRC = [(256, 320), (192, 256), (320, 384), (384, 448), (0, 64), (64, 128), None, None, (704, 768), (768, 832)]


def e_prep(k, PS, P, CB, CW, cc, ident, O6D, FMD):
    k.stage_begin()
    tf = cc[:, 2, :]; tr = cc[:, 3, :]; blk = cc[:, 8, :]
    cb = k.sb([128, 48], name="cb"); cw = k.sb([128, 5, 768], name="cw")
    k.dma(cb, cb[:, 0:32], None, CB[:, :]); k.dma(cw, cw[:], None, CW[:, :, :])
    k.op("act", lambda e: e.activation(out=cb[:, 32:40], in_=cb[:, 16:24], func=AF.Exp), [cb], [cb])
    k.op("dve", lambda e: e.tensor_scalar(out=cb[:, 32:40], in0=cb[:, 32:40], scalar1=-1.0, scalar2=None, op0=ALU.mult), [cb], [cb])
    NB = 2
    m4s = [k.sb([128, 1024], name="m4") for _ in range(NB)]; g4s = [k.sb([128, 1024], name="g4") for _ in range(NB)]
    mgs = [k.sb([128, 16], name="mg") for _ in range(NB)]; ggs = [k.sb([128, 16], name="gg") for _ in range(NB)]
    q5s = [k.sb([128, 5, 768], name="q5") for _ in range(NB)]
    cvs = [k.sb([128, 768], name="cv") for _ in range(NB)]; sqs = [k.sb([128, 512], name="sq") for _ in range(NB)]
    wks = [k.sb([128, 128], name="wk") for _ in range(NB)]
    outs = [[k.sb([128, W6], name="o6") for _ in range(4)] for _ in range(NB)]
    fmts = [[k.sb([64, 10, 128], name="fmt") for _ in range(4)] for _ in range(NB)]
    ksb = [[k.sb([128, 128], name="ksb") for _ in range(4)] for _ in range(NB)]
    Pt = P.rearrange("(n p) c -> n p c", p=128)
    segs = [(0, NCTX), (NCTX, T_)]
    def tile_body(t):
        i = t % NB
        psg = PS[i]
        m4 = m4s[i]; g4 = g4s[i]; mg = mgs[i]; gg = ggs[i]; q5 = q5s[i]; c = cvs[i]; s2 = sqs[i]; s = wks[i]
        r0 = t*128
        k.dma(m4, m4[:], P, P[r0:r0+128, 768:1792]); k.dma(g4, g4[:], P, P[r0:r0+128, 1808:2832], q="pool")
        k.dma(mg, mg[:], P, P[r0:r0+128, 1792:1808]); k.dma(gg, gg[:], P, P[r0:r0+128, 2832:2848], q="pool")
        seg = segs[0] if r0 < NCTX else segs[1]
        edge = (r0 - 2 < seg[0]) or (r0 + 130 > seg[1])
        if edge:
            k.op("pool", lambda e: e.memset(q5[:], 0.0), [], [q5])
        for kk in range(5):
            a0 = r0 + kk - 2; a1 = a0 + 128
            lo = max(a0, seg[0]); hi = min(a1, seg[1])
            k.dma(q5, q5[lo-a0:hi-a0, kk, :], P, P[lo:hi, 1808:2576], q="sp" if kk % 2 == 0 else "pool")
        k.op("dve", lambda e: e.tensor_mul(out=q5[:], in0=q5[:], in1=cw[:]), [q5, cw], [q5])
        k.op("dve", lambda e: e.reduce_sum(out=c[:], in_=q5[:].rearrange("p k c -> p c k"), axis=AX.X), [q5], [c])
        k.op("act", lambda e: e.activation(out=c[:], in_=c[:], func=AF.Silu), [c], [c])
        k.op("dve", lambda e: e.tensor_mul(out=s2[:], in0=c[:, 0:512], in1=c[:, 0:512]), [c], [s2])
        k.op("dve", lambda e: e.reduce_sum(out=s[:, 64:72], in_=s2[:].rearrange("p (h d) -> p h d", h=8), axis=AX.X), [s2], [s])
        k.op("dve", lambda e: e.tensor_scalar(out=s[:, 64:72], in0=s[:, 64:72], scalar1=EPS, scalar2=None, op0=ALU.add), [s], [s])
        k.op("act", lambda e: e.activation(out=s[:, 72:80], in_=s[:, 64:72], func=AF.Sqrt), [s], [s])
        k.op("dve", lambda e: e.reciprocal(out=s[:, 80:88], in_=s[:, 72:80]), [s], [s])
        k.op("dve", lambda e: e.tensor_add(out=s[:, 0:8], in0=mg[:, 8:16], in1=cb[:, 8:16]), [mg, cb], [s])
        k.op("act", lambda e: e.activation(out=s[:, 8:16], in_=s[:, 0:8], func=AF.Exp, scale=-1.0), [s], [s])
        k.op("act", lambda e: e.activation(out=s[:, 8:16], in_=s[:, 8:16], func=AF.Ln, bias=1.0, scale=1.0), [s], [s])
        k.op("dve", lambda e: e.tensor_scalar(out=s[:, 8:16], in0=s[:, 8:16], scalar1=-1.0, scalar2=None, op0=ALU.mult), [s], [s])
        k.op("pe", lambda e: e.matmul(psg[:, 0:4], tf, s[:, 8:12], start=True, stop=True), [cc, s], [psg])
        k.op("pe", lambda e: e.matmul(psg[:, 4:8], tr, s[:, 12:16], start=True, stop=True), [cc, s], [psg])
        k.op("pe", lambda e: e.matmul(psg[:, 8:16], blk, s[:, 8:16], start=True, stop=True), [cc, s], [psg])
        k.op("dve", lambda e: e.tensor_copy(s[:, 16:24], psg[:, 0:8]), [psg], [s])
        k.op("act", lambda e: e.activation(out=s[:, 48:56], in_=psg[:, 8:16], func=AF.Exp), [psg], [s])
        k.op("act", lambda e: e.activation(out=s[:, 40:48], in_=s[:, 16:24], func=AF.Exp), [s], [s])
        k.op("dve", lambda e: e.tensor_add(out=s[:, 24:32], in0=mg[:, 0:8], in1=cb[:, 0:8]), [mg, cb], [s])
        k.op("dve", lambda e: e.tensor_sub(out=s[:, 24:32], in0=s[:, 24:32], in1=s[:, 16:24]), [s], [s])
        k.op("act", lambda e: e.activation(out=s[:, 32:40], in_=s[:, 24:32], func=AF.Exp), [s], [s])
        k.op("act", lambda e: e.activation(out=s[:, 88:96], in_=gg[:, 8:16], func=AF.Sigmoid), [gg], [s])
        k.op("dve", lambda e: e.tensor_add(out=s[:, 96:104], in0=gg[:, 0:8], in1=cb[:, 24:32]), [gg, cb], [s])
        k.op("act", lambda e: e.activation(out=s[:, 96:104], in_=s[:, 96:104], func=AF.Exp), [s], [s])
        k.op("act", lambda e: e.activation(out=s[:, 96:104], in_=s[:, 96:104], func=AF.Ln, bias=1.0, scale=1.0), [s], [s])
        k.op("dve", lambda e: e.tensor_mul(out=s[:, 104:112], in0=s[:, 96:104], in1=cb[:, 32:40]), [s, cb], [s])
        k.op("pe", lambda e: e.matmul(psg[:, 16:24], tf, s[:, 104:112], start=True, stop=True), [cc, s], [psg])
        k.op("pe", lambda e: e.matmul(psg[:, 24:32], tr, s[:, 104:112], start=True, stop=True), [cc, s], [psg])
        k.op("pe", lambda e: e.matmul(psg[:, 32:40], blk, s[:, 104:112], start=True, stop=True), [cc, s], [psg])
        k.op("dve", lambda e: e.tensor_copy(s[:, 112:116], psg[:, 16:20]), [psg], [s])
        k.op("dve", lambda e: e.tensor_copy(s[:, 116:120], psg[:, 28:32]), [psg], [s])
        k.op("dve", lambda e: e.tensor_sub(out=s[:, 120:124], in0=psg[:, 24:28], in1=s[:, 104:108]), [psg, s], [s])
        k.op("dve", lambda e: e.tensor_sub(out=s[:, 124:128], in0=psg[:, 20:24], in1=s[:, 108:112]), [psg, s], [s])
        k.op("act", lambda e: e.activation(out=s[:, 120:128], in_=s[:, 120:128], func=AF.Exp), [s], [s])
        k.op("act", lambda e: e.activation(out=s[:, 56:64], in_=s[:, 112:120], func=AF.Exp), [s], [s])
        k.op("act", lambda e: e.activation(out=s2[:, 0:8], in_=psg[:, 32:40], func=AF.Exp), [psg], [s2])
        for h in range(4):
            o = outs[i][h]; fmt = fmts[i][h]; kst = ksb[i][h]
            hs = slice(h*64, (h+1)*64)
            O = lambda n: o[:, O6[n][0]:O6[n][1]]
            for d in range(2):
                j = d*4 + h
                KG0 = O6["KG0"][0] + d*130
                k.op("dve", lambda e: e.tensor_scalar(out=o[:, d*64:(d+1)*64], in0=m4[:, hs], scalar1=s[:, 40+j:41+j], scalar2=None, op0=ALU.mult), [m4, s], [o])
                k.op("pool", lambda e: e.tensor_scalar(out=kst[:, d*64:(d+1)*64], in0=m4[:, 256+h*64:256+(h+1)*64], scalar1=s[:, 32+j:33+j], scalar2=0.125, op0=ALU.mult, op1=ALU.mult), [m4, s], [kst])
                k.op("pool", lambda e: e.tensor_copy(o[:, KG0:KG0+64], kst[:, d*64:(d+1)*64]), [kst], [o])
                k.op("dve", lambda e: e.tensor_copy(o[:, KG0+128:KG0+129], s[:, 48+j:49+j]), [s], [o])
                k.op("dve", lambda e: e.tensor_copy(o[:, KG0+129:KG0+130], s2[:, j:j+1]), [s2], [o])
            k.op("act", lambda e: e.activation(out=O("sigo"), in_=m4[:, 768+h*64:768+(h+1)*64], func=AF.Sigmoid), [m4], [o])
            k.op("dve", lambda e: e.tensor_scalar(out=O("qn"), in0=c[:, hs], scalar1=s[:, 80+h:81+h], scalar2=0.125, op0=ALU.mult, op1=ALU.mult), [c, s], [o])
            k.op("dve", lambda e: e.tensor_scalar(out=O("kn"), in0=c[:, 256+h*64:256+(h+1)*64], scalar1=s[:, 84+h:85+h], scalar2=None, op0=ALU.mult), [c, s], [o])
            gv = c[:, 512+h*64:512+(h+1)*64]
            for d in range(2):
                j = d*4 + h
                KG0 = O6["KG0"][0] + d*130
                kb = o[:, 320+d*64:320+(d+1)*64]
                k.op("dve", lambda e: e.tensor_scalar(out=kb, in0=O("kn"), scalar1=s[:, 88+j:89+j], scalar2=None, op0=ALU.mult), [o, s], [o])
                k.op("pool", lambda e: e.tensor_scalar(out=o[:, 448+d*128:448+d*128+64], in0=gv, scalar1=s[:, 88+j:89+j], scalar2=None, op0=ALU.mult), [c, s], [o])
                k.op("dve", lambda e: e.tensor_scalar(out=o[:, 448+d*128+64:448+(d+1)*128], in0=kb, scalar1=s[:, 56+j:57+j], scalar2=None, op0=ALU.mult), [o, s], [o])
                k.op("pool", lambda e: e.tensor_scalar(out=o[:, 704+d*64:704+(d+1)*64], in0=O("qn"), scalar1=s[:, 56+j:57+j], scalar2=None, op0=ALU.mult), [o, s], [o])
                k.op("dve", lambda e: e.tensor_scalar(out=o[:, KG0+64:KG0+128], in0=O("kn"), scalar1=s[:, 120+j:121+j], scalar2=None, op0=ALU.mult), [o, s], [o])
                k.op("dve", lambda e: e.tensor_copy(o[:, 832+d:833+d], s[:, 112+j:113+j]), [s], [o])
            k.op("act", lambda e: e.activation(out=O("siluz"), in_=g4[:, 768+h*64:768+(h+1)*64], func=AF.Silu), [g4], [o])
            k.dma(O6D, O6D[h, r0:r0+128, :], o, o[:], skip_w=True)
            pA = PS[2 + i*3]; pB = PS[3 + i*3]; pC = PS[4 + i*3]
            for bi, src in enumerate(FM_SRC):
                if src is None:
                    src_ap = kst[:, (bi-6)*64:(bi-5)*64]; src_t = kst
                else:
                    src_ap = o[:, src[0]:src[1]]; src_t = o
                pp = (pA, pB, pC)[bi // 4]; off = (bi % 4)*128
                k.op("pe", lambda e: e.transpose(pp[0:64, off:off+128], src_ap, ident[:]), [src_t, ident], [pp])
            k.op("act", lambda e: e.copy(fmt[:, 0:4, :], pA[0:64, :].rearrange("p (c t) -> p c t", c=4)), [pA], [fmt])
            k.op("dve", lambda e: e.tensor_copy(fmt[:, 4:8, :], pB[0:64, :].rearrange("p (c t) -> p c t", c=4)), [pB], [fmt])
            k.op("act", lambda e: e.copy(fmt[:, 8:10, :], pC[0:64, 0:256].rearrange("p (c t) -> p c t", c=2)), [pC], [fmt])
            k.dma(FMD, FMD[h, t], fmt, fmt[:], q="pool", skip_w=True)
    for t0 in range(0, NT_, NB):
        k.interleave([(lambda t=t: tile_body(t)) for t in range(t0, min(NT_, t0 + NB))])
    k.stage_end()


def e_intra(k, PS, O6D, FMD, cc, O7D, O7T):
    k.stage_begin()
    ident, ones, trif, trir, trifs, trirs, ntrifs, ntrirs = [cc[:, i, :] for i in range(8)]
    class C: pass
    ch = []
    for c in range(4):
        o = C()
        o.P = [k.sb([128, 128], name="P") for _ in range(2)]; o.PT = [k.sb([128, 128], name="PT") for _ in range(2)]
        o.Y = [k.sb([128, 128], name="Y") for _ in range(2)]
        o.M1 = k.sb([128, 128], name="M1"); o.M2 = k.sb([128, 128], name="M2"); o.E2 = k.sb([128, 128], name="E2"); o.dg = k.sb([128, 128], name="dg")
        o.out = k.sb([128, 3, 128], name="out"); o.wt = k.sb([64, 128], name="wt")
        o.ps = [PS[c*2], PS[c*2+1]]
        ch.append(o)
    fms = [[k.sb([64, 8, 128], name="fm") for _ in range(2)] for _ in range(2)]
    tms = [[k.sb([128, 258], name="tm") for _ in range(2)] for _ in range(2)]
    for h in range(4):
      for tp in range(NT_ // 2):
        chains = []
        for j in range(2):
            t = tp*2 + j
            fm = fms[j][tp % 2]; tm = tms[j][tp % 2]
            k.dma(fm, fm[:], FMD, FMD[h, t, :, 0:8, :])
            k.dma(tm, tm[:, 0:256], O6D, O6D[h, t*128:(t+1)*128, 448:704], q="pool")
            k.dma(tm, tm[:, 256:258], O6D, O6D[h, t*128:(t+1)*128, 832:834], q="pool")
            for d in range(2):
                chains.append((ch[j*2+d], t, d, fm, tm))
        for (o, t, d, fm, tm) in chains:
            m1, m2, m3 = (ntrirs, ntrifs, trif) if d == 0 else (ntrifs, ntrirs, trir)
            gcol = tm[:, 256+d:257+d]
            k.op("dve", lambda e: e.tensor_scalar(out=o.dg[:], in0=ident, scalar1=gcol, scalar2=None, op0=ALU.mult), [cc, tm], [o.dg])
            k.op("pe", lambda e: e.matmul(o.ps[0][:, 0:128], ones, o.dg[:], start=True, stop=True), [cc, o.dg], [o.ps[0]])
            k.op("dve", lambda e: e.tensor_scalar(out=o.E2[:], in0=o.ps[0][:, 0:128], scalar1=gcol, scalar2=0.0, op0=ALU.subtract, op1=ALU.min), [o.ps[0], tm], [o.E2])
            k.op("dve", lambda e: e.tensor_scalar(out=o.M1[:], in0=o.ps[0][:, 0:128], scalar1=gcol, scalar2=0.0, op0=ALU.subtract, op1=ALU.max), [o.ps[0], tm], [o.M1])
            k.op("act", lambda e: e.activation(out=o.E2[:], in_=o.E2[:], func=AF.Exp), [o.E2], [o.E2])
            k.op("act", lambda e: e.activation(out=o.M1[:], in_=o.M1[:], func=AF.Exp, scale=-1.0), [o.M1], [o.M1])
            k.op("pool", lambda e: e.tensor_mul(out=o.M1[:], in0=o.M1[:], in1=m1), [o.M1, cc], [o.M1])
            k.op("pool", lambda e: e.tensor_mul(out=o.M2[:], in0=o.E2[:], in1=m2), [o.E2, cc], [o.M2])
            k.op("pool", lambda e: e.tensor_mul(out=o.E2[:], in0=o.E2[:], in1=m3), [o.E2, cc], [o.E2])
        for (o, t, d, fm, tm) in chains:
            mA = trif if d == 0 else trir
            knT = fm[:, 0, :]; qnT = fm[:, 1, :]; kbT = fm[:, 2+d, :]; qsT = fm[:, 4+d, :]; ksT = fm[:, 6+d, :]
            p0 = V(o.ps[0], 0, 128); p1 = V(o.ps[1], 0, 128)
            k.op("pe", lambda e: e.matmul(p0[:], kbT, knT, start=True, stop=True), [fm], [p0])
            k.op("pe", lambda e: e.matmul(p1[:], knT, kbT, start=True, stop=True), [fm], [p1])
            k.op("dve", lambda e: e.tensor_mul(out=o.P[0][:], in0=p0[:], in1=o.M1[:]), [p0, o.M1], [o.P[0]])
            k.op("dve", lambda e: e.tensor_mul(out=o.PT[0][:], in0=p1[:], in1=o.M2[:]), [p1, o.M2], [o.PT[0]])
            k.op("pe", lambda e: e.matmul(p0[:], knT, qnT, start=True, stop=True), [fm], [p0])
            k.op("pe", lambda e: e.matmul(p1[:], ksT, qsT, start=True, stop=True), [fm], [p1])
            k.op("dve", lambda e: e.tensor_mul(out=o.out[:, 1, :], in0=p0[:], in1=o.E2[:]), [p0, o.E2], [o.out])
            k.op("dve", lambda e: e.tensor_mul(out=o.out[:, 2, :], in0=p1[:], in1=mA), [p1, cc], [o.out])
            k.op("act", lambda e: e.copy(o.Y[0][:], tm[:, d*128:(d+1)*128]), [tm], [o.Y[0]])
        for m in range(6):
            a = m % 2; b = 1 - a
            for (o, t, d, fm, tm) in chains:
                p0 = V(o.ps[0], 0, 128); p1 = V(o.ps[1], 0, 128)
                k.op("pe", lambda e: e.matmul(p0[:], o.PT[a][:], o.Y[a][:], start=True, stop=True), [o.PT[a], o.Y[a]], [p0])
                dst = o.Y[b][:] if m < 5 else o.out[:, 0, :]
                dstT = o.Y[b] if m < 5 else o.out
                k.op("dve", lambda e: e.tensor_add(out=dst, in0=p0[:], in1=o.Y[a][:]), [p0, o.Y[a]], [dstT])
                if m < 5:
                    k.op("pe", lambda e: e.matmul(p1[:], o.P[a][:], o.PT[a][:], start=True, stop=True), [o.P[a], o.PT[a]], [p1])
                    k.op("act", lambda e: e.copy(o.PT[b][:], p1[:]), [p1], [o.PT[b]])
            if m < 4:
                for (o, t, d, fm, tm) in chains:
                    p0 = V(o.ps[0], 0, 128)
                    k.op("pe", lambda e: e.matmul(p0[:], o.PT[a][:], o.P[a][:], start=True, stop=True), [o.PT[a], o.P[a]], [p0])
                    k.op("act", lambda e: e.copy(o.P[b][:], p0[:]), [p0], [o.P[b]])
        for (o, t, d, fm, tm) in chains:
            k.dma(O7D, O7D[h, t*128:(t+1)*128, d, :, :], o.out, o.out[:], q="sp" if d == 0 else "pool", skip_w=True)
            p1 = V(o.ps[1], 0, 128)
            k.op("pe", lambda e: e.transpose(p1[0:64, :], o.out[:, 0, 64:128], ident), [o.out, cc], [p1])
            k.op("act", lambda e: e.copy(o.wt[:], p1[0:64, :]), [p1], [o.wt])
            k.dma(O7T, O7T[h, d, :, t*128:(t+1)*128], o.wt, o.wt[:], q="pool", skip_w=True)
    k.stage_end()


def e_stepglue(k, P, O6D, FMD, O7D, O7T, STEP):
    k.stage_begin()
    qi = 0
    def cp(dst, src, src_t):
        nonlocal qi
        k.dma(STEP, dst, src_t, src, q="sp" if qi % 2 == 0 else "pool", skip_w=True); qi += 1
    for h in range(4):
        S = STEP[h]
        S2 = S.rearrange("p (t two) d w -> p t two d w", two=2)
        o6c = O6D[h].rearrange("(c p) w -> p c w", p=64)
        o7c = O7D[h].rearrange("(c p) d j w -> p c d j w", p=64)
        o7t2 = O7D[h].rearrange("(t two p) d j w -> p t two d j w", two=2, p=64)
        pc = P.rearrange("(c p) w -> p c w", p=64)
        for d in range(2):
            for (blk_i, col) in ((4+d, 0), (8+d, 128)):
                for two in range(2):
                    cp(S2[:, :, two, d, col:col+64], FMD[h][:, :, blk_i, two*64:(two+1)*64].rearrange("t f k -> f t k"), FMD)
            cp(S[:, :, d, 64:128], O7T[h, d].rearrange("f (c k) -> f c k", k=64), O7T)
            kg0 = O6["KG0"][0] + d*130
            cp(S[:, :, d, 192:322], o6c[:, :, kg0:kg0+130], O6D)
            cp(S[:, :, d, 322:386], pc[:, :, 1280 + h*64:1280 + (h+1)*64], P)
            for two in range(2):
                cp(S2[:, :, two, d, 387:451], o7t2[:, :, two, d, 2, two*64:(two+1)*64], O7D)
                cp(S2[:, :, two, d, 515:579], o7t2[:, :, two, d, 1, two*64:(two+1)*64], O7D)
            cp(S[:, :, d, 451:515], o7c[:, :, d, 0, 0:64], O7D)
    k.stage_end()


def e_scan(k, PS, STEP, O8D):
    k.stage_begin()
    BS = 4; nblk = NC_ // BS
    R = []
    for s_ in range(2):
        r = dict(bufs=[[k.sb([64, BS, W8], name="blk") for _ in range(2)] for _ in range(2)],
                 obufs=[[k.sb([64, BS, 128], name="ob") for _ in range(2)] for _ in range(2)],
                 C1=[k.sb([64, 65], name="C1") for _ in range(2)], Sg=[k.sb([64, 64], name="S") for _ in range(2)],
                 tmpc=[k.sb([64, 65], name="tc") for _ in range(2)], vnew=[k.sb([64, 64], name="vn") for _ in range(2)],
                 rr=[k.sb([64, 4], name="rr") for _ in range(2)])
        psA = [PS[4*s_], PS[4*s_+1]]; psB = [PS[4*s_+2], PS[4*s_+3]]
        r["pso"] = [V(psA[d], 0, 65) for d in range(2)]; r["psc"] = [V(psA[d], 128, 193) for d in range(2)]
        r["psw"] = [V(psB[d], 0, 64) for d in range(2)]; r["pss"] = [V(psB[d], 128, 192) for d in range(2)]; r["psg"] = [V(psB[d], 256, 320) for d in range(2)]
        R.append(r)

    def head_body(h, r):
        bufs = r["bufs"]; obufs = r["obufs"]; C1 = r["C1"]; Sg = r["Sg"]; tmpc = r["tmpc"]; vnew = r["vnew"]; rr = r["rr"]
        pso = r["pso"]; psc = r["psc"]; psw = r["psw"]; pss = r["pss"]; psg = r["psg"]
        for d in range(2):
            k.op("dve", lambda e: e.memset(C1[d][:], 0.0), [], [C1[d]])
            k.op("dve", lambda e: e.memset(Sg[d][:], 0.0), [], [Sg[d]])
        for b in range(nblk):
            mb = [b, 0 if b == 0 else nblk - b]
            B = [bufs[d][b % 2] for d in range(2)]; OB = [obufs[d][b % 2] for d in range(2)]
            for d in range(2):
                k.op("pool", lambda e: e.memset(B[d][:, :, 386:387], 1.0), [], [B[d]])
                k.dma(B[d], B[d][:, :, 0:386], STEP, STEP[h, :, mb[d]*BS:(mb[d]+1)*BS, d, 0:386], q="sp" if d == 0 else "pool")
                k.dma(B[d], B[d][:, :, 387:579], STEP, STEP[h, :, mb[d]*BS:(mb[d]+1)*BS, d, 387:579], q="sp" if d == 0 else "pool")
            for jj in range(BS):
                sl = [jj, BS - 1 - jj]
                F = lambda d, n: B[d][:, sl[d], S8[n][0]:S8[n][1]]
                R2 = range(2)
                for d in R2:
                    k.op("pe", lambda e: e.matmul(psw[d][:64], F(d, "WT"), Sg[d][:], start=True, stop=True), [B[d], Sg[d]], [psw[d]])
                for d in R2:
                    k.op("pe", lambda e: e.matmul(pso[d][:64], F(d, "QsT"), C1[d][:], start=True, stop=False), [B[d], C1[d]], [pso[d]])
                    k.op("pe", lambda e: e.matmul(pso[d][:64], F(d, "AT"), F(d, "V1"), start=False, stop=True), [B[d]], [pso[d]])
                    k.op("pe", lambda e: e.matmul(psc[d][:64], F(d, "Ks"), F(d, "V1"), start=True, stop=True), [B[d]], [psc[d]])
                for d in R2:
                    k.op("dve", lambda e: e.tensor_sub(out=vnew[d][:], in0=F(d, "U"), in1=psw[d][:64]), [B[d], psw[d]], [vnew[d]])
                for d in R2:
                    k.op("pe", lambda e: e.matmul(psg[d][:64], F(d, "QgT"), Sg[d][:], start=True, stop=False), [B[d], Sg[d]], [psg[d]])
                    k.op("pe", lambda e: e.matmul(psg[d][:64], F(d, "AqkT"), vnew[d][:], start=False, stop=True), [B[d], vnew[d]], [psg[d]])
                    k.op("pe", lambda e: e.matmul(pss[d][:64], F(d, "Kd"), vnew[d][:], start=True, stop=True), [B[d], vnew[d]], [pss[d]])
                for d in R2:
                    k.op("dve", lambda e: e.tensor_add(out=tmpc[d][:], in0=psc[d][:64], in1=C1[d][:]), [psc[d], C1[d]], [tmpc[d]])
                    k.op("dve", lambda e: e.tensor_scalar(out=C1[d][:], in0=tmpc[d][:], scalar1=F(d, "sm"), scalar2=None, op0=ALU.mult), [tmpc[d], B[d]], [C1[d]])
                    k.op("dve", lambda e: e.scalar_tensor_tensor(out=Sg[d][:], in0=Sg[d][:], scalar=F(d, "sg"), in1=pss[d][:64], op0=ALU.mult, op1=ALU.add), [Sg[d], B[d], pss[d]], [Sg[d]])
                for d in R2:
                    k.op("act", lambda e: e.activation(out=rr[d][:, 0:1], in_=pso[d][:64, 64:65], func=AF.Abs), [pso[d]], [rr[d]])
                    k.op("dve", lambda e: e.tensor_scalar(out=rr[d][:, 2:3], in0=rr[d][:, 0:1], scalar1=1.0, scalar2=None, op0=ALU.max), [rr[d]], [rr[d]])
                    k.op("dve", lambda e: e.reciprocal(out=rr[d][:, 1:2], in_=rr[d][:, 2:3]), [rr[d]], [rr[d]])
                    k.op("act", lambda e: e.activation(out=OB[d][:, sl[d], 0:64], in_=pso[d][:64, 0:64], func=AF.Copy, scale=rr[d][:, 1:2]), [pso[d], rr[d]], [OB[d]])
                    k.op("act", lambda e: e.copy(OB[d][:, sl[d], 64:128], psg[d][:64]), [psg[d]], [OB[d]])
            for d in range(2):
                k.dma(O8D, O8D[h, :, mb[d]*BS:(mb[d]+1)*BS, d, :], OB[d], OB[d][:], q="sp", skip_w=True)

    for h0 in range(0, 4, 2):
        k.interleave([(lambda h=h0: head_body(h, R[0])), (lambda h=h0 + 1: head_body(h, R[1]))])
    k.stage_end()


def e_outproj(k, PS, AXD, O8D, O6D, GAIN2, X, modrow, sel, ident, W, XMID):
    k.stage_begin()
    gn = k.sb([128, 512], name="gn"); w = k.sb([128, 8, 1024], BF16, name="w")
    gate = [k.sb([128, 1024], name="gate") for _ in range(2)]
    k.dma(gn, gn[:], None, GAIN2[:, :])
    for cls in range(2):
        bcast_rows(k, PS, gate[cls], V(modrow, 2*1024, 3*1024), 1 if cls == 0 else 0, sel)
    for c in range(8):
        k.dma(w, w[:, c, :], None, W[c*128:(c+1)*128, :], q="pool")
    B = [dict(mix=k.sb([128, 1024], name="mix"), a=k.sb([128, 512], name="a"), b=k.sb([128, 512], name="b"), g=k.sb([128, 512], name="g"),
              sq=k.sb([128, 512], name="sq"), s=k.sb([128, 32], name="s"), x=k.sb([128, 1024], name="x"), mT=k.sb([128, 8, 128], BF16, name="mT"),
              o=k.sb([128, 1024], name="o")) for _ in range(2)]
    pi = 0
    def tile_body(t):
        nonlocal pi
        cls = 0 if t < 2 else 1
        b = B[t % 2]; mix = b['mix']; a = b['a']; bb = b['b']; g = b['g']; s = b['s']; x = b['x']; mT = b['mT']; o = b['o']
        rows = slice(t*128, (t+1)*128)
        k.dma(mix, mix[:, 0:512], AXD, AXD[rows, :])
        k.dma(x, x[:], X, X[rows, :])
        qi = 0
        for h in range(4):
            k.dma(g, g[:, h*64:(h+1)*64], O6D, O6D[h, rows, 128:192], q="pool")
            k.dma(g, g[:, 256+h*64:256+(h+1)*64], O6D, O6D[h, rows, 834:898], q="pool")
            for d in range(2):
                dst_t = a if d == 0 else bb
                for half in range(2):
                    dst = dst_t[half*64:(half+1)*64, :].rearrange("p (g hh w) -> p g hh w", g=2, hh=4)[:, :, h, :]
                    src = O8D[h, :, 2*t + half, d, :].rearrange("p (g w) -> p g w", g=2)
                    k.dma(dst_t, dst, O8D, src, q="sp" if qi % 2 == 0 else "pool"); qi += 1
        k.op("dve", lambda e: e.tensor_add(out=a[:], in0=a[:], in1=bb[:]), [a, bb], [a])
        k.op("dve", lambda e: e.tensor_mul(out=b['sq'][:], in0=a[:], in1=a[:]), [a], [b['sq']])
        k.op("dve", lambda e: e.reduce_sum(out=s[:, 0:8], in_=b['sq'][:].rearrange("p (h d) -> p h d", h=8), axis=AX.X), [b['sq']], [s])
        k.op("dve", lambda e: e.tensor_scalar(out=s[:, 0:8], in0=s[:, 0:8], scalar1=1.0/64, scalar2=EPS, op0=ALU.mult, op1=ALU.add), [s], [s])
        k.op("act", lambda e: e.activation(out=s[:, 8:16], in_=s[:, 0:8], func=AF.Sqrt), [s], [s])
        k.op("dve", lambda e: e.reciprocal(out=s[:, 16:24], in_=s[:, 8:16]), [s], [s])
        for h in range(8):
            k.op("dve" if h % 2 == 0 else "pool", lambda e: e.tensor_scalar(out=a[:, h*64:(h+1)*64], in0=a[:, h*64:(h+1)*64], scalar1=s[:, 16+h:17+h], scalar2=None, op0=ALU.mult), [a, s], [a])
        k.op("dve", lambda e: e.tensor_mul(out=a[:], in0=a[:], in1=gn[:]), [a, gn], [a])
        k.op("dve", lambda e: e.tensor_mul(out=mix[:, 512:1024], in0=a[:], in1=g[:]), [a, g], [mix])
        for half in range(2):
            pt = PS[t % 2]
            for c in range(4):
                cc_ = half*4 + c
                k.op("pe", lambda e: e.transpose(pt[:, c*128:(c+1)*128], mix[:, cc_*128:(cc_+1)*128], ident[:]), [mix, ident], [pt])
            if half == 0:
                k.op("act", lambda e: e.copy(mT[:, 0:4, :], pt[:].rearrange("p (c t) -> p c t", c=4)), [pt], [mT])
            else:
                k.op("dve", lambda e: e.tensor_copy(mT[:, 4:8, :], pt[:].rearrange("p (c t) -> p c t", c=4)), [pt], [mT])
        for nb in range(2):
            ps = PS[2 + pi % 4]; pi += 1
            for c in range(8):
                k.op("pe", lambda e: e.matmul(ps[:], mT[:, c, :], w[:, c, nb*512:(nb+1)*512], start=(c == 0), stop=(c == 7)), [mT, w], [ps])
            k.op("dve", lambda e: e.tensor_mul(out=o[:, nb*512:(nb+1)*512], in0=ps[:], in1=gate[cls][:, nb*512:(nb+1)*512]), [ps, gate[cls]], [o])
        k.op("pool", lambda e: e.tensor_add(out=o[:], in0=o[:], in1=x[:]), [o, x], [o])
        k.dma(XMID, XMID[rows, :], o, o[:], skip_w=True)
    for t0 in range(NT_):
        tile_body(t0)
    k.stage_end()


def e_topk(k, AFFT, VALS, IDX):
    k.stage_begin()
    for (c0, n, kk, o0, nm) in [(NCTX, T_ - NCTX, 1024, 0, "l"), (0, NCTX, 32, 1024, "c")]:
        a = k.sb([16, n], name="a" + nm); wk = k.sb([16, n], name="w" + nm)
        vals = k.sb([16, kk], name="v" + nm); idx = k.sb([16, kk], U32, name="i" + nm)
        k.dma(a, a[:], AFFT, AFFT[:, c0:c0+n])
        k.op("dve", lambda e: e.tensor_copy(wk[:], a[:]), [a], [wk])
        for r in range(kk // 8):
            k.op("dve", lambda e: e.max(out=vals[:, r*8:(r+1)*8], in_=wk[:]), [wk], [vals])
            k.op("dve", lambda e: e.max_index(out=idx[:, r*8:(r+1)*8], in_max=vals[:, r*8:(r+1)*8], in_values=wk[:]), [vals, wk], [idx])
            k.op("dve", lambda e: e.match_replace(out=wk[:], in_to_replace=vals[:, r*8:(r+1)*8], in_values=wk[:], imm_value=-1.0), [vals, wk], [wk])
        k.dma(VALS, VALS[:, o0:o0+kk], vals, vals[:], skip_w=True); k.dma(IDX, IDX[:, o0:o0+kk], idx, idx[:], skip_w=True)
    k.stage_end()


def e_expert(k, PS, H2, VALS, IDX, W1, W3, W2, ident, ACC):
    k.stage_begin()
    z = k.sb([128, 1024], name="z")
    k.op("dve", lambda e: e.memset(z[:], 0.0), [], [z])
    for t in range(NT_):
        k.dma(ACC, ACC[t*128:(t+1)*128, :], z, z[:], q="sp" if t % 2 == 0 else "pool", skip_w=True)
    RT = 9
    parts = [(0, 9)]
    PR = 9 * 128
    w2 = k.sb([128, 16, 1024], BF16, name="w2"); vl = k.sb([128, RT], name="vl"); ix = k.sb([128, RT], U32, name="ix")
    hid = k.sb([128, 16, PR], BF16, name="hid"); xt = k.sb([128, 8, PR], BF16, name="xt")
    xg = [k.sb([128, 1024], name="xg") for _ in range(2)]
    w1c = [k.sb([128, 8, 128], BF16, name="w1c") for _ in range(3)]; w3c = [k.sb([128, 8, 128], BF16, name="w3c") for _ in range(3)]
    sg = [k.sb([128, 512], name="sg") for _ in range(2)]
    ysb = [k.sb([128, 1024], name="ysb") for _ in range(2)]
    ph1 = [PS[0], PS[1]]; ph3 = [PS[2], PS[3]]; py = [PS[4], PS[5], PS[6], PS[7]]
    ci = 0; hi = 0; yi = 0; gi = 0
    first_scatter = True
    for e_ in range(16):
        k.dma(w2, w2[:], None, W2[e_].rearrange("(c p) n -> p c n", p=128), q="pool")
        k.op("dve", lambda e: e.memset(vl[:], 0.0), [], [vl])
        k.op("pool", lambda e: e.memset(ix[:], 0), [], [ix])
        k.dma(vl, vl[:, 0:8], VALS, VALS[e_, 0:1024].rearrange("(t p) -> p t", p=128), allow_slow_non_contiguous=True)
        k.dma(vl, vl[0:32, 8:9], VALS, VALS[e_, 1024:1056].rearrange("(t p) -> p t", p=32), allow_slow_non_contiguous=True)
        k.dma(ix, ix[:, 0:8], IDX, IDX[e_, 0:1024].rearrange("(t p) -> p t", p=128), allow_slow_non_contiguous=True)
        k.dma(ix, ix[0:32, 8:9], IDX, IDX[e_, 1024:1056].rearrange("(t p) -> p t", p=32), allow_slow_non_contiguous=True)
        for (t0, t1) in parts:
            nr = (t1 - t0) * 128
            for tt in range(t0, t1):
                g_ = xg[gi % 2]; gi += 1
                k.dma(g_, g_[:], H2, H2[:, :], q="pool", element_offset=(NCTX*1024 if tt < 8 else 0),
                      indirect=dict(in_offset=bass.IndirectOffsetOnAxis(ap=ix[:, tt:tt+1], axis=0), idx_t=ix))
                lr = (tt - t0) * 128
                for half in range(2):
                    pt = py[half]
                    for c in range(4):
                        cc_ = half*4 + c
                        k.op("pe", lambda e: e.transpose(pt[:, c*128:(c+1)*128], g_[:, cc_*128:(cc_+1)*128], ident[:]), [g_, ident], [pt])
                    k.op("act" if half == 0 else "dve", (lambda e: e.copy(xt[:, 0:4, lr:lr+128], pt[:].rearrange("p (c t) -> p c t", c=4))) if half == 0 else
                         (lambda e: e.tensor_copy(xt[:, 4:8, lr:lr+128], pt[:].rearrange("p (c t) -> p c t", c=4))), [pt], [xt])
            for fc in range(16):
                a1 = w1c[ci % 3]; a3 = w3c[ci % 3]; ci += 1
                k.dma(a1, a1[:], None, W1[e_].rearrange("(c p) f -> p c f", p=128)[:, :, fc*128:(fc+1)*128], q="pool")
                k.dma(a3, a3[:], None, W3[e_].rearrange("(c p) f -> p c f", p=128)[:, :, fc*128:(fc+1)*128], q="pool")
                for rb in range(0, nr, 512):
                    rn = min(512, nr - rb)
                    p1 = ph1[hi % 2]; p3 = ph3[hi % 2]; s_ = sg[hi % 2]; hi += 1
                    for c in range(8):
                        k.op("pe", lambda e: e.matmul(p1[:, 0:rn], a1[:, c, :], xt[:, c, rb:rb+rn], start=(c == 0), stop=(c == 7)), [a1, xt], [p1])
                    for c in range(8):
                        k.op("pe", lambda e: e.matmul(p3[:, 0:rn], a3[:, c, :], xt[:, c, rb:rb+rn], start=(c == 0), stop=(c == 7)), [a3, xt], [p3])
                    k.op("act", lambda e: e.activation(out=s_[:, 0:rn], in_=p1[:, 0:rn], func=AF.Silu), [p1], [s_])
                    k.op("dve", lambda e: e.tensor_mul(out=hid[:, fc, rb:rb+rn], in0=s_[:, 0:rn], in1=p3[:, 0:rn]), [s_, p3], [hid])
            for tt in range(t0, t1):
                ys = ysb[yi % 2]; yi += 1
                lr = (tt - t0) * 128
                for nb in range(2):
                    pp = py[(yi*2 + nb) % 4]
                    for fc in range(16):
                        k.op("pe", lambda e: e.matmul(pp[:], hid[:, fc, lr:lr+128], w2[:, fc, nb*512:(nb+1)*512], start=(fc == 0), stop=(fc == 15)), [hid, w2], [pp])
                    if nb == 0:
                        k.op("dve", lambda e: e.tensor_scalar(out=ys[:, 0:512], in0=pp[:], scalar1=vl[:, tt:tt+1], scalar2=None, op0=ALU.mult), [pp, vl], [ys])
                    else:
                        k.op("act", lambda e: e.activation(out=ys[:, 512:1024], in_=pp[:], func=AF.Copy, scale=vl[:, tt:tt+1]), [pp, vl], [ys])
                if tt < 8:
                    k.dma(ACC, ACC[:, :], ys, ys[:], q="pool", compute_op=ALU.add, element_offset=NCTX*1024,
                          indirect=dict(out_offset=bass.IndirectOffsetOnAxis(ap=ix[:, tt:tt+1], axis=0), idx_t=ix))
                else:
                    k.dma(ACC, ACC[:, :], ys, ys[0:32, :], q="pool", compute_op=ALU.add, element_offset=0,
                          indirect=dict(out_offset=bass.IndirectOffsetOnAxis(ap=ix[0:32, tt:tt+1], axis=0), idx_t=ix))
    k.stage_end()


def e_combine(k, PS, XMID, ACC, modrow, sel, OUTT, out_rows0):
    k.stage_begin()
    gate = [k.sb([128, 1024], name="gate") for _ in range(2)]
    for cls in range(2):
        bcast_rows(k, PS, gate[cls], V(modrow, 5*1024, 6*1024), 1 if cls == 0 else 0, sel)
    xs = [k.sb([128, 1024], name="x") for _ in range(2)]; ac = [k.sb([128, 1024], name="ac") for _ in range(2)]
    for t in range(out_rows0 // 128, NT_):
        cls = 0 if t < 2 else 1
        x = xs[t % 2]; a = ac[t % 2]; rows = slice(t*128, (t+1)*128)
        k.dma(x, x[:], XMID, XMID[rows, :]); k.dma(a, a[:], ACC, ACC[rows, :], q="pool")
        k.op("dve", lambda e: e.tensor_mul(out=a[:], in0=a[:], in1=gate[cls][:]), [a, gate[cls]], [a])
        k.op("pool", lambda e: e.tensor_add(out=x[:], in0=x[:], in1=a[:]), [x, a], [x])
        k.dma(OUTT, OUTT[t*128 - out_rows0:(t+1)*128 - out_rows0, :], x, x[:], skip_w=True)
    k.stage_end()


def build_fused(nlayer=2, upto=None):
    k = K()
    TT = T_
    def IN(name, shape, dt=F32):
        t = T(k, k.dram_in(name, shape, dt), name); t.is_dram = True
        return t
    XCAT = IN("xcat", [TT, 1024]); CT = IN("ct", [128, 8, 2]); IDENT = IN("ident", [128, 128]); SEL = IN("sel", [2, 2, 128])
    CS = IN("cs", [TT, 320]); SN = IN("sn", [TT, 320]); CC = IN("cc", [9, 128, 128])
    L = []
    for l in range(nlayer):
        L.append(dict(MODW=IN(f"modw{l}", [1024, 6144]), MODB=IN(f"modb{l}", [2, 6144]), N1G=IN(f"n1g{l}", [128, 1024]), WIN=IN(f"win{l}", [1024, 2848]),
                      QKG=IN(f"qkg{l}", [128, 640]), CB=IN(f"cb{l}", [128, 32]), CW=IN(f"cw{l}", [128, 5, 768]), GAIN2=IN(f"gain2{l}", [128, 512]),
                      WOUT=IN(f"wout{l}", [1024, 1024]), N2G=IN(f"n2g{l}", [128, 1024]), RW=IN(f"rw{l}", [1024, 16]),
                      W1=IN(f"w1{l}", [16, 1024, 2048]), W3=IN(f"w3{l}", [16, 1024, 2048]), W2=IN(f"w2{l}", [16, 2048, 1024])))
    OUT = k.dram_out("out", [TT - NCTX, 1024])
    PS = [k.ps([128, 512], name=f"bank{i}") for i in range(8)]
    ident = k.sb([128, 128], name="ident"); sel = k.sb([2, 2, 128], name="sel"); ctsb = k.sb([128, 8, 2], name="ct")
    cc = k.sb([128, 9, 128], name="cc")
    modrow = k.sb([2, 6144], name="modrow")
    k.dma(ident, ident[:], None, IDENT[:, :]); k.dma(sel, sel[:], None, SEL[:, :, :]); k.dma(ctsb, ctsb[:], None, CT[:, :, :])
    for i in range(9):
        k.dma(cc, cc[:, i, :], None, CC[i])
    k.op("act", lambda e: e.activation(out=ctsb[:], in_=ctsb[:], func=AF.Silu), [ctsb], [ctsb])
    D = lambda n, s, dt=F32: k.dram_tmp(n, s, dt)
    P = D("P", [TT, 2848]); QKT = D("QKT", [640, TT], BF16); AXD = D("AXD", [TT, 512]); O6D = D("O6D", [4, TT, W6]); FMD = D("FMD", [4, NT_, 64, 10, 128])
    O7D = D("O7D", [4, TT, 2, 3, 128]); O7T = D("O7T", [4, 2, 64, TT]); STEP = D("STEP", [4, 64, NC_, 2, W8]); O8D = D("O8D", [4, 64, NC_, 2, 128])
    XMID = D("XMID", [TT, 1024]); H2 = D("H2", [TT, 1024]); AFFT = D("AFFT", [16, TT]); VALS = D("VALS", [16, 1152]); IDX = D("IDX", [16, 1152], U32)
    ACC = D("ACC", [TT, 1024]); XN = D("XN", [TT, 1024])
    X = XCAT
    for l in range(nlayer):
        W = L[l]
        e_mod(k, PS, ctsb, W["MODW"], W["MODB"], modrow)
        e_normlin(k, PS, X, W["N1G"], modrow, 1, 0, sel, ident, W["WIN"], 2848, P, QKT=QKT, qk=dict(GAIN=W["QKG"], CS=CS, SN=SN))
        e_attn(k, PS, QKT, P, AXD)
        e_prep(k, PS, P, W["CB"], W["CW"], cc, ident, O6D, FMD)
        e_intra(k, PS, O6D, FMD, cc, O7D, O7T)
        e_stepglue(k, P, O6D, FMD, O7D, O7T, STEP)
        e_scan(k, PS, STEP, O8D)
        e_outproj(k, PS, AXD, O8D, O6D, W["GAIN2"], X, modrow, sel, ident, W["WOUT"], XMID)
        if upto == "xmid":
            k.stage_begin(); xx = [k.sb([128, 1024], name="cpy") for _ in range(2)]
            for t in range(2, NT_):
                k.dma(xx[t % 2], xx[t % 2][:], XMID, XMID[t*128:(t+1)*128, :]); k.dma(OUT, OUT[(t-2)*128:(t-1)*128, :], xx[t % 2], xx[t % 2][:], skip_w=True)
            k.stage_end(); break
        e_normlin(k, PS, XMID, W["N2G"], modrow, 4, 3, sel, ident, W["RW"], 16, None, H=H2, softmax=True, YT=AFFT)
        e_topk(k, AFFT, VALS, IDX)
        e_expert(k, PS, H2, VALS, IDX, W["W1"], W["W3"], W["W2"], ident, ACC)
        last = (l == nlayer - 1)
        e_combine(k, PS, XMID, ACC, modrow, sel, OUT if last else XN, NCTX if last else 0)
        X = XN
    k.finish([OUT])
    k.close()
    return k

def _rep(v, p=128):
    return np.ascontiguousarray(np.broadcast_to(np.asarray(v, np.float32)[None], (p,) + tuple(np.shape(v))))

def _consts():
    f32 = np.float32
    n = 8192
    row = (np.arange(n) // 64).astype(f32); col = (np.arange(n) % 64).astype(f32)
    inv = (10000.0 ** (-np.arange(16, dtype=f32) / 16)).astype(f32)
    ang = np.stack([row, col], -1)[..., None] * inv
    tile = lambda a: np.ascontiguousarray(np.broadcast_to(a[:, None], (a.shape[0], 10, 2, 16))).reshape(a.shape[0], 320)
    cs = np.concatenate([np.ones((256, 320), f32), tile(np.cos(ang).astype(f32))]); sn = np.concatenate([np.zeros((256, 320), f32), tile(np.sin(ang).astype(f32))])
    sel = np.zeros((2, 2, 128), f32); sel[0, 0] = 1; sel[1, 1] = 1
    s = np.arange(128)[:, None]; t = np.arange(128)[None, :]
    same = (s // 64) == (t // 64)
    trif = (same & (s <= t)).astype(f32); trir = (same & (s >= t)).astype(f32); eye = np.eye(128, dtype=f32)
    cc = np.stack([eye, np.ones((128, 128), f32), trif, trir, trif - eye, trir - eye, eye - trif, eye - trir, same.astype(f32)])
    return dict(cs=cs, sn=sn, sel=sel, cc=cc, ident=eye)

def make_in_maps(inp, nlayer=2):
    f32 = np.float32
    C = _consts()
    ims = []
    for b in range(2):
        ct = np.stack([inp['c'][b], inp['c_ctx']], 1).reshape(8, 128, 2).transpose(1, 0, 2)
        m = {"xcat": np.concatenate([inp['ctx'][b], inp['x'][b]], 0), "ct": np.ascontiguousarray(ct, dtype=f32)}
        m.update(C)
        for l in range(nlayer):
            cb = np.concatenate([inp['mlstm_i_bias'][l].reshape(8), inp['mlstm_f_bias'][l].reshape(8), inp['gdn_a_log'][l].reshape(8), inp['gdn_dt_bias'][l].reshape(8)])
            m.update({f"modw{l}": inp['mod_w'][l], f"modb{l}": _rep(inp['mod_b'][l], 2), f"n1g{l}": _rep(inp['norm1_g'][l]), f"win{l}": inp['w_in'][l],
                      f"qkg{l}": _rep(np.concatenate([np.tile(inp['q_norm_g'][l], 8), np.tile(inp['k_norm_g'][l], 2)])),
                      f"cb{l}": _rep(cb), f"cw{l}": _rep(inp['gdn_conv_w'][l]),
                      f"gain2{l}": _rep(np.concatenate([inp['mlstm_out_g'][l].reshape(256), np.tile(inp['gdn_out_g'][l], 4)])),
                      f"wout{l}": inp['w_out'][l], f"n2g{l}": _rep(inp['norm2_g'][l]), f"rw{l}": inp['router_w'][l],
                      f"w1{l}": inp['w1'][l], f"w3{l}": inp['w3'][l], f"w2{l}": inp['w2'][l]})
        ims.append({k_: np.ascontiguousarray(v, dtype=f32) for k_, v in m.items()})
    return ims

_K = {}
def kernel(**inputs):
    inp = {k_: np.asarray(v, np.float32) for k_, v in inputs.items()}
    if 2 not in _K:
        _K[2] = build_fused(2)
    res = run_bass_kernel_spmd(_K[2].nc, make_in_maps(inp, 2), core_ids=[0, 1])
    return np.ascontiguousarray(np.stack([res.results[b]["out"] for b in range(2)]).astype(np.float32))
```

```python
import numpy as np
import threading
from contextlib import ExitStack
import concourse.bass as bass
import concourse.mybir as mybir
from concourse.bass_utils import run_bass_kernel_spmd

F32 = mybir.dt.float32
I32 = mybir.dt.int32
U32 = mybir.dt.uint32
BF16 = mybir.dt.bfloat16
ALU = mybir.AluOpType
AF = mybir.ActivationFunctionType
AX = mybir.AxisListType
EPS = 1e-6
SAME_ENGINE_FIFO = False


class T:
    def __init__(self, k, ap, name):
        self.k = k; self.ap = ap; self.name = name
        self.w = None; self.r = {}
        self.dsem = {}; self.dn = 0; self.is_dram = False

    def __getitem__(self, idx):
        return self.ap[idx]

    def rearrange(self, *a, **kw):
        return self.ap.rearrange(*a, **kw)


class V:
    def __init__(s, t, a0, a1):
        s.t = t; s.base = t; s.a0 = a0; s.a1 = a1

    def __getitem__(s, idx):
        return s.t.ap[:, s.a0:s.a1][idx]


class K:
    def __init__(self):
        self.nc = bass.Bass("TRN2", target_bir_lowering=False)
        self.es = ExitStack()
        self.st = None
        nc = self.nc
        self.es.enter_context(nc.allow_low_precision('bf16 matmul operands, fp32 PSUM accumulation'))
        self.eng = {"pe": nc.tensor, "act": nc.scalar, "dve": nc.vector, "pool": nc.gpsimd, "sp": nc.sync}
        self.sem = {}; self.cnt = {}
        for e in ["pe", "act", "dve", "pool"]:
            self.sem[e] = self.es.enter_context(nc.semaphore("s_" + e)); self.cnt[e] = 0
        self.seen = {e: {} for e in self.eng}
        self.ntile = 0
        self.dsems = {}
        self.free_sems = {'sp': [], 'pool': []}
        self.stage_tiles = []
        self.nsem = 0

    def dram_in(self, name, shape, dt=F32):
        return self.nc.dram_tensor(name, list(shape), dt, kind="ExternalInput").ap()

    def dram_out(self, name, shape, dt=F32):
        t = T(self, self.nc.dram_tensor(name, list(shape), dt, kind="ExternalOutput").ap(), name); t.is_dram = True
        return t

    def dram_tmp(self, name, shape, dt=F32):
        t = T(self, self.nc.dram_tensor(name, list(shape), dt, kind="Internal").ap(), name); t.is_dram = True
        return t

    def sb(self, shape, dt=F32, name=None):
        self.ntile += 1
        name = (name or "t") + f"_{self.ntile}"
        stk = self.st if self.st is not None else self.es
        h = stk.enter_context(self.nc.sbuf_tensor("S_" + name, list(shape), dt))
        t = T(self, h[:], name)
        if self.st is not None:
            self.stage_tiles.append(t)
        return t

    def ps(self, shape, dt=F32, name=None):
        self.ntile += 1
        name = (name or "p") + f"_{self.ntile}"
        h = self.es.enter_context(self.nc.psum_tensor("P_" + name, list(shape), dt))
        return T(self, h[:], name)

    def stage_begin(self):
        assert self.st is None
        self.st = ExitStack(); self.stage_tiles = []

    def stage_end(self):
        self.barrier()
        for t in self.stage_tiles:
            for q_, sm_ in t.dsem.items():
                self.free_sems[q_].append(sm_)
        self.st.close(); self.st = None; self.stage_tiles = []

    def barrier(self):
        toks = [(self.sem[c], self.cnt[c]) for c in self.sem if self.cnt[c] > 0]
        toks += [(s, 16 * n) for (s, n) in self.dsems.values() if n > 0]
        for e in self.eng:
            self._wait(e, toks)

    def _get_dsem(self, t, q):
        if q not in t.dsem:
            if self.free_sems[q]:
                t.dsem[q] = self.free_sems[q].pop()
            else:
                self.nsem += 1
                sm_ = self.es.enter_context(self.nc.semaphore(f"d{self.nsem}{q}"))
                t.dsem[q] = sm_
                self.dsems[id(sm_)] = [sm_, 0]
        return t.dsem[q]

    def _deps(self, reads, writes):
        toks = []
        for t in reads:
            if t.w is not None:
                toks.append(t.w)
        for t in writes:
            if t.w is not None:
                toks.append(t.w)
            toks.extend(t.r.values())
        return toks

    def _wait(self, e, toks, skip_sem=None):
        eng = self.eng[e]
        best = {}
        for (s, v) in toks:
            if skip_sem is not None and s is skip_sem:
                continue
            if best.get(id(s), (None, 0))[1] < v:
                best[id(s)] = (s, v)
        for (s, v) in best.values():
            if self.seen[e].get(id(s), 0) < v:
                eng.wait_ge(s, v)
                self.seen[e][id(s)] = v

    def _mark(self, tok, reads, writes):
        for t in reads:
            t.r[id(tok[0])] = tok
        for t in writes:
            t.w = tok; t.r = {}

    def op(self, e, fn, reads, writes):
        reads = [getattr(t, "base", t) for t in reads if t is not None]
        writes = [getattr(t, "base", t) for t in writes]
        toks = self._deps(reads, writes)
        self._wait(e, toks, skip_sem=self.sem[e] if (e == "pe" or SAME_ENGINE_FIFO) else None)
        ins = fn(self.eng[e])
        self.cnt[e] += 1
        ins.then_inc(self.sem[e], 1)
        tok = (self.sem[e], self.cnt[e])
        self._mark(tok, reads, writes)
        self._yield()
        return tok

    def dma(self, out_t, out_ap, in_t, in_ap, q="sp", skip_w=False, indirect=None, **kw):
        reads = [in_t] if in_t is not None else []
        if indirect is not None and indirect.get("idx_t") is not None:
            reads.append(indirect["idx_t"])
        writes = [out_t] if out_t is not None else []
        toks = self._deps(reads, [] if skip_w else writes)
        self._wait(q, toks)
        if out_t is not None and not out_t.is_dram:
            owner = out_t
        elif in_t is not None and not in_t.is_dram:
            owner = in_t
        else:
            owner = out_t if out_t is not None else in_t
        sem = self._get_dsem(owner, q)
        ent = self.dsems[id(sem)]
        ent[1] += 1
        if indirect is None:
            ins = self.eng[q].dma_start(out=out_ap, in_=in_ap, **kw)
        else:
            ins = self.eng[q].indirect_dma_start(out=out_ap, out_offset=indirect.get("out_offset"), in_=in_ap,
                                                 in_offset=indirect.get("in_offset"), **kw)
        ins.then_inc(sem, 16)
        tok = (sem, 16 * ent[1])
        self._mark(tok, reads, writes)
        self._yield()
        return tok

    def _yield(self):
        il = getattr(self, "_il", None)
        if il is None:
            return
        me = threading.current_thread()
        if me not in il["threads"]:
            return
        with il["cv"]:
            i = il["threads"].index(me)
            self._pass_turn(il, i)
            while il["turn"] is not me:
                il["cv"].wait()

    def _pass_turn(self, il, i):
        n = len(il["threads"])
        for d in range(1, n + 1):
            t = il["threads"][(i + d) % n]
            if t in il["live"]:
                il["turn"] = t
                break
        il["cv"].notify_all()

    def interleave(self, fns):
        if len(fns) == 1:
            fns[0](); return
        il = {"cv": threading.Condition(), "threads": [], "live": set(), "turn": None, "err": []}
        def runner(fn):
            me = threading.current_thread()
            with il["cv"]:
                while il["turn"] is not me:
                    il["cv"].wait()
            try:
                fn()
            except BaseException as e:
                il["err"].append(e)
            with il["cv"]:
                il["live"].discard(me)
                if il["live"]:
                    self._pass_turn(il, il["threads"].index(me))
                else:
                    il["turn"] = None; il["cv"].notify_all()
        ths = [threading.Thread(target=runner, args=(fn,)) for fn in fns]
        il["threads"] = ths; il["live"] = set(ths)
        self._il = il
        for t in ths:
            t.start()
        with il["cv"]:
            il["turn"] = ths[0]; il["cv"].notify_all()
        for t in ths:
            t.join()
        self._il = None
        if il["err"]:
            raise il["err"][0]

    def finish(self, out_tiles):
        self.barrier()

    def close(self):
        if self.st is not None:
            self.st.close()
        self.es.close()


CUM = [0, 512, 640, 768, 1024, 1280, 1536, 1792, 1800, 1808, 2064, 2320, 2576, 2832, 2840, 2848]
import os
T_ = int(os.environ.get('FZ_T', '8448')); NT_ = T_ // 128; NC_ = T_ // 64; NCTX = 256

OUT6 = dict(Qs=(0, 128), Ks=(128, 256), eb=(256, 258), sigo=(258, 322), qn=(322, 386), kn=(386, 450), kb=(450, 578),
            Rm=(578, 834), Qg=(834, 962), Kd=(962, 1090), G=(1090, 1092), eG=(1092, 1094), siluz=(1094, 1158), gv=(1158, 1222),
            ebl=(1222, 1224), eGl=(1224, 1226))
W6 = 1226
S8 = dict(QsT=(0, 64), WT=(64, 128), QgT=(128, 192), Ks=(192, 256), V1=(256, 321), AT=(321, 385), U=(385, 449), Kd=(449, 513),
          AqkT=(513, 577), sm=(577, 578), sg=(578, 579))
W8 = 579


def bcast_rows(k, PS, dst, src2, r, sel, n=1024):
    for nb in range(0, n, 512):
        ps = PS[(nb // 512) % 2]
        k.op("pe", lambda e: e.matmul(ps[:, 0:512], sel[:, r, :], src2[:, nb:nb+512], start=True, stop=True), [sel, src2], [ps])
        k.op("dve", lambda e: e.tensor_copy(dst[:, nb:nb+512], ps[:, 0:512]), [ps], [dst])


def e_mod(k, PS, ctsb, MODW, MODB, modrow):
    k.stage_begin()
    ws = [k.sb([128, 8, 512], name="mw") for _ in range(2)]
    bi = k.sb([2, 6144], name="mb")
    k.dma(bi, bi[:], None, MODB[:, :])
    for nb in range(12):
        w = ws[nb % 2]; ps = PS[nb % 2]
        k.dma(w, w[:], None, MODW.rearrange("(c p) n -> p c n", p=128)[:, :, nb*512:(nb+1)*512], q="sp" if nb % 2 == 0 else "pool")
        for c in range(8):
            k.op("pe", lambda e: e.matmul(ps[0:2, 0:512], ctsb[:, c, :], w[:, c, :], start=(c == 0), stop=(c == 7)), [ctsb, w], [ps])
        k.op("dve", lambda e: e.tensor_add(out=modrow[:, nb*512:(nb+1)*512], in0=ps[0:2, 0:512], in1=bi[:, nb*512:(nb+1)*512]), [ps, bi], [modrow])
    k.stage_end()


def e_normlin(k, PS, X, GREP, modrow, isc, ish, sel, ident, W, N, Y, H=None, softmax=False, YT=None, QKT=None, qk=None):
    k.stage_begin()
    g = k.sb([128, 1024], name="g"); k.dma(g, g[:], None, GREP[:, :])
    A = [k.sb([128, 1024], name="A") for _ in range(2)]; sh = [k.sb([128, 1024], name="sh") for _ in range(2)]
    for cls in range(2):
        r = 1 if cls == 0 else 0
        bcast_rows(k, PS, A[cls], V(modrow, isc*1024, (isc+1)*1024), r, sel)
        bcast_rows(k, PS, sh[cls], V(modrow, ish*1024, (ish+1)*1024), r, sel)
        k.op("dve", lambda e: e.scalar_tensor_tensor(out=A[cls][:], in0=A[cls][:], scalar=1.0, in1=g[:], op0=ALU.add, op1=ALU.mult), [A[cls], g], [A[cls]])
    lowp = N > 16
    w = k.sb([128, 8, N], BF16 if lowp else F32, name="w")
    for c in range(8):
        k.dma(w, w[:, c, :], None, W[c*128:(c+1)*128, :], q="pool" if lowp else ("sp" if c % 2 == 0 else "pool"))
    xs = [k.sb([128, 1024], name="x") for _ in range(2)]; hs = [k.sb([128, 1024], name="h") for _ in range(2)]
    hT = [k.sb([128, 8, 128], BF16 if lowp else F32, name="hT") for _ in range(2)]
    ys = [k.sb([128, N], name="y") for _ in range(2)]
    st = [k.sb([128, 40], name="st") for _ in range(2)]
    if qk is not None:
        gn = k.sb([128, 640], name="gn"); k.dma(gn, gn[:], None, qk["GAIN"][:, :])
        QB = [dict(cs=k.sb([128, 320], name="cs"), sn=k.sb([128, 320], name="sn"), sq=k.sb([128, 640], name="sq"), o=k.sb([128, 640], name="o"),
                   t1=k.sb([128, 320], name="t1"), t2=k.sb([128, 320], name="t2"), oT=k.sb([128, 5, 128], BF16, name="oT")) for _ in range(2)]
        v5 = lambda ap: ap.rearrange("p (h a f j) -> p h a f j", h=10, a=2, f=2)
        v4 = lambda ap: ap.rearrange("p (h a j) -> p h a j", h=10, a=2)
    if YT is not None:
        yts = [k.sb([16, 128], name="yt") for _ in range(2)]
    nb_ = (N + 511) // 512
    pi = 0
    def tile_body(rt):
        nonlocal pi
        cls = 0 if rt < 2 else 1
        x = xs[rt % 2]; h = hs[rt % 2]; s = st[rt % 2]; ht = hT[rt % 2]; y = ys[rt % 2]
        rows = slice(rt*128, (rt+1)*128)
        k.dma(x, x[:], X, X[rows, :])
        k.op("dve", lambda e: e.memset(s[:, 0:8], 0.0), [], [s])
        k.op("act", lambda e: e.activation(out=h[:], in_=x[:], func=AF.Square, accum_out=s[:, 0:1]), [x, s], [h, s])
        k.op("dve", lambda e: e.tensor_scalar(out=s[:, 1:2], in0=s[:, 0:1], scalar1=1.0/1024, scalar2=EPS, op0=ALU.mult, op1=ALU.add), [s], [s])
        k.op("act", lambda e: e.activation(out=s[:, 7:8], in_=s[:, 1:2], func=AF.Sqrt), [s], [s])
        k.op("dve", lambda e: e.reciprocal(out=s[:, 2:3], in_=s[:, 7:8]), [s], [s])
        k.op("dve", lambda e: e.scalar_tensor_tensor(out=h[:], in0=x[:], scalar=s[:, 2:3], in1=A[cls][:], op0=ALU.mult, op1=ALU.mult), [x, s, A[cls]], [h])
        k.op("pool", lambda e: e.tensor_add(out=h[:], in0=h[:], in1=sh[cls][:]), [h, sh[cls]], [h])
        if H is not None:
            k.dma(H, H[rows, :], h, h[:], q="pool", skip_w=True)
        for half in range(2):
            pt = PS[rt % 2]
            for c in range(4):
                cc = half*4 + c
                k.op("pe", lambda e: e.transpose(pt[:, c*128:(c+1)*128], h[:, cc*128:(cc+1)*128], ident[:]), [h, ident], [pt])
            if half == 0:
                k.op("act", lambda e: e.copy(ht[:, 0:4, :], pt[:].rearrange("p (c t) -> p c t", c=4)), [pt], [ht])
            else:
                k.op("dve", lambda e: e.tensor_copy(ht[:, 4:8, :], pt[:].rearrange("p (c t) -> p c t", c=4)), [pt], [ht])
        for b in range(nb_):
            n0 = b*512; n1 = min(N, n0+512); ps = PS[2 + (rt % 2) + 2*(b % 2)]
            for c in range(8):
                k.op("pe", lambda e: e.matmul(ps[:, 0:n1-n0], ht[:, c, :], w[:, c, n0:n1], start=(c == 0), stop=(c == 7)), [ht, w], [ps])
            if b % 2 == 0:
                k.op("dve", lambda e: e.tensor_copy(y[:, n0:n1], ps[:, 0:n1-n0]), [ps], [y])
            else:
                k.op("act", lambda e: e.copy(y[:, n0:n1], ps[:, 0:n1-n0]), [ps], [y])
        if softmax:
            k.op("dve", lambda e: e.reduce_max(out=s[:, 3:4], in_=y[:], axis=AX.X), [y], [s])
            k.op("dve", lambda e: e.tensor_scalar(out=s[:, 4:5], in0=s[:, 3:4], scalar1=-1.0, scalar2=None, op0=ALU.mult), [s], [s])
            k.op("dve", lambda e: e.memset(s[:, 5:6], 0.0), [], [s])
            k.op("act", lambda e: e.activation(out=y[:], in_=y[:], func=AF.Exp, bias=s[:, 4:5], scale=1.0, accum_out=s[:, 5:6]), [y, s], [y, s])
            k.op("dve", lambda e: e.reciprocal(out=s[:, 6:7], in_=s[:, 5:6]), [s], [s])
            k.op("dve", lambda e: e.tensor_scalar(out=y[:], in0=y[:], scalar1=s[:, 6:7], scalar2=None, op0=ALU.mult), [y, s], [y])
        if YT is not None:
            yt = yts[rt % 2]; pt = PS[6 + rt % 2]
            k.op("pe", lambda e: e.transpose(pt[0:16, 0:128], y[:, 0:16], ident[:]), [y, ident], [pt])
            k.op("dve", lambda e: e.tensor_copy(yt[:], pt[0:16, 0:128]), [pt], [yt])
            k.dma(YT, YT[:, rows], yt, yt[:], q="pool", skip_w=True)
        if Y is not None:
            k.dma(Y, Y[rows, :], y, y[:], skip_w=True)
        if qk is not None:
            b = QB[rt % 2]; o = b['o']
            k.dma(b['cs'], b['cs'][:], None, qk["CS"][rows, :], q="pool"); k.dma(b['sn'], b['sn'][:], None, qk["SN"][rows, :], q="pool")
            xq = V(y, 0, 640)
            k.op("dve", lambda e: e.tensor_mul(out=b['sq'][:], in0=xq[:], in1=xq[:]), [y], [b['sq']])
            k.op("dve", lambda e: e.reduce_sum(out=s[:, 8:18], in_=b['sq'][:].rearrange("p (h d) -> p h d", h=10), axis=AX.X), [b['sq']], [s])
            k.op("dve", lambda e: e.tensor_scalar(out=s[:, 8:18], in0=s[:, 8:18], scalar1=1.0/64, scalar2=EPS, op0=ALU.mult, op1=ALU.add), [s], [s])
            k.op("act", lambda e: e.activation(out=s[:, 18:28], in_=s[:, 8:18], func=AF.Sqrt), [s], [s])
            k.op("dve", lambda e: e.reciprocal(out=s[:, 28:38], in_=s[:, 18:28]), [s], [s])
            for hh in range(10):
                k.op("dve" if hh % 2 == 0 else "pool", lambda e: e.tensor_scalar(out=b['sq'][:, hh*64:(hh+1)*64], in0=xq[:, hh*64:(hh+1)*64], scalar1=s[:, 28+hh:29+hh], scalar2=None, op0=ALU.mult), [y, s], [b['sq']])
            xn = b['sq']
            k.op("dve", lambda e: e.tensor_mul(out=xn[:], in0=xn[:], in1=gn[:]), [xn, gn], [xn])
            x1 = v5(xn[:])[:, :, :, 0, :]; x2 = v5(xn[:])[:, :, :, 1, :]
            o1 = v5(o[:])[:, :, :, 0, :]; o2 = v5(o[:])[:, :, :, 1, :]
            cs = v4(b['cs'][:]); sn = v4(b['sn'][:]); t1 = v4(b['t1'][:]); t2 = v4(b['t2'][:])
            k.op("dve", lambda e: e.tensor_mul(out=t1, in0=x1, in1=cs), [xn, b['cs']], [b['t1']])
            k.op("pool", lambda e: e.tensor_mul(out=t2, in0=x2, in1=sn), [xn, b['sn']], [b['t2']])
            k.op("dve", lambda e: e.tensor_sub(out=o1, in0=t1, in1=t2), [b['t1'], b['t2']], [o])
            k.op("dve", lambda e: e.tensor_mul(out=t1, in0=x2, in1=cs), [xn, b['cs']], [b['t1']])
            k.op("pool", lambda e: e.tensor_mul(out=t2, in0=x1, in1=sn), [xn, b['sn']], [b['t2']])
            k.op("dve", lambda e: e.tensor_add(out=o2, in0=t1, in1=t2), [b['t1'], b['t2']], [o])
            pt = PS[6 + rt % 2]
            for c in range(4):
                k.op("pe", lambda e: e.transpose(pt[:, c*128:(c+1)*128], o[:, c*128:(c+1)*128], ident[:]), [o, ident], [pt])
            k.op("act", lambda e: e.copy(b['oT'][:, 0:4, :], pt[:].rearrange("p (c t) -> p c t", c=4)), [pt], [b['oT']])
            pt2 = PS[6 + rt % 2]
            k.op("pe", lambda e: e.transpose(pt2[:, 0:128], o[:, 512:640], ident[:]), [o, ident], [pt2])
            k.op("dve", lambda e: e.tensor_copy(b['oT'][:, 4, :], pt2[:, 0:128]), [pt2], [b['oT']])
            k.dma(QKT, QKT.ap.rearrange("(c p) t -> p c t", p=128)[:, :, rows], b['oT'], b['oT'][:], q="pool", skip_w=True)
    for t0 in range(0, T_ // 128, 2):
        k.interleave([(lambda t=t: tile_body(t)) for t in range(t0, min(T_ // 128, t0 + 2))])
    k.stage_end()


def e_attn(k, PS, QKT, P, AXD):
    k.stage_begin()
    T = T_; NT = NT_
    kts = [k.sb([64, T], BF16, name="kt") for _ in range(2)]
    v1s = [k.sb([128, NT, 65], BF16, name="v1") for _ in range(2)]
    vtmp = k.sb([128, NT, 64], name="vtmp")
    for g in range(2):
        k.dma(kts[g], kts[g][:], QKT, QKT[512 + g*64:512 + (g+1)*64, :])
        k.op("dve", lambda e: e.memset(v1s[g][:, :, 64:65], 1.0), [], [v1s[g]])
        k.dma(vtmp, vtmp[:], P, P.ap.rearrange("(n p) c -> p n c", p=128)[:, :, 640 + g*64:640 + (g+1)*64], q="pool")
        k.op("dve", lambda e: e.tensor_copy(v1s[g][:, :, 0:64], vtmp[:]), [vtmp], [v1s[g]])
    qts = [k.sb([64, 512], BF16, name="q") for _ in range(2)]
    pts = [k.sb([128, 512], BF16, name="pt") for _ in range(3)]
    osb = [k.sb([128, 4, 64], name="osb") for _ in range(2)]
    rs = [k.sb([128, 8], name="rs") for _ in range(2)]
    pss = PS[0:2]; pso = PS[2:6]
    blocks = [(0, NCTX, 0, NCTX // 128)] + [(q0, 512, 0, NT) for q0 in range(NCTX, T, 512)]
    it = 0; bi = 0
    pending = None
    for h in range(8):
        g = h // 4; kt = kts[g]; v1 = v1s[g]
        for (q0, qn, k0, k1) in blocks:
            qt = qts[bi % 2]; ob = osb[bi % 2]; r = rs[bi % 2]; bi += 1
            nqs = qn // 128
            k.dma(qt, qt[:, 0:qn], QKT, QKT[h*64:(h+1)*64, q0:q0+qn])
            for kk in range(k0, k1):
                ps = pss[it % 2]; pt = pts[it % 3]; it += 1
                k.op("pe", lambda e: e.matmul(ps[:, 0:qn], kt[:, kk*128:(kk+1)*128], qt[:, 0:qn], start=True, stop=True), [kt, qt], [ps])
                k.op("act", lambda e: e.activation(out=pt[:, 0:qn], in_=ps[:, 0:qn], func=AF.Exp, scale=0.125), [ps], [pt])
                if pending is not None:
                    pending()

                def mk(pt=pt, v1=v1, kk=kk, k0=k0, k1=k1, nqs=nqs, r=r, ob=ob, q0=q0, qn=qn, h=h):
                    def run():
                        for qs in range(nqs):
                            k.op("pe", lambda e: e.matmul(pso[qs][:, 0:65], pt[:, qs*128:(qs+1)*128], v1[:, kk, :], start=(kk == k0), stop=(kk == k1-1)), [pt, v1], [pso[qs]])
                        if kk == k1 - 1:
                            for qs in range(nqs):
                                k.op("dve", lambda e: e.reciprocal(out=r[:, qs:qs+1], in_=pso[qs][:, 64:65]), [pso[qs]], [r])
                                k.op("dve", lambda e: e.tensor_scalar(out=ob[:, qs, :], in0=pso[qs][:, 0:64], scalar1=r[:, qs:qs+1], scalar2=None, op0=ALU.mult), [pso[qs], r], [ob])
                            k.dma(AXD, AXD[q0:q0+qn, h*64:(h+1)*64].rearrange("(s p) d -> p s d", p=128), ob, ob[:, 0:nqs, :], q="pool", skip_w=True)
                    return run
                pending = mk()
    if pending is not None:
        pending()
    k.stage_end()


O6 = dict(Qs=(0, 128), sigo=(128, 192), qn=(192, 256), kn=(256, 320), kb=(320, 448), Rm=(448, 704), Qg=(704, 832), G=(832, 834),
          siluz=(834, 898), KG0=(898, 1028), KG1=(1028, 1158))
W6 = 1158
S8 = dict(QsT=(0, 64), WT=(64, 128), QgT=(128, 192), Ks=(192, 256), Kd=(256, 320), sm=(320, 321), sg=(321, 322), V1=(322, 387),
          AT=(387, 451), U=(451, 515), AqkT=(515, 579))
W8 = 579
FM_SRC = [(256, 320), (192, 256), (320, 384), (384, 448), (0, 64), (64, 128), None, None, (704, 768), (768, 832)]


def e_prep(k, PS, P, CB, CW, cc, ident, O6D, FMD):
    k.stage_begin()
    tf = cc[:, 2, :]; tr = cc[:, 3, :]; blk = cc[:, 8, :]
    cb = k.sb([128, 48], name="cb"); cw = k.sb([128, 5, 768], name="cw")
    k.dma(cb, cb[:, 0:32], None, CB[:, :]); k.dma(cw, cw[:], None, CW[:, :, :])
    k.op("act", lambda e: e.activation(out=cb[:, 32:40], in_=cb[:, 16:24], func=AF.Exp), [cb], [cb])
    k.op("dve", lambda e: e.tensor_scalar(out=cb[:, 32:40], in0=cb[:, 32:40], scalar1=-1.0, scalar2=None, op0=ALU.mult), [cb], [cb])
    NB = 2
    m4s = [k.sb([128, 1024], name="m4") for _ in range(NB)]; g4s = [k.sb([128, 1024], name="g4") for _ in range(NB)]
    mgs = [k.sb([128, 16], name="mg") for _ in range(NB)]; ggs = [k.sb([128, 16], name="gg") for _ in range(NB)]
    q5s = [k.sb([128, 5, 768], name="q5") for _ in range(NB)]
    cvs = [k.sb([128, 768], name="cv") for _ in range(NB)]; sqs = [k.sb([128, 512], name="sq") for _ in range(NB)]
    wks = [k.sb([128, 128], name="wk") for _ in range(NB)]
    outs = [[k.sb([128, W6], name="o6") for _ in range(4)] for _ in range(NB)]
    fmts = [[k.sb([64, 10, 128], name="fmt") for _ in range(4)] for _ in range(NB)]
    ksb = [[k.sb([128, 128], name="ksb") for _ in range(4)] for _ in range(NB)]
    Pt = P.rearrange("(n p) c -> n p c", p=128)
    segs = [(0, NCTX), (NCTX, T_)]
    def tile_body(t):
        i = t % NB
        psg = PS[i]
        m4 = m4s[i]; g4 = g4s[i]; mg = mgs[i]; gg = ggs[i]; q5 = q5s[i]; c = cvs[i]; s2 = sqs[i]; s = wks[i]
        r0 = t*128
        k.dma(m4, m4[:], P, P[r0:r0+128, 768:1792]); k.dma(g4, g4[:], P, P[r0:r0+128, 1808:2832], q="pool")
        k.dma(mg, mg[:], P, P[r0:r0+128, 1792:1808]); k.dma(gg, gg[:], P, P[r0:r0+128, 2832:2848], q="pool")
        seg = segs[0] if r0 < NCTX else segs[1]
        edge = (r0 - 2 < seg[0]) or (r0 + 130 > seg[1])
        if edge:
            k.op("pool", lambda e: e.memset(q5[:], 0.0), [], [q5])
        for kk in range(5):
            a0 = r0 + kk - 2; a1 = a0 + 128
            lo = max(a0, seg[0]); hi = min(a1, seg[1])
            k.dma(q5, q5[lo-a0:hi-a0, kk, :], P, P[lo:hi, 1808:2576], q="sp" if kk % 2 == 0 else "pool")
        k.op("dve", lambda e: e.tensor_mul(out=q5[:], in0=q5[:], in1=cw[:]), [q5, cw], [q5])
        k.op("dve", lambda e: e.reduce_sum(out=c[:], in_=q5[:].rearrange("p k c -> p c k"), axis=AX.X), [q5], [c])
        k.op("act", lambda e: e.activation(out=c[:], in_=c[:], func=AF.Silu), [c], [c])
        k.op("dve", lambda e: e.tensor_mul(out=s2[:], in0=c[:, 0:512], in1=c[:, 0:512]), [c], [s2])
        k.op("dve", lambda e: e.reduce_sum(out=s[:, 64:72], in_=s2[:].rearrange("p (h d) -> p h d", h=8), axis=AX.X), [s2], [s])
        k.op("dve", lambda e: e.tensor_scalar(out=s[:, 64:72], in0=s[:, 64:72], scalar1=EPS, scalar2=None, op0=ALU.add), [s], [s])
        k.op("act", lambda e: e.activation(out=s[:, 72:80], in_=s[:, 64:72], func=AF.Sqrt), [s], [s])
        k.op("dve", lambda e: e.reciprocal(out=s[:, 80:88], in_=s[:, 72:80]), [s], [s])
        k.op("dve", lambda e: e.tensor_add(out=s[:, 0:8], in0=mg[:, 8:16], in1=cb[:, 8:16]), [mg, cb], [s])
        k.op("act", lambda e: e.activation(out=s[:, 8:16], in_=s[:, 0:8], func=AF.Exp, scale=-1.0), [s], [s])
        k.op("act", lambda e: e.activation(out=s[:, 8:16], in_=s[:, 8:16], func=AF.Ln, bias=1.0, scale=1.0), [s], [s])
        k.op("dve", lambda e: e.tensor_scalar(out=s[:, 8:16], in0=s[:, 8:16], scalar1=-1.0, scalar2=None, op0=ALU.mult), [s], [s])
        k.op("pe", lambda e: e.matmul(psg[:, 0:4], tf, s[:, 8:12], start=True, stop=True), [cc, s], [psg])
        k.op("pe", lambda e: e.matmul(psg[:, 4:8], tr, s[:, 12:16], start=True, stop=True), [cc, s], [psg])
        k.op("pe", lambda e: e.matmul(psg[:, 8:16], blk, s[:, 8:16], start=True, stop=True), [cc, s], [psg])
        k.op("dve", lambda e: e.tensor_copy(s[:, 16:24], psg[:, 0:8]), [psg], [s])
        k.op("act", lambda e: e.activation(out=s[:, 48:56], in_=psg[:, 8:16], func=AF.Exp), [psg], [s])
        k.op("act", lambda e: e.activation(out=s[:, 40:48], in_=s[:, 16:24], func=AF.Exp), [s], [s])
        k.op("dve", lambda e: e.tensor_add(out=s[:, 24:32], in0=mg[:, 0:8], in1=cb[:, 0:8]), [mg, cb], [s])
        k.op("dve", lambda e: e.tensor_sub(out=s[:, 24:32], in0=s[:, 24:32], in1=s[:, 16:24]), [s], [s])
        k.op("act", lambda e: e.activation(out=s[:, 32:40], in_=s[:, 24:32], func=AF.Exp), [s], [s])
        k.op("act", lambda e: e.activation(out=s[:, 88:96], in_=gg[:, 8:16], func=AF.Sigmoid), [gg], [s])
        k.op("dve", lambda e: e.tensor_add(out=s[:, 96:104], in0=gg[:, 0:8], in1=cb[:, 24:32]), [gg, cb], [s])
        k.op("act", lambda e: e.activation(out=s[:, 96:104], in_=s[:, 96:104], func=AF.Exp), [s], [s])
        k.op("act", lambda e: e.activation(out=s[:, 96:104], in_=s[:, 96:104], func=AF.Ln, bias=1.0, scale=1.0), [s], [s])
        k.op("dve", lambda e: e.tensor_mul(out=s[:, 104:112], in0=s[:, 96:104], in1=cb[:, 32:40]), [s, cb], [s])
        k.op("pe", lambda e: e.matmul(psg[:, 16:24], tf, s[:, 104:112], start=True, stop=True), [cc, s], [psg])
        k.op("pe", lambda e: e.matmul(psg[:, 24:32], tr, s[:, 104:112], start=True, stop=True), [cc, s], [psg])
        k.op("pe", lambda e: e.matmul(psg[:, 32:40], blk, s[:, 104:112], start=True, stop=True), [cc, s], [psg])
        k.op("dve", lambda e: e.tensor_copy(s[:, 112:116], psg[:, 16:20]), [psg], [s])
        k.op("dve", lambda e: e.tensor_copy(s[:, 116:120], psg[:, 28:32]), [psg], [s])
        k.op("dve", lambda e: e.tensor_sub(out=s[:, 120:124], in0=psg[:, 24:28], in1=s[:, 104:108]), [psg, s], [s])
        k.op("dve", lambda e: e.tensor_sub(out=s[:, 124:128], in0=psg[:, 20:24], in1=s[:, 108:112]), [psg, s], [s])
        k.op("act", lambda e: e.activation(out=s[:, 120:128], in_=s[:, 120:128], func=AF.Exp), [s], [s])
        k.op("act", lambda e: e.activation(out=s[:, 56:64], in_=s[:, 112:120], func=AF.Exp), [s], [s])
        k.op("act", lambda e: e.activation(out=s2[:, 0:8], in_=psg[:, 32:40], func=AF.Exp), [psg], [s2])
        for h in range(4):
            o = outs[i][h]; fmt = fmts[i][h]; kst = ksb[i][h]
            hs = slice(h*64, (h+1)*64)
            O = lambda n: o[:, O6[n][0]:O6[n][1]]
            for d in range(2):
                j = d*4 + h
                KG0 = O6["KG0"][0] + d*130
                k.op("dve", lambda e: e.tensor_scalar(out=o[:, d*64:(d+1)*64], in0=m4[:, hs], scalar1=s[:, 40+j:41+j], scalar2=None, op0=ALU.mult), [m4, s], [o])
                k.op("pool", lambda e: e.tensor_scalar(out=kst[:, d*64:(d+1)*64], in0=m4[:, 256+h*64:256+(h+1)*64], scalar1=s[:, 32+j:33+j], scalar2=0.125, op0=ALU.mult, op1=ALU.mult), [m4, s], [kst])
                k.op("pool", lambda e: e.tensor_copy(o[:, KG0:KG0+64], kst[:, d*64:(d+1)*64]), [kst], [o])
                k.op("dve", lambda e: e.tensor_copy(o[:, KG0+128:KG0+129], s[:, 48+j:49+j]), [s], [o])
                k.op("dve", lambda e: e.tensor_copy(o[:, KG0+129:KG0+130], s2[:, j:j+1]), [s2], [o])
            k.op("act", lambda e: e.activation(out=O("sigo"), in_=m4[:, 768+h*64:768+(h+1)*64], func=AF.Sigmoid), [m4], [o])
            k.op("dve", lambda e: e.tensor_scalar(out=O("qn"), in0=c[:, hs], scalar1=s[:, 80+h:81+h], scalar2=0.125, op0=ALU.mult, op1=ALU.mult), [c, s], [o])
            k.op("dve", lambda e: e.tensor_scalar(out=O("kn"), in0=c[:, 256+h*64:256+(h+1)*64], scalar1=s[:, 84+h:85+h], scalar2=None, op0=ALU.mult), [c, s], [o])
            gv = c[:, 512+h*64:512+(h+1)*64]
            for d in range(2):
                j = d*4 + h
                KG0 = O6["KG0"][0] + d*130
                kb = o[:, 320+d*64:320+(d+1)*64]
                k.op("dve", lambda e: e.tensor_scalar(out=kb, in0=O("kn"), scalar1=s[:, 88+j:89+j], scalar2=None, op0=ALU.mult), [o, s], [o])
                k.op("pool", lambda e: e.tensor_scalar(out=o[:, 448+d*128:448+d*128+64], in0=gv, scalar1=s[:, 88+j:89+j], scalar2=None, op0=ALU.mult), [c, s], [o])
                k.op("dve", lambda e: e.tensor_scalar(out=o[:, 448+d*128+64:448+(d+1)*128], in0=kb, scalar1=s[:, 56+j:57+j], scalar2=None, op0=ALU.mult), [o, s], [o])
                k.op("pool", lambda e: e.tensor_scalar(out=o[:, 704+d*64:704+(d+1)*64], in0=O("qn"), scalar1=s[:, 56+j:57+j], scalar2=None, op0=ALU.mult), [o, s], [o])
                k.op("dve", lambda e: e.tensor_scalar(out=o[:, KG0+64:KG0+128], in0=O("kn"), scalar1=s[:, 120+j:121+j], scalar2=None, op0=ALU.mult), [o, s], [o])
                k.op("dve", lambda e: e.tensor_copy(o[:, 832+d:833+d], s[:, 112+j:113+j]), [s], [o])
            k.op("act", lambda e: e.activation(out=O("siluz"), in_=g4[:, 768+h*64:768+(h+1)*64], func=AF.Silu), [g4], [o])
            k.dma(O6D, O6D[h, r0:r0+128, :], o, o[:], skip_w=True)
            pA = PS[2 + i*3]; pB = PS[3 + i*3]; pC = PS[4 + i*3]
            for bi, src in enumerate(FM_SRC):
                if src is None:
                    src_ap = kst[:, (bi-6)*64:(bi-5)*64]; src_t = kst
                else:
                    src_ap = o[:, src[0]:src[1]]; src_t = o
                pp = (pA, pB, pC)[bi // 4]; off = (bi % 4)*128
                k.op("pe", lambda e: e.transpose(pp[0:64, off:off+128], src_ap, ident[:]), [src_t, ident], [pp])
            k.op("act", lambda e: e.copy(fmt[:, 0:4, :], pA[0:64, :].rearrange("p (c t) -> p c t", c=4)), [pA], [fmt])
            k.op("dve", lambda e: e.tensor_copy(fmt[:, 4:8, :], pB[0:64, :].rearrange("p (c t) -> p c t", c=4)), [pB], [fmt])
            k.op("act", lambda e: e.copy(fmt[:, 8:10, :], pC[0:64, 0:256].rearrange("p (c t) -> p c t", c=2)), [pC], [fmt])
            k.dma(FMD, FMD[h, t], fmt, fmt[:], q="pool", skip_w=True)
    for t0 in range(0, NT_, NB):
        k.interleave([(lambda t=t: tile_body(t)) for t in range(t0, min(NT_, t0 + NB))])
    k.stage_end()


def e_intra(k, PS, O6D, FMD, cc, O7D, O7T):
    k.stage_begin()
    ident, ones, trif, trir, trifs, trirs, ntrifs, ntrirs = [cc[:, i, :] for i in range(8)]
    class C: pass
    ch = []
    for c in range(4):
        o = C()
        o.P = [k.sb([128, 128], name="P") for _ in range(2)]; o.PT = [k.sb([128, 128], name="PT") for _ in range(2)]
        o.Y = [k.sb([128, 128], name="Y") for _ in range(2)]
        o.M1 = k.sb([128, 128], name="M1"); o.M2 = k.sb([128, 128], name="M2"); o.E2 = k.sb([128, 128], name="E2"); o.dg = k.sb([128, 128], name="dg")
        o.out = k.sb([128, 3, 128], name="out"); o.wt = k.sb([64, 128], name="wt")
        o.ps = [PS[c*2], PS[c*2+1]]
        ch.append(o)
    fms = [[k.sb([64, 8, 128], name="fm") for _ in range(2)] for _ in range(2)]
    tms = [[k.sb([128, 258], name="tm") for _ in range(2)] for _ in range(2)]
    for h in range(4):
      for tp in range(NT_ // 2):
        chains = []
        for j in range(2):
            t = tp*2 + j
            fm = fms[j][tp % 2]; tm = tms[j][tp % 2]
            k.dma(fm, fm[:], FMD, FMD[h, t, :, 0:8, :])
            k.dma(tm, tm[:, 0:256], O6D, O6D[h, t*128:(t+1)*128, 448:704], q="pool")
            k.dma(tm, tm[:, 256:258], O6D, O6D[h, t*128:(t+1)*128, 832:834], q="pool")
            for d in range(2):
                chains.append((ch[j*2+d], t, d, fm, tm))
        for (o, t, d, fm, tm) in chains:
            m1, m2, m3 = (ntrirs, ntrifs, trif) if d == 0 else (ntrifs, ntrirs, trir)
            gcol = tm[:, 256+d:257+d]
            k.op("dve", lambda e: e.tensor_scalar(out=o.dg[:], in0=ident, scalar1=gcol, scalar2=None, op0=ALU.mult), [cc, tm], [o.dg])
            k.op("pe", lambda e: e.matmul(o.ps[0][:, 0:128], ones, o.dg[:], start=True, stop=True), [cc, o.dg], [o.ps[0]])
            k.op("dve", lambda e: e.tensor_scalar(out=o.E2[:], in0=o.ps[0][:, 0:128], scalar1=gcol, scalar2=0.0, op0=ALU.subtract, op1=ALU.min), [o.ps[0], tm], [o.E2])
            k.op("dve", lambda e: e.tensor_scalar(out=o.M1[:], in0=o.ps[0][:, 0:128], scalar1=gcol, scalar2=0.0, op0=ALU.subtract, op1=ALU.max), [o.ps[0], tm], [o.M1])
            k.op("act", lambda e: e.activation(out=o.E2[:], in_=o.E2[:], func=AF.Exp), [o.E2], [o.E2])
            k.op("act", lambda e: e.activation(out=o.M1[:], in_=o.M1[:], func=AF.Exp, scale=-1.0), [o.M1], [o.M1])
            k.op("pool", lambda e: e.tensor_mul(out=o.M1[:], in0=o.M1[:], in1=m1), [o.M1, cc], [o.M1])
            k.op("pool", lambda e: e.tensor_mul(out=o.M2[:], in0=o.E2[:], in1=m2), [o.E2, cc], [o.M2])
            k.op("pool", lambda e: e.tensor_mul(out=o.E2[:], in0=o.E2[:], in1=m3), [o.E2, cc], [o.E2])
        for (o, t, d, fm, tm) in chains:
            mA = trif if d == 0 else trir
            knT = fm[:, 0, :]; qnT = fm[:, 1, :]; kbT = fm[:, 2+d, :]; qsT = fm[:, 4+d, :]; ksT = fm[:, 6+d, :]
            p0 = V(o.ps[0], 0, 128); p1 = V(o.ps[1], 0, 128)
            k.op("pe", lambda e: e.matmul(p0[:], kbT, knT, start=True, stop=True), [fm], [p0])
            k.op("pe", lambda e: e.matmul(p1[:], knT, kbT, start=True, stop=True), [fm], [p1])
            k.op("dve", lambda e: e.tensor_mul(out=o.P[0][:], in0=p0[:], in1=o.M1[:]), [p0, o.M1], [o.P[0]])
            k.op("dve", lambda e: e.tensor_mul(out=o.PT[0][:], in0=p1[:], in1=o.M2[:]), [p1, o.M2], [o.PT[0]])
            k.op("pe", lambda e: e.matmul(p0[:], knT, qnT, start=True, stop=True), [fm], [p0])
            k.op("pe", lambda e: e.matmul(p1[:], ksT, qsT, start=True, stop=True), [fm], [p1])
            k.op("dve", lambda e: e.tensor_mul(out=o.out[:, 1, :], in0=p0[:], in1=o.E2[:]), [p0, o.E2], [o.out])
            k.op("dve", lambda e: e.tensor_mul(out=o.out[:, 2, :], in0=p1[:], in1=mA), [p1, cc], [o.out])
            k.op("act", lambda e: e.copy(o.Y[0][:], tm[:, d*128:(d+1)*128]), [tm], [o.Y[0]])
        for m in range(6):
            a = m % 2; b = 1 - a
            for (o, t, d, fm, tm) in chains:
                p0 = V(o.ps[0], 0, 128); p1 = V(o.ps[1], 0, 128)
                k.op("pe", lambda e: e.matmul(p0[:], o.PT[a][:], o.Y[a][:], start=True, stop=True), [o.PT[a], o.Y[a]], [p0])
                dst = o.Y[b][:] if m < 5 else o.out[:, 0, :]
                dstT = o.Y[b] if m < 5 else o.out
                k.op("dve", lambda e: e.tensor_add(out=dst, in0=p0[:], in1=o.Y[a][:]), [p0, o.Y[a]], [dstT])
                if m < 5:
                    k.op("pe", lambda e: e.matmul(p1[:], o.P[a][:], o.PT[a][:], start=True, stop=True), [o.P[a], o.PT[a]], [p1])
                    k.op("act", lambda e: e.copy(o.PT[b][:], p1[:]), [p1], [o.PT[b]])
            if m < 4:
                for (o, t, d, fm, tm) in chains:
                    p0 = V(o.ps[0], 0, 128)
                    k.op("pe", lambda e: e.matmul(p0[:], o.PT[a][:], o.P[a][:], start=True, stop=True), [o.PT[a], o.P[a]], [p0])
                    k.op("act", lambda e: e.copy(o.P[b][:], p0[:]), [p0], [o.P[b]])
        for (o, t, d, fm, tm) in chains:
            k.dma(O7D, O7D[h, t*128:(t+1)*128, d, :, :], o.out, o.out[:], q="sp" if d == 0 else "pool", skip_w=True)
            p1 = V(o.ps[1], 0, 128)
            k.op("pe", lambda e: e.transpose(p1[0:64, :], o.out[:, 0, 64:128], ident), [o.out, cc], [p1])
            k.op("act", lambda e: e.copy(o.wt[:], p1[0:64, :]), [p1], [o.wt])
            k.dma(O7T, O7T[h, d, :, t*128:(t+1)*128], o.wt, o.wt[:], q="pool", skip_w=True)
    k.stage_end()


def e_stepglue(k, P, O6D, FMD, O7D, O7T, STEP):
    k.stage_begin()
    qi = 0
    def cp(dst, src, src_t):
        nonlocal qi
        k.dma(STEP, dst, src_t, src, q="sp" if qi % 2 == 0 else "pool", skip_w=True); qi += 1
    for h in range(4):
        S = STEP[h]
        S2 = S.rearrange("p (t two) d w -> p t two d w", two=2)
        o6c = O6D[h].rearrange("(c p) w -> p c w", p=64)
        o7c = O7D[h].rearrange("(c p) d j w -> p c d j w", p=64)
        o7t2 = O7D[h].rearrange("(t two p) d j w -> p t two d j w", two=2, p=64)
        pc = P.rearrange("(c p) w -> p c w", p=64)
        for d in range(2):
            for (blk_i, col) in ((4+d, 0), (8+d, 128)):
                for two in range(2):
                    cp(S2[:, :, two, d, col:col+64], FMD[h][:, :, blk_i, two*64:(two+1)*64].rearrange("t f k -> f t k"), FMD)
            cp(S[:, :, d, 64:128], O7T[h, d].rearrange("f (c k) -> f c k", k=64), O7T)
            kg0 = O6["KG0"][0] + d*130
            cp(S[:, :, d, 192:322], o6c[:, :, kg0:kg0+130], O6D)
            cp(S[:, :, d, 322:386], pc[:, :, 1280 + h*64:1280 + (h+1)*64], P)
            for two in range(2):
                cp(S2[:, :, two, d, 387:451], o7t2[:, :, two, d, 2, two*64:(two+1)*64], O7D)
                cp(S2[:, :, two, d, 515:579], o7t2[:, :, two, d, 1, two*64:(two+1)*64], O7D)
            cp(S[:, :, d, 451:515], o7c[:, :, d, 0, 0:64], O7D)
    k.stage_end()


def e_scan(k, PS, STEP, O8D):
    k.stage_begin()
    BS = 4; nblk = NC_ // BS
    R = []
    for s_ in range(2):
        r = dict(bufs=[[k.sb([64, BS, W8], name="blk") for _ in range(2)] for _ in range(2)],
                 obufs=[[k.sb([64, BS, 128], name="ob") for _ in range(2)] for _ in range(2)],
                 C1=[k.sb([64, 65], name="C1") for _ in range(2)], Sg=[k.sb([64, 64], name="S") for _ in range(2)],
                 tmpc=[k.sb([64, 65], name="tc") for _ in range(2)], vnew=[k.sb([64, 64], name="vn") for _ in range(2)],
                 rr=[k.sb([64, 4], name="rr") for _ in range(2)])
        psA = [PS[4*s_], PS[4*s_+1]]; psB = [PS[4*s_+2], PS[4*s_+3]]
        r["pso"] = [V(psA[d], 0, 65) for d in range(2)]; r["psc"] = [V(psA[d], 128, 193) for d in range(2)]
        r["psw"] = [V(psB[d], 0, 64) for d in range(2)]; r["pss"] = [V(psB[d], 128, 192) for d in range(2)]; r["psg"] = [V(psB[d], 256, 320) for d in range(2)]
        R.append(r)

    def head_body(h, r):
        bufs = r["bufs"]; obufs = r["obufs"]; C1 = r["C1"]; Sg = r["Sg"]; tmpc = r["tmpc"]; vnew = r["vnew"]; rr = r["rr"]
        pso = r["pso"]; psc = r["psc"]; psw = r["psw"]; pss = r["pss"]; psg = r["psg"]
        for d in range(2):
            k.op("dve", lambda e: e.memset(C1[d][:], 0.0), [], [C1[d]])
            k.op("dve", lambda e: e.memset(Sg[d][:], 0.0), [], [Sg[d]])
        for b in range(nblk):
            mb = [b, 0 if b == 0 else nblk - b]
            B = [bufs[d][b % 2] for d in range(2)]; OB = [obufs[d][b % 2] for d in range(2)]
            for d in range(2):
                k.op("pool", lambda e: e.memset(B[d][:, :, 386:387], 1.0), [], [B[d]])
                k.dma(B[d], B[d][:, :, 0:386], STEP, STEP[h, :, mb[d]*BS:(mb[d]+1)*BS, d, 0:386], q="sp" if d == 0 else "pool")
                k.dma(B[d], B[d][:, :, 387:579], STEP, STEP[h, :, mb[d]*BS:(mb[d]+1)*BS, d, 387:579], q="sp" if d == 0 else "pool")
            for jj in range(BS):
                sl = [jj, BS - 1 - jj]
                F = lambda d, n: B[d][:, sl[d], S8[n][0]:S8[n][1]]
                R2 = range(2)
                for d in R2:
                    k.op("pe", lambda e: e.matmul(psw[d][:64], F(d, "WT"), Sg[d][:], start=True, stop=True), [B[d], Sg[d]], [psw[d]])
                for d in R2:
                    k.op("pe", lambda e: e.matmul(pso[d][:64], F(d, "QsT"), C1[d][:], start=True, stop=False), [B[d], C1[d]], [pso[d]])
                    k.op("pe", lambda e: e.matmul(pso[d][:64], F(d, "AT"), F(d, "V1"), start=False, stop=True), [B[d]], [pso[d]])
                    k.op("pe", lambda e: e.matmul(psc[d][:64], F(d, "Ks"), F(d, "V1"), start=True, stop=True), [B[d]], [psc[d]])
                for d in R2:
                    k.op("dve", lambda e: e.tensor_sub(out=vnew[d][:], in0=F(d, "U"), in1=psw[d][:64]), [B[d], psw[d]], [vnew[d]])
                for d in R2:
                    k.op("pe", lambda e: e.matmul(psg[d][:64], F(d, "QgT"), Sg[d][:], start=True, stop=False), [B[d], Sg[d]], [psg[d]])
                    k.op("pe", lambda e: e.matmul(psg[d][:64], F(d, "AqkT"), vnew[d][:], start=False, stop=True), [B[d], vnew[d]], [psg[d]])
                    k.op("pe", lambda e: e.matmul(pss[d][:64], F(d, "Kd"), vnew[d][:], start=True, stop=True), [B[d], vnew[d]], [pss[d]])
                for d in R2:
                    k.op("dve", lambda e: e.tensor_add(out=tmpc[d][:], in0=psc[d][:64], in1=C1[d][:]), [psc[d], C1[d]], [tmpc[d]])
                    k.op("dve", lambda e: e.tensor_scalar(out=C1[d][:], in0=tmpc[d][:], scalar1=F(d, "sm"), scalar2=None, op0=ALU.mult), [tmpc[d], B[d]], [C1[d]])
                    k.op("dve", lambda e: e.scalar_tensor_tensor(out=Sg[d][:], in0=Sg[d][:], scalar=F(d, "sg"), in1=pss[d][:64], op0=ALU.mult, op1=ALU.add), [Sg[d], B[d], pss[d]], [Sg[d]])
                for d in R2:
                    k.op("act", lambda e: e.activation(out=rr[d][:, 0:1], in_=pso[d][:64, 64:65], func=AF.Abs), [pso[d]], [rr[d]])
                    k.op("dve", lambda e: e.tensor_scalar(out=rr[d][:, 2:3], in0=rr[d][:, 0:1], scalar1=1.0, scalar2=None, op0=ALU.max), [rr[d]], [rr[d]])
                    k.op("dve", lambda e: e.reciprocal(out=rr[d][:, 1:2], in_=rr[d][:, 2:3]), [rr[d]], [rr[d]])
                    k.op("act", lambda e: e.activation(out=OB[d][:, sl[d], 0:64], in_=pso[d][:64, 0:64], func=AF.Copy, scale=rr[d][:, 1:2]), [pso[d], rr[d]], [OB[d]])
                    k.op("act", lambda e: e.copy(OB[d][:, sl[d], 64:128], psg[d][:64]), [psg[d]], [OB[d]])
            for d in range(2):
                k.dma(O8D, O8D[h, :, mb[d]*BS:(mb[d]+1)*BS, d, :], OB[d], OB[d][:], q="sp", skip_w=True)

    for h0 in range(0, 4, 2):
        k.interleave([(lambda h=h0: head_body(h, R[0])), (lambda h=h0 + 1: head_body(h, R[1]))])
    k.stage_end()


def e_outproj(k, PS, AXD, O8D, O6D, GAIN2, X, modrow, sel, ident, W, XMID):
    k.stage_begin()
    gn = k.sb([128, 512], name="gn"); w = k.sb([128, 8, 1024], BF16, name="w")
    gate = [k.sb([128, 1024], name="gate") for _ in range(2)]
    k.dma(gn, gn[:], None, GAIN2[:, :])
    for cls in range(2):
        bcast_rows(k, PS, gate[cls], V(modrow, 2*1024, 3*1024), 1 if cls == 0 else 0, sel)
    for c in range(8):
        k.dma(w, w[:, c, :], None, W[c*128:(c+1)*128, :], q="pool")
    B = [dict(mix=k.sb([128, 1024], name="mix"), a=k.sb([128, 512], name="a"), b=k.sb([128, 512], name="b"), g=k.sb([128, 512], name="g"),
              sq=k.sb([128, 512], name="sq"), s=k.sb([128, 32], name="s"), x=k.sb([128, 1024], name="x"), mT=k.sb([128, 8, 128], BF16, name="mT"),
              o=k.sb([128, 1024], name="o")) for _ in range(2)]
    pi = 0
    def tile_body(t):
        nonlocal pi
        cls = 0 if t < 2 else 1
        b = B[t % 2]; mix = b['mix']; a = b['a']; bb = b['b']; g = b['g']; s = b['s']; x = b['x']; mT = b['mT']; o = b['o']
        rows = slice(t*128, (t+1)*128)
        k.dma(mix, mix[:, 0:512], AXD, AXD[rows, :])
        k.dma(x, x[:], X, X[rows, :])
        qi = 0
        for h in range(4):
            k.dma(g, g[:, h*64:(h+1)*64], O6D, O6D[h, rows, 128:192], q="pool")
            k.dma(g, g[:, 256+h*64:256+(h+1)*64], O6D, O6D[h, rows, 834:898], q="pool")
            for d in range(2):
                dst_t = a if d == 0 else bb
                for half in range(2):
                    dst = dst_t[half*64:(half+1)*64, :].rearrange("p (g hh w) -> p g hh w", g=2, hh=4)[:, :, h, :]
                    src = O8D[h, :, 2*t + half, d, :].rearrange("p (g w) -> p g w", g=2)
                    k.dma(dst_t, dst, O8D, src, q="sp" if qi % 2 == 0 else "pool"); qi += 1
        k.op("dve", lambda e: e.tensor_add(out=a[:], in0=a[:], in1=bb[:]), [a, bb], [a])
        k.op("dve", lambda e: e.tensor_mul(out=b['sq'][:], in0=a[:], in1=a[:]), [a], [b['sq']])
        k.op("dve", lambda e: e.reduce_sum(out=s[:, 0:8], in_=b['sq'][:].rearrange("p (h d) -> p h d", h=8), axis=AX.X), [b['sq']], [s])
        k.op("dve", lambda e: e.tensor_scalar(out=s[:, 0:8], in0=s[:, 0:8], scalar1=1.0/64, scalar2=EPS, op0=ALU.mult, op1=ALU.add), [s], [s])
        k.op("act", lambda e: e.activation(out=s[:, 8:16], in_=s[:, 0:8], func=AF.Sqrt), [s], [s])
        k.op("dve", lambda e: e.reciprocal(out=s[:, 16:24], in_=s[:, 8:16]), [s], [s])
        for h in range(8):
            k.op("dve" if h % 2 == 0 else "pool", lambda e: e.tensor_scalar(out=a[:, h*64:(h+1)*64], in0=a[:, h*64:(h+1)*64], scalar1=s[:, 16+h:17+h], scalar2=None, op0=ALU.mult), [a, s], [a])
        k.op("dve", lambda e: e.tensor_mul(out=a[:], in0=a[:], in1=gn[:]), [a, gn], [a])
        k.op("dve", lambda e: e.tensor_mul(out=mix[:, 512:1024], in0=a[:], in1=g[:]), [a, g], [mix])
        for half in range(2):
            pt = PS[t % 2]
            for c in range(4):
                cc_ = half*4 + c
                k.op("pe", lambda e: e.transpose(pt[:, c*128:(c+1)*128], mix[:, cc_*128:(cc_+1)*128], ident[:]), [mix, ident], [pt])
            if half == 0:
                k.op("act", lambda e: e.copy(mT[:, 0:4, :], pt[:].rearrange("p (c t) -> p c t", c=4)), [pt], [mT])
            else:
                k.op("dve", lambda e: e.tensor_copy(mT[:, 4:8, :], pt[:].rearrange("p (c t) -> p c t", c=4)), [pt], [mT])
        for nb in range(2):
            ps = PS[2 + pi % 4]; pi += 1
            for c in range(8):
                k.op("pe", lambda e: e.matmul(ps[:], mT[:, c, :], w[:, c, nb*512:(nb+1)*512], start=(c == 0), stop=(c == 7)), [mT, w], [ps])
            k.op("dve", lambda e: e.tensor_mul(out=o[:, nb*512:(nb+1)*512], in0=ps[:], in1=gate[cls][:, nb*512:(nb+1)*512]), [ps, gate[cls]], [o])
        k.op("pool", lambda e: e.tensor_add(out=o[:], in0=o[:], in1=x[:]), [o, x], [o])
        k.dma(XMID, XMID[rows, :], o, o[:], skip_w=True)
    for t0 in range(NT_):
        tile_body(t0)
    k.stage_end()


def e_topk(k, AFFT, VALS, IDX):
    k.stage_begin()
    for (c0, n, kk, o0, nm) in [(NCTX, T_ - NCTX, 1024, 0, "l"), (0, NCTX, 32, 1024, "c")]:
        a = k.sb([16, n], name="a" + nm); wk = k.sb([16, n], name="w" + nm)
        vals = k.sb([16, kk], name="v" + nm); idx = k.sb([16, kk], U32, name="i" + nm)
        k.dma(a, a[:], AFFT, AFFT[:, c0:c0+n])
        k.op("dve", lambda e: e.tensor_copy(wk[:], a[:]), [a], [wk])
        for r in range(kk // 8):
            k.op("dve", lambda e: e.max(out=vals[:, r*8:(r+1)*8], in_=wk[:]), [wk], [vals])
            k.op("dve", lambda e: e.max_index(out=idx[:, r*8:(r+1)*8], in_max=vals[:, r*8:(r+1)*8], in_values=wk[:]), [vals, wk], [idx])
            k.op("dve", lambda e: e.match_replace(out=wk[:], in_to_replace=vals[:, r*8:(r+1)*8], in_values=wk[:], imm_value=-1.0), [vals, wk], [wk])
        k.dma(VALS, VALS[:, o0:o0+kk], vals, vals[:], skip_w=True); k.dma(IDX, IDX[:, o0:o0+kk], idx, idx[:], skip_w=True)
    k.stage_end()


def e_expert(k, PS, H2, VALS, IDX, W1, W3, W2, ident, ACC):
    k.stage_begin()
    z = k.sb([128, 1024], name="z")
    k.op("dve", lambda e: e.memset(z[:], 0.0), [], [z])
    for t in range(NT_):
        k.dma(ACC, ACC[t*128:(t+1)*128, :], z, z[:], q="sp" if t % 2 == 0 else "pool", skip_w=True)
    RT = 9
    parts = [(0, 9)]
    PR = 9 * 128
    w2 = k.sb([128, 16, 1024], BF16, name="w2"); vl = k.sb([128, RT], name="vl"); ix = k.sb([128, RT], U32, name="ix")
    hid = k.sb([128, 16, PR], BF16, name="hid"); xt = k.sb([128, 8, PR], BF16, name="xt")
    xg = [k.sb([128, 1024], name="xg") for _ in range(2)]
    w1c = [k.sb([128, 8, 128], BF16, name="w1c") for _ in range(3)]; w3c = [k.sb([128, 8, 128], BF16, name="w3c") for _ in range(3)]
    sg = [k.sb([128, 512], name="sg") for _ in range(2)]
    ysb = [k.sb([128, 1024], name="ysb") for _ in range(2)]
    ph1 = [PS[0], PS[1]]; ph3 = [PS[2], PS[3]]; py = [PS[4], PS[5], PS[6], PS[7]]
    ci = 0; hi = 0; yi = 0; gi = 0
    first_scatter = True
    for e_ in range(16):
        k.dma(w2, w2[:], None, W2[e_].rearrange("(c p) n -> p c n", p=128), q="pool")
        k.op("dve", lambda e: e.memset(vl[:], 0.0), [], [vl])
        k.op("pool", lambda e: e.memset(ix[:], 0), [], [ix])
        k.dma(vl, vl[:, 0:8], VALS, VALS[e_, 0:1024].rearrange("(t p) -> p t", p=128), allow_slow_non_contiguous=True)
        k.dma(vl, vl[0:32, 8:9], VALS, VALS[e_, 1024:1056].rearrange("(t p) -> p t", p=32), allow_slow_non_contiguous=True)
        k.dma(ix, ix[:, 0:8], IDX, IDX[e_, 0:1024].rearrange("(t p) -> p t", p=128), allow_slow_non_contiguous=True)
        k.dma(ix, ix[0:32, 8:9], IDX, IDX[e_, 1024:1056].rearrange("(t p) -> p t", p=32), allow_slow_non_contiguous=True)
        for (t0, t1) in parts:
            nr = (t1 - t0) * 128
            for tt in range(t0, t1):
                g_ = xg[gi % 2]; gi += 1
                k.dma(g_, g_[:], H2, H2[:, :], q="pool", element_offset=(NCTX*1024 if tt < 8 else 0),
                      indirect=dict(in_offset=bass.IndirectOffsetOnAxis(ap=ix[:, tt:tt+1], axis=0), idx_t=ix))
                lr = (tt - t0) * 128
                for half in range(2):
                    pt = py[half]
                    for c in range(4):
                        cc_ = half*4 + c
                        k.op("pe", lambda e: e.transpose(pt[:, c*128:(c+1)*128], g_[:, cc_*128:(cc_+1)*128], ident[:]), [g_, ident], [pt])
                    k.op("act" if half == 0 else "dve", (lambda e: e.copy(xt[:, 0:4, lr:lr+128], pt[:].rearrange("p (c t) -> p c t", c=4))) if half == 0 else
                         (lambda e: e.tensor_copy(xt[:, 4:8, lr:lr+128], pt[:].rearrange("p (c t) -> p c t", c=4))), [pt], [xt])
            for fc in range(16):
                a1 = w1c[ci % 3]; a3 = w3c[ci % 3]; ci += 1
                k.dma(a1, a1[:], None, W1[e_].rearrange("(c p) f -> p c f", p=128)[:, :, fc*128:(fc+1)*128], q="pool")
                k.dma(a3, a3[:], None, W3[e_].rearrange("(c p) f -> p c f", p=128)[:, :, fc*128:(fc+1)*128], q="pool")
                for rb in range(0, nr, 512):
                    rn = min(512, nr - rb)
                    p1 = ph1[hi % 2]; p3 = ph3[hi % 2]; s_ = sg[hi % 2]; hi += 1
                    for c in range(8):
                        k.op("pe", lambda e: e.matmul(p1[:, 0:rn], a1[:, c, :], xt[:, c, rb:rb+rn], start=(c == 0), stop=(c == 7)), [a1, xt], [p1])
                    for c in range(8):
                        k.op("pe", lambda e: e.matmul(p3[:, 0:rn], a3[:, c, :], xt[:, c, rb:rb+rn], start=(c == 0), stop=(c == 7)), [a3, xt], [p3])
                    k.op("act", lambda e: e.activation(out=s_[:, 0:rn], in_=p1[:, 0:rn], func=AF.Silu), [p1], [s_])
                    k.op("dve", lambda e: e.tensor_mul(out=hid[:, fc, rb:rb+rn], in0=s_[:, 0:rn], in1=p3[:, 0:rn]), [s_, p3], [hid])
            for tt in range(t0, t1):
                ys = ysb[yi % 2]; yi += 1
                lr = (tt - t0) * 128
                for nb in range(2):
                    pp = py[(yi*2 + nb) % 4]
                    for fc in range(16):
                        k.op("pe", lambda e: e.matmul(pp[:], hid[:, fc, lr:lr+128], w2[:, fc, nb*512:(nb+1)*512], start=(fc == 0), stop=(fc == 15)), [hid, w2], [pp])
                    if nb == 0:
                        k.op("dve", lambda e: e.tensor_scalar(out=ys[:, 0:512], in0=pp[:], scalar1=vl[:, tt:tt+1], scalar2=None, op0=ALU.mult), [pp, vl], [ys])
                    else:
                        k.op("act", lambda e: e.activation(out=ys[:, 512:1024], in_=pp[:], func=AF.Copy, scale=vl[:, tt:tt+1]), [pp, vl], [ys])
                if tt < 8:
                    k.dma(ACC, ACC[:, :], ys, ys[:], q="pool", compute_op=ALU.add, element_offset=NCTX*1024,
                          indirect=dict(out_offset=bass.IndirectOffsetOnAxis(ap=ix[:, tt:tt+1], axis=0), idx_t=ix))
                else:
                    k.dma(ACC, ACC[:, :], ys, ys[0:32, :], q="pool", compute_op=ALU.add, element_offset=0,
                          indirect=dict(out_offset=bass.IndirectOffsetOnAxis(ap=ix[0:32, tt:tt+1], axis=0), idx_t=ix))
    k.stage_end()


def e_combine(k, PS, XMID, ACC, modrow, sel, OUTT, out_rows0):
    k.stage_begin()
    gate = [k.sb([128, 1024], name="gate") for _ in range(2)]
    for cls in range(2):
        bcast_rows(k, PS, gate[cls], V(modrow, 5*1024, 6*1024), 1 if cls == 0 else 0, sel)
    xs = [k.sb([128, 1024], name="x") for _ in range(2)]; ac = [k.sb([128, 1024], name="ac") for _ in range(2)]
    for t in range(out_rows0 // 128, NT_):
        cls = 0 if t < 2 else 1
        x = xs[t % 2]; a = ac[t % 2]; rows = slice(t*128, (t+1)*128)
        k.dma(x, x[:], XMID, XMID[rows, :]); k.dma(a, a[:], ACC, ACC[rows, :], q="pool")
        k.op("dve", lambda e: e.tensor_mul(out=a[:], in0=a[:], in1=gate[cls][:]), [a, gate[cls]], [a])
        k.op("pool", lambda e: e.tensor_add(out=x[:], in0=x[:], in1=a[:]), [x, a], [x])
        k.dma(OUTT, OUTT[t*128 - out_rows0:(t+1)*128 - out_rows0, :], x, x[:], skip_w=True)
    k.stage_end()


def build_fused(nlayer=2, upto=None):
    k = K()
    TT = T_
    def IN(name, shape, dt=F32):
        t = T(k, k.dram_in(name, shape, dt), name); t.is_dram = True
        return t
    XCAT = IN("xcat", [TT, 1024]); CT = IN("ct", [128, 8, 2]); IDENT = IN("ident", [128, 128]); SEL = IN("sel", [2, 2, 128])
    CS = IN("cs", [TT, 320]); SN = IN("sn", [TT, 320]); CC = IN("cc", [9, 128, 128])
    L = []
    for l in range(nlayer):
        L.append(dict(MODW=IN(f"modw{l}", [1024, 6144]), MODB=IN(f"modb{l}", [2, 6144]), N1G=IN(f"n1g{l}", [128, 1024]), WIN=IN(f"win{l}", [1024, 2848]),
                      QKG=IN(f"qkg{l}", [128, 640]), CB=IN(f"cb{l}", [128, 32]), CW=IN(f"cw{l}", [128, 5, 768]), GAIN2=IN(f"gain2{l}", [128, 512]),
                      WOUT=IN(f"wout{l}", [1024, 1024]), N2G=IN(f"n2g{l}", [128, 1024]), RW=IN(f"rw{l}", [1024, 16]),
                      W1=IN(f"w1{l}", [16, 1024, 2048]), W3=IN(f"w3{l}", [16, 1024, 2048]), W2=IN(f"w2{l}", [16, 2048, 1024])))
    OUT = k.dram_out("out", [TT - NCTX, 1024])
    PS = [k.ps([128, 512], name=f"bank{i}") for i in range(8)]
    ident = k.sb([128, 128], name="ident"); sel = k.sb([2, 2, 128], name="sel"); ctsb = k.sb([128, 8, 2], name="ct")
    cc = k.sb([128, 9, 128], name="cc")
    modrow = k.sb([2, 6144], name="modrow")
    k.dma(ident, ident[:], None, IDENT[:, :]); k.dma(sel, sel[:], None, SEL[:, :, :]); k.dma(ctsb, ctsb[:], None, CT[:, :, :])
    for i in range(9):
        k.dma(cc, cc[:, i, :], None, CC[i])
    k.op("act", lambda e: e.activation(out=ctsb[:], in_=ctsb[:], func=AF.Silu), [ctsb], [ctsb])
    D = lambda n, s, dt=F32: k.dram_tmp(n, s, dt)
    P = D("P", [TT, 2848]); QKT = D("QKT", [640, TT], BF16); AXD = D("AXD", [TT, 512]); O6D = D("O6D", [4, TT, W6]); FMD = D("FMD", [4, NT_, 64, 10, 128])
    O7D = D("O7D", [4, TT, 2, 3, 128]); O7T = D("O7T", [4, 2, 64, TT]); STEP = D("STEP", [4, 64, NC_, 2, W8]); O8D = D("O8D", [4, 64, NC_, 2, 128])
    XMID = D("XMID", [TT, 1024]); H2 = D("H2", [TT, 1024]); AFFT = D("AFFT", [16, TT]); VALS = D("VALS", [16, 1152]); IDX = D("IDX", [16, 1152], U32)
    ACC = D("ACC", [TT, 1024]); XN = D("XN", [TT, 1024])
    X = XCAT
    for l in range(nlayer):
        W = L[l]
        e_mod(k, PS, ctsb, W["MODW"], W["MODB"], modrow)
        e_normlin(k, PS, X, W["N1G"], modrow, 1, 0, sel, ident, W["WIN"], 2848, P, QKT=QKT, qk=dict(GAIN=W["QKG"], CS=CS, SN=SN))
        e_attn(k, PS, QKT, P, AXD)
        e_prep(k, PS, P, W["CB"], W["CW"], cc, ident, O6D, FMD)
        e_intra(k, PS, O6D, FMD, cc, O7D, O7T)
        e_stepglue(k, P, O6D, FMD, O7D, O7T, STEP)
        e_scan(k, PS, STEP, O8D)
        e_outproj(k, PS, AXD, O8D, O6D, W["GAIN2"], X, modrow, sel, ident, W["WOUT"], XMID)
        if upto == "xmid":
            k.stage_begin(); xx = [k.sb([128, 1024], name="cpy") for _ in range(2)]
            for t in range(2, NT_):
                k.dma(xx[t % 2], xx[t % 2][:], XMID, XMID[t*128:(t+1)*128, :]); k.dma(OUT, OUT[(t-2)*128:(t-1)*128, :], xx[t % 2], xx[t % 2][:], skip_w=True)
            k.stage_end(); break
        e_normlin(k, PS, XMID, W["N2G"], modrow, 4, 3, sel, ident, W["RW"], 16, None, H=H2, softmax=True, YT=AFFT)
        e_topk(k, AFFT, VALS, IDX)
        e_expert(k, PS, H2, VALS, IDX, W["W1"], W["W3"], W["W2"], ident, ACC)
        last = (l == nlayer - 1)
        e_combine(k, PS, XMID, ACC, modrow, sel, OUT if last else XN, NCTX if last else 0)
        X = XN
    k.finish([OUT])
    k.close()
    return k

def _rep(v, p=128):
    return np.ascontiguousarray(np.broadcast_to(np.asarray(v, np.float32)[None], (p,) + tuple(np.shape(v))))

def _consts():
    f32 = np.float32
    n = 8192
    row = (np.arange(n) // 64).astype(f32); col = (np.arange(n) % 64).astype(f32)
    inv = (10000.0 ** (-np.arange(16, dtype=f32) / 16)).astype(f32)
    ang = np.stack([row, col], -1)[..., None] * inv
    tile = lambda a: np.ascontiguousarray(np.broadcast_to(a[:, None], (a.shape[0], 10, 2, 16))).reshape(a.shape[0], 320)
    cs = np.concatenate([np.ones((256, 320), f32), tile(np.cos(ang).astype(f32))]); sn = np.concatenate([np.zeros((256, 320), f32), tile(np.sin(ang).astype(f32))])
    sel = np.zeros((2, 2, 128), f32); sel[0, 0] = 1; sel[1, 1] = 1
    s = np.arange(128)[:, None]; t = np.arange(128)[None, :]
    same = (s // 64) == (t // 64)
    trif = (same & (s <= t)).astype(f32); trir = (same & (s >= t)).astype(f32); eye = np.eye(128, dtype=f32)
    cc = np.stack([eye, np.ones((128, 128), f32), trif, trir, trif - eye, trir - eye, eye - trif, eye - trir, same.astype(f32)])
    return dict(cs=cs, sn=sn, sel=sel, cc=cc, ident=eye)

def make_in_maps(inp, nlayer=2):
    f32 = np.float32
    C = _consts()
    ims = []
    for b in range(2):
        ct = np.stack([inp['c'][b], inp['c_ctx']], 1).reshape(8, 128, 2).transpose(1, 0, 2)
        m = {"xcat": np.concatenate([inp['ctx'][b], inp['x'][b]], 0), "ct": np.ascontiguousarray(ct, dtype=f32)}
        m.update(C)
        for l in range(nlayer):
            cb = np.concatenate([inp['mlstm_i_bias'][l].reshape(8), inp['mlstm_f_bias'][l].reshape(8), inp['gdn_a_log'][l].reshape(8), inp['gdn_dt_bias'][l].reshape(8)])
            m.update({f"modw{l}": inp['mod_w'][l], f"modb{l}": _rep(inp['mod_b'][l], 2), f"n1g{l}": _rep(inp['norm1_g'][l]), f"win{l}": inp['w_in'][l],
                      f"qkg{l}": _rep(np.concatenate([np.tile(inp['q_norm_g'][l], 8), np.tile(inp['k_norm_g'][l], 2)])),
                      f"cb{l}": _rep(cb), f"cw{l}": _rep(inp['gdn_conv_w'][l]),
                      f"gain2{l}": _rep(np.concatenate([inp['mlstm_out_g'][l].reshape(256), np.tile(inp['gdn_out_g'][l], 4)])),
                      f"wout{l}": inp['w_out'][l], f"n2g{l}": _rep(inp['norm2_g'][l]), f"rw{l}": inp['router_w'][l],
                      f"w1{l}": inp['w1'][l], f"w3{l}": inp['w3'][l], f"w2{l}": inp['w2'][l]})
        ims.append({k_: np.ascontiguousarray(v, dtype=f32) for k_, v in m.items()})
    return ims

_K = {}
def kernel(**inputs):
    inp = {k_: np.asarray(v, np.float32) for k_, v in inputs.items()}
    if 2 not in _K:
        _K[2] = build_fused(2)
    res = run_bass_kernel_spmd(_K[2].nc, make_in_maps(inp, 2), core_ids=[0, 1])
    return np.ascontiguousarray(np.stack([res.results[b]["out"] for b in range(2)]).astype(np.float32))
```

```python
import numpy as np
import threading
from contextlib import ExitStack
import concourse.bass as bass
import concourse.mybir as mybir
from concourse.bass_utils import run_bass_kernel_spmd

F32 = mybir.dt.float32
I32 = mybir.dt.int32
U32 = mybir.dt.uint32
BF16 = mybir.dt.bfloat16
ALU = mybir.AluOpType
AF = mybir.ActivationFunctionType
AX = mybir.AxisListType
EPS = 1e-6
SAME_ENGINE_FIFO = False


class T:
    def __init__(self, k, ap, name):
        self.k = k; self.ap = ap; self.name = name
        self.w = None; self.r = {}
        self.dsem = {}; self.dn = 0; self.is_dram = False

    def __getitem__(self, idx):
        return self.ap[idx]

    def rearrange(self, *a, **kw):
        return self.ap.rearrange(*a, **kw)


class V:
    def __init__(s, t, a0, a1):
        s.t = t; s.base = t; s.a0 = a0; s.a1 = a1

    def __getitem__(s, idx):
        return s.t.ap[:, s.a0:s.a1][idx]


class K:
    def __init__(self):
        self.nc = bass.Bass("TRN2", target_bir_lowering=False)
        self.es = ExitStack()
        self.st = None
        nc = self.nc
        self.es.enter_context(nc.allow_low_precision('bf16 matmul operands, fp32 PSUM accumulation'))
        self.eng = {"pe": nc.tensor, "act": nc.scalar, "dve": nc.vector, "pool": nc.gpsimd, "sp": nc.sync}
        self.sem = {}; self.cnt = {}
        for e in ["pe", "act", "dve", "pool"]:
            self.sem[e] = self.es.enter_context(nc.semaphore("s_" + e)); self.cnt[e] = 0
        self.seen = {e: {} for e in self.eng}
        self.ntile = 0
        self.dsems = {}
        self.free_sems = {'sp': [], 'pool': []}
        self.stage_tiles = []
        self.nsem = 0

    def dram_in(self, name, shape, dt=F32):
        return self.nc.dram_tensor(name, list(shape), dt, kind="ExternalInput").ap()

    def dram_out(self, name, shape, dt=F32):
        t = T(self, self.nc.dram_tensor(name, list(shape), dt, kind="ExternalOutput").ap(), name); t.is_dram = True
        return t

    def dram_tmp(self, name, shape, dt=F32):
        t = T(self, self.nc.dram_tensor(name, list(shape), dt, kind="Internal").ap(), name); t.is_dram = True
        return t

    def sb(self, shape, dt=F32, name=None):
        self.ntile += 1
        name = (name or "t") + f"_{self.ntile}"
        stk = self.st if self.st is not None else self.es
        h = stk.enter_context(self.nc.sbuf_tensor("S_" + name, list(shape), dt))
        t = T(self, h[:], name)
        if self.st is not None:
            self.stage_tiles.append(t)
        return t

    def ps(self, shape, dt=F32, name=None):
        self.ntile += 1
        name = (name or "p") + f"_{self.ntile}"
        h = self.es.enter_context(self.nc.psum_tensor("P_" + name, list(shape), dt))
        return T(self, h[:], name)

    def stage_begin(self):
        assert self.st is None
        self.st = ExitStack(); self.stage_tiles = []

    def stage_end(self):
        self.barrier()
        for t in self.stage_tiles:
            for q_, sm_ in t.dsem.items():
                self.free_sems[q_].append(sm_)
        self.st.close(); self.st = None; self.stage_tiles = []

    def barrier(self):
        toks = [(self.sem[c], self.cnt[c]) for c in self.sem if self.cnt[c] > 0]
        toks += [(s, 16 * n) for (s, n) in self.dsems.values() if n > 0]
        for e in self.eng:
            self._wait(e, toks)

    def _get_dsem(self, t, q):
        if q not in t.dsem:
            if self.free_sems[q]:
                t.dsem[q] = self.free_sems[q].pop()
            else:
                self.nsem += 1
                sm_ = self.es.enter_context(self.nc.semaphore(f"d{self.nsem}{q}"))
                t.dsem[q] = sm_
                self.dsems[id(sm_)] = [sm_, 0]
        return t.dsem[q]

    def _deps(self, reads, writes):
        toks = []
        for t in reads:
            if t.w is not None:
                toks.append(t.w)
        for t in writes:
            if t.w is not None:
                toks.append(t.w)
            toks.extend(t.r.values())
        return toks

    def _wait(self, e, toks, skip_sem=None):
        eng = self.eng[e]
        best = {}
        for (s, v) in toks:
            if skip_sem is not None and s is skip_sem:
                continue
            if best.get(id(s), (None, 0))[1] < v:
                best[id(s)] = (s, v)
        for (s, v) in best.values():
            if self.seen[e].get(id(s), 0) < v:
                eng.wait_ge(s, v)
                self.seen[e][id(s)] = v

    def _mark(self, tok, reads, writes):
        for t in reads:
            t.r[id(tok[0])] = tok
        for t in writes:
            t.w = tok; t.r = {}

    def op(self, e, fn, reads, writes):
        reads = [getattr(t, "base", t) for t in reads if t is not None]
        writes = [getattr(t, "base", t) for t in writes]
        toks = self._deps(reads, writes)
        self._wait(e, toks, skip_sem=self.sem[e] if (e == "pe" or SAME_ENGINE_FIFO) else None)
        ins = fn(self.eng[e])
        self.cnt[e] += 1
        ins.then_inc(self.sem[e], 1)
        tok = (self.sem[e], self.cnt[e])
        self._mark(tok, reads, writes)
        self._yield()
        return tok

    def dma(self, out_t, out_ap, in_t, in_ap, q="sp", skip_w=False, indirect=None, **kw):
        reads = [in_t] if in_t is not None else []
        if indirect is not None and indirect.get("idx_t") is not None:
            reads.append(indirect["idx_t"])
        writes = [out_t] if out_t is not None else []
        toks = self._deps(reads, [] if skip_w else writes)
        self._wait(q, toks)
        if out_t is not None and not out_t.is_dram:
            owner = out_t
        elif in_t is not None and not in_t.is_dram:
            owner = in_t
        else:
            owner = out_t if out_t is not None else in_t
        sem = self._get_dsem(owner, q)
        ent = self.dsems[id(sem)]
        ent[1] += 1
        if indirect is None:
            ins = self.eng[q].dma_start(out=out_ap, in_=in_ap, **kw)
        else:
            ins = self.eng[q].indirect_dma_start(out=out_ap, out_offset=indirect.get("out_offset"), in_=in_ap,
                                                 in_offset=indirect.get("in_offset"), **kw)
        ins.then_inc(sem, 16)
        tok = (sem, 16 * ent[1])
        self._mark(tok, reads, writes)
        self._yield()
        return tok

    def _yield(self):
        il = getattr(self, "_il", None)
        if il is None:
            return
        me = threading.current_thread()
        if me not in il["threads"]:
            return
        with il["cv"]:
            i = il["threads"].index(me)
            self._pass_turn(il, i)
            while il["turn"] is not me:
                il["cv"].wait()

    def _pass_turn(self, il, i):
        n = len(il["threads"])
        for d in range(1, n + 1):
            t = il["threads"][(i + d) % n]
            if t in il["live"]:
                il["turn"] = t
                break
        il["cv"].notify_all()

    def interleave(self, fns):
        if len(fns) == 1:
            fns[0](); return
        il = {"cv": threading.Condition(), "threads": [], "live": set(), "turn": None, "err": []}
        def runner(fn):
            me = threading.current_thread()
            with il["cv"]:
                while il["turn"] is not me:
                    il["cv"].wait()
            try:
                fn()
            except BaseException as e:
                il["err"].append(e)
            with il["cv"]:
                il["live"].discard(me)
                if il["live"]:
                    self._pass_turn(il, il["threads"].index(me))
                else:
                    il["turn"] = None; il["cv"].notify_all()
        ths = [threading.Thread(target=runner, args=(fn,)) for fn in fns]
        il["threads"] = ths; il["live"] = set(ths)
        self._il = il
        for t in ths:
            t.start()
        with il["cv"]:
            il["turn"] = ths[0]; il["cv"].notify_all()
        for t in ths:
            t.join()
        self._il = None
        if il["err"]:
            raise il["err"][0]

    def finish(self, out_tiles):
        self.barrier()

    def close(self):
        if self.st is not None:
            self.st.close()
        self.es.close()


CUM = [0, 512, 640, 768, 1024, 1280, 1536, 1792, 1800, 1808, 2064, 2320, 2576, 2832, 2840, 2848]
import os
T_ = int(os.environ.get('FZ_T', '8448')); NT_ = T_ // 128; NC_ = T_ // 64; NCTX = 256

OUT6 = dict(Qs=(0, 128), Ks=(128, 256), eb=(256, 258), sigo=(258, 322), qn=(322, 386), kn=(386, 450), kb=(450, 578),
            Rm=(578, 834), Qg=(834, 962), Kd=(962, 1090), G=(1090, 1092), eG=(1092, 1094), siluz=(1094, 1158), gv=(1158, 1222),
            ebl=(1222, 1224), eGl=(1224, 1226))
W6 = 1226
S8 = dict(QsT=(0, 64), WT=(64, 128), QgT=(128, 192), Ks=(192, 256), V1=(256, 321), AT=(321, 385), U=(385, 449), Kd=(449, 513),
          AqkT=(513, 577), sm=(577, 578), sg=(578, 579))
W8 = 579


def bcast_rows(k, PS, dst, src2, r, sel, n=1024):
    for nb in range(0, n, 512):
        ps = PS[(nb // 512) % 2]
        k.op("pe", lambda e: e.matmul(ps[:, 0:512], sel[:, r, :], src2[:, nb:nb+512], start=True, stop=True), [sel, src2], [ps])
        k.op("dve", lambda e: e.tensor_copy(dst[:, nb:nb+512], ps[:, 0:512]), [ps], [dst])


def e_mod(k, PS, ctsb, MODW, MODB, modrow):
    k.stage_begin()
    ws = [k.sb([128, 8, 512], name="mw") for _ in range(2)]
    bi = k.sb([2, 6144], name="mb")
    k.dma(bi, bi[:], None, MODB[:, :])
    for nb in range(12):
        w = ws[nb % 2]; ps = PS[nb % 2]
        k.dma(w, w[:], None, MODW.rearrange("(c p) n -> p c n", p=128)[:, :, nb*512:(nb+1)*512], q="sp" if nb % 2 == 0 else "pool")
        for c in range(8):
            k.op("pe", lambda e: e.matmul(ps[0:2, 0:512], ctsb[:, c, :], w[:, c, :], start=(c == 0), stop=(c == 7)), [ctsb, w], [ps])
        k.op("dve", lambda e: e.tensor_add(out=modrow[:, nb*512:(nb+1)*512], in0=ps[0:2, 0:512], in1=bi[:, nb*512:(nb+1)*512]), [ps, bi], [modrow])
    k.stage_end()


def e_normlin(k, PS, X, GREP, modrow, isc, ish, sel, ident, W, N, Y, H=None, softmax=False, YT=None, QKT=None, qk=None):
    k.stage_begin()
    g = k.sb([128, 1024], name="g"); k.dma(g, g[:], None, GREP[:, :])
    A = [k.sb([128, 1024], name="A") for _ in range(2)]; sh = [k.sb([128, 1024], name="sh") for _ in range(2)]
    for cls in range(2):
        r = 1 if cls == 0 else 0
        bcast_rows(k, PS, A[cls], V(modrow, isc*1024, (isc+1)*1024), r, sel)
        bcast_rows(k, PS, sh[cls], V(modrow, ish*1024, (ish+1)*1024), r, sel)
        k.op("dve", lambda e: e.scalar_tensor_tensor(out=A[cls][:], in0=A[cls][:], scalar=1.0, in1=g[:], op0=ALU.add, op1=ALU.mult), [A[cls], g], [A[cls]])
    lowp = N > 16
    w = k.sb([128, 8, N], BF16 if lowp else F32, name="w")
    for c in range(8):
        k.dma(w, w[:, c, :], None, W[c*128:(c+1)*128, :], q="pool" if lowp else ("sp" if c % 2 == 0 else "pool"))
    xs = [k.sb([128, 1024], name="x") for _ in range(2)]; hs = [k.sb([128, 1024], name="h") for _ in range(2)]
    hT = [k.sb([128, 8, 128], BF16 if lowp else F32, name="hT") for _ in range(2)]
    ys = [k.sb([128, N], name="y") for _ in range(2)]
    st = [k.sb([128, 40], name="st") for _ in range(2)]
    if qk is not None:
        gn = k.sb([128, 640], name="gn"); k.dma(gn, gn[:], None, qk["GAIN"][:, :])
        QB = [dict(cs=k.sb([128, 320], name="cs"), sn=k.sb([128, 320], name="sn"), sq=k.sb([128, 640], name="sq"), o=k.sb([128, 640], name="o"),
                   t1=k.sb([128, 320], name="t1"), t2=k.sb([128, 320], name="t2"), oT=k.sb([128, 5, 128], BF16, name="oT")) for _ in range(2)]
        v5 = lambda ap: ap.rearrange("p (h a f j) -> p h a f j", h=10, a=2, f=2)
        v4 = lambda ap: ap.rearrange("p (h a j) -> p h a j", h=10, a=2)
    if YT is not None:
        yts = [k.sb([16, 128], name="yt") for _ in range(2)]
    nb_ = (N + 511) // 512
    pi = 0
    def tile_body(rt):
        nonlocal pi
        cls = 0 if rt < 2 else 1
        x = xs[rt % 2]; h = hs[rt % 2]; s = st[rt % 2]; ht = hT[rt % 2]; y = ys[rt % 2]
        rows = slice(rt*128, (rt+1)*128)
        k.dma(x, x[:], X, X[rows, :])
        k.op("dve", lambda e: e.memset(s[:, 0:8], 0.0), [], [s])
        k.op("act", lambda e: e.activation(out=h[:], in_=x[:], func=AF.Square, accum_out=s[:, 0:1]), [x, s], [h, s])
        k.op("dve", lambda e: e.tensor_scalar(out=s[:, 1:2], in0=s[:, 0:1], scalar1=1.0/1024, scalar2=EPS, op0=ALU.mult, op1=ALU.add), [s], [s])
        k.op("act", lambda e: e.activation(out=s[:, 7:8], in_=s[:, 1:2], func=AF.Sqrt), [s], [s])
        k.op("dve", lambda e: e.reciprocal(out=s[:, 2:3], in_=s[:, 7:8]), [s], [s])
        k.op("dve", lambda e: e.scalar_tensor_tensor(out=h[:], in0=x[:], scalar=s[:, 2:3], in1=A[cls][:], op0=ALU.mult, op1=ALU.mult), [x, s, A[cls]], [h])
        k.op("pool", lambda e: e.tensor_add(out=h[:], in0=h[:], in1=sh[cls][:]), [h, sh[cls]], [h])
        if H is not None:
            k.dma(H, H[rows, :], h, h[:], q="pool", skip_w=True)
        for half in range(2):
            pt = PS[rt % 2]
            for c in range(4):
                cc = half*4 + c
                k.op("pe", lambda e: e.transpose(pt[:, c*128:(c+1)*128], h[:, cc*128:(cc+1)*128], ident[:]), [h, ident], [pt])
            if half == 0:
                k.op("act", lambda e: e.copy(ht[:, 0:4, :], pt[:].rearrange("p (c t) -> p c t", c=4)), [pt], [ht])
            else:
                k.op("dve", lambda e: e.tensor_copy(ht[:, 4:8, :], pt[:].rearrange("p (c t) -> p c t", c=4)), [pt], [ht])
        for b in range(nb_):
            n0 = b*512; n1 = min(N, n0+512); ps = PS[2 + (rt % 2) + 2*(b % 2)]
            for c in range(8):
                k.op("pe", lambda e: e.matmul(ps[:, 0:n1-n0], ht[:, c, :], w[:, c, n0:n1], start=(c == 0), stop=(c == 7)), [ht, w], [ps])
            if b % 2 == 0:
                k.op("dve", lambda e: e.tensor_copy(y[:, n0:n1], ps[:, 0:n1-n0]), [ps], [y])
            else:
                k.op("act", lambda e: e.copy(y[:, n0:n1], ps[:, 0:n1-n0]), [ps], [y])
        if softmax:
            k.op("dve", lambda e: e.reduce_max(out=s[:, 3:4], in_=y[:], axis=AX.X), [y], [s])
            k.op("dve", lambda e: e.tensor_scalar(out=s[:, 4:5], in0=s[:, 3:4], scalar1=-1.0, scalar2=None, op0=ALU.mult), [s], [s])
            k.op("dve", lambda e: e.memset(s[:, 5:6], 0.0), [], [s])
            k.op("act", lambda e: e.activation(out=y[:], in_=y[:], func=AF.Exp, bias=s[:, 4:5], scale=1.0, accum_out=s[:, 5:6]), [y, s], [y, s])
            k.op("dve", lambda e: e.reciprocal(out=s[:, 6:7], in_=s[:, 5:6]), [s], [s])
            k.op("dve", lambda e: e.tensor_scalar(out=y[:], in0=y[:], scalar1=s[:, 6:7], scalar2=None, op0=ALU.mult), [y, s], [y])
        if YT is not None:
            yt = yts[rt % 2]; pt = PS[6 + rt % 2]
            k.op("pe", lambda e: e.transpose(pt[0:16, 0:128], y[:, 0:16], ident[:]), [y, ident], [pt])
            k.op("dve", lambda e: e.tensor_copy(yt[:], pt[0:16, 0:128]), [pt], [yt])
            k.dma(YT, YT[:, rows], yt, yt[:], q="pool", skip_w=True)
        if Y is not None:
            k.dma(Y, Y[rows, :], y, y[:], skip_w=True)
        if qk is not None:
            b = QB[rt % 2]; o = b['o']
            k.dma(b['cs'], b['cs'][:], None, qk["CS"][rows, :], q="pool"); k.dma(b['sn'], b['sn'][:], None, qk["SN"][rows, :], q="pool")
            xq = V(y, 0, 640)
            k.op("dve", lambda e: e.tensor_mul(out=b['sq'][:], in0=xq[:], in1=xq[:]), [y], [b['sq']])
            k.op("dve", lambda e: e.reduce_sum(out=s[:, 8:18], in_=b['sq'][:].rearrange("p (h d) -> p h d", h=10), axis=AX.X), [b['sq']], [s])
            k.op("dve", lambda e: e.tensor_scalar(out=s[:, 8:18], in0=s[:, 8:18], scalar1=1.0/64, scalar2=EPS, op0=ALU.mult, op1=ALU.add), [s], [s])
            k.op("act", lambda e: e.activation(out=s[:, 18:28], in_=s[:, 8:18], func=AF.Sqrt), [s], [s])
            k.op("dve", lambda e: e.reciprocal(out=s[:, 28:38], in_=s[:, 18:28]), [s], [s])
            for hh in range(10):
                k.op("dve" if hh % 2 == 0 else "pool", lambda e: e.tensor_scalar(out=b['sq'][:, hh*64:(hh+1)*64], in0=xq[:, hh*64:(hh+1)*64], scalar1=s[:, 28+hh:29+hh], scalar2=None, op0=ALU.mult), [y, s], [b['sq']])
            xn = b['sq']
            k.op("dve", lambda e: e.tensor_mul(out=xn[:], in0=xn[:], in1=gn[:]), [xn, gn], [xn])
            x1 = v5(xn[:])[:, :, :, 0, :]; x2 = v5(xn[:])[:, :, :, 1, :]
            o1 = v5(o[:])[:, :, :, 0, :]; o2 = v5(o[:])[:, :, :, 1, :]
            cs = v4(b['cs'][:]); sn = v4(b['sn'][:]); t1 = v4(b['t1'][:]); t2 = v4(b['t2'][:])
            k.op("dve", lambda e: e.tensor_mul(out=t1, in0=x1, in1=cs), [xn, b['cs']], [b['t1']])
            k.op("pool", lambda e: e.tensor_mul(out=t2, in0=x2, in1=sn), [xn, b['sn']], [b['t2']])
            k.op("dve", lambda e: e.tensor_sub(out=o1, in0=t1, in1=t2), [b['t1'], b['t2']], [o])
            k.op("dve", lambda e: e.tensor_mul(out=t1, in0=x2, in1=cs), [xn, b['cs']], [b['t1']])
            k.op("pool", lambda e: e.tensor_mul(out=t2, in0=x1, in1=sn), [xn, b['sn']], [b['t2']])
            k.op("dve", lambda e: e.tensor_add(out=o2, in0=t1, in1=t2), [b['t1'], b['t2']], [o])
            pt = PS[6 + rt % 2]
            for c in range(4):
                k.op("pe", lambda e: e.transpose(pt[:, c*128:(c+1)*128], o[:, c*128:(c+1)*128], ident[:]), [o, ident], [pt])
            k.op("act", lambda e: e.copy(b['oT'][:, 0:4, :], pt[:].rearrange("p (c t) -> p c t", c=4)), [pt], [b['oT']])
            pt2 = PS[6 + rt % 2]
            k.op("pe", lambda e: e.transpose(pt2[:, 0:128], o[:, 512:640], ident[:]), [o, ident], [pt2])
            k.op("dve", lambda e: e.tensor_copy(b['oT'][:, 4, :], pt2[:, 0:128]), [pt2], [b['oT']])
            k.dma(QKT, QKT.ap.rearrange("(c p) t -> p c t", p=128)[:, :, rows], b['oT'], b['oT'][:], q="pool", skip_w=True)
    for t0 in range(0, T_ // 128, 2):
        k.interleave([(lambda t=t: tile_body(t)) for t in range(t0, min(T_ // 128, t0 + 2))])
    k.stage_end()


def e_attn(k, PS, QKT, P, AXD):
    k.stage_begin()
    T = T_; NT = NT_
    kts = [k.sb([64, T], BF16, name="kt") for _ in range(2)]
    v1s = [k.sb([128, NT, 65], BF16, name="v1") for _ in range(2)]
    vtmp = k.sb([128, NT, 64], name="vtmp")
    for g in range(2):
        k.dma(kts[g], kts[g][:], QKT, QKT[512 + g*64:512 + (g+1)*64, :])
        k.op("dve", lambda e: e.memset(v1s[g][:, :, 64:65], 1.0), [], [v1s[g]])
        k.dma(vtmp, vtmp[:], P, P.ap.rearrange("(n p) c -> p n c", p=128)[:, :, 640 + g*64:640 + (g+1)*64], q="pool")
        k.op("dve", lambda e: e.tensor_copy(v1s[g][:, :, 0:64], vtmp[:]), [vtmp], [v1s[g]])
    qts = [k.sb([64, 512], BF16, name="q") for _ in range(2)]
    pts = [k.sb([128, 512], BF16, name="pt") for _ in range(3)]
    osb = [k.sb([128, 4, 64], name="osb") for _ in range(2)]
    rs = [k.sb([128, 8], name="rs") for _ in range(2)]
    pss = PS[0:2]; pso = PS[2:6]
    blocks = [(0, NCTX, 0, NCTX // 128)] + [(q0, 512, 0, NT) for q0 in range(NCTX, T, 512)]
    it = 0; bi = 0
    pending = None
    for h in range(8):
        g = h // 4; kt = kts[g]; v1 = v1s[g]
        for (q0, qn, k0, k1) in blocks:
            qt = qts[bi % 2]; ob = osb[bi % 2]; r = rs[bi % 2]; bi += 1
            nqs = qn // 128
            k.dma(qt, qt[:, 0:qn], QKT, QKT[h*64:(h+1)*64, q0:q0+qn])
            for kk in range(k0, k1):
                ps = pss[it % 2]; pt = pts[it % 3]; it += 1
                k.op("pe", lambda e: e.matmul(ps[:, 0:qn], kt[:, kk*128:(kk+1)*128], qt[:, 0:qn], start=True, stop=True), [kt, qt], [ps])
                k.op("act", lambda e: e.activation(out=pt[:, 0:qn], in_=ps[:, 0:qn], func=AF.Exp, scale=0.125), [ps], [pt])
                if pending is not None:
                    pending()

                def mk(pt=pt, v1=v1, kk=kk, k0=k0, k1=k1, nqs=nqs, r=r, ob=ob, q0=q0, qn=qn, h=h):
                    def run():
                        for qs in range(nqs):
                            k.op("pe", lambda e: e.matmul(pso[qs][:, 0:65], pt[:, qs*128:(qs+1)*128], v1[:, kk, :], start=(kk == k0), stop=(kk == k1-1)), [pt, v1], [pso[qs]])
                        if kk == k1 - 1:
                            for qs in range(nqs):
                                k.op("dve", lambda e: e.reciprocal(out=r[:, qs:qs+1], in_=pso[qs][:, 64:65]), [pso[qs]], [r])
                                k.op("dve", lambda e: e.tensor_scalar(out=ob[:, qs, :], in0=pso[qs][:, 0:64], scalar1=r[:, qs:qs+1], scalar2=None, op0=ALU.mult), [pso[qs], r], [ob])
                            k.dma(AXD, AXD[q0:q0+qn, h*64:(h+1)*64].rearrange("(s p) d -> p s d", p=128), ob, ob[:, 0:nqs, :], q="pool", skip_w=True)
                    return run
                pending = mk()
    if pending is not None:
        pending()
    k.stage_end()


O6 = dict(Qs=(0, 128), sigo=(128, 192), qn=(192, 256), kn=(256, 320), kb=(320, 448), Rm=(448, 704), Qg=(704, 832), G=(832, 834),
          siluz=(834, 898), KG0=(898, 1028), KG1=(1028, 1158))
W6 = 1158
S8 = dict(QsT=(0, 64), WT=(64, 128), QgT=(128, 192), Ks=(192, 256), Kd=(256, 320), sm=(320, 321), sg=(321, 322), V1=(322, 387),
          AT=(387, 451), U=(451, 515), AqkT=(515, 579))
W8 = 579
FM_SRC = [(256, 320), (192, 256), (320, 384), (384, 448), (0, 64), (64, 128), None, None, (704, 768), (768, 832)]


def e_prep(k, PS, P, CB, CW, cc, ident, O6D, FMD):
    k.stage_begin()
    tf = cc[:, 2, :]; tr = cc[:, 3, :]; blk = cc[:, 8, :]
    cb = k.sb([128, 48], name="cb"); cw = k.sb([128, 5, 768], name="cw")
    k.dma(cb, cb[:, 0:32], None, CB[:, :]); k.dma(cw, cw[:], None, CW[:, :, :])
    k.op("act", lambda e: e.activation(out=cb[:, 32:40], in_=cb[:, 16:24], func=AF.Exp), [cb], [cb])
    k.op("dve", lambda e: e.tensor_scalar(out=cb[:, 32:40], in0=cb[:, 32:40], scalar1=-1.0, scalar2=None, op0=ALU.mult), [cb], [cb])
    NB = 2
    m4s = [k.sb([128, 1024], name="m4") for _ in range(NB)]; g4s = [k.sb([128, 1024], name="g4") for _ in range(NB)]
    mgs = [k.sb([128, 16], name="mg") for _ in range(NB)]; ggs = [k.sb([128, 16], name="gg") for _ in range(NB)]
    q5s = [k.sb([128, 5, 768], name="q5") for _ in range(NB)]
    cvs = [k.sb([128, 768], name="cv") for _ in range(NB)]; sqs = [k.sb([128, 512], name="sq") for _ in range(NB)]
    wks = [k.sb([128, 128], name="wk") for _ in range(NB)]
    outs = [[k.sb([128, W6], name="o6") for _ in range(4)] for _ in range(NB)]
    fmts = [[k.sb([64, 10, 128], name="fmt") for _ in range(4)] for _ in range(NB)]
    ksb = [[k.sb([128, 128], name="ksb") for _ in range(4)] for _ in range(NB)]
    Pt = P.rearrange("(n p) c -> n p c", p=128)
    segs = [(0, NCTX), (NCTX, T_)]
    def tile_body(t):
        i = t % NB
        psg = PS[i]
        m4 = m4s[i]; g4 = g4s[i]; mg = mgs[i]; gg = ggs[i]; q5 = q5s[i]; c = cvs[i]; s2 = sqs[i]; s = wks[i]
        r0 = t*128
        k.dma(m4, m4[:], P, P[r0:r0+128, 768:1792]); k.dma(g4, g4[:], P, P[r0:r0+128, 1808:2832], q="pool")
        k.dma(mg, mg[:], P, P[r0:r0+128, 1792:1808]); k.dma(gg, gg[:], P, P[r0:r0+128, 2832:2848], q="pool")
        seg = segs[0] if r0 < NCTX else segs[1]
        edge = (r0 - 2 < seg[0]) or (r0 + 130 > seg[1])
        if edge:
            k.op("pool", lambda e: e.memset(q5[:], 0.0), [], [q5])
        for kk in range(5):
            a0 = r0 + kk - 2; a1 = a0 + 128
            lo = max(a0, seg[0]); hi = min(a1, seg[1])
            k.dma(q5, q5[lo-a0:hi-a0, kk, :], P, P[lo:hi, 1808:2576], q="sp" if kk % 2 == 0 else "pool")
        k.op("dve", lambda e: e.tensor_mul(out=q5[:], in0=q5[:], in1=cw[:]), [q5, cw], [q5])
        k.op("dve", lambda e: e.reduce_sum(out=c[:], in_=q5[:].rearrange("p k c -> p c k"), axis=AX.X), [q5], [c])
        k.op("act", lambda e: e.activation(out=c[:], in_=c[:], func=AF.Silu), [c], [c])
        k.op("dve", lambda e: e.tensor_mul(out=s2[:], in0=c[:, 0:512], in1=c[:, 0:512]), [c], [s2])
        k.op("dve", lambda e: e.reduce_sum(out=s[:, 64:72], in_=s2[:].rearrange("p (h d) -> p h d", h=8), axis=AX.X), [s2], [s])
        k.op("dve", lambda e: e.tensor_scalar(out=s[:, 64:72], in0=s[:, 64:72], scalar1=EPS, scalar2=None, op0=ALU.add), [s], [s])
        k.op("act", lambda e: e.activation(out=s[:, 72:80], in_=s[:, 64:72], func=AF.Sqrt), [s], [s])
        k.op("dve", lambda e: e.reciprocal(out=s[:, 80:88], in_=s[:, 72:80]), [s], [s])
        k.op("dve", lambda e: e.tensor_add(out=s[:, 0:8], in0=mg[:, 8:16], in1=cb[:, 8:16]), [mg, cb], [s])
        k.op("act", lambda e: e.activation(out=s[:, 8:16], in_=s[:, 0:8], func=AF.Exp, scale=-1.0), [s], [s])
        k.op("act", lambda e: e.activation(out=s[:, 8:16], in_=s[:, 8:16], func=AF.Ln, bias=1.0, scale=1.0), [s], [s])
        k.op("dve", lambda e: e.tensor_scalar(out=s[:, 8:16], in0=s[:, 8:16], scalar1=-1.0, scalar2=None, op0=ALU.mult), [s], [s])
        k.op("pe", lambda e: e.matmul(psg[:, 0:4], tf, s[:, 8:12], start=True, stop=True), [cc, s], [psg])
        k.op("pe", lambda e: e.matmul(psg[:, 4:8], tr, s[:, 12:16], start=True, stop=True), [cc, s], [psg])
        k.op("pe", lambda e: e.matmul(psg[:, 8:16], blk, s[:, 8:16], start=True, stop=True), [cc, s], [psg])
        k.op("dve", lambda e: e.tensor_copy(s[:, 16:24], psg[:, 0:8]), [psg], [s])
        k.op("act", lambda e: e.activation(out=s[:, 48:56], in_=psg[:, 8:16], func=AF.Exp), [psg], [s])
        k.op("act", lambda e: e.activation(out=s[:, 40:48], in_=s[:, 16:24], func=AF.Exp), [s], [s])
        k.op("dve", lambda e: e.tensor_add(out=s[:, 24:32], in0=mg[:, 0:8], in1=cb[:, 0:8]), [mg, cb], [s])
        k.op("dve", lambda e: e.tensor_sub(out=s[:, 24:32], in0=s[:, 24:32], in1=s[:, 16:24]), [s], [s])
        k.op("act", lambda e: e.activation(out=s[:, 32:40], in_=s[:, 24:32], func=AF.Exp), [s], [s])
        k.op("act", lambda e: e.activation(out=s[:, 88:96], in_=gg[:, 8:16], func=AF.Sigmoid), [gg], [s])
        k.op("dve", lambda e: e.tensor_add(out=s[:, 96:104], in0=gg[:, 0:8], in1=cb[:, 24:32]), [gg, cb], [s])
        k.op("act", lambda e: e.activation(out=s[:, 96:104], in_=s[:, 96:104], func=AF.Exp), [s], [s])
        k.op("act", lambda e: e.activation(out=s[:, 96:104], in_=s[:, 96:104], func=AF.Ln, bias=1.0, scale=1.0), [s], [s])
        k.op("dve", lambda e: e.tensor_mul(out=s[:, 104:112], in0=s[:, 96:104], in1=cb[:, 32:40]), [s, cb], [s])
        k.op("pe", lambda e: e.matmul(psg[:, 16:24], tf, s[:, 104:112], start=True, stop=True), [cc, s], [psg])
        k.op("pe", lambda e: e.matmul(psg[:, 24:32], tr, s[:, 104:112], start=True, stop=True), [cc, s], [psg])
        k.op("pe", lambda e: e.matmul(psg[:, 32:40], blk, s[:, 104:112], start=True, stop=True), [cc, s], [psg])
        k.op("dve", lambda e: e.tensor_copy(s[:, 112:116], psg[:, 16:20]), [psg], [s])
        k.op("dve", lambda e: e.tensor_copy(s[:, 116:120], psg[:, 28:32]), [psg], [s])
        k.op("dve", lambda e: e.tensor_sub(out=s[:, 120:124], in0=psg[:, 24:28], in1=s[:, 104:108]), [psg, s], [s])
        k.op("dve", lambda e: e.tensor_sub(out=s[:, 124:128], in0=psg[:, 20:24], in1=s[:, 108:112]), [psg, s], [s])
        k.op("act", lambda e: e.activation(out=s[:, 120:128], in_=s[:, 120:128], func=AF.Exp), [s], [s])
        k.op("act", lambda e: e.activation(out=s[:, 56:64], in_=s[:, 112:120], func=AF.Exp), [s], [s])
        k.op("act", lambda e: e.activation(out=s2[:, 0:8], in_=psg[:, 32:40], func=AF.Exp), [psg], [s2])
        for h in range(4):
            o = outs[i][h]; fmt = fmts[i][h]; kst = ksb[i][h]
            hs = slice(h*64, (h+1)*64)
            O = lambda n: o[:, O6[n][0]:O6[n][1]]
            for d in range(2):
                j = d*4 + h
                KG0 = O6["KG0"][0] + d*130
                k.op("dve", lambda e: e.tensor_scalar(out=o[:, d*64:(d+1)*64], in0=m4[:, hs], scalar1=s[:, 40+j:41+j], scalar2=None, op0=ALU.mult), [m4, s], [o])
                k.op("pool", lambda e: e.tensor_scalar(out=kst[:, d*64:(d+1)*64], in0=m4[:, 256+h*64:256+(h+1)*64], scalar1=s[:, 32+j:33+j], scalar2=0.125, op0=ALU.mult, op1=ALU.mult), [m4, s], [kst])
                k.op("pool", lambda e: e.tensor_copy(o[:, KG0:KG0+64], kst[:, d*64:(d+1)*64]), [kst], [o])
                k.op("dve", lambda e: e.tensor_copy(o[:, KG0+128:KG0+129], s[:, 48+j:49+j]), [s], [o])
                k.op("dve", lambda e: e.tensor_copy(o[:, KG0+129:KG0+130], s2[:, j:j+1]), [s2], [o])
            k.op("act", lambda e: e.activation(out=O("sigo"), in_=m4[:, 768+h*64:768+(h+1)*64], func=AF.Sigmoid), [m4], [o])
            k.op("dve", lambda e: e.tensor_scalar(out=O("qn"), in0=c[:, hs], scalar1=s[:, 80+h:81+h], scalar2=0.125, op0=ALU.mult, op1=ALU.mult), [c, s], [o])
            k.op("dve", lambda e: e.tensor_scalar(out=O("kn"), in0=c[:, 256+h*64:256+(h+1)*64], scalar1=s[:, 84+h:85+h], scalar2=None, op0=ALU.mult), [c, s], [o])
            gv = c[:, 512+h*64:512+(h+1)*64]
            for d in range(2):
                j = d*4 + h
                KG0 = O6["KG0"][0] + d*130
                kb = o[:, 320+d*64:320+(d+1)*64]
                k.op("dve", lambda e: e.tensor_scalar(out=kb, in0=O("kn"), scalar1=s[:, 88+j:89+j], scalar2=None, op0=ALU.mult), [o, s], [o])
                k.op("pool", lambda e: e.tensor_scalar(out=o[:, 448+d*128:448+d*128+64], in0=gv, scalar1=s[:, 88+j:89+j], scalar2=None, op0=ALU.mult), [c, s], [o])
                k.op("dve", lambda e: e.tensor_scalar(out=o[:, 448+d*128+64:448+(d+1)*128], in0=kb, scalar1=s[:, 56+j:57+j], scalar2=None, op0=ALU.mult), [o, s], [o])
                k.op("pool", lambda e: e.tensor_scalar(out=o[:, 704+d*64:704+(d+1)*64], in0=O("qn"), scalar1=s[:, 56+j:57+j], scalar2=None, op0=ALU.mult), [o, s], [o])
                k.op("dve", lambda e: e.tensor_scalar(out=o[:, KG0+64:KG0+128], in0=O("kn"), scalar1=s[:, 120+j:121+j], scalar2=None, op0=ALU.mult), [o, s], [o])
                k.op("dve", lambda e: e.tensor_copy(o[:, 832+d:833+d], s[:, 112+j:113+j]), [s], [o])
            k.op("act", lambda e: e.activation(out=O("siluz"), in_=g4[:, 768+h*64:768+(h+1)*64], func=AF.Silu), [g4], [o])
            k.dma(O6D, O6D[h, r0:r0+128, :], o, o[:], skip_w=True)
            pA = PS[2 + i*3]; pB = PS[3 + i*3]; pC = PS[4 + i*3]
            for bi, src in enumerate(FM_SRC):
                if src is None:
                    src_ap = kst[:, (bi-6)*64:(bi-5)*64]; src_t = kst
                else:
                    src_ap = o[:, src[0]:src[1]]; src_t = o
                pp = (pA, pB, pC)[bi // 4]; off = (bi % 4)*128
                k.op("pe", lambda e: e.transpose(pp[0:64, off:off+128], src_ap, ident[:]), [src_t, ident], [pp])
            k.op("act", lambda e: e.copy(fmt[:, 0:4, :], pA[0:64, :].rearrange("p (c t) -> p c t", c=4)), [pA], [fmt])
            k.op("dve", lambda e: e.tensor_copy(fmt[:, 4:8, :], pB[0:64, :].rearrange("p (c t) -> p c t", c=4)), [pB], [fmt])
            k.op("act", lambda e: e.copy(fmt[:, 8:10, :], pC[0:64, 0:256].rearrange("p (c t) -> p c t", c=2)), [pC], [fmt])
            k.dma(FMD, FMD[h, t], fmt, fmt[:], q="pool", skip_w=True)
    for t0 in range(0, NT_, NB):
        k.interleave([(lambda t=t: tile_body(t)) for t in range(t0, min(NT_, t0 + NB))])
    k.stage_end()


def e_intra(k, PS, O6D, FMD, cc, O7D, O7T):
    k.stage_begin()
    ident, ones, trif, trir, trifs, trirs, ntrifs, ntrirs = [cc[:, i, :] for i in range(8)]
    class C: pass
    ch = []
    for c in range(4):
        o = C()
        o.P = [k.sb([128, 128], name="P") for _ in range(2)]; o.PT = [k.sb([128, 128], name="PT") for _ in range(2)]
        o.Y = [k.sb([128, 128], name="Y") for _ in range(2)]
        o.M1 = k.sb([128, 128], name="M1"); o.M2 = k.sb([128, 128], name="M2"); o.E2 = k.sb([128, 128], name="E2"); o.dg = k.sb([128, 128], name="dg")
        o.out = k.sb([128, 3, 128], name="out"); o.wt = k.sb([64, 128], name="wt")
        o.ps = [PS[c*2], PS[c*2+1]]
        ch.append(o)
    fms = [[k.sb([64, 8, 128], name="fm") for _ in range(2)] for _ in range(2)]
    tms = [[k.sb([128, 258], name="tm") for _ in range(2)] for _ in range(2)]
    for h in range(4):
      for tp in range(NT_ // 2):
        chains = []
        for j in range(2):
            t = tp*2 + j
            fm = fms[j][tp % 2]; tm = tms[j][tp % 2]
            k.dma(fm, fm[:], FMD, FMD[h, t, :, 0:8, :])
            k.dma(tm, tm[:, 0:256], O6D, O6D[h, t*128:(t+1)*128, 448:704], q="pool")
            k.dma(tm, tm[:, 256:258], O6D, O6D[h, t*128:(t+1)*128, 832:834], q="pool")
            for d in range(2):
                chains.append((ch[j*2+d], t, d, fm, tm))
        for (o, t, d, fm, tm) in chains:
            m1, m2, m3 = (ntrirs, ntrifs, trif) if d == 0 else (ntrifs, ntrirs, trir)
            gcol = tm[:, 256+d:257+d]
            k.op("dve", lambda e: e.tensor_scalar(out=o.dg[:], in0=ident, scalar1=gcol, scalar2=None, op0=ALU.mult), [cc, tm], [o.dg])
            k.op("pe", lambda e: e.matmul(o.ps[0][:, 0:128], ones, o.dg[:], start=True, stop=True), [cc, o.dg], [o.ps[0]])
            k.op("dve", lambda e: e.tensor_scalar(out=o.E2[:], in0=o.ps[0][:, 0:128], scalar1=gcol, scalar2=0.0, op0=ALU.subtract, op1=ALU.min), [o.ps[0], tm], [o.E2])
            k.op("dve", lambda e: e.tensor_scalar(out=o.M1[:], in0=o.ps[0][:, 0:128], scalar1=gcol, scalar2=0.0, op0=ALU.subtract, op1=ALU.max), [o.ps[0], tm], [o.M1])
            k.op("act", lambda e: e.activation(out=o.E2[:], in_=o.E2[:], func=AF.Exp), [o.E2], [o.E2])
            k.op("act", lambda e: e.activation(out=o.M1[:], in_=o.M1[:], func=AF.Exp, scale=-1.0), [o.M1], [o.M1])
            k.op("pool", lambda e: e.tensor_mul(out=o.M1[:], in0=o.M1[:], in1=m1), [o.M1, cc], [o.M1])
            k.op("pool", lambda e: e.tensor_mul(out=o.M2[:], in0=o.E2[:], in1=m2), [o.E2, cc], [o.M2])
            k.op("pool", lambda e: e.tensor_mul(out=o.E2[:], in0=o.E2[:], in1=m3), [o.E2, cc], [o.E2])
        for (o, t, d, fm, tm) in chains:
            mA = trif if d == 0 else trir
            knT = fm[:, 0, :]; qnT = fm[:, 1, :]; kbT = fm[:, 2+d, :]; qsT = fm[:, 4+d, :]; ksT = fm[:, 6+d, :]
            p0 = V(o.ps[0], 0, 128); p1 = V(o.ps[1], 0, 128)
            k.op("pe", lambda e: e.matmul(p0[:], kbT, knT, start=True, stop=True), [fm], [p0])
            k.op("pe", lambda e: e.matmul(p1[:], knT, kbT, start=True, stop=True), [fm], [p1])
            k.op("dve", lambda e: e.tensor_mul(out=o.P[0][:], in0=p0[:], in1=o.M1[:]), [p0, o.M1], [o.P[0]])
            k.op("dve", lambda e: e.tensor_mul(out=o.PT[0][:], in0=p1[:], in1=o.M2[:]), [p1, o.M2], [o.PT[0]])
            k.op("pe", lambda e: e.matmul(p0[:], knT, qnT, start=True, stop=True), [fm], [p0])
            k.op("pe", lambda e: e.matmul(p1[:], ksT, qsT, start=True, stop=True), [fm], [p1])
            k.op("dve", lambda e: e.tensor_mul(out=o.out[:, 1, :], in0=p0[:], in1=o.E2[:]), [p0, o.E2], [o.out])
            k.op("dve", lambda e: e.tensor_mul(out=o.out[:, 2, :], in0=p1[:], in1=mA), [p1, cc], [o.out])
            k.op("act", lambda e: e.copy(o.Y[0][:], tm[:, d*128:(d+1)*128]), [tm], [o.Y[0]])
        for m in range(6):
            a = m % 2; b = 1 - a
            for (o, t, d, fm, tm) in chains:
                p0 = V(o.ps[0], 0, 128); p1 = V(o.ps[1], 0, 128)
                k.op("pe", lambda e: e.matmul(p0[:], o.PT[a][:], o.Y[a][:], start=True, stop=True), [o.PT[a], o.Y[a]], [p0])
                dst = o.Y[b][:] if m < 5 else o.out[:, 0, :]
                dstT = o.Y[b] if m < 5 else o.out
                k.op("dve", lambda e: e.tensor_add(out=dst, in0=p0[:], in1=o.Y[a][:]), [p0, o.Y[a]], [dstT])
                if m < 5:
                    k.op("pe", lambda e: e.matmul(p1[:], o.P[a][:], o.PT[a][:], start=True, stop=True), [o.P[a], o.PT[a]], [p1])
                    k.op("act", lambda e: e.copy(o.PT[b][:], p1[:]), [p1], [o.PT[b]])
            if m < 4:
                for (o, t, d, fm, tm) in chains:
                    p0 = V(o.ps[0], 0, 128)
                    k.op("pe", lambda e: e.matmul(p0[:], o.PT[a][:], o.P[a][:], start=True, stop=True), [o.PT[a], o.P[a]], [p0])
                    k.op("act", lambda e: e.copy(o.P[b][:], p0[:]), [p0], [o.P[b]])
        for (o, t, d, fm, tm) in chains:
            k.dma(O7D, O7D[h, t*128:(t+1)*128, d, :, :], o.out, o.out[:], q="sp" if d == 0 else "pool", skip_w=True)
            p1 = V(o.ps[1], 0, 128)
            k.op("pe", lambda e: e.transpose(p1[0:64, :], o.out[:, 0, 64:128], ident), [o.out, cc], [p1])
            k.op("act", lambda e: e.copy(o.wt[:], p1[0:64, :]), [p1], [o.wt])
            k.dma(O7T, O7T[h, d, :, t*128:(t+1)*128], o.wt, o.wt[:], q="pool", skip_w=True)
    k.stage_end()


def e_stepglue(k, P, O6D, FMD, O7D, O7T, STEP):
    k.stage_begin()
    qi = 0
    def cp(dst, src, src_t):
        nonlocal qi
        k.dma(STEP, dst, src_t, src, q="sp" if qi % 2 == 0 else "pool", skip_w=True); qi += 1
    for h in range(4):
        S = STEP[h]
        S2 = S.rearrange("p (t two) d w -> p t two d w", two=2)
        o6c = O6D[h].rearrange("(c p) w -> p c w", p=64)
        o7c = O7D[h].rearrange("(c p) d j w -> p c d j w", p=64)
        o7t2 = O7D[h].rearrange("(t two p) d j w -> p t two d j w", two=2, p=64)
        pc = P.rearrange("(c p) w -> p c w", p=64)
        for d in range(2):
            for (blk_i, col) in ((4+d, 0), (8+d, 128)):
                for two in range(2):
                    cp(S2[:, :, two, d, col:col+64], FMD[h][:, :, blk_i, two*64:(two+1)*64].rearrange("t f k -> f t k"), FMD)
            cp(S[:, :, d, 64:128], O7T[h, d].rearrange("f (c k) -> f c k", k=64), O7T)
            kg0 = O6["KG0"][0] + d*130
            cp(S[:, :, d, 192:322], o6c[:, :, kg0:kg0+130], O6D)
            cp(S[:, :, d, 322:386], pc[:, :, 1280 + h*64:1280 + (h+1)*64], P)
            for two in range(2):
                cp(S2[:, :, two, d, 387:451], o7t2[:, :, two, d, 2, two*64:(two+1)*64], O7D)
                cp(S2[:, :, two, d, 515:579], o7t2[:, :, two, d, 1, two*64:(two+1)*64], O7D)
            cp(S[:, :, d, 451:515], o7c[:, :, d, 0, 0:64], O7D)
    k.stage_end()


def e_scan(k, PS, STEP, O8D):
    k.stage_begin()
    BS = 4; nblk = NC_ // BS
    R = []
    for s_ in range(2):
        r = dict(bufs=[[k.sb([64, BS, W8], name="blk") for _ in range(2)] for _ in range(2)],
                 obufs=[[k.sb([64, BS, 128], name="ob") for _ in range(2)] for _ in range(2)],
                 C1=[k.sb([64, 65], name="C1") for _ in range(2)], Sg=[k.sb([64, 64], name="S") for _ in range(2)],
                 tmpc=[k.sb([64, 65], name="tc") for _ in range(2)], vnew=[k.sb([64, 64], name="vn") for _ in range(2)],
                 rr=[k.sb([64, 4], name="rr") for _ in range(2)])
        psA = [PS[4*s_], PS[4*s_+1]]; psB = [PS[4*s_+2], PS[4*s_+3]]
        r["pso"] = [V(psA[d], 0, 65) for d in range(2)]; r["psc"] = [V(psA[d], 128, 193) for d in range(2)]
        r["psw"] = [V(psB[d], 0, 64) for d in range(2)]; r["pss"] = [V(psB[d], 128, 192) for d in range(2)]; r["psg"] = [V(psB[d], 256, 320) for d in range(2)]
        R.append(r)

    def head_body(h, r):
        bufs = r["bufs"]; obufs = r["obufs"]; C1 = r["C1"]; Sg = r["Sg"]; tmpc = r["tmpc"]; vnew = r["vnew"]; rr = r["rr"]
        pso = r["pso"]; psc = r["psc"]; psw = r["psw"]; pss = r["pss"]; psg = r["psg"]
        for d in range(2):
            k.op("dve", lambda e: e.memset(C1[d][:], 0.0), [], [C1[d]])
            k.op("dve", lambda e: e.memset(Sg[d][:], 0.0), [], [Sg[d]])
        for b in range(nblk):
            mb = [b, 0 if b == 0 else nblk - b]
            B = [bufs[d][b % 2] for d in range(2)]; OB = [obufs[d][b % 2] for d in range(2)]
            for d in range(2):
                k.op("pool", lambda e: e.memset(B[d][:, :, 386:387], 1.0), [], [B[d]])
                k.dma(B[d], B[d][:, :, 0:386], STEP, STEP[h, :, mb[d]*BS:(mb[d]+1)*BS, d, 0:386], q="sp" if d == 0 else "pool")
                k.dma(B[d], B[d][:, :, 387:579], STEP, STEP[h, :, mb[d]*BS:(mb[d]+1)*BS, d, 387:579], q="sp" if d == 0 else "pool")
            for jj in range(BS):
                sl = [jj, BS - 1 - jj]
                F = lambda d, n: B[d][:, sl[d], S8[n][0]:S8[n][1]]
                R2 = range(2)
                for d in R2:
                    k.op("pe", lambda e: e.matmul(psw[d][:64], F(d, "WT"), Sg[d][:], start=True, stop=True), [B[d], Sg[d]], [psw[d]])
                for d in R2:
                    k.op("pe", lambda e: e.matmul(pso[d][:64], F(d, "QsT"), C1[d][:], start=True, stop=False), [B[d], C1[d]], [pso[d]])
                    k.op("pe", lambda e: e.matmul(pso[d][:64], F(d, "AT"), F(d, "V1"), start=False, stop=True), [B[d]], [pso[d]])
                    k.op("pe", lambda e: e.matmul(psc[d][:64], F(d, "Ks"), F(d, "V1"), start=True, stop=True), [B[d]], [psc[d]])
                for d in R2:
                    k.op("dve", lambda e: e.tensor_sub(out=vnew[d][:], in0=F(d, "U"), in1=psw[d][:64]), [B[d], psw[d]], [vnew[d]])
                for d in R2:
                    k.op("pe", lambda e: e.matmul(psg[d][:64], F(d, "QgT"), Sg[d][:], start=True, stop=False), [B[d], Sg[d]], [psg[d]])
                    k.op("pe", lambda e: e.matmul(psg[d][:64], F(d, "AqkT"), vnew[d][:], start=False, stop=True), [B[d], vnew[d]], [psg[d]])
                    k.op("pe", lambda e: e.matmul(pss[d][:64], F(d, "Kd"), vnew[d][:], start=True, stop=True), [B[d], vnew[d]], [pss[d]])
                for d in R2:
                    k.op("dve", lambda e: e.tensor_add(out=tmpc[d][:], in0=psc[d][:64], in1=C1[d][:]), [psc[d], C1[d]], [tmpc[d]])
                    k.op("dve", lambda e: e.tensor_scalar(out=C1[d][:], in0=tmpc[d][:], scalar1=F(d, "sm"), scalar2=None, op0=ALU.mult), [tmpc[d], B[d]], [C1[d]])
                    k.op("dve", lambda e: e.scalar_tensor_tensor(out=Sg[d][:], in0=Sg[d][:], scalar=F(d, "sg"), in1=pss[d][:64], op0=ALU.mult, op1=ALU.add), [Sg[d], B[d], pss[d]], [Sg[d]])
                for d in R2:
                    k.op("act", lambda e: e.activation(out=rr[d][:, 0:1], in_=pso[d][:64, 64:65], func=AF.Abs), [pso[d]], [rr[d]])
                    k.op("dve", lambda e: e.tensor_scalar(out=rr[d][:, 2:3], in0=rr[d][:, 0:1], scalar1=1.0, scalar2=None, op0=ALU.max), [rr[d]], [rr[d]])
                    k.op("dve", lambda e: e.reciprocal(out=rr[d][:, 1:2], in_=rr[d][:, 2:3]), [rr[d]], [rr[d]])
                    k.op("act", lambda e: e.activation(out=OB[d][:, sl[d], 0:64], in_=pso[d][:64, 0:64], func=AF.Copy, scale=rr[d][:, 1:2]), [pso[d], rr[d]], [OB[d]])
                    k.op("act", lambda e: e.copy(OB[d][:, sl[d], 64:128], psg[d][:64]), [psg[d]], [OB[d]])
            for d in range(2):
                k.dma(O8D, O8D[h, :, mb[d]*BS:(mb[d]+1)*BS, d, :], OB[d], OB[d][:], q="sp", skip_w=True)

    for h0 in range(0, 4, 2):
        k.interleave([(lambda h=h0: head_body(h, R[0])), (lambda h=h0 + 1: head_body(h, R[1]))])
    k.stage_end()


def e_outproj(k, PS, AXD, O8D, O6D, GAIN2, X, modrow, sel, ident, W, XMID):
    k.stage_begin()
    gn = k.sb([128, 512], name="gn"); w = k.sb([128, 8, 1024], BF16, name="w")
    gate = [k.sb([128, 1024], name="gate") for _ in range(2)]
    k.dma(gn, gn[:], None, GAIN2[:, :])
    for cls in range(2):
        bcast_rows(k, PS, gate[cls], V(modrow, 2*1024, 3*1024), 1 if cls == 0 else 0, sel)
    for c in range(8):
        k.dma(w, w[:, c, :], None, W[c*128:(c+1)*128, :], q="pool")
    B = [dict(mix=k.sb([128, 1024], name="mix"), a=k.sb([128, 512], name="a"), b=k.sb([128, 512], name="b"), g=k.sb([128, 512], name="g"),
              sq=k.sb([128, 512], name="sq"), s=k.sb([128, 32], name="s"), x=k.sb([128, 1024], name="x"), mT=k.sb([128, 8, 128], BF16, name="mT"),
              o=k.sb([128, 1024], name="o")) for _ in range(2)]
    pi = 0
    def tile_body(t):
        nonlocal pi
        cls = 0 if t < 2 else 1
        b = B[t % 2]; mix = b['mix']; a = b['a']; bb = b['b']; g = b['g']; s = b['s']; x = b['x']; mT = b['mT']; o = b['o']
        rows = slice(t*128, (t+1)*128)
        k.dma(mix, mix[:, 0:512], AXD, AXD[rows, :])
        k.dma(x, x[:], X, X[rows, :])
        qi = 0
        for h in range(4):
            k.dma(g, g[:, h*64:(h+1)*64], O6D, O6D[h, rows, 128:192], q="pool")
            k.dma(g, g[:, 256+h*64:256+(h+1)*64], O6D, O6D[h, rows, 834:898], q="pool")
            for d in range(2):
                dst_t = a if d == 0 else bb
                for half in range(2):
                    dst = dst_t[half*64:(half+1)*64, :].rearrange("p (g hh w) -> p g hh w", g=2, hh=4)[:, :, h, :]
                    src = O8D[h, :, 2*t + half, d, :].rearrange("p (g w) -> p g w", g=2)
                    k.dma(dst_t, dst, O8D, src, q="sp" if qi % 2 == 0 else "pool"); qi += 1
        k.op("dve", lambda e: e.tensor_add(out=a[:], in0=a[:], in1=bb[:]), [a, bb], [a])
        k.op("dve", lambda e: e.tensor_mul(out=b['sq'][:], in0=a[:], in1=a[:]), [a], [b['sq']])
        k.op("dve", lambda e: e.reduce_sum(out=s[:, 0:8], in_=b['sq'][:].rearrange("p (h d) -> p h d", h=8), axis=AX.X), [b['sq']], [s])
        k.op("dve", lambda e: e.tensor_scalar(out=s[:, 0:8], in0=s[:, 0:8], scalar1=1.0/64, scalar2=EPS, op0=ALU.mult, op1=ALU.add), [s], [s])
        k.op("act", lambda e: e.activation(out=s[:, 8:16], in_=s[:, 0:8], func=AF.Sqrt), [s], [s])
        k.op("dve", lambda e: e.reciprocal(out=s[:, 16:24], in_=s[:, 8:16]), [s], [s])
        for h in range(8):
            k.op("dve" if h % 2 == 0 else "pool", lambda e: e.tensor_scalar(out=a[:, h*64:(h+1)*64], in0=a[:, h*64:(h+1)*64], scalar1=s[:, 16+h:17+h], scalar2=None, op0=ALU.mult), [a, s], [a])
        k.op("dve", lambda e: e.tensor_mul(out=a[:], in0=a[:], in1=gn[:]), [a, gn], [a])
        k.op("dve", lambda e: e.tensor_mul(out=mix[:, 512:1024], in0=a[:], in1=g[:]), [a, g], [mix])
        for half in range(2):
            pt = PS[t % 2]
            for c in range(4):
                cc_ = half*4 + c
                k.op("pe", lambda e: e.transpose(pt[:, c*128:(c+1)*128], mix[:, cc_*128:(cc_+1)*128], ident[:]), [mix, ident], [pt])
            if half == 0:
                k.op("act", lambda e: e.copy(mT[:, 0:4, :], pt[:].rearrange("p (c t) -> p c t", c=4)), [pt], [mT])
            else:
                k.op("dve", lambda e: e.tensor_copy(mT[:, 4:8, :], pt[:].rearrange("p (c t) -> p c t", c=4)), [pt], [mT])
        for nb in range(2):
            ps = PS[2 + (t % 2) + 2*nb]
            for c in range(8):
                k.op("pe", lambda e: e.matmul(ps[:], mT[:, c, :], w[:, c, nb*512:(nb+1)*512], start=(c == 0), stop=(c == 7)), [mT, w], [ps])
            k.op("dve", lambda e: e.tensor_mul(out=o[:, nb*512:(nb+1)*512], in0=ps[:], in1=gate[cls][:, nb*512:(nb+1)*512]), [ps, gate[cls]], [o])
        k.op("pool", lambda e: e.tensor_add(out=o[:], in0=o[:], in1=x[:]), [o, x], [o])
        k.dma(XMID, XMID[rows, :], o, o[:], skip_w=True)
    for t0 in range(0, NT_, 2):
        k.interleave([(lambda t=t: tile_body(t)) for t in range(t0, min(NT_, t0 + 2))])
    k.stage_end()


def e_topk(k, AFFT, VALS, IDX):
    k.stage_begin()
    for (c0, n, kk, o0, nm) in [(NCTX, T_ - NCTX, 1024, 0, "l"), (0, NCTX, 32, 1024, "c")]:
        a = k.sb([16, n], name="a" + nm); wk = k.sb([16, n], name="w" + nm)
        vals = k.sb([16, kk], name="v" + nm); idx = k.sb([16, kk], U32, name="i" + nm)
        k.dma(a, a[:], AFFT, AFFT[:, c0:c0+n])
        k.op("dve", lambda e: e.tensor_copy(wk[:], a[:]), [a], [wk])
        for r in range(kk // 8):
            k.op("dve", lambda e: e.max(out=vals[:, r*8:(r+1)*8], in_=wk[:]), [wk], [vals])
            k.op("dve", lambda e: e.max_index(out=idx[:, r*8:(r+1)*8], in_max=vals[:, r*8:(r+1)*8], in_values=wk[:]), [vals, wk], [idx])
            k.op("dve", lambda e: e.match_replace(out=wk[:], in_to_replace=vals[:, r*8:(r+1)*8], in_values=wk[:], imm_value=-1.0), [vals, wk], [wk])
        k.dma(VALS, VALS[:, o0:o0+kk], vals, vals[:], skip_w=True); k.dma(IDX, IDX[:, o0:o0+kk], idx, idx[:], skip_w=True)
    k.stage_end()


def e_expert(k, PS, H2, VALS, IDX, W1, W3, W2, ident, ACC):
    k.stage_begin()
    z = k.sb([128, 1024], name="z")
    k.op("dve", lambda e: e.memset(z[:], 0.0), [], [z])
    for t in range(NT_):
        k.dma(ACC, ACC[t*128:(t+1)*128, :], z, z[:], q="sp" if t % 2 == 0 else "pool", skip_w=True)
    RT = 9
    parts = [(0, 9)]
    PR = 9 * 128
    w2 = k.sb([128, 16, 1024], BF16, name="w2"); vl = k.sb([128, RT], name="vl"); ix = k.sb([128, RT], U32, name="ix")
    hid = k.sb([128, 16, PR], BF16, name="hid"); xt = k.sb([128, 8, PR], BF16, name="xt")
    xg = [k.sb([128, 1024], name="xg") for _ in range(2)]
    w1c = [k.sb([128, 8, 128], BF16, name="w1c") for _ in range(3)]; w3c = [k.sb([128, 8, 128], BF16, name="w3c") for _ in range(3)]
    sg = [k.sb([128, 512], name="sg") for _ in range(2)]
    ysb = [k.sb([128, 1024], name="ysb") for _ in range(2)]
    ph1 = [PS[0], PS[1]]; ph3 = [PS[2], PS[3]]; py = [PS[4], PS[5], PS[6], PS[7]]
    ci = 0; hi = 0; yi = 0; gi = 0
    first_scatter = True
    for e_ in range(16):
        k.dma(w2, w2[:], None, W2[e_].rearrange("(c p) n -> p c n", p=128), q="pool")
        k.op("dve", lambda e: e.memset(vl[:], 0.0), [], [vl])
        k.op("pool", lambda e: e.memset(ix[:], 0), [], [ix])
        k.dma(vl, vl[:, 0:8], VALS, VALS[e_, 0:1024].rearrange("(t p) -> p t", p=128), allow_slow_non_contiguous=True)
        k.dma(vl, vl[0:32, 8:9], VALS, VALS[e_, 1024:1056].rearrange("(t p) -> p t", p=32), allow_slow_non_contiguous=True)
        k.dma(ix, ix[:, 0:8], IDX, IDX[e_, 0:1024].rearrange("(t p) -> p t", p=128), allow_slow_non_contiguous=True)
        k.dma(ix, ix[0:32, 8:9], IDX, IDX[e_, 1024:1056].rearrange("(t p) -> p t", p=32), allow_slow_non_contiguous=True)
        for (t0, t1) in parts:
            nr = (t1 - t0) * 128
            for tt in range(t0, t1):
                g_ = xg[gi % 2]; gi += 1
                k.dma(g_, g_[:], H2, H2[:, :], q="pool", element_offset=(NCTX*1024 if tt < 8 else 0),
                      indirect=dict(in_offset=bass.IndirectOffsetOnAxis(ap=ix[:, tt:tt+1], axis=0), idx_t=ix))
                lr = (tt - t0) * 128
                for half in range(2):
                    pt = py[half]
                    for c in range(4):
                        cc_ = half*4 + c
                        k.op("pe", lambda e: e.transpose(pt[:, c*128:(c+1)*128], g_[:, cc_*128:(cc_+1)*128], ident[:]), [g_, ident], [pt])
                    k.op("act" if half == 0 else "dve", (lambda e: e.copy(xt[:, 0:4, lr:lr+128], pt[:].rearrange("p (c t) -> p c t", c=4))) if half == 0 else
                         (lambda e: e.tensor_copy(xt[:, 4:8, lr:lr+128], pt[:].rearrange("p (c t) -> p c t", c=4))), [pt], [xt])
            for fc in range(16):
                a1 = w1c[ci % 3]; a3 = w3c[ci % 3]; ci += 1
                k.dma(a1, a1[:], None, W1[e_].rearrange("(c p) f -> p c f", p=128)[:, :, fc*128:(fc+1)*128], q="pool")
                k.dma(a3, a3[:], None, W3[e_].rearrange("(c p) f -> p c f", p=128)[:, :, fc*128:(fc+1)*128], q="pool")
                for rb in range(0, nr, 512):
                    rn = min(512, nr - rb)
                    p1 = ph1[hi % 2]; p3 = ph3[hi % 2]; s_ = sg[hi % 2]; hi += 1
                    for c in range(8):
                        k.op("pe", lambda e: e.matmul(p1[:, 0:rn], a1[:, c, :], xt[:, c, rb:rb+rn], start=(c == 0), stop=(c == 7)), [a1, xt], [p1])
                    for c in range(8):
                        k.op("pe", lambda e: e.matmul(p3[:, 0:rn], a3[:, c, :], xt[:, c, rb:rb+rn], start=(c == 0), stop=(c == 7)), [a3, xt], [p3])
                    k.op("act", lambda e: e.activation(out=s_[:, 0:rn], in_=p1[:, 0:rn], func=AF.Silu), [p1], [s_])
                    k.op("dve", lambda e: e.tensor_mul(out=hid[:, fc, rb:rb+rn], in0=s_[:, 0:rn], in1=p3[:, 0:rn]), [s_, p3], [hid])
            for tt in range(t0, t1):
                ys = ysb[yi % 2]; yi += 1
                lr = (tt - t0) * 128
                for nb in range(2):
                    pp = py[(yi*2 + nb) % 4]
                    for fc in range(16):
                        k.op("pe", lambda e: e.matmul(pp[:], hid[:, fc, lr:lr+128], w2[:, fc, nb*512:(nb+1)*512], start=(fc == 0), stop=(fc == 15)), [hid, w2], [pp])
                    if nb == 0:
                        k.op("dve", lambda e: e.tensor_scalar(out=ys[:, 0:512], in0=pp[:], scalar1=vl[:, tt:tt+1], scalar2=None, op0=ALU.mult), [pp, vl], [ys])
                    else:
                        k.op("act", lambda e: e.activation(out=ys[:, 512:1024], in_=pp[:], func=AF.Copy, scale=vl[:, tt:tt+1]), [pp, vl], [ys])
                if tt < 8:
                    k.dma(ACC, ACC[:, :], ys, ys[:], q="pool", compute_op=ALU.add, element_offset=NCTX*1024,
                          indirect=dict(out_offset=bass.IndirectOffsetOnAxis(ap=ix[:, tt:tt+1], axis=0), idx_t=ix))
                else:
                    k.dma(ACC, ACC[:, :], ys, ys[0:32, :], q="pool", compute_op=ALU.add, element_offset=0,
                          indirect=dict(out_offset=bass.IndirectOffsetOnAxis(ap=ix[0:32, tt:tt+1], axis=0), idx_t=ix))
    k.stage_end()


def e_combine(k, PS, XMID, ACC, modrow, sel, OUTT, out_rows0):
    k.stage_begin()
    gate = [k.sb([128, 1024], name="gate") for _ in range(2)]
    for cls in range(2):
        bcast_rows(k, PS, gate[cls], V(modrow, 5*1024, 6*1024), 1 if cls == 0 else 0, sel)
    xs = [k.sb([128, 1024], name="x") for _ in range(2)]; ac = [k.sb([128, 1024], name="ac") for _ in range(2)]
    for t in range(out_rows0 // 128, NT_):
        cls = 0 if t < 2 else 1
        x = xs[t % 2]; a = ac[t % 2]; rows = slice(t*128, (t+1)*128)
        k.dma(x, x[:], XMID, XMID[rows, :]); k.dma(a, a[:], ACC, ACC[rows, :], q="pool")
        k.op("dve", lambda e: e.tensor_mul(out=a[:], in0=a[:], in1=gate[cls][:]), [a, gate[cls]], [a])
        k.op("pool", lambda e: e.tensor_add(out=x[:], in0=x[:], in1=a[:]), [x, a], [x])
        k.dma(OUTT, OUTT[t*128 - out_rows0:(t+1)*128 - out_rows0, :], x, x[:], skip_w=True)
    k.stage_end()


def build_fused(nlayer=2, upto=None):
    k = K()
    TT = T_
    def IN(name, shape, dt=F32):
        t = T(k, k.dram_in(name, shape, dt), name); t.is_dram = True
        return t
    XCAT = IN("xcat", [TT, 1024]); CT = IN("ct", [128, 8, 2]); IDENT = IN("ident", [128, 128]); SEL = IN("sel", [2, 2, 128])
    CS = IN("cs", [TT, 320]); SN = IN("sn", [TT, 320]); CC = IN("cc", [9, 128, 128])
    L = []
    for l in range(nlayer):
        L.append(dict(MODW=IN(f"modw{l}", [1024, 6144]), MODB=IN(f"modb{l}", [2, 6144]), N1G=IN(f"n1g{l}", [128, 1024]), WIN=IN(f"win{l}", [1024, 2848]),
                      QKG=IN(f"qkg{l}", [128, 640]), CB=IN(f"cb{l}", [128, 32]), CW=IN(f"cw{l}", [128, 5, 768]), GAIN2=IN(f"gain2{l}", [128, 512]),
                      WOUT=IN(f"wout{l}", [1024, 1024]), N2G=IN(f"n2g{l}", [128, 1024]), RW=IN(f"rw{l}", [1024, 16]),
                      W1=IN(f"w1{l}", [16, 1024, 2048]), W3=IN(f"w3{l}", [16, 1024, 2048]), W2=IN(f"w2{l}", [16, 2048, 1024])))
    OUT = k.dram_out("out", [TT - NCTX, 1024])
    PS = [k.ps([128, 512], name=f"bank{i}") for i in range(8)]
    ident = k.sb([128, 128], name="ident"); sel = k.sb([2, 2, 128], name="sel"); ctsb = k.sb([128, 8, 2], name="ct")
    cc = k.sb([128, 9, 128], name="cc")
    modrow = k.sb([2, 6144], name="modrow")
    k.dma(ident, ident[:], None, IDENT[:, :]); k.dma(sel, sel[:], None, SEL[:, :, :]); k.dma(ctsb, ctsb[:], None, CT[:, :, :])
    for i in range(9):
        k.dma(cc, cc[:, i, :], None, CC[i])
    k.op("act", lambda e: e.activation(out=ctsb[:], in_=ctsb[:], func=AF.Silu), [ctsb], [ctsb])
    D = lambda n, s, dt=F32: k.dram_tmp(n, s, dt)
    P = D("P", [TT, 2848]); QKT = D("QKT", [640, TT], BF16); AXD = D("AXD", [TT, 512]); O6D = D("O6D", [4, TT, W6]); FMD = D("FMD", [4, NT_, 64, 10, 128])
    O7D = D("O7D", [4, TT, 2, 3, 128]); O7T = D("O7T", [4, 2, 64, TT]); STEP = D("STEP", [4, 64, NC_, 2, W8]); O8D = D("O8D", [4, 64, NC_, 2, 128])
    XMID = D("XMID", [TT, 1024]); H2 = D("H2", [TT, 1024]); AFFT = D("AFFT", [16, TT]); VALS = D("VALS", [16, 1152]); IDX = D("IDX", [16, 1152], U32)
    ACC = D("ACC", [TT, 1024]); XN = D("XN", [TT, 1024])
    X = XCAT
    for l in range(nlayer):
        W = L[l]
        e_mod(k, PS, ctsb, W["MODW"], W["MODB"], modrow)
        e_normlin(k, PS, X, W["N1G"], modrow, 1, 0, sel, ident, W["WIN"], 2848, P, QKT=QKT, qk=dict(GAIN=W["QKG"], CS=CS, SN=SN))
        e_attn(k, PS, QKT, P, AXD)
        e_prep(k, PS, P, W["CB"], W["CW"], cc, ident, O6D, FMD)
        e_intra(k, PS, O6D, FMD, cc, O7D, O7T)
        e_stepglue(k, P, O6D, FMD, O7D, O7T, STEP)
        e_scan(k, PS, STEP, O8D)
        e_outproj(k, PS, AXD, O8D, O6D, W["GAIN2"], X, modrow, sel, ident, W["WOUT"], XMID)
        if upto == "xmid":
            k.stage_begin(); xx = [k.sb([128, 1024], name="cpy") for _ in range(2)]
            for t in range(2, NT_):
                k.dma(xx[t % 2], xx[t % 2][:], XMID, XMID[t*128:(t+1)*128, :]); k.dma(OUT, OUT[(t-2)*128:(t-1)*128, :], xx[t % 2], xx[t % 2][:], skip_w=True)
            k.stage_end(); break
        e_normlin(k, PS, XMID, W["N2G"], modrow, 4, 3, sel, ident, W["RW"], 16, None, H=H2, softmax=True, YT=AFFT)
        e_topk(k, AFFT, VALS, IDX)
        e_expert(k, PS, H2, VALS, IDX, W["W1"], W["W3"], W["W2"], ident, ACC)
        last = (l == nlayer - 1)
        e_combine(k, PS, XMID, ACC, modrow, sel, OUT if last else XN, NCTX if last else 0)
        X = XN
    k.finish([OUT])
    k.close()
    return k

def _rep(v, p=128):
    return np.ascontiguousarray(np.broadcast_to(np.asarray(v, np.float32)[None], (p,) + tuple(np.shape(v))))

def _consts():
    f32 = np.float32
    n = 8192
    row = (np.arange(n) // 64).astype(f32); col = (np.arange(n) % 64).astype(f32)
    inv = (10000.0 ** (-np.arange(16, dtype=f32) / 16)).astype(f32)
    ang = np.stack([row, col], -1)[..., None] * inv
    tile = lambda a: np.ascontiguousarray(np.broadcast_to(a[:, None], (a.shape[0], 10, 2, 16))).reshape(a.shape[0], 320)
    cs = np.concatenate([np.ones((256, 320), f32), tile(np.cos(ang).astype(f32))]); sn = np.concatenate([np.zeros((256, 320), f32), tile(np.sin(ang).astype(f32))])
    sel = np.zeros((2, 2, 128), f32); sel[0, 0] = 1; sel[1, 1] = 1
    s = np.arange(128)[:, None]; t = np.arange(128)[None, :]
    same = (s // 64) == (t // 64)
    trif = (same & (s <= t)).astype(f32); trir = (same & (s >= t)).astype(f32); eye = np.eye(128, dtype=f32)
    cc = np.stack([eye, np.ones((128, 128), f32), trif, trir, trif - eye, trir - eye, eye - trif, eye - trir, same.astype(f32)])
    return dict(cs=cs, sn=sn, sel=sel, cc=cc, ident=eye)

def make_in_maps(inp, nlayer=2):
    f32 = np.float32
    C = _consts()
    ims = []
    for b in range(2):
        ct = np.stack([inp['c'][b], inp['c_ctx']], 1).reshape(8, 128, 2).transpose(1, 0, 2)
        m = {"xcat": np.concatenate([inp['ctx'][b], inp['x'][b]], 0), "ct": np.ascontiguousarray(ct, dtype=f32)}
        m.update(C)
        for l in range(nlayer):
            cb = np.concatenate([inp['mlstm_i_bias'][l].reshape(8), inp['mlstm_f_bias'][l].reshape(8), inp['gdn_a_log'][l].reshape(8), inp['gdn_dt_bias'][l].reshape(8)])
            m.update({f"modw{l}": inp['mod_w'][l], f"modb{l}": _rep(inp['mod_b'][l], 2), f"n1g{l}": _rep(inp['norm1_g'][l]), f"win{l}": inp['w_in'][l],
                      f"qkg{l}": _rep(np.concatenate([np.tile(inp['q_norm_g'][l], 8), np.tile(inp['k_norm_g'][l], 2)])),
                      f"cb{l}": _rep(cb), f"cw{l}": _rep(inp['gdn_conv_w'][l]),
                      f"gain2{l}": _rep(np.concatenate([inp['mlstm_out_g'][l].reshape(256), np.tile(inp['gdn_out_g'][l], 4)])),
                      f"wout{l}": inp['w_out'][l], f"n2g{l}": _rep(inp['norm2_g'][l]), f"rw{l}": inp['router_w'][l],
                      f"w1{l}": inp['w1'][l], f"w3{l}": inp['w3'][l], f"w2{l}": inp['w2'][l]})
        ims.append({k_: np.ascontiguousarray(v, dtype=f32) for k_, v in m.items()})
    return ims

_K = {}
def kernel(**inputs):
    inp = {k_: np.asarray(v, np.float32) for k_, v in inputs.items()}
    if 2 not in _K:
        _K[2] = build_fused(2)
    res = run_bass_kernel_spmd(_K[2].nc, make_in_maps(inp, 2), core_ids=[0, 1])
    return np.ascontiguousarray(np.stack([res.results[b]["out"] for b in range(2)]).astype(np.float32))
```
